# Optimizing a Trainium2 kernel written in Bass

```python
import jax, jax.numpy as jnp
from jax import lax
import numpy as np

D_MODEL = 1024
BATCH = 8
SEQ = 8192
DEPTH = 1

N_HEADS = 8
QK_NOPE_DIM = 64
QK_ROPE_DIM = 32
V_HEAD_DIM = 64
Q_LORA_RANK = 256
KV_LORA_RANK = 256
ROPE_THETA = 10000.0
ATTN_WIDTH = N_HEADS * V_HEAD_DIM
Q_BLOCK = 128
FOURIER_GROUPS = 8
FOURIER_GROUP_DIM = 64
FOURIER_WIDTH = FOURIER_GROUPS * FOURIER_GROUP_DIM
N_BRANCHES = 2
N_EXPERT_GROUPS = 4
EXPERTS_PER_GROUP = 8
N_EXPERTS = N_EXPERT_GROUPS * EXPERTS_PER_GROUP
TOP_K_INNER = 2
D_EXPERT = 256
N_ADA = 6
EPS = 1e-6

IN_SIZES = (Q_LORA_RANK, KV_LORA_RANK, QK_ROPE_DIM, FOURIER_WIDTH, N_BRANCHES * D_MODEL)
IN_SPLITS = tuple(int(v) for v in np.cumsum(IN_SIZES)[:-1])
IN_COLS = int(sum(IN_SIZES))

kernel_name = "hybrid_mla_fnet_hmoe_adaln_encoder"


def rmsnorm(x, g):
    xf = x.astype(jnp.float32)
    r = lax.rsqrt(jnp.mean(xf * xf, axis=-1, keepdims=True) + EPS)
    return (xf * r * g.astype(jnp.float32)).astype(x.dtype)


def modulate(h, shift, scale):
    return h * (1.0 + scale[:, None, :]) + shift[:, None, :]


def rope_angles(positions):
    inv_freq = ROPE_THETA ** (-jnp.arange(0, QK_ROPE_DIM, 2, dtype=jnp.float32) / QK_ROPE_DIM)
    ang = positions.astype(jnp.float32)[..., None] * inv_freq
    return jnp.cos(ang), jnp.sin(ang)


def apply_rope(x, cos, sin):
    half = QK_ROPE_DIM // 2
    x1, x2 = x[..., :half], x[..., half:]
    cos = cos.astype(x.dtype)
    sin = sin.astype(x.dtype)
    return jnp.concatenate([x1 * cos - x2 * sin, x2 * cos + x1 * sin], axis=-1)


def mla_attention(q_lat, kv_lat, k_rope, positions, g_q, g_kv, w_uq, w_uk, w_uv):
    B, S, _ = q_lat.shape
    q_lat = rmsnorm(q_lat, g_q)
    kv_lat = rmsnorm(kv_lat, g_kv)
    q = jnp.einsum('bsr,rhd->bshd', q_lat, w_uq)
    q_nope, q_rope = q[..., :QK_NOPE_DIM], q[..., QK_NOPE_DIM:]
    k_nope = jnp.einsum('bsr,rhd->bshd', kv_lat, w_uk)
    v = jnp.einsum('bsr,rhd->bshd', kv_lat, w_uv)
    cos, sin = rope_angles(positions)
    q_rope = apply_rope(q_rope, cos[:, :, None, :], sin[:, :, None, :])
    k_rope = apply_rope(k_rope, cos, sin)
    scale = (QK_NOPE_DIM + QK_ROPE_DIM) ** -0.5
    nb = S // Q_BLOCK
    qn_blk = (q_nope * scale).reshape(B, nb, Q_BLOCK, N_HEADS, QK_NOPE_DIM).transpose(1, 0, 2, 3, 4)
    qr_blk = (q_rope * scale).reshape(B, nb, Q_BLOCK, N_HEADS, QK_ROPE_DIM).transpose(1, 0, 2, 3, 4)

    def attend(blk):
        qn, qr = blk
        s = (jnp.einsum('bqhd,bkhd->bhqk', qn, k_nope)
             + jnp.einsum('bqhd,bkd->bhqk', qr, k_rope))
        p = jax.nn.softmax(s.astype(jnp.float32), axis=-1).astype(v.dtype)
        return jnp.einsum('bhqk,bkhd->bqhd', p, v)

    o = lax.map(attend, (qn_blk, qr_blk))
    return o.transpose(1, 0, 2, 3, 4).reshape(B, S, ATTN_WIDTH)


def fourier_mix(u):
    B, S, _ = u.shape
    ug = u.astype(jnp.float32).reshape(B, S, FOURIER_GROUPS, FOURIER_GROUP_DIM)
    f = jnp.fft.fft2(ug, axes=(1, 3), norm='ortho').real
    return f.reshape(B, S, FOURIER_WIDTH).astype(u.dtype)


def hierarchical_moe(h, w_rg, b_rg, w_re, b_re, w_gate, w_up, w_down):
    B, S, D = h.shape
    t = h.reshape(B * S, D)
    g_logits = (t @ w_rg).astype(jnp.float32) + b_rg.astype(jnp.float32)
    g_prob = jax.nn.softmax(g_logits, axis=-1)
    g_sel = jnp.argmax(g_prob, axis=-1)
    g_p = jnp.take_along_axis(g_prob, g_sel[:, None], axis=-1)[:, 0]
    e_logits = jnp.einsum('td,dge->tge', t, w_re).astype(jnp.float32) + b_re.astype(jnp.float32)
    e_logits = jnp.take_along_axis(e_logits, g_sel[:, None, None], axis=1)[:, 0]
    e_prob = jax.nn.softmax(e_logits, axis=-1)
    top_p, top_i = lax.top_k(e_prob, TOP_K_INNER)
    top_p = top_p / jnp.sum(top_p, axis=-1, keepdims=True)
    weights = g_p[:, None] * top_p
    global_idx = g_sel[:, None] * EXPERTS_PER_GROUP + top_i
    combine = jnp.sum(jax.nn.one_hot(global_idx, N_EXPERTS, dtype=jnp.float32)
                      * weights[..., None], axis=1).astype(t.dtype)
    out = jnp.zeros_like(t)
    for e in range(N_EXPERTS):
        hid = jax.nn.silu(t @ w_gate[e]) * (t @ w_up[e])
        out = out + combine[:, e:e + 1] * (hid @ w_down[e])
    return out.reshape(B, S, D)


def setup_inputs(seed: int = 0) -> dict:
    key = jax.random.key(seed)
    ks = jax.random.split(key, 32)
    f32 = jnp.float32
    L, D = DEPTH, D_MODEL

    def nrm(k, shape, fan_in, mult=1.0):
        return jax.random.normal(k, shape, f32) * (mult * fan_in ** -0.5)

    def gain(k, shape):
        return 1.0 + 0.02 * jax.random.normal(k, shape, f32)

    x = jax.random.normal(ks[0], (BATCH, SEQ, D), f32)
    c = jax.random.normal(ks[1], (BATCH, D), f32)
    positions = jnp.sort(jax.random.randint(ks[2], (BATCH, SEQ), 0, 4 * SEQ), axis=-1).astype(jnp.int32)
    return {
        "x": x,
        "c": c,
        "positions": positions,
        "w_ada": nrm(ks[3], (L, D, N_ADA * D), D, 0.5),
        "b_ada": 0.02 * jax.random.normal(ks[4], (L, N_ADA * D), f32),
        "g_norm_mix": gain(ks[5], (L, D)),
        "w_in": nrm(ks[6], (L, D, IN_COLS), D),
        "g_q_lat": gain(ks[7], (L, Q_LORA_RANK)),
        "g_kv_lat": gain(ks[8], (L, KV_LORA_RANK)),
        "w_uq": nrm(ks[9], (L, Q_LORA_RANK, N_HEADS, QK_NOPE_DIM + QK_ROPE_DIM), Q_LORA_RANK),
        "w_uk": nrm(ks[10], (L, KV_LORA_RANK, N_HEADS, QK_NOPE_DIM), KV_LORA_RANK),
        "w_uv": nrm(ks[11], (L, KV_LORA_RANK, N_HEADS, V_HEAD_DIM), KV_LORA_RANK),
        "w_branch_attn": nrm(ks[12], (L, ATTN_WIDTH, D), ATTN_WIDTH),
        "w_branch_fourier": nrm(ks[13], (L, FOURIER_WIDTH, D), FOURIER_WIDTH),
        "b_gates": 0.02 * jax.random.normal(ks[14], (L, N_BRANCHES * D), f32),
        "w_out": nrm(ks[15], (L, D, D), D),
        "g_norm_ffn": gain(ks[16], (L, D)),
        "w_router_group": nrm(ks[17], (L, D, N_EXPERT_GROUPS), D),
        "b_router_group": 0.01 * jax.random.normal(ks[18], (L, N_EXPERT_GROUPS), f32),
        "w_router_expert": nrm(ks[19], (L, D, N_EXPERT_GROUPS, EXPERTS_PER_GROUP), D),
        "b_router_expert": 0.01 * jax.random.normal(ks[20], (L, N_EXPERT_GROUPS, EXPERTS_PER_GROUP), f32),
        "w_expert_gate": nrm(ks[21], (L, N_EXPERTS, D, D_EXPERT), D),
        "w_expert_up": nrm(ks[22], (L, N_EXPERTS, D, D_EXPERT), D),
        "w_expert_down": nrm(ks[23], (L, N_EXPERTS, D_EXPERT, D), D_EXPERT),
        "w_ada_final": nrm(ks[24], (D, 2 * D), D, 0.5),
        "b_ada_final": 0.02 * jax.random.normal(ks[25], (2 * D,), f32),
        "g_norm_final": gain(ks[26], (D,)),
    }


def reference(x, c, positions, w_ada, b_ada, g_norm_mix, w_in, g_q_lat, g_kv_lat,
              w_uq, w_uk, w_uv, w_branch_attn, w_branch_fourier, b_gates, w_out,
              g_norm_ffn, w_router_group, b_router_group, w_router_expert, b_router_expert,
              w_expert_gate, w_expert_up, w_expert_down, w_ada_final, b_ada_final, g_norm_final):
    h = x
    c_act = jax.nn.silu(c)
    for l in range(DEPTH):
        mod = c_act @ w_ada[l] + b_ada[l]
        sh1, sc1, gt1, sh2, sc2, gt2 = jnp.split(mod, N_ADA, axis=-1)

        u = modulate(rmsnorm(h, g_norm_mix[l]), sh1, sc1)
        z = u @ w_in[l]
        q_lat, kv_lat, k_rope, f_in, gate_logits = jnp.split(z, IN_SPLITS, axis=-1)
        a = mla_attention(q_lat, kv_lat, k_rope, positions, g_q_lat[l], g_kv_lat[l],
                          w_uq[l], w_uk[l], w_uv[l]) @ w_branch_attn[l]
        fo = fourier_mix(f_in) @ w_branch_fourier[l]
        ga, gf = jnp.split(gate_logits + b_gates[l], N_BRANCHES, axis=-1)
        merged = jax.nn.sigmoid(ga) * a + jax.nn.sigmoid(gf) * fo
        h = h + gt1[:, None, :] * (merged @ w_out[l])

        v = modulate(rmsnorm(h, g_norm_ffn[l]), sh2, sc2)
        h = h + gt2[:, None, :] * hierarchical_moe(
            v, w_router_group[l], b_router_group[l], w_router_expert[l], b_router_expert[l],
            w_expert_gate[l], w_expert_up[l], w_expert_down[l])

    fmod = c_act @ w_ada_final + b_ada_final
    shf, scf = jnp.split(fmod, 2, axis=-1)
    return modulate(rmsnorm(h, g_norm_final), shf, scf)
```

```python
import math
from contextlib import ExitStack

import numpy as np
import ml_dtypes

import concourse.bass as bass
import concourse.mybir as mybir
from concourse.bass_utils import run_bass_kernel_spmd

F32 = mybir.dt.float32
BF16 = mybir.dt.bfloat16
I32 = mybir.dt.int32
U32 = mybir.dt.uint32
U8 = mybir.dt.uint8
AF = mybir.ActivationFunctionType
ALU = mybir.AluOpType
AX = mybir.AxisListType
DTSIZE = {F32: 4, BF16: 2, I32: 4, U32: 4, U8: 1}

S_TOK = 8192
D = 1024
NT = 64
NCH = 16
CH = 512
NH = 8
EPS = 1e-6
TWO_PI = 2.0 * math.pi
CW1 = 6.28125
CW2 = TWO_PI - CW1
ATT_SCALE = 96 ** -0.5
TR = 256
NST = 96
NSLOT = NST * TR


class Buf:
    __slots__ = ("name", "last_w", "readers")

    def __init__(self, name=""):
        self.name = name
        self.last_w = None
        self.readers = []


class Op:
    __slots__ = ("eng", "fn", "deps", "signal", "sig_idx", "is_dma", "dma_sem", "dma_val", "prev_dma")

    def __init__(self, eng, fn, is_dma=False):
        self.eng = eng
        self.fn = fn
        self.deps = []
        self.signal = False
        self.sig_idx = 0
        self.is_dma = is_dma
        self.dma_sem = None
        self.dma_val = 0
        self.prev_dma = None


ENGS = ("pe", "act", "dve", "pool", "sp")
DMA_RING = 8


def _nop(eng):
    return eng.nop()


_nop.is_nop = True


class Sched:
    def __init__(self, nc, same_engine_sync=True):
        self.nc = nc
        self.ops = {e: [] for e in ENGS}
        self.same = same_engine_sync
        self.dma_ops = {e: [] for e in ENGS}

    def op(self, eng, fn, reads=(), writes=(), is_dma=False):
        o = Op(eng, fn, is_dma)
        for b in reads:
            p = b.last_w
            if p is not None:
                if (not p.is_dma) and p.eng == eng and not is_dma:
                    if self.same and eng != "pe":
                        o.deps.append(p)
                else:
                    o.deps.append(p)
        for b in writes:
            p = b.last_w
            if p is not None and (p.is_dma or is_dma or p.eng != eng):
                o.deps.append(p)
            for r in b.readers:
                if r.is_dma or is_dma or r.eng != eng:
                    o.deps.append(r)
        for b in reads:
            b.readers.append(o)
        for b in writes:
            b.last_w = o
            b.readers = []
        if is_dma:
            lst = self.dma_ops[eng]
            n = len(lst)
            if n >= DMA_RING:
                o.prev_dma = lst[n - DMA_RING]
            o.dma_val = 16 * (n // DMA_RING + 1)
            o.dma_sem = n % DMA_RING
            lst.append(o)
        self.ops[eng].append(o)
        return o

    def dma(self, queue, out, in_, reads=(), writes=(), **kw):
        return self.op(queue, lambda e: e.dma_start(out=out, in_=in_, **kw), reads, writes, is_dma=True)

    def barrier(self):
        lasts = []
        for e in ENGS:
            if self.ops[e]:
                for o in reversed(self.ops[e]):
                    if not o.is_dma and not getattr(o.fn, "is_nop", False):
                        lasts.append(o)
                        break
            lasts.extend(self.dma_ops[e][-DMA_RING:])
        for e in ENGS:
            if True:
                o = self.op(e, _nop)
                for p in lasts:
                    if p.is_dma or p.eng != e:
                        o.deps.append(p)

    def finish(self):
        o = self.op("sp", _nop)
        for e in ENGS:
            o.deps.extend(self.dma_ops[e][-DMA_RING:])

    def emit(self, stack):
        nc = self.nc
        for e in ENGS:
            for o in self.ops[e]:
                for p in o.deps:
                    if not p.is_dma:
                        p.signal = True
        for e in ENGS:
            c = 0
            for o in self.ops[e]:
                if o.signal and not o.is_dma:
                    c += 1
                    o.sig_idx = c
        sems = {e: stack.enter_context(nc.semaphore("s_" + e)) for e in ENGS}
        dsems = {e: [stack.enter_context(nc.semaphore("d_%s_%d" % (e, i))) for i in range(DMA_RING)]
                 for e in ENGS if self.dma_ops[e]}
        block = stack.enter_context(nc.Block())
        engobj = {"pe": "tensor", "act": "scalar", "dve": "vector", "pool": "gpsimd", "sp": "sync"}

        def make(ename):
            ops = self.ops[ename]

            def body(eng):
                waited = {}
                dwaited = {}
                for o in ops:
                    deps = list(o.deps)
                    if o.prev_dma is not None:
                        deps.append(o.prev_dma)
                    for p in deps:
                        if p.is_dma:
                            key = (p.eng, p.dma_sem)
                            if dwaited.get(key, 0) >= p.dma_val:
                                continue
                            eng.wait_ge(dsems[p.eng][p.dma_sem], p.dma_val)
                            dwaited[key] = p.dma_val
                        else:
                            if waited.get(p.eng, 0) >= p.sig_idx:
                                continue
                            eng.wait_ge(sems[p.eng], p.sig_idx)
                            waited[p.eng] = p.sig_idx
                    ins = o.fn(eng)
                    if o.is_dma:
                        ins.then_inc(dsems[ename][o.dma_sem], 16)
                    elif o.signal:
                        ins.then_inc(sems[ename], 1)
            return body

        for ename in ENGS:
            if self.ops[ename]:
                getattr(block, engobj[ename])(make(ename))


class Arena:
    def __init__(self, tensor, size):
        self.t = tensor
        self.size = size
        self.off = 0

    def alloc(self, shape, dt):
        n = 1
        for s in shape[1:]:
            n *= s
        nbytes = n * DTSIZE[dt]
        off = (self.off + 63) // 64 * 64
        assert off + nbytes <= self.size, "arena overflow %d + %d > %d" % (off, nbytes, self.size)
        self.off = off + nbytes
        v = self.t[:, off:off + nbytes].bitcast(dt)
        if len(shape) > 2:
            names = ["d%d" % i for i in range(len(shape) - 1)]
            pat = "p (" + " ".join(names) + ") -> p " + " ".join(names)
            kw = {names[i]: shape[i + 1] for i in range(len(names))}
            v = v.rearrange(pat, **kw)
        return v

    def mark(self):
        return self.off

    def reset(self, m):
        self.off = m


class Ring:
    def __init__(self, items):
        self.items = items
        self.bufs = [Buf() for _ in items]
        self.i = 0

    def next(self):
        i = self.i
        self.i = (i + 1) % len(self.items)
        return self.items[i], self.bufs[i]


class Builder:
    def __init__(self, dbg=(), stop_after=None):
        self.dbg = set(dbg)
        self.stop_after = stop_after
        self.nc = bass.Bass("TRN2", target_bir_lowering=False)
        self.S = Sched(self.nc)
        self.deferred = []

    def din(self, name, shape, dt=F32):
        return self.nc.dram_tensor(name, list(shape), dt, kind="ExternalInput").ap()

    def scratch(self, name, shape, dt):
        kind = "ExternalOutput" if name in self.dbg else "Internal"
        return self.nc.dram_tensor(name, list(shape), dt, kind=kind).ap()

    def mm(self, out, lhsT, rhs, start, stop, reads, writes):
        return self.S.op("pe", lambda e: e.matmul(out, lhsT=lhsT, rhs=rhs, start=start, stop=stop), reads, writes)

    def tr(self, out, in_, ident, reads, writes):
        return self.S.op("pe", lambda e: e.transpose(out=out, in_=in_, identity=ident), reads, writes)

    def act(self, out, in_, func, reads, writes, **kw):
        return self.S.op("act", lambda e: e.activation(out=out, in_=in_, func=func, **kw), reads, writes)

    def ts(self, eng, out, in0, s1, s2, op0, op1, reads, writes):
        if op1 is None:
            return self.S.op(eng, lambda e: e.tensor_scalar(out=out, in0=in0, scalar1=s1, scalar2=None, op0=op0), reads, writes)
        return self.S.op(eng, lambda e: e.tensor_scalar(out=out, in0=in0, scalar1=s1, scalar2=s2, op0=op0, op1=op1), reads, writes)

    def tt(self, eng, out, in0, in1, op, reads, writes):
        return self.S.op(eng, lambda e: e.tensor_tensor(out=out, in0=in0, in1=in1, op=op), reads, writes)

    def stt(self, out, in0, scalar, in1, op0, op1, reads, writes):
        return self.S.op("dve", lambda e: e.scalar_tensor_tensor(out=out, in0=in0, scalar=scalar, in1=in1, op0=op0, op1=op1), reads, writes)

    def cp(self, eng, out, in_, reads, writes):
        if eng == "act":
            return self.S.op("act", lambda e: e.copy(out=out, in_=in_), reads, writes)
        return self.S.op(eng, lambda e: e.tensor_copy(out=out, in_=in_), reads, writes)

    def memset(self, eng, ap, val, writes):
        return self.S.op(eng, lambda e: e.memset(ap, val), (), writes)

    def psum(self):
        return self.pring.next()

    def defer(self, fn):
        self.deferred.append(fn)

    def flush(self):
        d, self.deferred = self.deferred, []
        for f in d:
            f()

    def build(self):
        nc = self.nc
        S = self.S
        with ExitStack() as st:
            self.st = st
            self.b_ys2 = Buf()
            self.b_wgu = Buf()
            self.b_wd = Buf()
            arena_t = st.enter_context(nc.sbuf_tensor("arena", [128, 200 * 1024], U8))
            self.A = Arena(arena_t, 200 * 1024)
            pts = [st.enter_context(nc.psum_tensor("ps%d" % i, [128, 512], F32)) for i in range(8)]
            self.pring = Ring(pts)
            self.declare_io()
            self.phase0()
            if self.stop_after != "p0":
                self.phase1()
            if self.stop_after not in ("p0", "p1"):
                S.barrier()
                self.phase2b()
            if self.stop_after not in ("p0", "p1", "p2b") and "noatt" not in self.dbg:
                S.barrier()
                self.phase2a()
            if self.stop_after not in ("p0", "p1", "p2b", "p2a"):
                S.barrier()
                self.phase3()
            if self.stop_after not in ("p0", "p1", "p2b", "p2a", "p3"):
                self.phase4()
            if self.stop_after not in ("p0", "p1", "p2b", "p2a", "p3", "p4"):
                self.phase5()
            S.barrier()
            S.finish()
            S.emit(st)
        return nc

    def declare_io(self):
        self.x = self.din("x", [S_TOK, D])
        self.ct = self.din("ct", [128, 8])
        self.pos = self.din("pos", [1, S_TOK], I32)
        self.w_ada = self.din("w_ada", [D, 6 * D])
        self.b_ada_t = self.din("b_ada_t", [128, 48])
        self.w_adaf = self.din("w_adaf", [D, 2 * D])
        self.b_adaf_t = self.din("b_adaf_t", [128, 16])
        self.gmix_t = self.din("gmix_t", [128, 8])
        self.gffn_t = self.din("gffn_t", [128, 8])
        self.gfin_t = self.din("gfin_t", [128, 8])
        self.w_in = self.din("w_in", [D, 3104])
        self.glat_t = self.din("glat_t", [128, 4])
        self.w_uq = self.din("w_uq", [256, 768])
        self.w_uk = self.din("w_uk", [256, 512])
        self.w_uv = self.din("w_uv", [256, 512])
        self.b_gates = self.din("b_gates", [1, 2048])
        self.ident = self.din("ident", [128, 128])
        self.invf = self.din("invf", [128, 1])
        self.out = self.nc.dram_tensor("out", [S_TOK, D], F32, kind="ExternalOutput").ap()
        self.US = self.scratch("US", [S_TOK, 512], BF16)
        self.SG = self.scratch("SG", [S_TOK, 2048], BF16)
        self.QT = self.scratch("QT", [NH, 96, S_TOK], BF16)
        self.KT = self.scratch("KT", [NH, 64, S_TOK], BF16)
        self.KTR = self.scratch("KTR", [32, S_TOK], BF16)
        self.VS = self.scratch("VS", [S_TOK, 512], BF16)
        self.DBG = self.scratch("DBG", [128, 4096], F32)
        self.OT = self.scratch("OT", [NH, 64, S_TOK], BF16)
        self.YS = self.scratch("YS", [128, 2, 64, 512], BF16)
        self.FO = self.scratch("FO", [S_TOK, D], BF16)
        self.H1 = self.scratch("H1", [S_TOK, D], F32)
        self.VN = self.scratch("VN", [S_TOK, D], BF16)
        self.XS = self.scratch("XS", [NSLOT, D], BF16)
        self.YS2 = self.scratch("YS2", [NSLOT, D], BF16)
        self.WGU = self.scratch("WGU", [32 * 128, 8 * 512], BF16)
        self.WD = self.scratch("WD", [32 * 128, 2 * D], BF16)
        self.RT = self.scratch("RT", [128, 1024], F32)
        self.w_ba = self.din("w_ba", [512, D])
        self.w_out = self.din("w_out", [D, D])
        self.w_rt = self.din("w_rt", [D, 36])
        self.b_rt = self.din("b_rt", [1, 36])
        self.ltri = self.din("ltri", [128, 128], BF16)
        self.tstart = self.din("tstart", [128, NST])
        self.piota = self.din("piota", [128, 1])
        self.w_eg = self.din("w_eg", [32, D, 256])
        self.w_eu = self.din("w_eu", [32, D, 256])
        self.w_ed = self.din("w_ed", [32, 256, D])
        self.tre = self.din("tre", [64, 128, 128], BF16)
        self.tim = self.din("tim", [64, 128, 128], BF16)
        self.d2 = self.din("d2", [128, 128], BF16)
        self.bdc = self.din("bdc", [128, 128])
        self.bds = self.din("bds", [128, 128])
        self.w_bf = self.din("w_bf", [512, D])

    def phase0(self):
        S, A = self.S, self.A
        self.ident_f = A.alloc([128, 128], F32); self.b_ident_f = Buf()
        self.ident_b = A.alloc([128, 128], BF16); self.b_ident_b = Buf()
        self.ones_f = A.alloc([128, 128], F32); self.b_ones_f = Buf()
        self.ones_b = A.alloc([128, 128], BF16); self.b_ones_b = Buf()
        self.modT = A.alloc([128, 64], F32); self.b_modT = Buf()
        self.vecs = A.alloc([128, 8, 8], F32); self.b_vecs = Buf()
        self.bc = A.alloc([128, 4, D], F32); self.b_bc = Buf()
        self.mhalf = A.alloc([128, 2], F32); self.b_mhalf = Buf()
        cact = A.alloc([128, 8, 2], F32); b_cact = Buf()
        ctt = A.alloc([128, 8], F32); b_ctt = Buf()
        small = A.alloc([128, 128], F32); b_small = Buf()
        self.small = small; self.b_small = b_small
        S.dma("sp", self.ident_f, self.ident[:, :], writes=[self.b_ident_f])
        S.dma("sp", ctt, self.ct[:, :], writes=[b_ctt])
        S.dma("sp", small[:, 0:48], self.b_ada_t[:, :], writes=[b_small])
        S.dma("sp", small[:, 48:64], self.b_adaf_t[:, :], writes=[b_small])
        S.dma("sp", small[:, 64:72], self.gmix_t[:, :], writes=[b_small])
        S.dma("sp", small[:, 72:80], self.gffn_t[:, :], writes=[b_small])
        S.dma("sp", small[:, 80:88], self.gfin_t[:, :], writes=[b_small])
        S.dma("sp", small[:, 88:92], self.glat_t[:, :], writes=[b_small])
        S.dma("sp", small[:, 92:93], self.invf[:, :], writes=[b_small])
        self.cp("dve", self.ident_b, self.ident_f, [self.b_ident_f], [self.b_ident_b])
        self.memset("pool", self.ones_f, 1.0, [self.b_ones_f])
        self.memset("pool", self.ones_b, 1.0, [self.b_ones_b])
        self.memset("pool", self.mhalf, -0.5, [self.b_mhalf])
        self.act(cact[:, :, 0], ctt, AF.Silu, [b_ctt], [b_cact])
        self.act(cact[:, :, 1], ctt, AF.Silu, [b_ctt], [b_cact])

        m0 = A.mark()
        wst = [A.alloc([128, 8, 1024], F32) for _ in range(2)]
        wring = Ring(wst)
        for piece in range(8):
            wt, bw = wring.next()
            if piece < 6:
                src = self.w_ada[:, piece * 1024:(piece + 1) * 1024]
            else:
                src = self.w_adaf[:, (piece - 6) * 1024:(piece - 5) * 1024]
            S.dma("sp", wt, src.rearrange("(k p) n -> p k n", p=128), writes=[bw])
            pt, bp = self.psum()
            pv = pt[:, 0:16].rearrange("p (a b) -> p a b", b=2)
            for jc in range(8):
                for k in range(8):
                    self.mm(pv[:, jc, :], wt[:, k, jc * 128:(jc + 1) * 128], cact[:, k, :], k == 0, k == 7,
                            [bw, b_cact], [bp])
            self.tt("dve", self.modT[:, piece * 8:(piece + 1) * 8], pv[:, :, 0], small[:, piece * 8:(piece + 1) * 8],
                    ALU.add, [bp, b_small], [self.b_modT])
        S.barrier()
        A.reset(m0)
        mT = self.modT
        V = self.vecs
        bv = self.b_vecs
        self.stt(V[:, 0, :], mT[:, 8:16], 1.0, small[:, 64:72], ALU.add, ALU.mult, [self.b_modT, b_small], [bv])
        self.cp("dve", V[:, 1, :], mT[:, 0:8], [self.b_modT], [bv])
        self.stt(V[:, 2, :], mT[:, 32:40], 1.0, small[:, 72:80], ALU.add, ALU.mult, [self.b_modT, b_small], [bv])
        self.cp("dve", V[:, 3, :], mT[:, 24:32], [self.b_modT], [bv])
        self.stt(V[:, 4, :], mT[:, 56:64], 1.0, small[:, 80:88], ALU.add, ALU.mult, [self.b_modT, b_small], [bv])
        diag_r = Ring([A.alloc([128, 128], F32) for _ in range(2)])
        srcs = [mT[:, 16:24], mT[:, 40:48], V[:, 4, :], mT[:, 48:56]]
        for i, sv in enumerate(srcs):
            for j in range(8):
                dg, bd = diag_r.next()
                self.ts("dve", dg, self.ident_f, sv[:, j:j + 1], None, ALU.mult, None,
                        [self.b_ident_f, self.b_modT, bv], [bd])
                pt, bp = self.psum()
                self.mm(pt[:, 0:128], self.ones_f, dg, True, True, [self.b_ones_f, bd], [bp])
                self.cp("act", self.bc[:, i, j * 128:(j + 1) * 128], pt[:, 0:128], [bp], [self.b_bc])

        self.m_persist = A.mark()
        self.cosT = A.alloc([128, S_TOK], BF16); self.b_cosT = Buf()
        self.sinT = A.alloc([128, S_TOK], BF16); self.b_sinT = Buf()
        m1 = A.mark()
        PW = 2048
        pi_r = Ring([A.alloc([128, PW], I32) for _ in range(2)])
        ang_r = Ring([A.alloc([128, PW], F32) for _ in range(2)])
        ki_r = Ring([A.alloc([128, PW], I32) for _ in range(1)])
        kf_r = Ring([A.alloc([128, PW], F32) for _ in range(1)])
        r_r = Ring([A.alloc([128, PW], F32) for _ in range(2)])
        ab_r = Ring([A.alloc([128, PW], F32) for _ in range(1)])
        R = slice(64, 96)
        invf = small[R, 92:93]
        for pc in range(S_TOK // PW):
            sl = slice(pc * PW, (pc + 1) * PW)
            pi, bpi = pi_r.next()
            S.dma("sp", pi[R, :], self.pos[0:1, sl].partition_broadcast(32), writes=[bpi])
            ang, bang = ang_r.next()
            self.cp("dve", ang[R, :], pi[R, :], [bpi], [bang])
            self.ts("dve", ang[R, :], ang[R, :], invf, None, ALU.mult, None, [bang, b_small], [bang])
            ki, bki = ki_r.next()
            self.ts("dve", ki[R, :], ang[R, :], 1.0 / TWO_PI, None, ALU.mult, None, [bang], [bki])
            kf, bkf = kf_r.next()
            self.cp("dve", kf[R, :], ki[R, :], [bki], [bkf])
            r, br = r_r.next()
            self.stt(r[R, :], kf[R, :], -CW1, ang[R, :], ALU.mult, ALU.add, [bkf, bang], [br])
            self.stt(r[R, :], kf[R, :], -CW2, r[R, :], ALU.mult, ALU.add, [bkf, br], [br])
            self.ts("dve", r[R, :], r[R, :], math.pi, -math.pi, ALU.min, ALU.max, [br], [br])
            self.act(self.sinT[R, sl], r[R, :], AF.Sin, [br], [self.b_sinT])
            ab, bab = ab_r.next()
            self.stt(ab[R, :], r[R, :], -1.0, r[R, :], ALU.mult, ALU.max, [br], [bab])
            self.act(self.cosT[R, sl], ab[R, :], AF.Sin, [bab], [self.b_cosT], scale=-1.0, bias=math.pi / 2)
        S.barrier()
        A.reset(m1)

        self.Wlat = A.alloc([128, 8, 512], BF16)
        self.Wkr = A.alloc([128, 8, 2, 96], BF16)
        self.Wf = A.alloc([128, 8, 512], BF16)
        self.Wg = A.alloc([128, 8, 2048], BF16)
        self.Wuq = A.alloc([128, 2, NH, 96], BF16)
        self.WuqR = A.alloc([128, 2, NH, 96], BF16)
        self.Wuk = A.alloc([128, 2, 512], BF16)
        self.Wuv = A.alloc([128, 2, 512], BF16)
        self.bg_b = A.alloc([128, 2048], BF16)
        self.b_W1 = Buf()
        bW = self.b_W1
        m2 = A.mark()
        stg = Ring([A.alloc([128, 8, 512], F32) for _ in range(2)])
        self.memset("pool", self.Wkr, 0.0, [bW])
        self.memset("pool", self.WuqR, 0.0, [bW])
        win = self.w_in

        def wsrc(c0, c1):
            return win[:, c0:c1].rearrange("(k p) n -> p k n", p=128)
        t, b = stg.next()
        S.dma("sp", t, wsrc(0, 512), writes=[b])
        self.cp("dve", self.Wlat, t, [b], [bW])
        t, b = stg.next()
        S.dma("sp", t[:, :, 0:32], wsrc(512, 544), writes=[b])
        self.cp("dve", self.Wkr[:, :, 0, 64:96], t[:, :, 0:32], [b], [bW])
        self.ts("dve", self.Wkr[:, :, 1, 64:80], t[:, :, 16:32], -1.0, None, ALU.mult, None, [b], [bW])
        self.cp("dve", self.Wkr[:, :, 1, 80:96], t[:, :, 0:16], [b], [bW])
        t, b = stg.next()
        S.dma("sp", t, wsrc(544, 1056), writes=[b])
        self.cp("act", self.Wf, t, [b], [bW])
        for g in range(4):
            t, b = stg.next()
            S.dma("sp", t, wsrc(1056 + g * 512, 1056 + (g + 1) * 512), writes=[b])
            self.cp("dve" if g % 2 == 0 else "act", self.Wg[:, :, g * 512:(g + 1) * 512], t, [b], [bW])
        tq2 = A.alloc([128, 2, 768], F32); btq2 = Buf()
        S.dma("sp", tq2, self.w_uq.rearrange("(j p) n -> p j n", p=128), writes=[btq2])
        tq2v = tq2.rearrange("p j (h d) -> p j h d", h=NH)
        self.cp("dve", self.Wuq, tq2v, [btq2], [bW])
        self.ts("dve", self.WuqR[:, :, :, 64:80], tq2v[:, :, :, 80:96], -1.0, None, ALU.mult, None, [btq2], [bW])
        self.cp("dve", self.WuqR[:, :, :, 80:96], tq2v[:, :, :, 64:80], [btq2], [bW])
        t, b = stg.next()
        S.dma("sp", t[:, 0:2, :], self.w_uk.rearrange("(j p) n -> p j n", p=128), writes=[b])
        self.cp("dve", self.Wuk, t[:, 0:2, :], [b], [bW])
        t, b = stg.next()
        S.dma("sp", t[:, 0:2, :], self.w_uv.rearrange("(j p) n -> p j n", p=128), writes=[b])
        self.cp("dve", self.Wuv, t[:, 0:2, :], [b], [bW])
        t, b = stg.next()
        tb = t[0:1, 0:4, :].rearrange("p a b -> p (a b)")
        S.dma("sp", tb, self.b_gates[0:1, :], writes=[b])
        self.cp("dve", self.bg_b[0:1, :], tb, [b], [bW])
        S.barrier()
        A.reset(m2)
        if "DBG" in self.dbg:
            S.dma("sp", self.DBG[:, 0:64], self.modT, reads=[self.b_modT], writes=[Buf()])
            S.dma("sp", self.DBG[:, 64:128], self.vecs.rearrange("p a b -> p (a b)"), reads=[self.b_vecs], writes=[Buf()])
            S.dma("sp", self.DBG[:, 1024:2048], self.bc[:, 0, :], reads=[self.b_bc], writes=[Buf()])
            S.dma("sp", self.DBG[:, 2048:3072], self.bc[:, 2, :], reads=[self.b_bc], writes=[Buf()])

    def phase1(self):
        S, A = self.S, self.A
        m0 = A.mark()
        bW = self.b_W1
        V = self.vecs
        small = self.small
        xr = Ring([A.alloc([128, D], F32) for _ in range(3)])
        junk = A.alloc([128, D], BF16); b_junk = Buf()
        xnr = Ring([A.alloc([128, D], BF16) for _ in range(2)])
        uTr = Ring([A.alloc([128, 8, CH], BF16) for _ in range(2)])
        latTr = Ring([A.alloc([128, 4, CH], BF16) for _ in range(2)])
        stat = A.alloc([128, NT, 4], F32)
        stat2 = A.alloc([128, NT, 4], F32)
        znr = Ring([A.alloc([128, 512], BF16) for _ in range(2)])
        vsr = Ring([A.alloc([128, 512], BF16) for _ in range(3)])
        usr = Ring([A.alloc([128, 512], BF16) for _ in range(3)])
        sgr = Ring([A.alloc([128, 2048], BF16) for _ in range(3)])
        qtr = Ring([A.alloc([128, CH], BF16) for _ in range(3)])
        ktr = Ring([A.alloc([128, CH], BF16) for _ in range(3)])
        t1r = Ring([A.alloc([128, CH], F32) for _ in range(2)])
        t2r = Ring([A.alloc([128, CH], F32) for _ in range(2)])
        xT = {}
        state = {}

        def load_x(t):
            xt, bx = xr.next()
            S.dma("sp", xt, self.x[t * 128:(t + 1) * 128, :], writes=[bx])
            xT[t] = (xt, bx)

        xns = {}
        zns = {}

        def stageA1(t):
            xt, bx = xT.pop(t)
            bst = Buf()
            self.act(junk, xt, AF.Square, [bx], [b_junk, bst], accum_out=stat[:, t, 0:1])
            self.ts("dve", stat[:, t, 1:2], stat[:, t, 0:1], 1.0 / D, EPS, ALU.mult, ALU.add, [bst], [bst])
            self.tt("pool", stat[:, t, 2:3], stat[:, t, 1:2], self.mhalf[:, 0:1], ALU.pow, [bst, self.b_mhalf], [bst])
            xn, bxn = xnr.next()
            self.act(xn, xt, AF.Copy, [bx, bst], [bxn], scale=stat[:, t, 2:3])
            xns[t] = (xn, bxn)

        def stageA2(t):
            c, s = divmod(t, 4)
            if s == 0:
                state[c] = (uTr.next(), latTr.next())
            (uT, buT), (latT, blatT) = state[c]
            xn, bxn = xns.pop(t)
            pt, bp = self.psum()
            pv = pt[:, :].bitcast(BF16).rearrange("p (k n) -> p k n", k=8)
            for k in range(8):
                self.tr(pv[:, k, :], xn[:, k * 128:(k + 1) * 128], self.ident_b, [bxn, self.b_ident_b], [bp])
            for k in range(8):
                self.ts("dve", uT[:, k, s * 128:(s + 1) * 128], pv[:, k, :], V[:, 0, k:k + 1], V[:, 1, k:k + 1],
                        ALU.mult, ALU.add, [bp, self.b_vecs], [buT])

        def stageB1(t):
            c, s = divmod(t, 4)
            (uT, buT), (latT, blatT) = state[c]
            ts_ = slice(s * 128, (s + 1) * 128)
            pt, bp = self.psum()
            for k in range(8):
                self.mm(pt[:, :], uT[:, k, ts_], self.Wlat[:, k, :], k == 0, k == 7, [buT, bW], [bp])
            bst = Buf()
            self.act(junk[:, 0:256], pt[:, 0:256], AF.Square, [bp], [b_junk, bst], accum_out=stat2[:, t, 0:1])
            self.act(junk[:, 256:512], pt[:, 256:512], AF.Square, [bp], [b_junk, bst], accum_out=stat2[:, t, 1:2])
            self.ts("dve", stat2[:, t, 0:2], stat2[:, t, 0:2], 1.0 / 256, EPS, ALU.mult, ALU.add, [bst], [bst])
            self.tt("pool", stat2[:, t, 2:4], stat2[:, t, 0:2], self.mhalf[:, 0:2], ALU.pow,
                    [bst, self.b_mhalf], [bst])
            zn, bzn = znr.next()
            self.ts("dve", zn[:, 0:256], pt[:, 0:256], stat2[:, t, 2:3], None, ALU.mult, None, [bp, bst], [bzn])
            self.act(zn[:, 256:512], pt[:, 256:512], AF.Copy, [bp, bst], [bzn], scale=stat2[:, t, 3:4])
            zns[t] = (zn, bzn)

        def stageB2(t):
            c, s = divmod(t, 4)
            (uT, buT), (latT, blatT) = state[c]
            ts_ = slice(s * 128, (s + 1) * 128)
            zn, bzn = zns.pop(t)
            pt2, bp2 = self.psum()
            pv2 = pt2[:, 0:256].bitcast(BF16).rearrange("p (k n) -> p k n", k=4)
            for j in range(4):
                self.tr(pv2[:, j, :], zn[:, j * 128:(j + 1) * 128], self.ident_b, [bzn, self.b_ident_b], [bp2])
            for j in range(4):
                self.ts("dve", latT[:, j, ts_], pv2[:, j, :], small[:, 88 + j:89 + j], None, ALU.mult, None,
                        [bp2, self.b_small], [blatT])

        def stageC(t):
            c, s = divmod(t, 4)
            (uT, buT), (latT, blatT) = state[c]
            ts_ = slice(s * 128, (s + 1) * 128)
            rows = slice(t * 128, (t + 1) * 128)
            pt, bp = self.psum()
            for j in range(2):
                self.mm(pt[:, :], latT[:, 2 + j, ts_], self.Wuv[:, j, :], j == 0, j == 1, [blatT, bW], [bp])
            vs, bvs = vsr.next()
            self.cp("act", vs, pt[:, :], [bp], [bvs])
            self.defer(lambda: S.dma("sp", self.VS[rows, :], vs, reads=[bvs], writes=[Buf()]))
            pt, bp = self.psum()
            for k in range(8):
                self.mm(pt[:, :], uT[:, k, ts_], self.Wf[:, k, :], k == 0, k == 7, [buT, bW], [bp])
            us, bus = usr.next()
            self.cp("dve", us, pt[:, :], [bp], [bus])
            self.defer(lambda: S.dma("sp", self.US[rows, :], us, reads=[bus], writes=[Buf()]))
            sg, bsg = sgr.next()
            for g in range(4):
                pt, bp = self.psum()
                gs = slice(g * 512, (g + 1) * 512)
                for k in range(8):
                    self.mm(pt[:, :], uT[:, k, ts_], self.Wg[:, k, gs], k == 0, False, [buT, bW], [bp])
                self.mm(pt[:, :], self.ones_b[0:1, :], self.bg_b[0:1, gs], False, True, [self.b_ones_b, bW], [bp])
                self.act(sg[:, gs], pt[:, :], AF.Sigmoid, [bp], [bsg])
            self.defer(lambda: S.dma("sp", self.SG[rows, :], sg, reads=[bsg], writes=[Buf()]))

        def stageD(c):
            (uT, buT), (latT, blatT) = state[c]
            cs = slice(c * CH, (c + 1) * CH)
            R = slice(64, 96)
            for h in range(NH):
                pa, bpa = self.psum()
                for j in range(2):
                    self.mm(pa[0:96, :], self.Wuq[:, j, h, :], latT[:, j, :], j == 0, j == 1, [bW, blatT], [bpa])
                pb, bpb = self.psum()
                for j in range(2):
                    self.mm(pb[0:96, :], self.WuqR[:, j, h, :], latT[:, j, :], j == 0, j == 1, [bW, blatT], [bpb])
                qt, bqt = qtr.next()
                self.act(qt[0:64, :], pa[0:64, :], AF.Copy, [bpa], [bqt], scale=ATT_SCALE)
                t1, bt1 = t1r.next()
                t2, bt2 = t2r.next()
                self.stt(t1[R, :], pa[R, :], ATT_SCALE, self.cosT[R, cs], ALU.mult, ALU.mult, [bpa, self.b_cosT], [bt1])
                self.stt(t2[R, :], pb[R, :], ATT_SCALE, self.sinT[R, cs], ALU.mult, ALU.mult, [bpb, self.b_sinT], [bt2])
                self.tt("pool", qt[R, :], t1[R, :], t2[R, :], ALU.add, [bt1, bt2], [bqt])
                S.dma("sp", self.QT[h, :, cs], qt[0:96, :], reads=[bqt], writes=[Buf()])
                pk, bpk = self.psum()
                for j in range(2):
                    self.mm(pk[0:64, :], self.Wuk[:, j, h * 64:(h + 1) * 64], latT[:, 2 + j, :], j == 0, j == 1,
                            [bW, blatT], [bpk])
                kt, bkt = ktr.next()
                self.cp("act", kt[0:64, :], pk[0:64, :], [bpk], [bkt])
                S.dma("sp", self.KT[h, :, cs], kt[0:64, :], reads=[bkt], writes=[Buf()])
            pa, bpa = self.psum()
            for k in range(8):
                self.mm(pa[0:96, :], self.Wkr[:, k, 0, :], uT[:, k, :], k == 0, k == 7, [bW, buT], [bpa])
            pb, bpb = self.psum()
            for k in range(8):
                self.mm(pb[0:96, :], self.Wkr[:, k, 1, :], uT[:, k, :], k == 0, k == 7, [bW, buT], [bpb])
            t1, bt1 = t1r.next()
            t2, bt2 = t2r.next()
            self.tt("dve", t1[R, :], pa[R, :], self.cosT[R, cs], ALU.mult, [bpa, self.b_cosT], [bt1])
            self.tt("dve", t2[R, :], pb[R, :], self.sinT[R, cs], ALU.mult, [bpb, self.b_sinT], [bt2])
            kt, bkt = ktr.next()
            self.tt("pool", kt[R, :], t1[R, :], t2[R, :], ALU.add, [bt1, bt2], [bkt])
            S.dma("sp", self.KTR[:, cs], kt[R, :], reads=[bkt], writes=[Buf()])

        load_x(0)
        load_x(1)
        stages = [stageA1, stageA2, stageB1, stageB2, stageC]
        for i in range(NT + len(stages) - 1):
            if i + 2 < NT:
                load_x(i + 2)
            self.flush()
            for k_, fn_ in enumerate(stages):
                if 0 <= i - k_ < NT:
                    fn_(i - k_)
            tl = i - (len(stages) - 1)
            if 0 <= tl < NT and tl % 4 == 3:
                stageD(tl // 4)
        self.flush()
        S.barrier()
        A.reset(self.m_persist)


    def phase2b(self):
        S, A = self.S, self.A
        m0 = A.mark()
        WCS = A.alloc([128, 4, 2, D], BF16); bWCS = Buf()
        D2 = A.alloc([128, 128], BF16); bD2 = Buf()
        m1 = A.mark()
        wbf = A.alloc([128, 4, D], F32); bwbf = Buf()
        bd = A.alloc([128, 2, 128], F32); bbd = Buf()
        S.dma("sp", wbf, self.w_bf.rearrange("(j p) n -> p j n", p=128), writes=[bwbf])
        S.dma("sp", bd[:, 0, :], self.bdc[:, :], writes=[bbd])
        S.dma("sp", bd[:, 1, :], self.bds[:, :], writes=[bbd])
        S.dma("sp", D2, self.d2[:, :], writes=[bD2])
        for j in range(4):
            for r in range(2):
                for hf in range(2):
                    ps, bps = self.psum()
                    hs = slice(hf * 512, (hf + 1) * 512)
                    self.mm(ps[:, :], bd[:, r, :], wbf[:, j, hs], True, True, [bbd, bwbf], [bps])
                    self.cp("act" if hf else "dve", WCS[:, j, r, hs], ps[:, :], [bps], [bWCS])
        S.barrier()
        A.reset(m1)
        tbr = Ring([A.alloc([128, 2, 128], BF16) for _ in range(6)])
        unr = Ring([A.alloc([128, 512], BF16) for _ in range(6)])
        yor = Ring([A.alloc([128, 2, 512], BF16) for _ in range(3)])
        USv = self.US.rearrange("(n1 n2) c -> n2 n1 c", n2=64)
        TREv = self.tre.rearrange("n a b -> a n b")
        TIMv = self.tim.rearrange("n a b -> a n b")
        ld1 = {}

        def load1(n2):
            tb, btb = tbr.next()
            S.dma("sp", tb[:, 0, :], self.tre[n2, :, :], writes=[btb])
            S.dma("sp", tb[:, 1, :], self.tim[n2, :, :], writes=[btb])
            un, bun = unr.next()
            S.dma("sp", un, USv[n2, :, :], writes=[bun])
            ld1[n2] = (tb, btb, un, bun)
        PF = 4
        for n2 in range(PF):
            load1(n2)
        for n2 in range(64):
            if n2 + PF < 64:
                load1(n2 + PF)
            tb, btb, un, bun = ld1.pop(n2)
            yo, byo = yor.next()
            for r in range(2):
                ps, bps = self.psum()
                self.mm(ps[:, :], tb[:, r, :], un, True, True, [btb, bun], [bps])
                self.cp("act" if r else "dve", yo[:, r, :], ps[:, :], [bps], [byo])
            S.dma("sp", self.YS[:, :, n2, :], yo, reads=[byo], writes=[Buf()])
        S.barrier()
        ysr = Ring([A.alloc([128, 512], BF16) for _ in range(8)])
        gtr = Ring([A.alloc([128, 4, 2, 2, 64], BF16) for _ in range(3)])
        for_ = Ring([A.alloc([128, D], BF16) for _ in range(4)])
        FOv = self.FO.rearrange("(k2 k1) d -> k1 k2 d", k1=128)
        ld2 = {}

        def load2(k1):
            ys, bys = ysr.next()
            S.dma("sp", ys, self.YS[k1, :, :, :].rearrange("r n c -> (r n) c"), writes=[bys])
            ld2[k1] = (ys, bys)
        PF2 = 6
        for k1 in range(PF2):
            load2(k1)
        gts = {}

        def d2stage(kp):
            gt, bgt = gtr.next()
            for pr in range(2):
                k1 = 2 * kp + pr
                if k1 + PF2 < 128:
                    load2(k1 + PF2)
                ys, bys = ld2.pop(k1)
                ps, bps = self.psum()
                for j in range(4):
                    self.mm(ps[:, j * 128:(j + 1) * 128], ys[:, j * 128:(j + 1) * 128], D2, True, True,
                            [bys, bD2], [bps])
                self.cp("dve" if pr else "act", gt[:, :, :, pr, :],
                        ps[:, :].rearrange("p (j q k) -> p j q k", j=4, q=2), [bps], [bgt])
            gts[kp] = (gt, bgt)

        def proj(kp):
            gt, bgt = gts.pop(kp)
            fo, bfo = for_.next()
            for hf in range(2):
                hs = slice(hf * 512, (hf + 1) * 512)
                ps, bps = self.psum()
                n = 0
                for j in range(4):
                    for q in range(2):
                        self.mm(ps[:, :], gt[:, j, q, :, :].rearrange("p a b -> p (a b)"), WCS[:, j, q, hs],
                                n == 0, n == 7, [bgt, bWCS], [bps])
                        n += 1
                self.cp("act" if hf else "dve", fo[:, hs], ps[:, :], [bps], [bfo])
            self.defer(lambda: S.dma("sp", FOv[2 * kp, :, :], fo[0:64, :], reads=[bfo], writes=[Buf()]))
            self.defer(lambda: S.dma("sp", FOv[2 * kp + 1, :, :], fo[64:128, :], reads=[bfo], writes=[Buf()]))

        d2stage(0)
        for kp in range(64):
            self.flush()
            if kp + 1 < 64:
                d2stage(kp + 1)
            proj(kp)
        self.flush()
        S.barrier()
        A.reset(m0)

    def phase2a(self):
        S, A = self.S, self.A
        m0 = A.mark()
        pts = self.pring.items
        sring = Ring(pts[0:6])
        oring = Ring(pts[6:8])
        Vall = A.alloc([128, NT, NH, 65], BF16); bV = Buf()
        vst = Ring([A.alloc([128, 8, 512], BF16) for _ in range(2)])
        Kr = Ring([A.alloc([128, S_TOK], BF16) for _ in range(2)])
        Qr = Ring([A.alloc([128, CH], BF16) for _ in range(3)])
        Pr = Ring([A.alloc([128, CH], BF16) for _ in range(4)])
        srr = Ring([A.alloc([128, CH], F32) for _ in range(2)])
        recr = Ring([A.alloc([128, CH], F32) for _ in range(2)])
        onr = Ring([A.alloc([128, CH], BF16) for _ in range(2)])
        self.memset("pool", Vall[:, :, :, 64:65], 1.0, [bV])
        for g in range(8):
            vs, bvs = vst.next()
            S.dma("sp", vs, self.VS[g * 1024:(g + 1) * 1024, :].rearrange("(t p) n -> p t n", p=128), writes=[bvs])
            self.cp("pool" if g % 2 else "dve", Vall[:, g * 8:(g + 1) * 8, :, 0:64],
                    vs.rearrange("p t (h d) -> p t h d", h=NH), [bvs], [bV])
        kbufs = []
        for i in range(2):
            kb, bk = Kr.next()
            brope = Buf()
            S.dma("sp", kb[64:96, :], self.KTR[:, :], writes=[brope])
            kbufs.append((kb, bk, brope))
        LAG = 2
        cst = Ring([A.alloc([128, 2048], F32) for _ in range(3)])
        cbf = Ring([A.alloc([128, 2048], BF16) for _ in range(2)])
        WGUv = self.WGU.rearrange("r (k f) -> r k f", k=8)
        units = [(e_, kind) for e_ in range(32) for kind in range(3)]
        cld = {}

        def conv_load(u):
            if u >= len(units):
                return
            e_, kind = units[u]
            t_, b_ = cst.next()
            if kind == 0:
                S.dma("sp", t_.rearrange("p (k f) -> p k f", k=8),
                      self.w_eg[e_, :, :].rearrange("(k p) f -> p k f", p=128), writes=[b_])
            elif kind == 1:
                S.dma("sp", t_.rearrange("p (k f) -> p k f", k=8),
                      self.w_eu[e_, :, :].rearrange("(k p) f -> p k f", p=128), writes=[b_])
            else:
                S.dma("sp", t_.rearrange("p (j d) -> p j d", j=2),
                      self.w_ed[e_, :, :].rearrange("(j p) d -> p j d", p=128), writes=[b_])
            cld[u] = (t_, b_)

        def conv_cast(u):
            if u >= len(units):
                return
            e_, kind = units[u]
            t_, b_ = cld.pop(u)
            o_, bo_ = cbf.next()
            self.cp("pool", o_, t_, [b_], [bo_])
            rows = slice(e_ * 128, (e_ + 1) * 128)
            if kind == 0:
                S.dma("sp", WGUv[rows, :, 0:256], o_.rearrange("p (k f) -> p k f", k=8), reads=[bo_], writes=[Buf()])
            elif kind == 1:
                S.dma("sp", WGUv[rows, :, 256:512], o_.rearrange("p (k f) -> p k f", k=8), reads=[bo_], writes=[Buf()])
            else:
                S.dma("sp", self.WD[rows, :], o_, reads=[bo_], writes=[Buf()])

        conv_load(0)
        zt = A.alloc([128, 4096], BF16); bzt = Buf()
        self.memset("pool", zt, 0.0, [bzt])
        XSz = self.XS.rearrange("(p r) d -> p (r d)", p=128)
        nz = (NSLOT // 128) * D // 4096

        def zero_fill(i):
            if 0 <= i < nz:
                S.dma("sp", XSz[:, i * 4096:(i + 1) * 4096], zt, reads=[bzt], writes=[Buf()])

        def load_k(h):
            kb, bk, brope = kbufs[h % 2]
            S.dma("sp", kb[0:64, :], self.KT[h, :, :], writes=[bk])

        load_k(0)
        for h in range(NH):
            kb, bk, brope = kbufs[h % 2]
            if h + 1 < NH:
                load_k(h + 1)
            qts = {}

            def load_q(qc):
                qt, bq = Qr.next()
                S.dma("sp", qt[0:96, :], self.QT[h, :, qc * CH:(qc + 1) * CH], writes=[bq])
                qts[qc] = (qt, bq)
            load_q(0)
            load_q(1)
            for qc in range(NCH):
                if qc + 2 < NCH:
                    load_q(qc + 2)
                conv_load(h * NCH + qc + 1)
                conv_cast(h * NCH + qc)
                zero_fill(h * NCH + qc - 70)
                qt, bq = qts.pop(qc)
                po, bpo = oring.next()
                pend = []

                def pv(kt, pT, bpT):
                    self.mm(po[0:65, :], Vall[:, kt, h, :], pT, kt == 0, kt == NT - 1, [bV, bpT], [bpo])
                for kt in range(NT):
                    ps, bps = sring.next()
                    self.mm(ps[:, :], kb[0:96, kt * 128:(kt + 1) * 128], qt[0:96, :], True, True,
                            [bk, brope, bq], [bps])
                    pT, bpT = Pr.next()
                    self.act(pT, ps[:, :], AF.Exp, [bps], [bpT])
                    pend.append((kt, pT, bpT))
                    if len(pend) > LAG:
                        pv(*pend.pop(0))
                while pend:
                    pv(*pend.pop(0))
                sr, bsr = srr.next()
                self.cp("dve", sr[64:65, :], po[64:65, :], [bpo], [bsr])
                pb, bpb = sring.next()
                self.mm(pb[0:64, :], self.ones_f[64:65, 0:64], sr[64:65, :], True, True, [self.b_ones_f, bsr], [bpb])
                rec, brec = recr.next()
                self.S.op("dve", lambda e, o_=rec[0:64, :], i_=pb[0:64, :]: e.reciprocal(out=o_, in_=i_), [bpb], [brec])
                on, bon = onr.next()
                self.tt("dve", on[0:64, :], po[0:64, :], rec[0:64, :], ALU.mult, [bpo, brec], [bon])
                S.dma("sp", self.OT[h, :, qc * CH:(qc + 1) * CH], on[0:64, :], reads=[bon], writes=[Buf()])
        S.barrier()
        A.reset(m0)

    def phase3(self):
        S, A = self.S, self.A
        V = self.vecs
        self.w12 = A.alloc([128, NT, 2], F32); self.b_w12 = Buf()
        self.slot_i = A.alloc([128, 2, NT], I32); self.b_slot = Buf()
        self.idxw = A.alloc([128, NST], I32); self.b_idxw = Buf()
        self.m_persist = A.mark()
        M12 = A.alloc([128, 2, NT, 32], BF16); bM = Buf()
        rank = A.alloc([128, NT, 32], F32); brank = Buf()
        Rr = A.alloc([128, 32], F32); bR = Buf()
        m0 = A.mark()
        Wba = A.alloc([128, 4, D], BF16)
        Wout = A.alloc([128, 8, D], BF16)
        Wr = A.alloc([128, 8, 36], BF16)
        br_b = A.alloc([128, 36], BF16)
        Ltri = A.alloc([128, 128], BF16)
        AB2 = A.alloc([128, 2, D], F32)
        bW = Buf()
        m1 = A.mark()
        stg = Ring([A.alloc([128, 8, 512], F32) for _ in range(2)])
        for hf in range(2):
            t, b = stg.next()
            S.dma("sp", t[:, 0:4, :], self.w_ba[:, hf * 512:(hf + 1) * 512].rearrange("(j p) n -> p j n", p=128), writes=[b])
            self.cp("dve", Wba[:, :, hf * 512:(hf + 1) * 512], t[:, 0:4, :], [b], [bW])
        for hf in range(2):
            t, b = stg.next()
            S.dma("sp", t, self.w_out[:, hf * 512:(hf + 1) * 512].rearrange("(k p) n -> p k n", p=128), writes=[b])
            self.tt("dve", Wout[:, :, hf * 512:(hf + 1) * 512], t,
                    self.bc[:, 0, hf * 512:(hf + 1) * 512].unsqueeze(1).broadcast_to([128, 8, 512]), ALU.mult,
                    [b, self.b_bc], [bW])
        t, b = stg.next()
        S.dma("sp", t[:, :, 0:36], self.w_rt.rearrange("(k p) n -> p k n", p=128), writes=[b])
        self.cp("dve", Wr, t[:, :, 0:36], [b], [bW])
        t, b = stg.next()
        S.dma("sp", t[0:1, 0, 0:36], self.b_rt[0:1, :], writes=[b])
        self.cp("dve", br_b[0:1, :], t[0:1, 0, 0:36], [b], [bW])
        S.dma("sp", Ltri, self.ltri[:, :], writes=[bW])
        self.memset("dve", Rr, 0.0, [bR])
        diag_r = Ring([A.alloc([128, 128], F32) for _ in range(2)])
        for i in range(2):
            for j in range(8):
                dg, bd = diag_r.next()
                self.ts("dve", dg, self.ident_f, V[:, 2 + i, j:j + 1], None, ALU.mult, None,
                        [self.b_ident_f, self.b_vecs], [bd])
                pt, bp = self.psum()
                self.mm(pt[:, 0:128], self.ones_f, dg, True, True, [self.b_ones_f, bd], [bp])
                self.cp("act", AB2[:, i, j * 128:(j + 1) * 128], pt[:, 0:128], [bp], [bW])
        S.barrier()
        A.reset(m1)
        oTr = Ring([A.alloc([128, 4, CH], BF16) for _ in range(2)])
        for_ = Ring([A.alloc([128, D], BF16) for _ in range(3)])
        sgr = Ring([A.alloc([128, 2 * D], BF16) for _ in range(3)])
        xr = Ring([A.alloc([128, D], F32) for _ in range(5)])
        m1r = Ring([A.alloc([128, D], BF16) for _ in range(2)])
        m2r = Ring([A.alloc([128, D], BF16) for _ in range(2)])
        mgr = Ring([A.alloc([128, D], BF16) for _ in range(2)])
        mTr = Ring([A.alloc([128, 8, 128], BF16) for _ in range(2)])
        h1r = Ring([A.alloc([128, D], F32) for _ in range(3)])
        hnr = Ring([A.alloc([128, D], F32) for _ in range(2)])
        vnr = Ring([A.alloc([128, D], BF16) for _ in range(3)])
        vTr = Ring([A.alloc([128, 8, 128], BF16) for _ in range(2)])
        junk = A.alloc([128, D], BF16); b_junk = Buf()
        stat = A.alloc([128, NT, 4], F32)
        lg4r = Ring([A.alloc([128, 4, 36], F32) for _ in range(2)])
        rs = A.alloc([128, 1024], F32); brs = Buf()

        st = {}
        chunk = {}
        lgs = {}

        def loads(t):
            c, s_ = divmod(t, 4)
            rows = slice(t * 128, (t + 1) * 128)
            if s_ == 0:
                oT, boT = oTr.next()
                for h in range(NH):
                    S.dma("sp", oT[(h % 2) * 64:(h % 2) * 64 + 64, h // 2, :], self.OT[h, :, c * CH:(c + 1) * CH],
                          writes=[boT])
                chunk[c] = (oT, boT)
            fo, bfo = for_.next()
            S.dma("sp", fo, self.FO[rows, :], writes=[bfo])
            sg, bsg = sgr.next()
            S.dma("sp", sg, self.SG[rows, :], writes=[bsg])
            xt, bx = xr.next()
            S.dma("sp", xt, self.x[rows, :], writes=[bx])
            st[t] = dict(fo=(fo, bfo), sg=(sg, bsg), x=(xt, bx))

        def stage1(t):
            c, s_ = divmod(t, 4)
            oT, boT = chunk[c]
            d = st[t]
            fo, bfo = d["fo"]; sg, bsg = d["sg"]
            ts_ = slice(s_ * 128, (s_ + 1) * 128)
            m1, bm1 = m1r.next()
            for hf in range(2):
                hs = slice(hf * 512, (hf + 1) * 512)
                ps, bps = self.psum()
                for j in range(4):
                    self.mm(ps[:, :], oT[:, j, ts_], Wba[:, j, hs], j == 0, j == 3, [boT, bW], [bps])
                self.tt("dve", m1[:, hs], ps[:, :], sg[:, hs], ALU.mult, [bps, bsg], [bm1])
            m2, bm2 = d["m2"]
            mg, bmg = mgr.next()
            self.tt("dve", mg, m1, m2, ALU.add, [bm1, bm2], [bmg])
            d["mg"] = (mg, bmg)

        def stage1p(t):
            d = st[t]
            fo, bfo = d["fo"]; sg, bsg = d["sg"]
            m2, bm2 = m2r.next()
            self.tt("pool", m2, fo, sg[:, D:2 * D], ALU.mult, [bfo, bsg], [bm2])
            d["m2"] = (m2, bm2)

        def stage2(t):
            d = st[t]
            mg, bmg = d["mg"]
            ps, bps = self.psum()
            pv = ps[:, :].bitcast(BF16).rearrange("p (k n) -> p k n", k=8)
            for k in range(8):
                self.tr(pv[:, k, :], mg[:, k * 128:(k + 1) * 128], self.ident_b, [bmg, self.b_ident_b], [bps])
            mT, bmT = mTr.next()
            self.cp("act", mT, pv, [bps], [bmT])
            d["mT"] = (mT, bmT)

        def stage3(t):
            d = st[t]
            mT, bmT = d["mT"]
            xt, bx = d["x"]
            rows = slice(t * 128, (t + 1) * 128)
            h1, bh1 = h1r.next()
            for hf in range(2):
                hs = slice(hf * 512, (hf + 1) * 512)
                ps, bps = self.psum()
                for k in range(8):
                    self.mm(ps[:, :], mT[:, k, :], Wout[:, k, hs], k == 0, k == 7, [bmT, bW], [bps])
                self.tt("dve", h1[:, hs], ps[:, :], xt[:, hs], ALU.add, [bps, bx], [bh1])
            self.defer(lambda: S.dma("sp", self.H1[rows, :], h1, reads=[bh1], writes=[Buf()]))
            bst = Buf()
            self.act(junk, h1, AF.Square, [bh1], [b_junk, bst], accum_out=stat[:, t, 0:1])
            self.ts("dve", stat[:, t, 1:2], stat[:, t, 0:1], 1.0 / D, EPS, ALU.mult, ALU.add, [bst], [bst])
            self.tt("pool", stat[:, t, 2:3], stat[:, t, 1:2], self.mhalf[:, 0:1], ALU.pow, [bst, self.b_mhalf], [bst])
            d["h1"] = (h1, bh1, bst)

        def stage4(t):
            d = st[t]
            h1, bh1, bst = d["h1"]
            rows = slice(t * 128, (t + 1) * 128)
            hn, bhn = hnr.next()
            self.act(hn, h1, AF.Copy, [bh1, bst], [bhn], scale=stat[:, t, 2:3])
            self.tt("dve", hn, hn, AB2[:, 0, :], ALU.mult, [bhn, bW], [bhn])
            vn, bvn = vnr.next()
            self.tt("pool", vn, hn, AB2[:, 1, :], ALU.add, [bhn, bW], [bvn])
            self.defer(lambda: S.dma("sp", self.VN[rows, :], vn, reads=[bvn], writes=[Buf()]))
            d["vn"] = (vn, bvn)

        def stage5(t):
            d = st[t]
            vn, bvn = d["vn"]
            ps, bps = self.psum()
            pv = ps[:, :].bitcast(BF16).rearrange("p (k n) -> p k n", k=8)
            for k in range(8):
                self.tr(pv[:, k, :], vn[:, k * 128:(k + 1) * 128], self.ident_b, [bvn, self.b_ident_b], [bps])
            vT, bvT = vTr.next()
            self.cp("act", vT, pv, [bps], [bvT])
            d["vT"] = (vT, bvT)

        def stageC(t):
            c, s_ = divmod(t, 4)
            d = st.pop(t)
            vT, bvT = d["vT"]
            if s_ == 0:
                lgs[c] = lg4r.next()
            lg4, blg = lgs[c]
            ps, bps = self.psum()
            for k in range(8):
                self.mm(ps[:, 0:36], vT[:, k, :], Wr[:, k, :], k == 0, False, [bvT, bW], [bps])
            self.mm(ps[:, 0:36], self.ones_b[0:1, :], br_b[0:1, :], False, True, [self.b_ones_b, bW], [bps])
            self.cp("dve", lg4[:, s_, :], ps[:, 0:36], [bps], [blg])
            if s_ == 3:
                pending.append(route(c))

        def bc(ap, shape, axis):
            return ap.unsqueeze(axis).broadcast_to(shape)

        def route(c):
            lg4, blg = lgs.pop(c)
            t0 = c * 4
            G = lg4[:, :, 0:4]
            E = lg4[:, :, 4:36].rearrange("p t (g e) -> p t g e", g=4)
            o = [0]

            def sc(n, shape):
                v = rs[:, o[0]:o[0] + n]
                o[0] += n
                if len(shape) == 2:
                    return v.rearrange("p (a b) -> p a b", a=shape[0])
                if len(shape) == 3:
                    return v.rearrange("p (a b c) -> p a b c", a=shape[0], b=shape[1])
                return v
            R_ = [blg, brs]
            W_ = [brs]
            dve = self.S.op
            gmax = sc(4, (4,))
            dve("dve", lambda e: e.reduce_max(out=gmax, in_=G, axis=AX.X), R_, W_)
            gm = sc(16, (4, 4))
            self.tt("dve", gm, G, bc(gmax, [128, 4, 4], 2), ALU.is_ge, R_, W_)
            gd = sc(16, (4, 4))
            self.tt("dve", gd, G, bc(gmax, [128, 4, 4], 2), ALU.subtract, R_, W_)
            ge = sc(16, (4, 4))
            self.act(ge, gd, AF.Exp, R_, W_)
            gsum = sc(4, (4,))
            dve("dve", lambda e: e.reduce_sum(out=gsum, in_=ge, axis=AX.X), R_, W_)
            tmp = sc(128, (4, 4, 8))
            self.tt("dve", tmp, E, bc(gm, [128, 4, 4, 8], 3), ALU.mult, R_, W_)
            esel = sc(32, (4, 8))
            dve("dve", lambda e: e.reduce_sum(out=esel, in_=tmp.rearrange("p t g e -> p t e g"), axis=AX.X), R_, W_)
            yield
            top8 = sc(32, (4, 8))
            for i in range(4):
                dve("dve", lambda e, i=i: e.max(out=top8[:, i, :], in_=esel[:, i, :]), R_, W_)
            sel = sc(32, (4, 8))
            self.tt("dve", sel, esel, top8[:, :, 1:2].broadcast_to([128, 4, 8]), ALU.is_ge, R_, W_)
            sel1 = sc(32, (4, 8))
            self.tt("dve", sel1, esel, top8[:, :, 0:1].broadcast_to([128, 4, 8]), ALU.is_ge, R_, W_)
            sel2 = sc(32, (4, 8))
            self.tt("dve", sel2, sel, sel1, ALU.subtract, R_, W_)
            ed = sc(32, (4, 8))
            self.tt("dve", ed, esel, top8[:, :, 0:1].broadcast_to([128, 4, 8]), ALU.subtract, R_, W_)
            ex = sc(32, (4, 8))
            self.act(ex, ed, AF.Exp, R_, W_)
            yield
            sx = sc(32, (4, 8))
            self.tt("dve", sx, sel, ex, ALU.mult, R_, W_)
            den = sc(4, (4,))
            dve("dve", lambda e: e.reduce_sum(out=den, in_=sx, axis=AX.X), R_, W_)
            gd2 = sc(4, (4,))
            self.tt("dve", gd2, gsum, den, ALU.mult, R_, W_)
            coef = sc(4, (4,))
            dve("dve", lambda e: e.reciprocal(out=coef, in_=gd2), R_, W_)
            w8 = sc(32, (4, 8))
            self.tt("dve", w8, sx, bc(coef, [128, 4, 8], 2), ALU.mult, R_, W_)
            tw = sc(32, (4, 8))
            self.tt("dve", tw, sel1, w8, ALU.mult, R_, W_)
            dve("dve", lambda e: e.reduce_sum(out=self.w12[:, t0:t0 + 4, 0], in_=tw, axis=AX.X), R_, [brs, self.b_w12])
            tw2 = sc(32, (4, 8))
            self.tt("dve", tw2, sel2, w8, ALU.mult, R_, W_)
            dve("dve", lambda e: e.reduce_sum(out=self.w12[:, t0:t0 + 4, 1], in_=tw2, axis=AX.X), R_, [brs, self.b_w12])
            yield
            M1 = M12[:, 0, t0:t0 + 4, :].rearrange("p t (g e) -> p t g e", g=4)
            M2 = M12[:, 1, t0:t0 + 4, :].rearrange("p t (g e) -> p t g e", g=4)
            self.tt("dve", M1, bc(gm, [128, 4, 4, 8], 3), bc(sel1, [128, 4, 4, 8], 2), ALU.mult, R_, [brs, bM])
            self.tt("dve", M2, bc(gm, [128, 4, 4, 8], 3), bc(sel2, [128, 4, 4, 8], 2), ALU.mult, R_, [brs, bM])
            Mt = rs[:, 512:512 + 64].bitcast(BF16).rearrange("p (t e) -> p t e", t=4)
            self.tt("dve", Mt, M12[:, 0, t0:t0 + 4, :], M12[:, 1, t0:t0 + 4, :], ALU.add, [bM, brs], W_)
            for i in range(4):
                ps, bps = self.psum()
                self.mm(ps[:, 0:32], Ltri, Mt[:, i, :], True, True, [bW, brs], [bps])
                self.mm(ps[:, 32:64], self.ones_b, Mt[:, i, :], True, True, [self.b_ones_b, brs], [bps])
                self.tt("dve", rank[:, t0 + i, :], ps[:, 0:32], Rr, ALU.add, [bps, bR], [brank])
                self.tt("dve", Rr, Rr, ps[:, 32:64], ALU.add, [bps, bR], [bR])
            yield

        loads(0)
        loads(1)
        stages = [stage1, stage2, stage3, stage4, stage5, stageC]
        order = [3, 2, 0, 1, 4, 5]
        pending = []

        def pump():
            for g_ in list(pending):
                try:
                    next(g_)
                except StopIteration:
                    pending.remove(g_)
        for i in range(NT + len(stages) - 1):
            if i + 2 < NT:
                loads(i + 2)
            self.flush()
            if i < NT:
                stage1p(i)
            for k_ in order:
                if 0 <= i - k_ < NT:
                    stages[k_](i - k_)
            pump()
        while pending:
            pump()
        self.flush()
        S.barrier()
        A.reset(m0)
        cnt = A.alloc([128, 32], F32)
        ci = A.alloc([128, 32], I32)
        pad = A.alloc([128, 32], F32)
        cs = [A.alloc([128, 32], F32) for _ in range(2)]
        base = A.alloc([128, 32], F32)
        sf = A.alloc([128, NT, 32], F32)
        prod = A.alloc([128, NT, 32], F32)
        slf = A.alloc([128, 2, NT], F32)
        tst = A.alloc([128, NST], F32)
        acc = A.alloc([128, NST], F32)
        pio = A.alloc([128, 1], F32)
        bb = Buf()
        RW = ([bb, bR, brank, bM], [bb])
        S.dma("sp", tst, self.tstart[:, :], writes=[bb])
        S.dma("sp", pio, self.piota[:, :], writes=[bb])
        self.ts("dve", cnt, Rr, float(TR - 1), 1.0 / TR, ALU.add, ALU.mult, *RW)
        self.ts("dve", ci, cnt, -0.5 + 0.5 / TR, None, ALU.add, None, *RW)
        self.cp("dve", pad, ci, *RW)
        self.ts("dve", pad, pad, float(TR), None, ALU.mult, None, *RW)
        cur = cs[0]
        self.cp("dve", cur, pad, *RW)
        k = 0
        for sh in (1, 2, 4, 8, 16):
            nxt = cs[1 - k]
            self.cp("dve", nxt[:, 0:sh], cur[:, 0:sh], *RW)
            self.tt("dve", nxt[:, sh:32], cur[:, sh:32], cur[:, 0:32 - sh], ALU.add, *RW)
            cur = nxt
            k = 1 - k
        bend = cur
        self.tt("dve", base, bend, pad, ALU.subtract, *RW)
        self.tt("dve", sf, rank, base.unsqueeze(1).broadcast_to([128, NT, 32]), ALU.add, *RW)
        for q in range(2):
            self.tt("dve", prod, sf, M12[:, q, :, :], ALU.mult, *RW)
            self.S.op("dve", lambda e, q=q: e.reduce_sum(out=slf[:, q, :], in_=prod, axis=AX.X), *RW)
        self.cp("dve", self.slot_i, slf, RW[0], [bb, self.b_slot])
        self.memset("dve", acc, 0.0, [bb])
        for e_ in range(32):
            self.stt(acc, tst, bend[:, e_:e_ + 1], acc, ALU.is_ge, ALU.add, *RW)
        self.ts("dve", acc, acc, 31.0, 128.0, ALU.min, ALU.mult, *RW)
        self.ts("dve", acc, acc, pio[:, 0:1], None, ALU.add, None, *RW)
        self.cp("dve", self.idxw, acc, RW[0], [bb, self.b_idxw])
        if "RT" in self.dbg:
            S.dma("sp", self.RT[:, 0:128], slf.rearrange("p a b -> p (a b)"), reads=[bb], writes=[Buf()])
            S.dma("sp", self.RT[:, 128:256], self.w12.rearrange("p a b -> p (a b)"), reads=[bb, self.b_w12], writes=[Buf()])
            S.dma("sp", self.RT[:, 256:256 + NST], acc, reads=[bb], writes=[Buf()])
            S.dma("sp", self.RT[:, 512:544], Rr, reads=[bb, bR], writes=[Buf()])
            S.dma("sp", self.RT[:, 544:576], bend, reads=[bb], writes=[Buf()])
        S.barrier()
        A.reset(self.m_persist)

    def idma(self, out, in_, idx, gather, reads, writes):
        if gather:
            fn = lambda e: e.indirect_dma_start(out=out, out_offset=None, in_=in_,
                                                in_offset=bass.IndirectOffsetOnAxis(ap=idx, axis=0))
        else:
            fn = lambda e: e.indirect_dma_start(out=out, out_offset=bass.IndirectOffsetOnAxis(ap=idx, axis=0),
                                                in_=in_, in_offset=None)
        return self.S.op("pool", fn, reads, writes, is_dma=True)

    def phase4(self):
        S, A = self.S, self.A
        m0 = A.mark()
        bwgu = self.b_wgu; bwd = self.b_wd
        vr = Ring([A.alloc([128, D], BF16) for _ in range(3)])
        bxs = Buf()
        for t in range(NT):
            v, bv = vr.next()
            S.dma("sp", v, self.VN[t * 128:(t + 1) * 128, :], writes=[bv])
            for q in range(2):
                self.idma(self.XS[:, :], v, self.slot_i[:, q, t:t + 1], False, [bv, self.b_slot], [Buf()])
        S.barrier()
        xsr = Ring([A.alloc([128, D], BF16) for _ in range(10)])
        wgr = Ring([A.alloc([128, 8, 512], BF16) for _ in range(5)])
        wdr = Ring([A.alloc([128, 2, D], BF16) for _ in range(5)])
        xTr = Ring([A.alloc([128, 8, 128], BF16) for _ in range(2)])
        sgl = Ring([A.alloc([128, 256], F32) for _ in range(2)])
        hdr = Ring([A.alloc([128, 256], BF16) for _ in range(2)])
        hTr = Ring([A.alloc([128, 2, 128], BF16) for _ in range(2)])
        ysr = Ring([A.alloc([128, D], BF16) for _ in range(3)])
        ld = {}

        def loads(s_):
            wg, bwg = wgr.next()
            self.idma(wg.rearrange("p k f -> p (k f)"), self.WGU[:, :], self.idxw[:, s_:s_ + 1], True,
                      [bwgu, self.b_idxw], [bwg])
            wd, bwd_ = wdr.next()
            self.idma(wd.rearrange("p j d -> p (j d)"), self.WD[:, :], self.idxw[:, s_:s_ + 1], True,
                      [bwd, self.b_idxw], [bwd_])
            xl = []
            for u in range(TR // 128):
                xs, bx = xsr.next()
                r0 = s_ * TR + u * 128
                S.dma("sp", xs, self.XS[r0:r0 + 128, :], reads=[bxs], writes=[bx])
                xl.append((xs, bx))
            ld[s_] = (xl, wg, bwg, wd, bwd_)

        SUB = TR // 128
        NU = NST * SUB
        stt_ = {}

        def e1(u):
            xl, wg, bwg, wd, bwd_ = ld[u // SUB]
            xs, bx = xl[u % SUB]
            ps, bps = self.psum()
            pv = ps[:, :].bitcast(BF16).rearrange("p (k n) -> p k n", k=8)
            for k in range(8):
                self.tr(pv[:, k, :], xs[:, k * 128:(k + 1) * 128], self.ident_b, [bx, self.b_ident_b], [bps])
            xT, bxT = xTr.next()
            self.cp("act", xT, pv, [bps], [bxT])
            stt_[u] = dict(xT=(xT, bxT))

        def e2(u):
            xl, wg, bwg, wd, bwd_ = ld[u // SUB]
            xT, bxT = stt_[u]["xT"]
            ps, bps = self.psum()
            for k in range(8):
                self.mm(ps[:, :], xT[:, k, :], wg[:, k, :], k == 0, k == 7, [bxT, bwg], [bps])
            sg, bsg = sgl.next()
            self.act(sg, ps[:, 0:256], AF.Silu, [bps], [bsg])
            hd, bhd = hdr.next()
            self.tt("dve", hd, ps[:, 256:512], sg, ALU.mult, [bps, bsg], [bhd])
            stt_[u]["hd"] = (hd, bhd)

        def e3(u):
            hd, bhd = stt_[u]["hd"]
            ps2, bps2 = self.psum()
            pv2 = ps2[:, 0:128].bitcast(BF16).rearrange("p (k n) -> p k n", k=2)
            for j in range(2):
                self.tr(pv2[:, j, :], hd[:, j * 128:(j + 1) * 128], self.ident_b, [bhd, self.b_ident_b], [bps2])
            hT, bhT = hTr.next()
            self.cp("act", hT, pv2, [bps2], [bhT])
            stt_[u]["hT"] = (hT, bhT)

        def e4(u):
            xl, wg, bwg, wd, bwd_ = ld[u // SUB]
            hT, bhT = stt_.pop(u)["hT"]
            r0 = u * 128
            ys, bys = ysr.next()
            for hf in range(2):
                hs = slice(hf * 512, (hf + 1) * 512)
                ps3, bps3 = self.psum()
                for j in range(2):
                    self.mm(ps3[:, :], hT[:, j, :], wd[:, j, hs], j == 0, j == 1, [bhT, bwd_], [bps3])
                self.tt("dve", ys[:, hs], ps3[:, :], self.bc[:, 1, hs], ALU.mult, [bps3, self.b_bc], [bys])
            self.defer(lambda: S.dma("sp", self.YS2[r0:r0 + 128, :], ys, reads=[bys], writes=[Buf()]))
            if u % SUB == SUB - 1:
                ld.pop(u // SUB)

        loads(0)
        loads(1)
        est = [e1, e2, e3, e4]
        eorder = [2, 0, 3, 1]
        for i in range(NU + 3):
            if i % SUB == 0 and i // SUB + 2 < NST:
                loads(i // SUB + 2)
            self.flush()
            for k_ in eorder:
                if 0 <= i - k_ < NU:
                    est[k_](i - k_)
        self.flush()
        S.barrier()
        A.reset(m0)

    def phase5(self):
        S, A = self.S, self.A
        m0 = A.mark()
        y1r = Ring([A.alloc([128, D], BF16) for _ in range(3)])
        y2r = Ring([A.alloc([128, D], BF16) for _ in range(3)])
        h1r = Ring([A.alloc([128, D], F32) for _ in range(3)])
        mr = Ring([A.alloc([128, D], F32) for _ in range(2)])
        h2r = Ring([A.alloc([128, D], F32) for _ in range(4)])
        outr = Ring([A.alloc([128, D], F32) for _ in range(4)])
        junk = A.alloc([128, D], BF16); b_junk = Buf()
        stat = A.alloc([128, NT, 4], F32)
        ld = {}

        def loads(t):
            y1, by1 = y1r.next()
            self.idma(y1, self.YS2[:, :], self.slot_i[:, 0, t:t + 1], True, [self.b_ys2, self.b_slot], [by1])
            y2, by2 = y2r.next()
            self.idma(y2, self.YS2[:, :], self.slot_i[:, 1, t:t + 1], True, [self.b_ys2, self.b_slot], [by2])
            h1, bh1 = h1r.next()
            S.dma("sp", h1, self.H1[t * 128:(t + 1) * 128, :], writes=[bh1])
            ld[t] = (y1, by1, y2, by2, h1, bh1)

        mid = {}
        mid0 = {}

        def s1(t):
            y1, by1, y2, by2, h1, bh1 = ld.pop(t)
            m, bm = mr.next()
            self.act(m, y1, AF.Copy, [by1, self.b_w12], [bm], scale=self.w12[:, t, 0:1])
            self.stt(m, y2, self.w12[:, t, 1:2], m, ALU.mult, ALU.add, [by2, self.b_w12, bm], [bm])
            h2, bh2 = h2r.next()
            self.tt("pool", h2, m, h1, ALU.add, [bm, bh1], [bh2])
            mid0[t] = (h2, bh2)

        def s1b(t):
            h2, bh2 = mid0.pop(t)
            bst = Buf()
            self.act(junk, h2, AF.Square, [bh2], [b_junk, bst], accum_out=stat[:, t, 0:1])
            self.ts("dve", stat[:, t, 1:2], stat[:, t, 0:1], 1.0 / D, EPS, ALU.mult, ALU.add, [bst], [bst])
            self.tt("pool", stat[:, t, 2:3], stat[:, t, 1:2], self.mhalf[:, 0:1], ALU.pow, [bst, self.b_mhalf], [bst])
            mid[t] = (h2, bh2, bst)

        def s2(t):
            h2, bh2, bst = mid.pop(t)
            o, bo = outr.next()
            self.stt(o, h2, stat[:, t, 2:3], self.bc[:, 2, :], ALU.mult, ALU.mult, [bh2, bst, self.b_bc], [bo])
            self.tt("dve", o, o, self.bc[:, 3, :], ALU.add, [bo, self.b_bc], [bo])
            self.defer(lambda: S.dma("sp", self.out[t * 128:(t + 1) * 128, :], o, reads=[bo], writes=[Buf()]))

        loads(0)
        loads(1)
        for i in range(NT + 2):
            if i + 2 < NT:
                loads(i + 2)
            self.flush()
            if 0 <= i - 2 < NT:
                s2(i - 2)
            if 0 <= i - 1 < NT:
                s1b(i - 1)
            if i < NT:
                s1(i)
        self.flush()
        S.barrier()
        A.reset(m0)


def host_inputs(inputs, b):
    f32 = np.float32

    def fm(v):
        v = np.asarray(v, f32).reshape(-1, 128)
        return np.ascontiguousarray(v.T)
    invf = np.zeros((128, 1), f32)
    inv = (10000.0 ** (-np.arange(0, 32, 2, dtype=f32) / 32)).astype(f32)
    for p in range(64, 96):
        invf[p, 0] = inv[(p - 64) % 16]
    m = {
        "x": np.ascontiguousarray(inputs["x"][b]),
        "ct": fm(inputs["c"][b]),
        "pos": np.ascontiguousarray(inputs["positions"][b].reshape(1, S_TOK).astype(np.int32)),
        "w_ada": np.ascontiguousarray(inputs["w_ada"][0]),
        "b_ada_t": fm(inputs["b_ada"][0]),
        "w_adaf": np.ascontiguousarray(inputs["w_ada_final"]),
        "b_adaf_t": fm(inputs["b_ada_final"]),
        "gmix_t": fm(inputs["g_norm_mix"][0]),
        "gffn_t": fm(inputs["g_norm_ffn"][0]),
        "gfin_t": fm(inputs["g_norm_final"]),
        "w_in": np.ascontiguousarray(inputs["w_in"][0]),
        "glat_t": np.ascontiguousarray(np.concatenate([fm(inputs["g_q_lat"][0]), fm(inputs["g_kv_lat"][0])], axis=1)),
        "w_uq": np.ascontiguousarray(inputs["w_uq"][0].reshape(256, 768)),
        "w_uk": np.ascontiguousarray(inputs["w_uk"][0].reshape(256, 512)),
        "w_uv": np.ascontiguousarray(inputs["w_uv"][0].reshape(256, 512)),
        "b_gates": np.ascontiguousarray(inputs["b_gates"][0].reshape(1, 2048)),
        "ident": np.eye(128, dtype=f32),
        "invf": invf,
        "w_bf": np.ascontiguousarray(inputs["w_branch_fourier"][0]),
        "w_ba": np.ascontiguousarray(inputs["w_branch_attn"][0]),
        "w_out": np.ascontiguousarray(inputs["w_out"][0]),
        "w_rt": np.ascontiguousarray(np.concatenate([inputs["w_router_group"][0],
                                                     inputs["w_router_expert"][0].reshape(D, 32)], axis=1)),
        "b_rt": np.ascontiguousarray(np.concatenate([inputs["b_router_group"][0].reshape(1, 4),
                                                     inputs["b_router_expert"][0].reshape(1, 32)], axis=1)),
        "w_eg": np.ascontiguousarray(inputs["w_expert_gate"][0]),
        "w_eu": np.ascontiguousarray(inputs["w_expert_up"][0]),
        "w_ed": np.ascontiguousarray(inputs["w_expert_down"][0]),
    }
    m.update(CONSTS)
    return m


def _make_consts():
    bf = ml_dtypes.bfloat16
    n1 = np.arange(128)[None, :, None]
    n2 = np.arange(64)[:, None, None]
    k1 = np.arange(128)[None, None, :]
    ph = (k1 * (64 * n1 + n2)) % 8192
    al = 2.0 * np.pi * ph / 8192.0
    sc = 1.0 / math.sqrt(8192.0)
    tre = (np.cos(al) * sc).astype(bf)
    tim = (-np.sin(al) * sc).astype(bf)
    a = np.arange(64)
    th = 2.0 * np.pi * ((a[:, None] * a[None, :]) % 64) / 64.0
    c, s_ = np.cos(th), np.sin(th)
    d2 = np.zeros((128, 128))
    d2[0:64, 0:64] = c
    d2[0:64, 64:128] = s_
    d2[64:128, 0:64] = s_
    d2[64:128, 64:128] = -c
    bdc = np.zeros((128, 128), np.float32)
    bds = np.zeros((128, 128), np.float32)
    for g in range(2):
        bdc[g * 64:(g + 1) * 64, g * 64:(g + 1) * 64] = c / 8.0
        bds[g * 64:(g + 1) * 64, g * 64:(g + 1) * 64] = -s_ / 8.0
    ltri = np.triu(np.ones((128, 128), np.float32), 1).astype(bf)
    tstart = np.tile((np.arange(NST, dtype=np.float32) * float(TR))[None, :], (128, 1))
    piota = np.arange(128, dtype=np.float32).reshape(128, 1)
    return {"tre": tre, "tim": tim, "d2": d2.astype(bf), "bdc": bdc, "bds": bds, "ltri": ltri,
            "tstart": np.ascontiguousarray(tstart), "piota": piota}


CONSTS = _make_consts()


def kernel(**inputs):
    bld = Builder()
    nc = bld.build()
    in_maps = [host_inputs(inputs, b) for b in range(8)]
    res = run_bass_kernel_spmd(nc, in_maps, core_ids=list(range(8)))
    return np.stack([np.asarray(r["out"]) for r in res.results], axis=0).astype(np.float32)
```

```python
import math
from contextlib import ExitStack

import numpy as np
import ml_dtypes

import concourse.bass as bass
import concourse.mybir as mybir
from concourse.bass_utils import run_bass_kernel_spmd

F32 = mybir.dt.float32
BF16 = mybir.dt.bfloat16
I32 = mybir.dt.int32
U32 = mybir.dt.uint32
U8 = mybir.dt.uint8
AF = mybir.ActivationFunctionType
ALU = mybir.AluOpType
AX = mybir.AxisListType
DTSIZE = {F32: 4, BF16: 2, I32: 4, U32: 4, U8: 1}

S_TOK = 8192
D = 1024
NT = 64
NCH = 16
CH = 512
NH = 8
EPS = 1e-6
TWO_PI = 2.0 * math.pi
CW1 = 6.28125
CW2 = TWO_PI - CW1
ATT_SCALE = 96 ** -0.5
TR = 256
NST = 96
P1_ORDER = [4, 3, 2, 1, 0]
P3_ORDER = [3, 2, 0, 1, 4, 5]
E_ORDER = [2, 0, 3, 1]
NSLOT = NST * TR


class Buf:
    __slots__ = ("name", "last_w", "readers")

    def __init__(self, name=""):
        self.name = name
        self.last_w = None
        self.readers = []


class Op:
    __slots__ = ("eng", "fn", "deps", "signal", "sig_idx", "is_dma", "dma_sem", "dma_val", "prev_dma")

    def __init__(self, eng, fn, is_dma=False):
        self.eng = eng
        self.fn = fn
        self.deps = []
        self.signal = False
        self.sig_idx = 0
        self.is_dma = is_dma
        self.dma_sem = None
        self.dma_val = 0
        self.prev_dma = None


ENGS = ("pe", "act", "dve", "pool", "sp")
DMA_RING = 8


def _nop(eng):
    return eng.nop()


_nop.is_nop = True


class Sched:
    def __init__(self, nc, same_engine_sync=True):
        self.nc = nc
        self.ops = {e: [] for e in ENGS}
        self.same = same_engine_sync
        self.dma_ops = {e: [] for e in ENGS}

    def op(self, eng, fn, reads=(), writes=(), is_dma=False):
        o = Op(eng, fn, is_dma)
        for b in reads:
            p = b.last_w
            if p is not None:
                if (not p.is_dma) and p.eng == eng and not is_dma:
                    if self.same and eng != "pe":
                        o.deps.append(p)
                else:
                    o.deps.append(p)
        for b in writes:
            p = b.last_w
            if p is not None and (p.is_dma or is_dma or p.eng != eng):
                o.deps.append(p)
            for r in b.readers:
                if r.is_dma or is_dma or r.eng != eng:
                    o.deps.append(r)
        for b in reads:
            b.readers.append(o)
        for b in writes:
            b.last_w = o
            b.readers = []
        if is_dma:
            lst = self.dma_ops[eng]
            n = len(lst)
            if n >= DMA_RING:
                o.prev_dma = lst[n - DMA_RING]
            o.dma_val = 16 * (n // DMA_RING + 1)
            o.dma_sem = n % DMA_RING
            lst.append(o)
        self.ops[eng].append(o)
        return o

    def dma(self, queue, out, in_, reads=(), writes=(), **kw):
        return self.op(queue, lambda e: e.dma_start(out=out, in_=in_, **kw), reads, writes, is_dma=True)

    def barrier(self):
        lasts = []
        for e in ENGS:
            if self.ops[e]:
                for o in reversed(self.ops[e]):
                    if not o.is_dma and not getattr(o.fn, "is_nop", False):
                        lasts.append(o)
                        break
            lasts.extend(self.dma_ops[e][-DMA_RING:])
        for e in ENGS:
            if True:
                o = self.op(e, _nop)
                for p in lasts:
                    if p.is_dma or p.eng != e:
                        o.deps.append(p)

    def finish(self):
        o = self.op("sp", _nop)
        for e in ENGS:
            o.deps.extend(self.dma_ops[e][-DMA_RING:])

    def emit(self, stack):
        nc = self.nc
        for e in ENGS:
            for o in self.ops[e]:
                for p in o.deps:
                    if not p.is_dma:
                        p.signal = True
        for e in ENGS:
            c = 0
            for o in self.ops[e]:
                if o.signal and not o.is_dma:
                    c += 1
                    o.sig_idx = c
        sems = {e: stack.enter_context(nc.semaphore("s_" + e)) for e in ENGS}
        dsems = {e: [stack.enter_context(nc.semaphore("d_%s_%d" % (e, i))) for i in range(DMA_RING)]
                 for e in ENGS if self.dma_ops[e]}
        block = stack.enter_context(nc.Block())
        engobj = {"pe": "tensor", "act": "scalar", "dve": "vector", "pool": "gpsimd", "sp": "sync"}

        def make(ename):
            ops = self.ops[ename]

            def body(eng):
                waited = {}
                dwaited = {}
                for o in ops:
                    deps = list(o.deps)
                    if o.prev_dma is not None:
                        deps.append(o.prev_dma)
                    for p in deps:
                        if p.is_dma:
                            key = (p.eng, p.dma_sem)
                            if dwaited.get(key, 0) >= p.dma_val:
                                continue
                            eng.wait_ge(dsems[p.eng][p.dma_sem], p.dma_val)
                            dwaited[key] = p.dma_val
                        else:
                            if waited.get(p.eng, 0) >= p.sig_idx:
                                continue
                            eng.wait_ge(sems[p.eng], p.sig_idx)
                            waited[p.eng] = p.sig_idx
                    ins = o.fn(eng)
                    if o.is_dma:
                        ins.then_inc(dsems[ename][o.dma_sem], 16)
                    elif o.signal:
                        ins.then_inc(sems[ename], 1)
            return body

        for ename in ENGS:
            if self.ops[ename]:
                getattr(block, engobj[ename])(make(ename))


class Arena:
    def __init__(self, tensor, size):
        self.t = tensor
        self.size = size
        self.off = 0

    def alloc(self, shape, dt):
        n = 1
        for s in shape[1:]:
            n *= s
        nbytes = n * DTSIZE[dt]
        off = (self.off + 63) // 64 * 64
        assert off + nbytes <= self.size, "arena overflow %d + %d > %d" % (off, nbytes, self.size)
        self.off = off + nbytes
        v = self.t[:, off:off + nbytes].bitcast(dt)
        if len(shape) > 2:
            names = ["d%d" % i for i in range(len(shape) - 1)]
            pat = "p (" + " ".join(names) + ") -> p " + " ".join(names)
            kw = {names[i]: shape[i + 1] for i in range(len(names))}
            v = v.rearrange(pat, **kw)
        return v

    def mark(self):
        return self.off

    def reset(self, m):
        self.off = m


class Ring:
    def __init__(self, items):
        self.items = items
        self.bufs = [Buf() for _ in items]
        self.i = 0

    def next(self):
        i = self.i
        self.i = (i + 1) % len(self.items)
        return self.items[i], self.bufs[i]


class Builder:
    def __init__(self, dbg=(), stop_after=None):
        self.dbg = set(dbg)
        self.stop_after = stop_after
        self.nc = bass.Bass("TRN2", target_bir_lowering=False)
        self.S = Sched(self.nc)
        self.deferred = []

    def din(self, name, shape, dt=F32):
        return self.nc.dram_tensor(name, list(shape), dt, kind="ExternalInput").ap()

    def scratch(self, name, shape, dt):
        kind = "ExternalOutput" if name in self.dbg else "Internal"
        return self.nc.dram_tensor(name, list(shape), dt, kind=kind).ap()

    def mm(self, out, lhsT, rhs, start, stop, reads, writes):
        return self.S.op("pe", lambda e: e.matmul(out, lhsT=lhsT, rhs=rhs, start=start, stop=stop), reads, writes)

    def tr(self, out, in_, ident, reads, writes):
        return self.S.op("pe", lambda e: e.transpose(out=out, in_=in_, identity=ident), reads, writes)

    def act(self, out, in_, func, reads, writes, **kw):
        return self.S.op("act", lambda e: e.activation(out=out, in_=in_, func=func, **kw), reads, writes)

    def ts(self, eng, out, in0, s1, s2, op0, op1, reads, writes):
        if op1 is None:
            return self.S.op(eng, lambda e: e.tensor_scalar(out=out, in0=in0, scalar1=s1, scalar2=None, op0=op0), reads, writes)
        return self.S.op(eng, lambda e: e.tensor_scalar(out=out, in0=in0, scalar1=s1, scalar2=s2, op0=op0, op1=op1), reads, writes)

    def tt(self, eng, out, in0, in1, op, reads, writes):
        return self.S.op(eng, lambda e: e.tensor_tensor(out=out, in0=in0, in1=in1, op=op), reads, writes)

    def stt(self, out, in0, scalar, in1, op0, op1, reads, writes):
        return self.S.op("dve", lambda e: e.scalar_tensor_tensor(out=out, in0=in0, scalar=scalar, in1=in1, op0=op0, op1=op1), reads, writes)

    def cp(self, eng, out, in_, reads, writes):
        if eng == "act":
            return self.S.op("act", lambda e: e.copy(out=out, in_=in_), reads, writes)
        return self.S.op(eng, lambda e: e.tensor_copy(out=out, in_=in_), reads, writes)

    def memset(self, eng, ap, val, writes):
        return self.S.op(eng, lambda e: e.memset(ap, val), (), writes)

    def psum(self):
        return self.pring.next()

    def defer(self, fn):
        self.deferred.append(fn)

    def flush(self):
        d, self.deferred = self.deferred, []
        for f in d:
            f()

    def build(self):
        nc = self.nc
        S = self.S
        with ExitStack() as st:
            self.st = st
            self.b_ys2 = Buf()
            self.b_wgu = Buf()
            self.b_wd = Buf()
            arena_t = st.enter_context(nc.sbuf_tensor("arena", [128, 200 * 1024], U8))
            self.A = Arena(arena_t, 200 * 1024)
            pts = [st.enter_context(nc.psum_tensor("ps%d" % i, [128, 512], F32)) for i in range(8)]
            self.pring = Ring(pts)
            self.declare_io()
            self.phase0()
            if self.stop_after != "p0":
                self.phase1()
            if self.stop_after not in ("p0", "p1"):
                S.barrier()
                self.phase2b()
            if self.stop_after not in ("p0", "p1", "p2b") and "noatt" not in self.dbg:
                S.barrier()
                self.phase2a()
            if self.stop_after not in ("p0", "p1", "p2b", "p2a"):
                S.barrier()
                self.phase3()
            if self.stop_after not in ("p0", "p1", "p2b", "p2a", "p3"):
                self.phase4()
            if self.stop_after not in ("p0", "p1", "p2b", "p2a", "p3", "p4"):
                self.phase5()
            S.barrier()
            S.finish()
            S.emit(st)
        return nc

    def declare_io(self):
        self.x = self.din("x", [S_TOK, D])
        self.ct = self.din("ct", [128, 8])
        self.pos = self.din("pos", [1, S_TOK], I32)
        self.w_ada = self.din("w_ada", [D, 6 * D])
        self.b_ada_t = self.din("b_ada_t", [128, 48])
        self.w_adaf = self.din("w_adaf", [D, 2 * D])
        self.b_adaf_t = self.din("b_adaf_t", [128, 16])
        self.gmix_t = self.din("gmix_t", [128, 8])
        self.gffn_t = self.din("gffn_t", [128, 8])
        self.gfin_t = self.din("gfin_t", [128, 8])
        self.w_in = self.din("w_in", [D, 3104])
        self.glat_t = self.din("glat_t", [128, 4])
        self.w_uq = self.din("w_uq", [256, 768])
        self.w_uk = self.din("w_uk", [256, 512])
        self.w_uv = self.din("w_uv", [256, 512])
        self.b_gates = self.din("b_gates", [1, 2048])
        self.ident = self.din("ident", [128, 128])
        self.invf = self.din("invf", [128, 1])
        self.out = self.nc.dram_tensor("out", [S_TOK, D], F32, kind="ExternalOutput").ap()
        self.US = self.scratch("US", [S_TOK, 512], BF16)
        self.SG = self.scratch("SG", [S_TOK, 2048], BF16)
        self.QT = self.scratch("QT", [NH, 96, S_TOK], BF16)
        self.KT = self.scratch("KT", [NH, 64, S_TOK], BF16)
        self.KTR = self.scratch("KTR", [32, S_TOK], BF16)
        self.VS = self.scratch("VS", [S_TOK, 512], BF16)
        self.DBG = self.scratch("DBG", [128, 4096], F32)
        self.OT = self.scratch("OT", [NH, 64, S_TOK], BF16)
        self.YS = self.scratch("YS", [128, 2, 64, 512], BF16)
        self.FO = self.scratch("FO", [S_TOK, D], BF16)
        self.H1 = self.scratch("H1", [S_TOK, D], F32)
        self.VN = self.scratch("VN", [S_TOK, D], BF16)
        self.XS = self.scratch("XS", [NSLOT, D], BF16)
        self.YS2 = self.scratch("YS2", [NSLOT, D], BF16)
        self.WGU = self.scratch("WGU", [32 * 128, 8 * 512], BF16)
        self.WD = self.scratch("WD", [32 * 128, 2 * D], BF16)
        self.RT = self.scratch("RT", [128, 1024], F32)
        self.w_ba = self.din("w_ba", [512, D])
        self.w_out = self.din("w_out", [D, D])
        self.w_rt = self.din("w_rt", [D, 36])
        self.b_rt = self.din("b_rt", [1, 36])
        self.ltri = self.din("ltri", [128, 128], BF16)
        self.tstart = self.din("tstart", [128, NST])
        self.piota = self.din("piota", [128, 1])
        self.w_eg = self.din("w_eg", [32, D, 256])
        self.w_eu = self.din("w_eu", [32, D, 256])
        self.w_ed = self.din("w_ed", [32, 256, D])
        self.tre = self.din("tre", [64, 128, 128], BF16)
        self.tim = self.din("tim", [64, 128, 128], BF16)
        self.d2 = self.din("d2", [128, 128], BF16)
        self.bdc = self.din("bdc", [128, 128])
        self.bds = self.din("bds", [128, 128])
        self.w_bf = self.din("w_bf", [512, D])

    def phase0(self):
        S, A = self.S, self.A
        self.ident_f = A.alloc([128, 128], F32); self.b_ident_f = Buf()
        self.ident_b = A.alloc([128, 128], BF16); self.b_ident_b = Buf()
        self.ones_f = A.alloc([128, 128], F32); self.b_ones_f = Buf()
        self.ones_b = A.alloc([128, 128], BF16); self.b_ones_b = Buf()
        self.modT = A.alloc([128, 64], F32); self.b_modT = Buf()
        self.vecs = A.alloc([128, 8, 8], F32); self.b_vecs = Buf()
        self.bc = A.alloc([128, 4, D], F32); self.b_bc = Buf()
        self.mhalf = A.alloc([128, 2], F32); self.b_mhalf = Buf()
        cact = A.alloc([128, 8, 2], F32); b_cact = Buf()
        ctt = A.alloc([128, 8], F32); b_ctt = Buf()
        small = A.alloc([128, 128], F32); b_small = Buf()
        self.small = small; self.b_small = b_small
        S.dma("sp", self.ident_f, self.ident[:, :], writes=[self.b_ident_f])
        S.dma("sp", ctt, self.ct[:, :], writes=[b_ctt])
        S.dma("sp", small[:, 0:48], self.b_ada_t[:, :], writes=[b_small])
        S.dma("sp", small[:, 48:64], self.b_adaf_t[:, :], writes=[b_small])
        S.dma("sp", small[:, 64:72], self.gmix_t[:, :], writes=[b_small])
        S.dma("sp", small[:, 72:80], self.gffn_t[:, :], writes=[b_small])
        S.dma("sp", small[:, 80:88], self.gfin_t[:, :], writes=[b_small])
        S.dma("sp", small[:, 88:92], self.glat_t[:, :], writes=[b_small])
        S.dma("sp", small[:, 92:93], self.invf[:, :], writes=[b_small])
        self.cp("dve", self.ident_b, self.ident_f, [self.b_ident_f], [self.b_ident_b])
        self.memset("pool", self.ones_f, 1.0, [self.b_ones_f])
        self.memset("pool", self.ones_b, 1.0, [self.b_ones_b])
        self.memset("pool", self.mhalf, -0.5, [self.b_mhalf])
        self.act(cact[:, :, 0], ctt, AF.Silu, [b_ctt], [b_cact])
        self.act(cact[:, :, 1], ctt, AF.Silu, [b_ctt], [b_cact])

        m0 = A.mark()
        wst = [A.alloc([128, 8, 1024], F32) for _ in range(2)]
        wring = Ring(wst)
        for piece in range(8):
            wt, bw = wring.next()
            if piece < 6:
                src = self.w_ada[:, piece * 1024:(piece + 1) * 1024]
            else:
                src = self.w_adaf[:, (piece - 6) * 1024:(piece - 5) * 1024]
            S.dma("sp", wt, src.rearrange("(k p) n -> p k n", p=128), writes=[bw])
            pt, bp = self.psum()
            pv = pt[:, 0:16].rearrange("p (a b) -> p a b", b=2)
            for jc in range(8):
                for k in range(8):
                    self.mm(pv[:, jc, :], wt[:, k, jc * 128:(jc + 1) * 128], cact[:, k, :], k == 0, k == 7,
                            [bw, b_cact], [bp])
            self.tt("dve", self.modT[:, piece * 8:(piece + 1) * 8], pv[:, :, 0], small[:, piece * 8:(piece + 1) * 8],
                    ALU.add, [bp, b_small], [self.b_modT])
        S.barrier()
        A.reset(m0)
        mT = self.modT
        V = self.vecs
        bv = self.b_vecs
        self.stt(V[:, 0, :], mT[:, 8:16], 1.0, small[:, 64:72], ALU.add, ALU.mult, [self.b_modT, b_small], [bv])
        self.cp("dve", V[:, 1, :], mT[:, 0:8], [self.b_modT], [bv])
        self.stt(V[:, 2, :], mT[:, 32:40], 1.0, small[:, 72:80], ALU.add, ALU.mult, [self.b_modT, b_small], [bv])
        self.cp("dve", V[:, 3, :], mT[:, 24:32], [self.b_modT], [bv])
        self.stt(V[:, 4, :], mT[:, 56:64], 1.0, small[:, 80:88], ALU.add, ALU.mult, [self.b_modT, b_small], [bv])
        diag_r = Ring([A.alloc([128, 128], F32) for _ in range(2)])
        srcs = [mT[:, 16:24], mT[:, 40:48], V[:, 4, :], mT[:, 48:56]]
        for i, sv in enumerate(srcs):
            for j in range(8):
                dg, bd = diag_r.next()
                self.ts("dve", dg, self.ident_f, sv[:, j:j + 1], None, ALU.mult, None,
                        [self.b_ident_f, self.b_modT, bv], [bd])
                pt, bp = self.psum()
                self.mm(pt[:, 0:128], self.ones_f, dg, True, True, [self.b_ones_f, bd], [bp])
                self.cp("act", self.bc[:, i, j * 128:(j + 1) * 128], pt[:, 0:128], [bp], [self.b_bc])

        self.m_persist = A.mark()
        self.cosT = A.alloc([128, S_TOK], BF16); self.b_cosT = Buf()
        self.sinT = A.alloc([128, S_TOK], BF16); self.b_sinT = Buf()
        m1 = A.mark()
        PW = 2048
        pi_r = Ring([A.alloc([128, PW], I32) for _ in range(2)])
        ang_r = Ring([A.alloc([128, PW], F32) for _ in range(2)])
        ki_r = Ring([A.alloc([128, PW], I32) for _ in range(1)])
        kf_r = Ring([A.alloc([128, PW], F32) for _ in range(1)])
        r_r = Ring([A.alloc([128, PW], F32) for _ in range(2)])
        ab_r = Ring([A.alloc([128, PW], F32) for _ in range(1)])
        R = slice(64, 96)
        invf = small[R, 92:93]
        for pc in range(S_TOK // PW):
            sl = slice(pc * PW, (pc + 1) * PW)
            pi, bpi = pi_r.next()
            S.dma("sp", pi[R, :], self.pos[0:1, sl].partition_broadcast(32), writes=[bpi])
            ang, bang = ang_r.next()
            self.cp("dve", ang[R, :], pi[R, :], [bpi], [bang])
            self.ts("dve", ang[R, :], ang[R, :], invf, None, ALU.mult, None, [bang, b_small], [bang])
            ki, bki = ki_r.next()
            self.ts("dve", ki[R, :], ang[R, :], 1.0 / TWO_PI, None, ALU.mult, None, [bang], [bki])
            kf, bkf = kf_r.next()
            self.cp("dve", kf[R, :], ki[R, :], [bki], [bkf])
            r, br = r_r.next()
            self.stt(r[R, :], kf[R, :], -CW1, ang[R, :], ALU.mult, ALU.add, [bkf, bang], [br])
            self.stt(r[R, :], kf[R, :], -CW2, r[R, :], ALU.mult, ALU.add, [bkf, br], [br])
            self.ts("dve", r[R, :], r[R, :], math.pi, -math.pi, ALU.min, ALU.max, [br], [br])
            self.act(self.sinT[R, sl], r[R, :], AF.Sin, [br], [self.b_sinT])
            ab, bab = ab_r.next()
            self.stt(ab[R, :], r[R, :], -1.0, r[R, :], ALU.mult, ALU.max, [br], [bab])
            self.act(self.cosT[R, sl], ab[R, :], AF.Sin, [bab], [self.b_cosT], scale=-1.0, bias=math.pi / 2)
        S.barrier()
        A.reset(m1)

        self.Wlat = A.alloc([128, 8, 512], BF16)
        self.Wkr = A.alloc([128, 8, 2, 96], BF16)
        self.Wf = A.alloc([128, 8, 512], BF16)
        self.Wg = A.alloc([128, 8, 2048], BF16)
        self.Wuq = A.alloc([128, 2, NH, 96], BF16)
        self.WuqR = A.alloc([128, 2, NH, 96], BF16)
        self.Wuk = A.alloc([128, 2, 512], BF16)
        self.Wuv = A.alloc([128, 2, 512], BF16)
        self.bg_b = A.alloc([128, 2048], BF16)
        self.b_W1 = Buf()
        bW = self.b_W1
        m2 = A.mark()
        stg = Ring([A.alloc([128, 8, 512], F32) for _ in range(2)])
        self.memset("pool", self.Wkr, 0.0, [bW])
        self.memset("pool", self.WuqR, 0.0, [bW])
        win = self.w_in

        def wsrc(c0, c1):
            return win[:, c0:c1].rearrange("(k p) n -> p k n", p=128)
        t, b = stg.next()
        S.dma("sp", t, wsrc(0, 512), writes=[b])
        self.cp("dve", self.Wlat, t, [b], [bW])
        t, b = stg.next()
        S.dma("sp", t[:, :, 0:32], wsrc(512, 544), writes=[b])
        self.cp("dve", self.Wkr[:, :, 0, 64:96], t[:, :, 0:32], [b], [bW])
        self.ts("dve", self.Wkr[:, :, 1, 64:80], t[:, :, 16:32], -1.0, None, ALU.mult, None, [b], [bW])
        self.cp("dve", self.Wkr[:, :, 1, 80:96], t[:, :, 0:16], [b], [bW])
        t, b = stg.next()
        S.dma("sp", t, wsrc(544, 1056), writes=[b])
        self.cp("act", self.Wf, t, [b], [bW])
        for g in range(4):
            t, b = stg.next()
            S.dma("sp", t, wsrc(1056 + g * 512, 1056 + (g + 1) * 512), writes=[b])
            self.cp("dve" if g % 2 == 0 else "act", self.Wg[:, :, g * 512:(g + 1) * 512], t, [b], [bW])
        tq2 = A.alloc([128, 2, 768], F32); btq2 = Buf()
        S.dma("sp", tq2, self.w_uq.rearrange("(j p) n -> p j n", p=128), writes=[btq2])
        tq2v = tq2.rearrange("p j (h d) -> p j h d", h=NH)
        self.cp("dve", self.Wuq, tq2v, [btq2], [bW])
        self.ts("dve", self.WuqR[:, :, :, 64:80], tq2v[:, :, :, 80:96], -1.0, None, ALU.mult, None, [btq2], [bW])
        self.cp("dve", self.WuqR[:, :, :, 80:96], tq2v[:, :, :, 64:80], [btq2], [bW])
        t, b = stg.next()
        S.dma("sp", t[:, 0:2, :], self.w_uk.rearrange("(j p) n -> p j n", p=128), writes=[b])
        self.cp("dve", self.Wuk, t[:, 0:2, :], [b], [bW])
        t, b = stg.next()
        S.dma("sp", t[:, 0:2, :], self.w_uv.rearrange("(j p) n -> p j n", p=128), writes=[b])
        self.cp("dve", self.Wuv, t[:, 0:2, :], [b], [bW])
        t, b = stg.next()
        tb = t[0:1, 0:4, :].rearrange("p a b -> p (a b)")
        S.dma("sp", tb, self.b_gates[0:1, :], writes=[b])
        self.cp("dve", self.bg_b[0:1, :], tb, [b], [bW])
        S.barrier()
        A.reset(m2)
        if "DBG" in self.dbg:
            S.dma("sp", self.DBG[:, 0:64], self.modT, reads=[self.b_modT], writes=[Buf()])
            S.dma("sp", self.DBG[:, 64:128], self.vecs.rearrange("p a b -> p (a b)"), reads=[self.b_vecs], writes=[Buf()])
            S.dma("sp", self.DBG[:, 1024:2048], self.bc[:, 0, :], reads=[self.b_bc], writes=[Buf()])
            S.dma("sp", self.DBG[:, 2048:3072], self.bc[:, 2, :], reads=[self.b_bc], writes=[Buf()])

    def phase1(self):
        S, A = self.S, self.A
        m0 = A.mark()
        bW = self.b_W1
        V = self.vecs
        small = self.small
        xr = Ring([A.alloc([128, D], F32) for _ in range(3)])
        junk = A.alloc([128, D], BF16); b_junk = Buf()
        xnr = Ring([A.alloc([128, D], BF16) for _ in range(2)])
        uTr = Ring([A.alloc([128, 8, CH], BF16) for _ in range(2)])
        latTr = Ring([A.alloc([128, 4, CH], BF16) for _ in range(2)])
        stat = A.alloc([128, NT, 4], F32)
        stat2 = A.alloc([128, NT, 4], F32)
        znr = Ring([A.alloc([128, 512], BF16) for _ in range(2)])
        vsr = Ring([A.alloc([128, 512], BF16) for _ in range(3)])
        usr = Ring([A.alloc([128, 512], BF16) for _ in range(3)])
        sgr = Ring([A.alloc([128, 2048], BF16) for _ in range(3)])
        qtr = Ring([A.alloc([128, CH], BF16) for _ in range(3)])
        ktr = Ring([A.alloc([128, CH], BF16) for _ in range(3)])
        t1r = Ring([A.alloc([128, CH], F32) for _ in range(2)])
        t2r = Ring([A.alloc([128, CH], F32) for _ in range(2)])
        xT = {}
        state = {}

        def load_x(t):
            xt, bx = xr.next()
            S.dma("sp", xt, self.x[t * 128:(t + 1) * 128, :], writes=[bx])
            xT[t] = (xt, bx)

        xns = {}
        zns = {}

        def stageA1(t):
            xt, bx = xT.pop(t)
            bst = Buf()
            self.act(junk, xt, AF.Square, [bx], [b_junk, bst], accum_out=stat[:, t, 0:1])
            self.ts("dve", stat[:, t, 1:2], stat[:, t, 0:1], 1.0 / D, EPS, ALU.mult, ALU.add, [bst], [bst])
            self.tt("pool", stat[:, t, 2:3], stat[:, t, 1:2], self.mhalf[:, 0:1], ALU.pow, [bst, self.b_mhalf], [bst])
            xn, bxn = xnr.next()
            self.act(xn, xt, AF.Copy, [bx, bst], [bxn], scale=stat[:, t, 2:3])
            xns[t] = (xn, bxn)

        def stageA2(t):
            c, s = divmod(t, 4)
            if s == 0:
                state[c] = (uTr.next(), latTr.next())
            (uT, buT), (latT, blatT) = state[c]
            xn, bxn = xns.pop(t)
            pt, bp = self.psum()
            pv = pt[:, :].bitcast(BF16).rearrange("p (k n) -> p k n", k=8)
            for k in range(8):
                self.tr(pv[:, k, :], xn[:, k * 128:(k + 1) * 128], self.ident_b, [bxn, self.b_ident_b], [bp])
            for k in range(8):
                self.ts("dve", uT[:, k, s * 128:(s + 1) * 128], pv[:, k, :], V[:, 0, k:k + 1], V[:, 1, k:k + 1],
                        ALU.mult, ALU.add, [bp, self.b_vecs], [buT])

        def stageB1(t):
            c, s = divmod(t, 4)
            (uT, buT), (latT, blatT) = state[c]
            ts_ = slice(s * 128, (s + 1) * 128)
            pt, bp = self.psum()
            for k in range(8):
                self.mm(pt[:, :], uT[:, k, ts_], self.Wlat[:, k, :], k == 0, k == 7, [buT, bW], [bp])
            bst = Buf()
            self.act(junk[:, 0:256], pt[:, 0:256], AF.Square, [bp], [b_junk, bst], accum_out=stat2[:, t, 0:1])
            self.act(junk[:, 256:512], pt[:, 256:512], AF.Square, [bp], [b_junk, bst], accum_out=stat2[:, t, 1:2])
            self.ts("dve", stat2[:, t, 0:2], stat2[:, t, 0:2], 1.0 / 256, EPS, ALU.mult, ALU.add, [bst], [bst])
            self.tt("pool", stat2[:, t, 2:4], stat2[:, t, 0:2], self.mhalf[:, 0:2], ALU.pow,
                    [bst, self.b_mhalf], [bst])
            zn, bzn = znr.next()
            self.ts("dve", zn[:, 0:256], pt[:, 0:256], stat2[:, t, 2:3], None, ALU.mult, None, [bp, bst], [bzn])
            self.act(zn[:, 256:512], pt[:, 256:512], AF.Copy, [bp, bst], [bzn], scale=stat2[:, t, 3:4])
            zns[t] = (zn, bzn)

        def stageB2(t):
            c, s = divmod(t, 4)
            (uT, buT), (latT, blatT) = state[c]
            ts_ = slice(s * 128, (s + 1) * 128)
            zn, bzn = zns.pop(t)
            pt2, bp2 = self.psum()
            pv2 = pt2[:, 0:256].bitcast(BF16).rearrange("p (k n) -> p k n", k=4)
            for j in range(4):
                self.tr(pv2[:, j, :], zn[:, j * 128:(j + 1) * 128], self.ident_b, [bzn, self.b_ident_b], [bp2])
            for j in range(4):
                self.ts("dve", latT[:, j, ts_], pv2[:, j, :], small[:, 88 + j:89 + j], None, ALU.mult, None,
                        [bp2, self.b_small], [blatT])

        def stageC(t):
            c, s = divmod(t, 4)
            (uT, buT), (latT, blatT) = state[c]
            ts_ = slice(s * 128, (s + 1) * 128)
            rows = slice(t * 128, (t + 1) * 128)
            pt, bp = self.psum()
            for j in range(2):
                self.mm(pt[:, :], latT[:, 2 + j, ts_], self.Wuv[:, j, :], j == 0, j == 1, [blatT, bW], [bp])
            vs, bvs = vsr.next()
            self.cp("act", vs, pt[:, :], [bp], [bvs])
            self.defer(lambda: S.dma("sp", self.VS[rows, :], vs, reads=[bvs], writes=[Buf()]))
            pt, bp = self.psum()
            for k in range(8):
                self.mm(pt[:, :], uT[:, k, ts_], self.Wf[:, k, :], k == 0, k == 7, [buT, bW], [bp])
            us, bus = usr.next()
            self.cp("dve", us, pt[:, :], [bp], [bus])
            self.defer(lambda: S.dma("sp", self.US[rows, :], us, reads=[bus], writes=[Buf()]))
            sg, bsg = sgr.next()
            for g in range(4):
                pt, bp = self.psum()
                gs = slice(g * 512, (g + 1) * 512)
                for k in range(8):
                    self.mm(pt[:, :], uT[:, k, ts_], self.Wg[:, k, gs], k == 0, False, [buT, bW], [bp])
                self.mm(pt[:, :], self.ones_b[0:1, :], self.bg_b[0:1, gs], False, True, [self.b_ones_b, bW], [bp])
                self.act(sg[:, gs], pt[:, :], AF.Sigmoid, [bp], [bsg])
            self.defer(lambda: S.dma("sp", self.SG[rows, :], sg, reads=[bsg], writes=[Buf()]))

        def stageD(c):
            (uT, buT), (latT, blatT) = state[c]
            cs = slice(c * CH, (c + 1) * CH)
            R = slice(64, 96)
            for h in range(NH):
                pa, bpa = self.psum()
                for j in range(2):
                    self.mm(pa[0:96, :], self.Wuq[:, j, h, :], latT[:, j, :], j == 0, j == 1, [bW, blatT], [bpa])
                pb, bpb = self.psum()
                for j in range(2):
                    self.mm(pb[0:96, :], self.WuqR[:, j, h, :], latT[:, j, :], j == 0, j == 1, [bW, blatT], [bpb])
                qt, bqt = qtr.next()
                self.act(qt[0:64, :], pa[0:64, :], AF.Copy, [bpa], [bqt], scale=ATT_SCALE)
                t1, bt1 = t1r.next()
                t2, bt2 = t2r.next()
                self.stt(t1[R, :], pa[R, :], ATT_SCALE, self.cosT[R, cs], ALU.mult, ALU.mult, [bpa, self.b_cosT], [bt1])
                self.stt(t2[R, :], pb[R, :], ATT_SCALE, self.sinT[R, cs], ALU.mult, ALU.mult, [bpb, self.b_sinT], [bt2])
                self.tt("pool", qt[R, :], t1[R, :], t2[R, :], ALU.add, [bt1, bt2], [bqt])
                S.dma("sp", self.QT[h, :, cs], qt[0:96, :], reads=[bqt], writes=[Buf()])
                pk, bpk = self.psum()
                for j in range(2):
                    self.mm(pk[0:64, :], self.Wuk[:, j, h * 64:(h + 1) * 64], latT[:, 2 + j, :], j == 0, j == 1,
                            [bW, blatT], [bpk])
                kt, bkt = ktr.next()
                self.cp("act", kt[0:64, :], pk[0:64, :], [bpk], [bkt])
                S.dma("sp", self.KT[h, :, cs], kt[0:64, :], reads=[bkt], writes=[Buf()])
            pa, bpa = self.psum()
            for k in range(8):
                self.mm(pa[0:96, :], self.Wkr[:, k, 0, :], uT[:, k, :], k == 0, k == 7, [bW, buT], [bpa])
            pb, bpb = self.psum()
            for k in range(8):
                self.mm(pb[0:96, :], self.Wkr[:, k, 1, :], uT[:, k, :], k == 0, k == 7, [bW, buT], [bpb])
            t1, bt1 = t1r.next()
            t2, bt2 = t2r.next()
            self.tt("dve", t1[R, :], pa[R, :], self.cosT[R, cs], ALU.mult, [bpa, self.b_cosT], [bt1])
            self.tt("dve", t2[R, :], pb[R, :], self.sinT[R, cs], ALU.mult, [bpb, self.b_sinT], [bt2])
            kt, bkt = ktr.next()
            self.tt("pool", kt[R, :], t1[R, :], t2[R, :], ALU.add, [bt1, bt2], [bkt])
            S.dma("sp", self.KTR[:, cs], kt[R, :], reads=[bkt], writes=[Buf()])

        load_x(0)
        load_x(1)
        stages = [stageA1, stageA2, stageB1, stageB2, stageC]
        for i in range(NT + len(stages) - 1):
            if i + 2 < NT:
                load_x(i + 2)
            self.flush()
            for k_ in P1_ORDER:
                if 0 <= i - k_ < NT:
                    stages[k_](i - k_)
            tl = i - (len(stages) - 1)
            if 0 <= tl < NT and tl % 4 == 3:
                stageD(tl // 4)
        self.flush()
        S.barrier()
        A.reset(self.m_persist)


    def phase2b(self):
        S, A = self.S, self.A
        m0 = A.mark()
        WCS = A.alloc([128, 4, 2, D], BF16); bWCS = Buf()
        D2 = A.alloc([128, 128], BF16); bD2 = Buf()
        m1 = A.mark()
        wbf = A.alloc([128, 4, D], F32); bwbf = Buf()
        bd = A.alloc([128, 2, 128], F32); bbd = Buf()
        S.dma("sp", wbf, self.w_bf.rearrange("(j p) n -> p j n", p=128), writes=[bwbf])
        S.dma("sp", bd[:, 0, :], self.bdc[:, :], writes=[bbd])
        S.dma("sp", bd[:, 1, :], self.bds[:, :], writes=[bbd])
        S.dma("sp", D2, self.d2[:, :], writes=[bD2])
        for j in range(4):
            for r in range(2):
                for hf in range(2):
                    ps, bps = self.psum()
                    hs = slice(hf * 512, (hf + 1) * 512)
                    self.mm(ps[:, :], bd[:, r, :], wbf[:, j, hs], True, True, [bbd, bwbf], [bps])
                    self.cp("act" if hf else "dve", WCS[:, j, r, hs], ps[:, :], [bps], [bWCS])
        S.barrier()
        A.reset(m1)
        tbr = Ring([A.alloc([128, 2, 128], BF16) for _ in range(6)])
        unr = Ring([A.alloc([128, 512], BF16) for _ in range(6)])
        yor = Ring([A.alloc([128, 2, 512], BF16) for _ in range(3)])
        USv = self.US.rearrange("(n1 n2) c -> n2 n1 c", n2=64)
        TREv = self.tre.rearrange("n a b -> a n b")
        TIMv = self.tim.rearrange("n a b -> a n b")
        ld1 = {}

        def load1(n2):
            tb, btb = tbr.next()
            S.dma("sp", tb[:, 0, :], self.tre[n2, :, :], writes=[btb])
            S.dma("sp", tb[:, 1, :], self.tim[n2, :, :], writes=[btb])
            un, bun = unr.next()
            S.dma("sp", un, USv[n2, :, :], writes=[bun])
            ld1[n2] = (tb, btb, un, bun)
        PF = 4
        for n2 in range(PF):
            load1(n2)
        for n2 in range(64):
            if n2 + PF < 64:
                load1(n2 + PF)
            tb, btb, un, bun = ld1.pop(n2)
            yo, byo = yor.next()
            for r in range(2):
                ps, bps = self.psum()
                self.mm(ps[:, :], tb[:, r, :], un, True, True, [btb, bun], [bps])
                self.cp("act" if r else "dve", yo[:, r, :], ps[:, :], [bps], [byo])
            S.dma("sp", self.YS[:, :, n2, :], yo, reads=[byo], writes=[Buf()])
        S.barrier()
        ysr = Ring([A.alloc([128, 512], BF16) for _ in range(8)])
        gtr = Ring([A.alloc([128, 4, 2, 2, 64], BF16) for _ in range(3)])
        for_ = Ring([A.alloc([128, D], BF16) for _ in range(4)])
        FOv = self.FO.rearrange("(k2 k1) d -> k1 k2 d", k1=128)
        ld2 = {}

        def load2(k1):
            ys, bys = ysr.next()
            S.dma("sp", ys, self.YS[k1, :, :, :].rearrange("r n c -> (r n) c"), writes=[bys])
            ld2[k1] = (ys, bys)
        PF2 = 6
        for k1 in range(PF2):
            load2(k1)
        gts = {}

        def d2stage(kp):
            gt, bgt = gtr.next()
            for pr in range(2):
                k1 = 2 * kp + pr
                if k1 + PF2 < 128:
                    load2(k1 + PF2)
                ys, bys = ld2.pop(k1)
                ps, bps = self.psum()
                for j in range(4):
                    self.mm(ps[:, j * 128:(j + 1) * 128], ys[:, j * 128:(j + 1) * 128], D2, True, True,
                            [bys, bD2], [bps])
                self.cp("dve" if pr else "act", gt[:, :, :, pr, :],
                        ps[:, :].rearrange("p (j q k) -> p j q k", j=4, q=2), [bps], [bgt])
            gts[kp] = (gt, bgt)

        def proj(kp):
            gt, bgt = gts.pop(kp)
            fo, bfo = for_.next()
            for hf in range(2):
                hs = slice(hf * 512, (hf + 1) * 512)
                ps, bps = self.psum()
                n = 0
                for j in range(4):
                    for q in range(2):
                        self.mm(ps[:, :], gt[:, j, q, :, :].rearrange("p a b -> p (a b)"), WCS[:, j, q, hs],
                                n == 0, n == 7, [bgt, bWCS], [bps])
                        n += 1
                self.cp("act" if hf else "dve", fo[:, hs], ps[:, :], [bps], [bfo])
            self.defer(lambda: S.dma("sp", FOv[2 * kp, :, :], fo[0:64, :], reads=[bfo], writes=[Buf()]))
            self.defer(lambda: S.dma("sp", FOv[2 * kp + 1, :, :], fo[64:128, :], reads=[bfo], writes=[Buf()]))

        d2stage(0)
        for kp in range(64):
            self.flush()
            if kp + 1 < 64:
                d2stage(kp + 1)
            proj(kp)
        self.flush()
        S.barrier()
        A.reset(m0)

    def phase2a(self):
        S, A = self.S, self.A
        m0 = A.mark()
        pts = self.pring.items
        sring = Ring(pts[0:6])
        oring = Ring(pts[6:8])
        Vall = A.alloc([128, NT, NH, 65], BF16); bV = Buf()
        vst = Ring([A.alloc([128, 8, 512], BF16) for _ in range(2)])
        Kr = Ring([A.alloc([128, S_TOK], BF16) for _ in range(2)])
        Qr = Ring([A.alloc([128, CH], BF16) for _ in range(3)])
        Pr = Ring([A.alloc([128, CH], BF16) for _ in range(4)])
        srr = Ring([A.alloc([128, CH], F32) for _ in range(2)])
        recr = Ring([A.alloc([128, CH], F32) for _ in range(2)])
        onr = Ring([A.alloc([128, CH], BF16) for _ in range(2)])
        self.memset("pool", Vall[:, :, :, 64:65], 1.0, [bV])
        for g in range(8):
            vs, bvs = vst.next()
            S.dma("sp", vs, self.VS[g * 1024:(g + 1) * 1024, :].rearrange("(t p) n -> p t n", p=128), writes=[bvs])
            self.cp("pool" if g % 2 else "dve", Vall[:, g * 8:(g + 1) * 8, :, 0:64],
                    vs.rearrange("p t (h d) -> p t h d", h=NH), [bvs], [bV])
        kbufs = []
        for i in range(2):
            kb, bk = Kr.next()
            brope = Buf()
            S.dma("sp", kb[64:96, :], self.KTR[:, :], writes=[brope])
            kbufs.append((kb, bk, brope))
        LAG = 2
        cst = Ring([A.alloc([128, 2048], F32) for _ in range(3)])
        cbf = Ring([A.alloc([128, 2048], BF16) for _ in range(2)])
        WGUv = self.WGU.rearrange("r (k f) -> r k f", k=8)
        units = [(e_, kind) for e_ in range(32) for kind in range(3)]
        cld = {}

        def conv_load(u):
            if u >= len(units):
                return
            e_, kind = units[u]
            t_, b_ = cst.next()
            if kind == 0:
                S.dma("sp", t_.rearrange("p (k f) -> p k f", k=8),
                      self.w_eg[e_, :, :].rearrange("(k p) f -> p k f", p=128), writes=[b_])
            elif kind == 1:
                S.dma("sp", t_.rearrange("p (k f) -> p k f", k=8),
                      self.w_eu[e_, :, :].rearrange("(k p) f -> p k f", p=128), writes=[b_])
            else:
                S.dma("sp", t_.rearrange("p (j d) -> p j d", j=2),
                      self.w_ed[e_, :, :].rearrange("(j p) d -> p j d", p=128), writes=[b_])
            cld[u] = (t_, b_)

        def conv_cast(u):
            if u >= len(units):
                return
            e_, kind = units[u]
            t_, b_ = cld.pop(u)
            o_, bo_ = cbf.next()
            self.cp("pool", o_, t_, [b_], [bo_])
            rows = slice(e_ * 128, (e_ + 1) * 128)
            if kind == 0:
                S.dma("sp", WGUv[rows, :, 0:256], o_.rearrange("p (k f) -> p k f", k=8), reads=[bo_], writes=[Buf()])
            elif kind == 1:
                S.dma("sp", WGUv[rows, :, 256:512], o_.rearrange("p (k f) -> p k f", k=8), reads=[bo_], writes=[Buf()])
            else:
                S.dma("sp", self.WD[rows, :], o_, reads=[bo_], writes=[Buf()])

        conv_load(0)
        zt = A.alloc([128, 4096], BF16); bzt = Buf()
        self.memset("pool", zt, 0.0, [bzt])
        XSz = self.XS.rearrange("(p r) d -> p (r d)", p=128)
        nz = (NSLOT // 128) * D // 4096

        def zero_fill(i):
            if 0 <= i < nz:
                S.dma("sp", XSz[:, i * 4096:(i + 1) * 4096], zt, reads=[bzt], writes=[Buf()])

        def load_k(h):
            kb, bk, brope = kbufs[h % 2]
            S.dma("sp", kb[0:64, :], self.KT[h, :, :], writes=[bk])

        load_k(0)
        for h in range(NH):
            kb, bk, brope = kbufs[h % 2]
            if h + 1 < NH:
                load_k(h + 1)
            qts = {}

            def load_q(qc):
                qt, bq = Qr.next()
                S.dma("sp", qt[0:96, :], self.QT[h, :, qc * CH:(qc + 1) * CH], writes=[bq])
                qts[qc] = (qt, bq)
            load_q(0)
            load_q(1)
            for qc in range(NCH):
                if qc + 2 < NCH:
                    load_q(qc + 2)
                conv_load(h * NCH + qc + 1)
                conv_cast(h * NCH + qc)
                zero_fill(h * NCH + qc - 70)
                qt, bq = qts.pop(qc)
                po, bpo = oring.next()
                pend = []

                def pv(kt, pT, bpT):
                    self.mm(po[0:65, :], Vall[:, kt, h, :], pT, kt == 0, kt == NT - 1, [bV, bpT], [bpo])
                for kt in range(NT):
                    ps, bps = sring.next()
                    self.mm(ps[:, :], kb[0:96, kt * 128:(kt + 1) * 128], qt[0:96, :], True, True,
                            [bk, brope, bq], [bps])
                    pT, bpT = Pr.next()
                    self.act(pT, ps[:, :], AF.Exp, [bps], [bpT])
                    pend.append((kt, pT, bpT))
                    if len(pend) > LAG:
                        pv(*pend.pop(0))
                while pend:
                    pv(*pend.pop(0))
                sr, bsr = srr.next()
                self.cp("dve", sr[64:65, :], po[64:65, :], [bpo], [bsr])
                pb, bpb = sring.next()
                self.mm(pb[0:64, :], self.ones_f[64:65, 0:64], sr[64:65, :], True, True, [self.b_ones_f, bsr], [bpb])
                rec, brec = recr.next()
                self.S.op("dve", lambda e, o_=rec[0:64, :], i_=pb[0:64, :]: e.reciprocal(out=o_, in_=i_), [bpb], [brec])
                on, bon = onr.next()
                self.tt("dve", on[0:64, :], po[0:64, :], rec[0:64, :], ALU.mult, [bpo, brec], [bon])
                S.dma("sp", self.OT[h, :, qc * CH:(qc + 1) * CH], on[0:64, :], reads=[bon], writes=[Buf()])
        S.barrier()
        A.reset(m0)

    def phase3(self):
        S, A = self.S, self.A
        V = self.vecs
        self.w12 = A.alloc([128, NT, 2], F32); self.b_w12 = Buf()
        self.slot_i = A.alloc([128, 2, NT], I32); self.b_slot = Buf()
        self.idxw = A.alloc([128, NST], I32); self.b_idxw = Buf()
        self.m_persist = A.mark()
        M12 = A.alloc([128, 2, NT, 32], BF16); bM = Buf()
        rank = A.alloc([128, NT, 32], F32); brank = Buf()
        Rr = A.alloc([128, 32], F32); bR = Buf()
        m0 = A.mark()
        Wba = A.alloc([128, 4, D], BF16)
        Wout = A.alloc([128, 8, D], BF16)
        Wr = A.alloc([128, 8, 36], BF16)
        br_b = A.alloc([128, 36], BF16)
        Ltri = A.alloc([128, 128], BF16)
        AB2 = A.alloc([128, 2, D], F32)
        bW = Buf()
        m1 = A.mark()
        stg = Ring([A.alloc([128, 8, 512], F32) for _ in range(2)])
        for hf in range(2):
            t, b = stg.next()
            S.dma("sp", t[:, 0:4, :], self.w_ba[:, hf * 512:(hf + 1) * 512].rearrange("(j p) n -> p j n", p=128), writes=[b])
            self.cp("dve", Wba[:, :, hf * 512:(hf + 1) * 512], t[:, 0:4, :], [b], [bW])
        for hf in range(2):
            t, b = stg.next()
            S.dma("sp", t, self.w_out[:, hf * 512:(hf + 1) * 512].rearrange("(k p) n -> p k n", p=128), writes=[b])
            self.tt("dve", Wout[:, :, hf * 512:(hf + 1) * 512], t,
                    self.bc[:, 0, hf * 512:(hf + 1) * 512].unsqueeze(1).broadcast_to([128, 8, 512]), ALU.mult,
                    [b, self.b_bc], [bW])
        t, b = stg.next()
        S.dma("sp", t[:, :, 0:36], self.w_rt.rearrange("(k p) n -> p k n", p=128), writes=[b])
        self.cp("dve", Wr, t[:, :, 0:36], [b], [bW])
        t, b = stg.next()
        S.dma("sp", t[0:1, 0, 0:36], self.b_rt[0:1, :], writes=[b])
        self.cp("dve", br_b[0:1, :], t[0:1, 0, 0:36], [b], [bW])
        S.dma("sp", Ltri, self.ltri[:, :], writes=[bW])
        self.memset("dve", Rr, 0.0, [bR])
        diag_r = Ring([A.alloc([128, 128], F32) for _ in range(2)])
        for i in range(2):
            for j in range(8):
                dg, bd = diag_r.next()
                self.ts("dve", dg, self.ident_f, V[:, 2 + i, j:j + 1], None, ALU.mult, None,
                        [self.b_ident_f, self.b_vecs], [bd])
                pt, bp = self.psum()
                self.mm(pt[:, 0:128], self.ones_f, dg, True, True, [self.b_ones_f, bd], [bp])
                self.cp("act", AB2[:, i, j * 128:(j + 1) * 128], pt[:, 0:128], [bp], [bW])
        S.barrier()
        A.reset(m1)
        oTr = Ring([A.alloc([128, 4, CH], BF16) for _ in range(2)])
        for_ = Ring([A.alloc([128, D], BF16) for _ in range(3)])
        sgr = Ring([A.alloc([128, 2 * D], BF16) for _ in range(3)])
        xr = Ring([A.alloc([128, D], F32) for _ in range(5)])
        m1r = Ring([A.alloc([128, D], BF16) for _ in range(2)])
        m2r = Ring([A.alloc([128, D], BF16) for _ in range(2)])
        mgr = Ring([A.alloc([128, D], BF16) for _ in range(2)])
        mTr = Ring([A.alloc([128, 8, 128], BF16) for _ in range(2)])
        h1r = Ring([A.alloc([128, D], F32) for _ in range(3)])
        hnr = Ring([A.alloc([128, D], F32) for _ in range(2)])
        vnr = Ring([A.alloc([128, D], BF16) for _ in range(3)])
        vTr = Ring([A.alloc([128, 8, 128], BF16) for _ in range(2)])
        junk = A.alloc([128, D], BF16); b_junk = Buf()
        stat = A.alloc([128, NT, 4], F32)
        lg4r = Ring([A.alloc([128, 4, 36], F32) for _ in range(2)])
        rs = A.alloc([128, 1024], F32); brs = Buf()

        st = {}
        chunk = {}
        lgs = {}

        def loads(t):
            c, s_ = divmod(t, 4)
            rows = slice(t * 128, (t + 1) * 128)
            if s_ == 0:
                oT, boT = oTr.next()
                for h in range(NH):
                    S.dma("sp", oT[(h % 2) * 64:(h % 2) * 64 + 64, h // 2, :], self.OT[h, :, c * CH:(c + 1) * CH],
                          writes=[boT])
                chunk[c] = (oT, boT)
            fo, bfo = for_.next()
            S.dma("sp", fo, self.FO[rows, :], writes=[bfo])
            sg, bsg = sgr.next()
            S.dma("sp", sg, self.SG[rows, :], writes=[bsg])
            xt, bx = xr.next()
            S.dma("sp", xt, self.x[rows, :], writes=[bx])
            st[t] = dict(fo=(fo, bfo), sg=(sg, bsg), x=(xt, bx))

        def stage1(t):
            c, s_ = divmod(t, 4)
            oT, boT = chunk[c]
            d = st[t]
            fo, bfo = d["fo"]; sg, bsg = d["sg"]
            ts_ = slice(s_ * 128, (s_ + 1) * 128)
            m1, bm1 = m1r.next()
            for hf in range(2):
                hs = slice(hf * 512, (hf + 1) * 512)
                ps, bps = self.psum()
                for j in range(4):
                    self.mm(ps[:, :], oT[:, j, ts_], Wba[:, j, hs], j == 0, j == 3, [boT, bW], [bps])
                self.tt("dve", m1[:, hs], ps[:, :], sg[:, hs], ALU.mult, [bps, bsg], [bm1])
            m2, bm2 = d["m2"]
            mg, bmg = mgr.next()
            self.tt("dve", mg, m1, m2, ALU.add, [bm1, bm2], [bmg])
            d["mg"] = (mg, bmg)

        def stage1p(t):
            d = st[t]
            fo, bfo = d["fo"]; sg, bsg = d["sg"]
            m2, bm2 = m2r.next()
            self.tt("pool", m2, fo, sg[:, D:2 * D], ALU.mult, [bfo, bsg], [bm2])
            d["m2"] = (m2, bm2)

        def stage2(t):
            d = st[t]
            mg, bmg = d["mg"]
            ps, bps = self.psum()
            pv = ps[:, :].bitcast(BF16).rearrange("p (k n) -> p k n", k=8)
            for k in range(8):
                self.tr(pv[:, k, :], mg[:, k * 128:(k + 1) * 128], self.ident_b, [bmg, self.b_ident_b], [bps])
            mT, bmT = mTr.next()
            self.cp("act", mT, pv, [bps], [bmT])
            d["mT"] = (mT, bmT)

        def stage3(t):
            d = st[t]
            mT, bmT = d["mT"]
            xt, bx = d["x"]
            rows = slice(t * 128, (t + 1) * 128)
            h1, bh1 = h1r.next()
            for hf in range(2):
                hs = slice(hf * 512, (hf + 1) * 512)
                ps, bps = self.psum()
                for k in range(8):
                    self.mm(ps[:, :], mT[:, k, :], Wout[:, k, hs], k == 0, k == 7, [bmT, bW], [bps])
                self.tt("dve", h1[:, hs], ps[:, :], xt[:, hs], ALU.add, [bps, bx], [bh1])
            self.defer(lambda: S.dma("sp", self.H1[rows, :], h1, reads=[bh1], writes=[Buf()]))
            bst = Buf()
            self.act(junk, h1, AF.Square, [bh1], [b_junk, bst], accum_out=stat[:, t, 0:1])
            self.ts("dve", stat[:, t, 1:2], stat[:, t, 0:1], 1.0 / D, EPS, ALU.mult, ALU.add, [bst], [bst])
            self.tt("pool", stat[:, t, 2:3], stat[:, t, 1:2], self.mhalf[:, 0:1], ALU.pow, [bst, self.b_mhalf], [bst])
            d["h1"] = (h1, bh1, bst)

        def stage4(t):
            d = st[t]
            h1, bh1, bst = d["h1"]
            rows = slice(t * 128, (t + 1) * 128)
            hn, bhn = hnr.next()
            self.act(hn, h1, AF.Copy, [bh1, bst], [bhn], scale=stat[:, t, 2:3])
            self.tt("dve", hn, hn, AB2[:, 0, :], ALU.mult, [bhn, bW], [bhn])
            vn, bvn = vnr.next()
            self.tt("pool", vn, hn, AB2[:, 1, :], ALU.add, [bhn, bW], [bvn])
            self.defer(lambda: S.dma("sp", self.VN[rows, :], vn, reads=[bvn], writes=[Buf()]))
            d["vn"] = (vn, bvn)

        def stage5(t):
            d = st[t]
            vn, bvn = d["vn"]
            ps, bps = self.psum()
            pv = ps[:, :].bitcast(BF16).rearrange("p (k n) -> p k n", k=8)
            for k in range(8):
                self.tr(pv[:, k, :], vn[:, k * 128:(k + 1) * 128], self.ident_b, [bvn, self.b_ident_b], [bps])
            vT, bvT = vTr.next()
            self.cp("act", vT, pv, [bps], [bvT])
            d["vT"] = (vT, bvT)

        def stageC(t):
            c, s_ = divmod(t, 4)
            d = st.pop(t)
            vT, bvT = d["vT"]
            if s_ == 0:
                lgs[c] = lg4r.next()
            lg4, blg = lgs[c]
            ps, bps = self.psum()
            for k in range(8):
                self.mm(ps[:, 0:36], vT[:, k, :], Wr[:, k, :], k == 0, False, [bvT, bW], [bps])
            self.mm(ps[:, 0:36], self.ones_b[0:1, :], br_b[0:1, :], False, True, [self.b_ones_b, bW], [bps])
            self.cp("dve", lg4[:, s_, :], ps[:, 0:36], [bps], [blg])
            if s_ == 3:
                pending.append(route(c))

        def bc(ap, shape, axis):
            return ap.unsqueeze(axis).broadcast_to(shape)

        def route(c):
            lg4, blg = lgs.pop(c)
            t0 = c * 4
            G = lg4[:, :, 0:4]
            E = lg4[:, :, 4:36].rearrange("p t (g e) -> p t g e", g=4)
            o = [0]

            def sc(n, shape):
                v = rs[:, o[0]:o[0] + n]
                o[0] += n
                if len(shape) == 2:
                    return v.rearrange("p (a b) -> p a b", a=shape[0])
                if len(shape) == 3:
                    return v.rearrange("p (a b c) -> p a b c", a=shape[0], b=shape[1])
                return v
            R_ = [blg, brs]
            W_ = [brs]
            dve = self.S.op
            gmax = sc(4, (4,))
            dve("dve", lambda e: e.reduce_max(out=gmax, in_=G, axis=AX.X), R_, W_)
            gm = sc(16, (4, 4))
            self.tt("dve", gm, G, bc(gmax, [128, 4, 4], 2), ALU.is_ge, R_, W_)
            gd = sc(16, (4, 4))
            self.tt("dve", gd, G, bc(gmax, [128, 4, 4], 2), ALU.subtract, R_, W_)
            ge = sc(16, (4, 4))
            self.act(ge, gd, AF.Exp, R_, W_)
            gsum = sc(4, (4,))
            dve("dve", lambda e: e.reduce_sum(out=gsum, in_=ge, axis=AX.X), R_, W_)
            tmp = sc(128, (4, 4, 8))
            self.tt("dve", tmp, E, bc(gm, [128, 4, 4, 8], 3), ALU.mult, R_, W_)
            esel = sc(32, (4, 8))
            dve("dve", lambda e: e.reduce_sum(out=esel, in_=tmp.rearrange("p t g e -> p t e g"), axis=AX.X), R_, W_)
            yield
            top8 = sc(32, (4, 8))
            for i in range(4):
                dve("dve", lambda e, i=i: e.max(out=top8[:, i, :], in_=esel[:, i, :]), R_, W_)
            sel = sc(32, (4, 8))
            self.tt("dve", sel, esel, top8[:, :, 1:2].broadcast_to([128, 4, 8]), ALU.is_ge, R_, W_)
            sel1 = sc(32, (4, 8))
            self.tt("dve", sel1, esel, top8[:, :, 0:1].broadcast_to([128, 4, 8]), ALU.is_ge, R_, W_)
            sel2 = sc(32, (4, 8))
            self.tt("dve", sel2, sel, sel1, ALU.subtract, R_, W_)
            ed = sc(32, (4, 8))
            self.tt("dve", ed, esel, top8[:, :, 0:1].broadcast_to([128, 4, 8]), ALU.subtract, R_, W_)
            ex = sc(32, (4, 8))
            self.act(ex, ed, AF.Exp, R_, W_)
            yield
            sx = sc(32, (4, 8))
            self.tt("dve", sx, sel, ex, ALU.mult, R_, W_)
            den = sc(4, (4,))
            dve("dve", lambda e: e.reduce_sum(out=den, in_=sx, axis=AX.X), R_, W_)
            gd2 = sc(4, (4,))
            self.tt("dve", gd2, gsum, den, ALU.mult, R_, W_)
            coef = sc(4, (4,))
            dve("dve", lambda e: e.reciprocal(out=coef, in_=gd2), R_, W_)
            w8 = sc(32, (4, 8))
            self.tt("dve", w8, sx, bc(coef, [128, 4, 8], 2), ALU.mult, R_, W_)
            tw = sc(32, (4, 8))
            self.tt("dve", tw, sel1, w8, ALU.mult, R_, W_)
            dve("dve", lambda e: e.reduce_sum(out=self.w12[:, t0:t0 + 4, 0], in_=tw, axis=AX.X), R_, [brs, self.b_w12])
            tw2 = sc(32, (4, 8))
            self.tt("dve", tw2, sel2, w8, ALU.mult, R_, W_)
            dve("dve", lambda e: e.reduce_sum(out=self.w12[:, t0:t0 + 4, 1], in_=tw2, axis=AX.X), R_, [brs, self.b_w12])
            yield
            M1 = M12[:, 0, t0:t0 + 4, :].rearrange("p t (g e) -> p t g e", g=4)
            M2 = M12[:, 1, t0:t0 + 4, :].rearrange("p t (g e) -> p t g e", g=4)
            self.tt("dve", M1, bc(gm, [128, 4, 4, 8], 3), bc(sel1, [128, 4, 4, 8], 2), ALU.mult, R_, [brs, bM])
            self.tt("dve", M2, bc(gm, [128, 4, 4, 8], 3), bc(sel2, [128, 4, 4, 8], 2), ALU.mult, R_, [brs, bM])
            Mt = rs[:, 512:512 + 64].bitcast(BF16).rearrange("p (t e) -> p t e", t=4)
            self.tt("dve", Mt, M12[:, 0, t0:t0 + 4, :], M12[:, 1, t0:t0 + 4, :], ALU.add, [bM, brs], W_)
            for i in range(4):
                ps, bps = self.psum()
                self.mm(ps[:, 0:32], Ltri, Mt[:, i, :], True, True, [bW, brs], [bps])
                self.mm(ps[:, 32:64], self.ones_b, Mt[:, i, :], True, True, [self.b_ones_b, brs], [bps])
                self.tt("dve", rank[:, t0 + i, :], ps[:, 0:32], Rr, ALU.add, [bps, bR], [brank])
                self.tt("dve", Rr, Rr, ps[:, 32:64], ALU.add, [bps, bR], [bR])
            yield

        loads(0)
        loads(1)
        stages = [stage1, stage2, stage3, stage4, stage5, stageC]
        order = P3_ORDER
        pending = []

        def pump():
            for g_ in list(pending):
                try:
                    next(g_)
                except StopIteration:
                    pending.remove(g_)
        for i in range(NT + len(stages) - 1):
            if i + 2 < NT:
                loads(i + 2)
            self.flush()
            if i < NT:
                stage1p(i)
            for k_ in order:
                if 0 <= i - k_ < NT:
                    stages[k_](i - k_)
            pump()
        while pending:
            pump()
        self.flush()
        S.barrier()
        A.reset(m0)
        cnt = A.alloc([128, 32], F32)
        ci = A.alloc([128, 32], I32)
        pad = A.alloc([128, 32], F32)
        cs = [A.alloc([128, 32], F32) for _ in range(2)]
        base = A.alloc([128, 32], F32)
        sf = A.alloc([128, NT, 32], F32)
        prod = A.alloc([128, NT, 32], F32)
        slf = A.alloc([128, 2, NT], F32)
        tst = A.alloc([128, NST], F32)
        acc = A.alloc([128, NST], F32)
        pio = A.alloc([128, 1], F32)
        bb = Buf()
        RW = ([bb, bR, brank, bM], [bb])
        S.dma("sp", tst, self.tstart[:, :], writes=[bb])
        S.dma("sp", pio, self.piota[:, :], writes=[bb])
        self.ts("dve", cnt, Rr, float(TR - 1), 1.0 / TR, ALU.add, ALU.mult, *RW)
        self.ts("dve", ci, cnt, -0.5 + 0.5 / TR, None, ALU.add, None, *RW)
        self.cp("dve", pad, ci, *RW)
        self.ts("dve", pad, pad, float(TR), None, ALU.mult, None, *RW)
        cur = cs[0]
        self.cp("dve", cur, pad, *RW)
        k = 0
        for sh in (1, 2, 4, 8, 16):
            nxt = cs[1 - k]
            self.cp("dve", nxt[:, 0:sh], cur[:, 0:sh], *RW)
            self.tt("dve", nxt[:, sh:32], cur[:, sh:32], cur[:, 0:32 - sh], ALU.add, *RW)
            cur = nxt
            k = 1 - k
        bend = cur
        self.tt("dve", base, bend, pad, ALU.subtract, *RW)
        self.tt("dve", sf, rank, base.unsqueeze(1).broadcast_to([128, NT, 32]), ALU.add, *RW)
        for q in range(2):
            self.tt("dve", prod, sf, M12[:, q, :, :], ALU.mult, *RW)
            self.S.op("dve", lambda e, q=q: e.reduce_sum(out=slf[:, q, :], in_=prod, axis=AX.X), *RW)
        self.cp("dve", self.slot_i, slf, RW[0], [bb, self.b_slot])
        self.memset("dve", acc, 0.0, [bb])
        for e_ in range(32):
            self.stt(acc, tst, bend[:, e_:e_ + 1], acc, ALU.is_ge, ALU.add, *RW)
        self.ts("dve", acc, acc, 31.0, 128.0, ALU.min, ALU.mult, *RW)
        self.ts("dve", acc, acc, pio[:, 0:1], None, ALU.add, None, *RW)
        self.cp("dve", self.idxw, acc, RW[0], [bb, self.b_idxw])
        if "RT" in self.dbg:
            S.dma("sp", self.RT[:, 0:128], slf.rearrange("p a b -> p (a b)"), reads=[bb], writes=[Buf()])
            S.dma("sp", self.RT[:, 128:256], self.w12.rearrange("p a b -> p (a b)"), reads=[bb, self.b_w12], writes=[Buf()])
            S.dma("sp", self.RT[:, 256:256 + NST], acc, reads=[bb], writes=[Buf()])
            S.dma("sp", self.RT[:, 512:544], Rr, reads=[bb, bR], writes=[Buf()])
            S.dma("sp", self.RT[:, 544:576], bend, reads=[bb], writes=[Buf()])
        S.barrier()
        A.reset(self.m_persist)

    def idma(self, out, in_, idx, gather, reads, writes):
        if gather:
            fn = lambda e: e.indirect_dma_start(out=out, out_offset=None, in_=in_,
                                                in_offset=bass.IndirectOffsetOnAxis(ap=idx, axis=0))
        else:
            fn = lambda e: e.indirect_dma_start(out=out, out_offset=bass.IndirectOffsetOnAxis(ap=idx, axis=0),
                                                in_=in_, in_offset=None)
        return self.S.op("pool", fn, reads, writes, is_dma=True)

    def phase4(self):
        S, A = self.S, self.A
        m0 = A.mark()
        bwgu = self.b_wgu; bwd = self.b_wd
        vr = Ring([A.alloc([128, D], BF16) for _ in range(3)])
        bxs = Buf()
        for t in range(NT):
            v, bv = vr.next()
            S.dma("sp", v, self.VN[t * 128:(t + 1) * 128, :], writes=[bv])
            for q in range(2):
                self.idma(self.XS[:, :], v, self.slot_i[:, q, t:t + 1], False, [bv, self.b_slot], [Buf()])
        S.barrier()
        xsr = Ring([A.alloc([128, D], BF16) for _ in range(10)])
        wgr = Ring([A.alloc([128, 8, 512], BF16) for _ in range(5)])
        wdr = Ring([A.alloc([128, 2, D], BF16) for _ in range(5)])
        xTr = Ring([A.alloc([128, 8, 128], BF16) for _ in range(2)])
        sgl = Ring([A.alloc([128, 256], F32) for _ in range(2)])
        hdr = Ring([A.alloc([128, 256], BF16) for _ in range(2)])
        hTr = Ring([A.alloc([128, 2, 128], BF16) for _ in range(2)])
        ysr = Ring([A.alloc([128, D], BF16) for _ in range(3)])
        ld = {}

        def loads(s_):
            wg, bwg = wgr.next()
            self.idma(wg.rearrange("p k f -> p (k f)"), self.WGU[:, :], self.idxw[:, s_:s_ + 1], True,
                      [bwgu, self.b_idxw], [bwg])
            wd, bwd_ = wdr.next()
            self.idma(wd.rearrange("p j d -> p (j d)"), self.WD[:, :], self.idxw[:, s_:s_ + 1], True,
                      [bwd, self.b_idxw], [bwd_])
            xl = []
            for u in range(TR // 128):
                xs, bx = xsr.next()
                r0 = s_ * TR + u * 128
                S.dma("sp", xs, self.XS[r0:r0 + 128, :], reads=[bxs], writes=[bx])
                xl.append((xs, bx))
            ld[s_] = (xl, wg, bwg, wd, bwd_)

        SUB = TR // 128
        NU = NST * SUB
        stt_ = {}

        def e1(u):
            xl, wg, bwg, wd, bwd_ = ld[u // SUB]
            xs, bx = xl[u % SUB]
            ps, bps = self.psum()
            pv = ps[:, :].bitcast(BF16).rearrange("p (k n) -> p k n", k=8)
            for k in range(8):
                self.tr(pv[:, k, :], xs[:, k * 128:(k + 1) * 128], self.ident_b, [bx, self.b_ident_b], [bps])
            xT, bxT = xTr.next()
            self.cp("act", xT, pv, [bps], [bxT])
            stt_[u] = dict(xT=(xT, bxT))

        def e2(u):
            xl, wg, bwg, wd, bwd_ = ld[u // SUB]
            xT, bxT = stt_[u]["xT"]
            ps, bps = self.psum()
            for k in range(8):
                self.mm(ps[:, :], xT[:, k, :], wg[:, k, :], k == 0, k == 7, [bxT, bwg], [bps])
            sg, bsg = sgl.next()
            self.act(sg, ps[:, 0:256], AF.Silu, [bps], [bsg])
            hd, bhd = hdr.next()
            self.tt("dve", hd, ps[:, 256:512], sg, ALU.mult, [bps, bsg], [bhd])
            stt_[u]["hd"] = (hd, bhd)

        def e3(u):
            hd, bhd = stt_[u]["hd"]
            ps2, bps2 = self.psum()
            pv2 = ps2[:, 0:128].bitcast(BF16).rearrange("p (k n) -> p k n", k=2)
            for j in range(2):
                self.tr(pv2[:, j, :], hd[:, j * 128:(j + 1) * 128], self.ident_b, [bhd, self.b_ident_b], [bps2])
            hT, bhT = hTr.next()
            self.cp("act", hT, pv2, [bps2], [bhT])
            stt_[u]["hT"] = (hT, bhT)

        def e4(u):
            xl, wg, bwg, wd, bwd_ = ld[u // SUB]
            hT, bhT = stt_.pop(u)["hT"]
            r0 = u * 128
            ys, bys = ysr.next()
            for hf in range(2):
                hs = slice(hf * 512, (hf + 1) * 512)
                ps3, bps3 = self.psum()
                for j in range(2):
                    self.mm(ps3[:, :], hT[:, j, :], wd[:, j, hs], j == 0, j == 1, [bhT, bwd_], [bps3])
                self.tt("dve", ys[:, hs], ps3[:, :], self.bc[:, 1, hs], ALU.mult, [bps3, self.b_bc], [bys])
            self.defer(lambda: S.dma("sp", self.YS2[r0:r0 + 128, :], ys, reads=[bys], writes=[Buf()]))
            if u % SUB == SUB - 1:
                ld.pop(u // SUB)

        loads(0)
        loads(1)
        est = [e1, e2, e3, e4]
        eorder = E_ORDER
        for i in range(NU + 3):
            if i % SUB == 0 and i // SUB + 2 < NST:
                loads(i // SUB + 2)
            self.flush()
            for k_ in eorder:
                if 0 <= i - k_ < NU:
                    est[k_](i - k_)
        self.flush()
        S.barrier()
        A.reset(m0)

    def phase5(self):
        S, A = self.S, self.A
        m0 = A.mark()
        y1r = Ring([A.alloc([128, D], BF16) for _ in range(3)])
        y2r = Ring([A.alloc([128, D], BF16) for _ in range(3)])
        h1r = Ring([A.alloc([128, D], F32) for _ in range(3)])
        mr = Ring([A.alloc([128, D], F32) for _ in range(2)])
        h2r = Ring([A.alloc([128, D], F32) for _ in range(4)])
        outr = Ring([A.alloc([128, D], F32) for _ in range(4)])
        junk = A.alloc([128, D], BF16); b_junk = Buf()
        stat = A.alloc([128, NT, 4], F32)
        ld = {}

        def loads(t):
            y1, by1 = y1r.next()
            self.idma(y1, self.YS2[:, :], self.slot_i[:, 0, t:t + 1], True, [self.b_ys2, self.b_slot], [by1])
            y2, by2 = y2r.next()
            self.idma(y2, self.YS2[:, :], self.slot_i[:, 1, t:t + 1], True, [self.b_ys2, self.b_slot], [by2])
            h1, bh1 = h1r.next()
            S.dma("sp", h1, self.H1[t * 128:(t + 1) * 128, :], writes=[bh1])
            ld[t] = (y1, by1, y2, by2, h1, bh1)

        mid = {}
        mid0 = {}

        def s1(t):
            y1, by1, y2, by2, h1, bh1 = ld.pop(t)
            m, bm = mr.next()
            self.act(m, y1, AF.Copy, [by1, self.b_w12], [bm], scale=self.w12[:, t, 0:1])
            self.stt(m, y2, self.w12[:, t, 1:2], m, ALU.mult, ALU.add, [by2, self.b_w12, bm], [bm])
            h2, bh2 = h2r.next()
            self.tt("pool", h2, m, h1, ALU.add, [bm, bh1], [bh2])
            mid0[t] = (h2, bh2)

        def s1b(t):
            h2, bh2 = mid0.pop(t)
            bst = Buf()
            self.act(junk, h2, AF.Square, [bh2], [b_junk, bst], accum_out=stat[:, t, 0:1])
            self.ts("dve", stat[:, t, 1:2], stat[:, t, 0:1], 1.0 / D, EPS, ALU.mult, ALU.add, [bst], [bst])
            self.tt("pool", stat[:, t, 2:3], stat[:, t, 1:2], self.mhalf[:, 0:1], ALU.pow, [bst, self.b_mhalf], [bst])
            mid[t] = (h2, bh2, bst)

        def s2(t):
            h2, bh2, bst = mid.pop(t)
            o, bo = outr.next()
            self.stt(o, h2, stat[:, t, 2:3], self.bc[:, 2, :], ALU.mult, ALU.mult, [bh2, bst, self.b_bc], [bo])
            self.tt("dve", o, o, self.bc[:, 3, :], ALU.add, [bo, self.b_bc], [bo])
            self.defer(lambda: S.dma("sp", self.out[t * 128:(t + 1) * 128, :], o, reads=[bo], writes=[Buf()]))

        loads(0)
        loads(1)
        for i in range(NT + 2):
            if i + 2 < NT:
                loads(i + 2)
            self.flush()
            if 0 <= i - 2 < NT:
                s2(i - 2)
            if 0 <= i - 1 < NT:
                s1b(i - 1)
            if i < NT:
                s1(i)
        self.flush()
        S.barrier()
        A.reset(m0)


def host_inputs(inputs, b):
    f32 = np.float32

    def fm(v):
        v = np.asarray(v, f32).reshape(-1, 128)
        return np.ascontiguousarray(v.T)
    invf = np.zeros((128, 1), f32)
    inv = (10000.0 ** (-np.arange(0, 32, 2, dtype=f32) / 32)).astype(f32)
    for p in range(64, 96):
        invf[p, 0] = inv[(p - 64) % 16]
    m = {
        "x": np.ascontiguousarray(inputs["x"][b]),
        "ct": fm(inputs["c"][b]),
        "pos": np.ascontiguousarray(inputs["positions"][b].reshape(1, S_TOK).astype(np.int32)),
        "w_ada": np.ascontiguousarray(inputs["w_ada"][0]),
        "b_ada_t": fm(inputs["b_ada"][0]),
        "w_adaf": np.ascontiguousarray(inputs["w_ada_final"]),
        "b_adaf_t": fm(inputs["b_ada_final"]),
        "gmix_t": fm(inputs["g_norm_mix"][0]),
        "gffn_t": fm(inputs["g_norm_ffn"][0]),
        "gfin_t": fm(inputs["g_norm_final"]),
        "w_in": np.ascontiguousarray(inputs["w_in"][0]),
        "glat_t": np.ascontiguousarray(np.concatenate([fm(inputs["g_q_lat"][0]), fm(inputs["g_kv_lat"][0])], axis=1)),
        "w_uq": np.ascontiguousarray(inputs["w_uq"][0].reshape(256, 768)),
        "w_uk": np.ascontiguousarray(inputs["w_uk"][0].reshape(256, 512)),
        "w_uv": np.ascontiguousarray(inputs["w_uv"][0].reshape(256, 512)),
        "b_gates": np.ascontiguousarray(inputs["b_gates"][0].reshape(1, 2048)),
        "ident": np.eye(128, dtype=f32),
        "invf": invf,
        "w_bf": np.ascontiguousarray(inputs["w_branch_fourier"][0]),
        "w_ba": np.ascontiguousarray(inputs["w_branch_attn"][0]),
        "w_out": np.ascontiguousarray(inputs["w_out"][0]),
        "w_rt": np.ascontiguousarray(np.concatenate([inputs["w_router_group"][0],
                                                     inputs["w_router_expert"][0].reshape(D, 32)], axis=1)),
        "b_rt": np.ascontiguousarray(np.concatenate([inputs["b_router_group"][0].reshape(1, 4),
                                                     inputs["b_router_expert"][0].reshape(1, 32)], axis=1)),
        "w_eg": np.ascontiguousarray(inputs["w_expert_gate"][0]),
        "w_eu": np.ascontiguousarray(inputs["w_expert_up"][0]),
        "w_ed": np.ascontiguousarray(inputs["w_expert_down"][0]),
    }
    m.update(CONSTS)
    return m


def _make_consts():
    bf = ml_dtypes.bfloat16
    n1 = np.arange(128)[None, :, None]
    n2 = np.arange(64)[:, None, None]
    k1 = np.arange(128)[None, None, :]
    ph = (k1 * (64 * n1 + n2)) % 8192
    al = 2.0 * np.pi * ph / 8192.0
    sc = 1.0 / math.sqrt(8192.0)
    tre = (np.cos(al) * sc).astype(bf)
    tim = (-np.sin(al) * sc).astype(bf)
    a = np.arange(64)
    th = 2.0 * np.pi * ((a[:, None] * a[None, :]) % 64) / 64.0
    c, s_ = np.cos(th), np.sin(th)
    d2 = np.zeros((128, 128))
    d2[0:64, 0:64] = c
    d2[0:64, 64:128] = s_
    d2[64:128, 0:64] = s_
    d2[64:128, 64:128] = -c
    bdc = np.zeros((128, 128), np.float32)
    bds = np.zeros((128, 128), np.float32)
    for g in range(2):
        bdc[g * 64:(g + 1) * 64, g * 64:(g + 1) * 64] = c / 8.0
        bds[g * 64:(g + 1) * 64, g * 64:(g + 1) * 64] = -s_ / 8.0
    ltri = np.triu(np.ones((128, 128), np.float32), 1).astype(bf)
    tstart = np.tile((np.arange(NST, dtype=np.float32) * float(TR))[None, :], (128, 1))
    piota = np.arange(128, dtype=np.float32).reshape(128, 1)
    return {"tre": tre, "tim": tim, "d2": d2.astype(bf), "bdc": bdc, "bds": bds, "ltri": ltri,
            "tstart": np.ascontiguousarray(tstart), "piota": piota}


CONSTS = _make_consts()


def kernel(**inputs):
    bld = Builder()
    nc = bld.build()
    in_maps = [host_inputs(inputs, b) for b in range(8)]
    res = run_bass_kernel_spmd(nc, in_maps, core_ids=list(range(8)))
    return np.stack([np.asarray(r["out"]) for r in res.results], axis=0).astype(np.float32)
```

```python
import math
from contextlib import ExitStack

import numpy as np
import ml_dtypes

import concourse.bass as bass
import concourse.mybir as mybir
from concourse.bass_utils import run_bass_kernel_spmd

F32 = mybir.dt.float32
BF16 = mybir.dt.bfloat16
I32 = mybir.dt.int32
U32 = mybir.dt.uint32
U8 = mybir.dt.uint8
AF = mybir.ActivationFunctionType
ALU = mybir.AluOpType
AX = mybir.AxisListType
DTSIZE = {F32: 4, BF16: 2, I32: 4, U32: 4, U8: 1}

S_TOK = 8192
D = 1024
NT = 64
NCH = 16
CH = 512
NH = 8
EPS = 1e-6
TWO_PI = 2.0 * math.pi
CW1 = 6.28125
CW2 = TWO_PI - CW1
ATT_SCALE = 96 ** -0.5
TR = 256
NST = 96
P1_ORDER = [4, 3, 2, 1, 0]
P3_ORDER = [3, 4, 2, 0, 1, 5, 6]
P3_PF = 2
E_ORDER = [2, 0, 3, 1]
NSLOT = NST * TR


class Buf:
    __slots__ = ("name", "last_w", "readers")

    def __init__(self, name=""):
        self.name = name
        self.last_w = None
        self.readers = []


class Op:
    __slots__ = ("eng", "fn", "deps", "signal", "sig_idx", "is_dma", "dma_sem", "dma_val", "prev_dma")

    def __init__(self, eng, fn, is_dma=False):
        self.eng = eng
        self.fn = fn
        self.deps = []
        self.signal = False
        self.sig_idx = 0
        self.is_dma = is_dma
        self.dma_sem = None
        self.dma_val = 0
        self.prev_dma = None


ENGS = ("pe", "act", "dve", "pool", "sp")
DMA_RING = 8


def _nop(eng):
    return eng.nop()


_nop.is_nop = True


class Sched:
    def __init__(self, nc, same_engine_sync=True):
        self.nc = nc
        self.ops = {e: [] for e in ENGS}
        self.same = same_engine_sync
        self.dma_ops = {e: [] for e in ENGS}

    def op(self, eng, fn, reads=(), writes=(), is_dma=False):
        o = Op(eng, fn, is_dma)
        for b in reads:
            p = b.last_w
            if p is not None:
                if (not p.is_dma) and p.eng == eng and not is_dma:
                    if self.same and eng != "pe":
                        o.deps.append(p)
                else:
                    o.deps.append(p)
        for b in writes:
            p = b.last_w
            if p is not None and (p.is_dma or is_dma or p.eng != eng):
                o.deps.append(p)
            for r in b.readers:
                if r.is_dma or is_dma or r.eng != eng:
                    o.deps.append(r)
        for b in reads:
            b.readers.append(o)
        for b in writes:
            b.last_w = o
            b.readers = []
        if is_dma:
            lst = self.dma_ops[eng]
            n = len(lst)
            if n >= DMA_RING:
                o.prev_dma = lst[n - DMA_RING]
            o.dma_val = 16 * (n // DMA_RING + 1)
            o.dma_sem = n % DMA_RING
            lst.append(o)
        self.ops[eng].append(o)
        return o

    def dma(self, queue, out, in_, reads=(), writes=(), **kw):
        return self.op(queue, lambda e: e.dma_start(out=out, in_=in_, **kw), reads, writes, is_dma=True)

    def barrier(self):
        lasts = []
        for e in ENGS:
            if self.ops[e]:
                for o in reversed(self.ops[e]):
                    if not o.is_dma and not getattr(o.fn, "is_nop", False):
                        lasts.append(o)
                        break
            lasts.extend(self.dma_ops[e][-DMA_RING:])
        for e in ENGS:
            if True:
                o = self.op(e, _nop)
                for p in lasts:
                    if p.is_dma or p.eng != e:
                        o.deps.append(p)

    def finish(self):
        o = self.op("sp", _nop)
        for e in ENGS:
            o.deps.extend(self.dma_ops[e][-DMA_RING:])

    def emit(self, stack):
        nc = self.nc
        for e in ENGS:
            for o in self.ops[e]:
                for p in o.deps:
                    if not p.is_dma:
                        p.signal = True
        for e in ENGS:
            c = 0
            for o in self.ops[e]:
                if o.signal and not o.is_dma:
                    c += 1
                    o.sig_idx = c
        sems = {e: stack.enter_context(nc.semaphore("s_" + e)) for e in ENGS}
        dsems = {e: [stack.enter_context(nc.semaphore("d_%s_%d" % (e, i))) for i in range(DMA_RING)]
                 for e in ENGS if self.dma_ops[e]}
        block = stack.enter_context(nc.Block())
        engobj = {"pe": "tensor", "act": "scalar", "dve": "vector", "pool": "gpsimd", "sp": "sync"}

        def make(ename):
            ops = self.ops[ename]

            def body(eng):
                waited = {}
                dwaited = {}
                for o in ops:
                    deps = list(o.deps)
                    if o.prev_dma is not None:
                        deps.append(o.prev_dma)
                    for p in deps:
                        if p.is_dma:
                            key = (p.eng, p.dma_sem)
                            if dwaited.get(key, 0) >= p.dma_val:
                                continue
                            eng.wait_ge(dsems[p.eng][p.dma_sem], p.dma_val)
                            dwaited[key] = p.dma_val
                        else:
                            if waited.get(p.eng, 0) >= p.sig_idx:
                                continue
                            eng.wait_ge(sems[p.eng], p.sig_idx)
                            waited[p.eng] = p.sig_idx
                    ins = o.fn(eng)
                    if o.is_dma:
                        ins.then_inc(dsems[ename][o.dma_sem], 16)
                    elif o.signal:
                        ins.then_inc(sems[ename], 1)
            return body

        for ename in ENGS:
            if self.ops[ename]:
                getattr(block, engobj[ename])(make(ename))


class Arena:
    def __init__(self, tensor, size):
        self.t = tensor
        self.size = size
        self.off = 0

    def alloc(self, shape, dt):
        n = 1
        for s in shape[1:]:
            n *= s
        nbytes = n * DTSIZE[dt]
        off = (self.off + 63) // 64 * 64
        assert off + nbytes <= self.size, "arena overflow %d + %d > %d" % (off, nbytes, self.size)
        self.off = off + nbytes
        v = self.t[:, off:off + nbytes].bitcast(dt)
        if len(shape) > 2:
            names = ["d%d" % i for i in range(len(shape) - 1)]
            pat = "p (" + " ".join(names) + ") -> p " + " ".join(names)
            kw = {names[i]: shape[i + 1] for i in range(len(names))}
            v = v.rearrange(pat, **kw)
        return v

    def mark(self):
        return self.off

    def reset(self, m):
        self.off = m


class Ring:
    def __init__(self, items):
        self.items = items
        self.bufs = [Buf() for _ in items]
        self.i = 0

    def next(self):
        i = self.i
        self.i = (i + 1) % len(self.items)
        return self.items[i], self.bufs[i]


class Builder:
    def __init__(self, dbg=(), stop_after=None):
        self.dbg = set(dbg)
        self.stop_after = stop_after
        self.nc = bass.Bass("TRN2", target_bir_lowering=False)
        self.S = Sched(self.nc)
        self.deferred = []

    def din(self, name, shape, dt=F32):
        return self.nc.dram_tensor(name, list(shape), dt, kind="ExternalInput").ap()

    def scratch(self, name, shape, dt):
        kind = "ExternalOutput" if name in self.dbg else "Internal"
        return self.nc.dram_tensor(name, list(shape), dt, kind=kind).ap()

    def mm(self, out, lhsT, rhs, start, stop, reads, writes):
        return self.S.op("pe", lambda e: e.matmul(out, lhsT=lhsT, rhs=rhs, start=start, stop=stop), reads, writes)

    def tr(self, out, in_, ident, reads, writes):
        return self.S.op("pe", lambda e: e.transpose(out=out, in_=in_, identity=ident), reads, writes)

    def act(self, out, in_, func, reads, writes, **kw):
        return self.S.op("act", lambda e: e.activation(out=out, in_=in_, func=func, **kw), reads, writes)

    def ts(self, eng, out, in0, s1, s2, op0, op1, reads, writes):
        if op1 is None:
            return self.S.op(eng, lambda e: e.tensor_scalar(out=out, in0=in0, scalar1=s1, scalar2=None, op0=op0), reads, writes)
        return self.S.op(eng, lambda e: e.tensor_scalar(out=out, in0=in0, scalar1=s1, scalar2=s2, op0=op0, op1=op1), reads, writes)

    def tt(self, eng, out, in0, in1, op, reads, writes):
        return self.S.op(eng, lambda e: e.tensor_tensor(out=out, in0=in0, in1=in1, op=op), reads, writes)

    def stt(self, out, in0, scalar, in1, op0, op1, reads, writes):
        return self.S.op("dve", lambda e: e.scalar_tensor_tensor(out=out, in0=in0, scalar=scalar, in1=in1, op0=op0, op1=op1), reads, writes)

    def cp(self, eng, out, in_, reads, writes):
        if eng == "act":
            return self.S.op("act", lambda e: e.copy(out=out, in_=in_), reads, writes)
        return self.S.op(eng, lambda e: e.tensor_copy(out=out, in_=in_), reads, writes)

    def memset(self, eng, ap, val, writes):
        return self.S.op(eng, lambda e: e.memset(ap, val), (), writes)

    def psum(self):
        return self.pring.next()

    def defer(self, fn):
        self.deferred.append(fn)

    def flush(self):
        d, self.deferred = self.deferred, []
        for f in d:
            f()

    def build(self):
        nc = self.nc
        S = self.S
        with ExitStack() as st:
            self.st = st
            self.b_ys2 = Buf()
            self.b_wgu = Buf()
            self.b_wd = Buf()
            arena_t = st.enter_context(nc.sbuf_tensor("arena", [128, 200 * 1024], U8))
            self.A = Arena(arena_t, 200 * 1024)
            pts = [st.enter_context(nc.psum_tensor("ps%d" % i, [128, 512], F32)) for i in range(8)]
            self.pring = Ring(pts)
            self.declare_io()
            self.phase0()
            if self.stop_after != "p0":
                self.phase1()
            if self.stop_after not in ("p0", "p1"):
                S.barrier()
                self.phase2b()
            if self.stop_after not in ("p0", "p1", "p2b") and "noatt" not in self.dbg:
                S.barrier()
                self.phase2a()
            if self.stop_after not in ("p0", "p1", "p2b", "p2a"):
                S.barrier()
                self.phase3()
            if self.stop_after not in ("p0", "p1", "p2b", "p2a", "p3"):
                self.phase4()
            if self.stop_after not in ("p0", "p1", "p2b", "p2a", "p3", "p4"):
                self.phase5()
            S.barrier()
            S.finish()
            S.emit(st)
        return nc

    def declare_io(self):
        self.x = self.din("x", [S_TOK, D])
        self.ct = self.din("ct", [128, 8])
        self.pos = self.din("pos", [1, S_TOK], I32)
        self.w_ada = self.din("w_ada", [D, 6 * D])
        self.b_ada_t = self.din("b_ada_t", [128, 48])
        self.w_adaf = self.din("w_adaf", [D, 2 * D])
        self.b_adaf_t = self.din("b_adaf_t", [128, 16])
        self.gmix_t = self.din("gmix_t", [128, 8])
        self.gffn_t = self.din("gffn_t", [128, 8])
        self.gfin_t = self.din("gfin_t", [128, 8])
        self.w_in = self.din("w_in", [D, 3104])
        self.glat_t = self.din("glat_t", [128, 4])
        self.w_uq = self.din("w_uq", [256, 768])
        self.w_uk = self.din("w_uk", [256, 512])
        self.w_uv = self.din("w_uv", [256, 512])
        self.b_gates = self.din("b_gates", [1, 2048])
        self.ident = self.din("ident", [128, 128])
        self.invf = self.din("invf", [128, 1])
        self.out = self.nc.dram_tensor("out", [S_TOK, D], F32, kind="ExternalOutput").ap()
        self.US = self.scratch("US", [S_TOK, 512], BF16)
        self.SG = self.scratch("SG", [S_TOK, 2048], BF16)
        self.QT = self.scratch("QT", [NH, 96, S_TOK], BF16)
        self.KT = self.scratch("KT", [NH, 64, S_TOK], BF16)
        self.KTR = self.scratch("KTR", [32, S_TOK], BF16)
        self.VS = self.scratch("VS", [S_TOK, 512], BF16)
        self.DBG = self.scratch("DBG", [128, 4096], F32)
        self.OT = self.scratch("OT", [NH, 64, S_TOK], BF16)
        self.YS = self.scratch("YS", [128, 2, 64, 512], BF16)
        self.FO = self.scratch("FO", [S_TOK, D], BF16)
        self.H1 = self.scratch("H1", [S_TOK, D], F32)
        self.VN = self.scratch("VN", [S_TOK, D], BF16)
        self.XS = self.scratch("XS", [NSLOT, D], BF16)
        self.YS2 = self.scratch("YS2", [NSLOT, D], BF16)
        self.WGU = self.scratch("WGU", [32 * 128, 8 * 512], BF16)
        self.WD = self.scratch("WD", [32 * 128, 2 * D], BF16)
        self.RT = self.scratch("RT", [128, 1024], F32)
        self.w_ba = self.din("w_ba", [512, D])
        self.w_out = self.din("w_out", [D, D])
        self.w_rt = self.din("w_rt", [D, 36])
        self.b_rt = self.din("b_rt", [1, 36])
        self.ltri = self.din("ltri", [128, 128], BF16)
        self.tstart = self.din("tstart", [128, NST])
        self.piota = self.din("piota", [128, 1])
        self.w_eg = self.din("w_eg", [32, D, 256])
        self.w_eu = self.din("w_eu", [32, D, 256])
        self.w_ed = self.din("w_ed", [32, 256, D])
        self.tri = self.din("tri", [64, 128, 2, 128], BF16)
        self.d2 = self.din("d2", [128, 128], BF16)
        self.bdc = self.din("bdc", [128, 128])
        self.bds = self.din("bds", [128, 128])
        self.w_bf = self.din("w_bf", [512, D])

    def phase0(self):
        S, A = self.S, self.A
        self.ident_f = A.alloc([128, 128], F32); self.b_ident_f = Buf()
        self.ident_b = A.alloc([128, 128], BF16); self.b_ident_b = Buf()
        self.ones_f = A.alloc([128, 128], F32); self.b_ones_f = Buf()
        self.ones_b = A.alloc([128, 128], BF16); self.b_ones_b = Buf()
        self.modT = A.alloc([128, 64], F32); self.b_modT = Buf()
        self.vecs = A.alloc([128, 8, 8], F32); self.b_vecs = Buf()
        self.bc = A.alloc([128, 4, D], F32); self.b_bc = Buf()
        self.mhalf = A.alloc([128, 2], F32); self.b_mhalf = Buf()
        cact = A.alloc([128, 8, 2], F32); b_cact = Buf()
        ctt = A.alloc([128, 8], F32); b_ctt = Buf()
        small = A.alloc([128, 128], F32); b_small = Buf()
        self.small = small; self.b_small = b_small
        S.dma("sp", self.ident_f, self.ident[:, :], writes=[self.b_ident_f])
        S.dma("sp", ctt, self.ct[:, :], writes=[b_ctt])
        S.dma("sp", small[:, 0:48], self.b_ada_t[:, :], writes=[b_small])
        S.dma("sp", small[:, 48:64], self.b_adaf_t[:, :], writes=[b_small])
        S.dma("sp", small[:, 64:72], self.gmix_t[:, :], writes=[b_small])
        S.dma("sp", small[:, 72:80], self.gffn_t[:, :], writes=[b_small])
        S.dma("sp", small[:, 80:88], self.gfin_t[:, :], writes=[b_small])
        S.dma("sp", small[:, 88:92], self.glat_t[:, :], writes=[b_small])
        S.dma("sp", small[:, 92:93], self.invf[:, :], writes=[b_small])
        self.cp("dve", self.ident_b, self.ident_f, [self.b_ident_f], [self.b_ident_b])
        self.memset("pool", self.ones_f, 1.0, [self.b_ones_f])
        self.memset("pool", self.ones_b, 1.0, [self.b_ones_b])
        self.memset("pool", self.mhalf, -0.5, [self.b_mhalf])
        self.act(cact[:, :, 0], ctt, AF.Silu, [b_ctt], [b_cact])
        self.act(cact[:, :, 1], ctt, AF.Silu, [b_ctt], [b_cact])

        m0 = A.mark()
        wring = Ring([A.alloc([128, 8, 512], F32) for _ in range(3)])
        Rrow = A.alloc([128, 8192], F32); bRrow = Buf()
        one2 = A.alloc([128, 2], F32); bone2 = Buf()
        self.memset("dve", one2, 1.0, [bone2])
        wl = {}

        def wload(piece):
            wt, bw = wring.next()
            if piece < 12:
                src = self.w_ada[:, piece * 512:(piece + 1) * 512]
            else:
                src = self.w_adaf[:, (piece - 12) * 512:(piece - 11) * 512]
            S.dma("sp", wt, src.rearrange("(k p) n -> p k n", p=128), writes=[bw])
            wl[piece] = (wt, bw)
        wload(0)
        wload(1)
        for piece in range(16):
            if piece + 2 < 16:
                wload(piece + 2)
            wt, bw = wl.pop(piece)
            pt, bp = self.psum()
            for k in range(8):
                self.mm(pt[0:2, :], cact[:, k, :], wt[:, k, :], k == 0, k == 7, [bw, b_cact], [bp])
            self.cp("act" if piece % 2 else "dve", Rrow[0:2, piece * 512:(piece + 1) * 512], pt[0:2, :], [bp], [bRrow])
        pt, bp = self.psum()
        pv = pt[:, 0:128].rearrange("p (a b) -> p a b", b=2)
        for j in range(64):
            self.mm(pv[:, j, :], Rrow[0:1, j * 128:(j + 1) * 128], one2[0:1, :], True, True, [bRrow, bone2], [bp])
        self.tt("dve", self.modT, pv[:, :, 0], small[:, 0:64], ALU.add, [bp, b_small], [self.b_modT])
        S.barrier()
        A.reset(m0)
        mT = self.modT
        V = self.vecs
        bv = self.b_vecs
        self.stt(V[:, 0, :], mT[:, 8:16], 1.0, small[:, 64:72], ALU.add, ALU.mult, [self.b_modT, b_small], [bv])
        self.cp("dve", V[:, 1, :], mT[:, 0:8], [self.b_modT], [bv])
        self.stt(V[:, 2, :], mT[:, 32:40], 1.0, small[:, 72:80], ALU.add, ALU.mult, [self.b_modT, b_small], [bv])
        self.cp("dve", V[:, 3, :], mT[:, 24:32], [self.b_modT], [bv])
        self.stt(V[:, 4, :], mT[:, 56:64], 1.0, small[:, 80:88], ALU.add, ALU.mult, [self.b_modT, b_small], [bv])
        diag_r = Ring([A.alloc([128, 128], F32) for _ in range(2)])
        srcs = [mT[:, 16:24], mT[:, 40:48], V[:, 4, :], mT[:, 48:56]]
        for i, sv in enumerate(srcs):
            for j in range(8):
                dg, bd = diag_r.next()
                self.ts("dve", dg, self.ident_f, sv[:, j:j + 1], None, ALU.mult, None,
                        [self.b_ident_f, self.b_modT, bv], [bd])
                pt, bp = self.psum()
                self.mm(pt[:, 0:128], self.ones_f, dg, True, True, [self.b_ones_f, bd], [bp])
                self.cp("act", self.bc[:, i, j * 128:(j + 1) * 128], pt[:, 0:128], [bp], [self.b_bc])

        self.m_persist = A.mark()
        self.cosT = A.alloc([128, S_TOK], BF16); self.b_cosT = Buf()
        self.sinT = A.alloc([128, S_TOK], BF16); self.b_sinT = Buf()
        m1 = A.mark()
        PW = 2048
        pi_r = Ring([A.alloc([128, PW], I32) for _ in range(2)])
        ang_r = Ring([A.alloc([128, PW], F32) for _ in range(2)])
        ki_r = Ring([A.alloc([128, PW], I32) for _ in range(1)])
        kf_r = Ring([A.alloc([128, PW], F32) for _ in range(1)])
        r_r = Ring([A.alloc([128, PW], F32) for _ in range(2)])
        ab_r = Ring([A.alloc([128, PW], F32) for _ in range(1)])
        R = slice(64, 96)
        invf = small[R, 92:93]
        for pc in range(S_TOK // PW):
            sl = slice(pc * PW, (pc + 1) * PW)
            pi, bpi = pi_r.next()
            S.dma("sp", pi[R, :], self.pos[0:1, sl].partition_broadcast(32), writes=[bpi])
            ang, bang = ang_r.next()
            self.cp("dve", ang[R, :], pi[R, :], [bpi], [bang])
            self.ts("dve", ang[R, :], ang[R, :], invf, None, ALU.mult, None, [bang, b_small], [bang])
            ki, bki = ki_r.next()
            self.ts("dve", ki[R, :], ang[R, :], 1.0 / TWO_PI, None, ALU.mult, None, [bang], [bki])
            kf, bkf = kf_r.next()
            self.cp("dve", kf[R, :], ki[R, :], [bki], [bkf])
            r, br = r_r.next()
            self.stt(r[R, :], kf[R, :], -CW1, ang[R, :], ALU.mult, ALU.add, [bkf, bang], [br])
            self.stt(r[R, :], kf[R, :], -CW2, r[R, :], ALU.mult, ALU.add, [bkf, br], [br])
            self.ts("dve", r[R, :], r[R, :], math.pi, -math.pi, ALU.min, ALU.max, [br], [br])
            self.act(self.sinT[R, sl], r[R, :], AF.Sin, [br], [self.b_sinT])
            ab, bab = ab_r.next()
            self.stt(ab[R, :], r[R, :], -1.0, r[R, :], ALU.mult, ALU.max, [br], [bab])
            self.act(self.cosT[R, sl], ab[R, :], AF.Sin, [bab], [self.b_cosT], scale=-1.0, bias=math.pi / 2)
        S.barrier()
        A.reset(m1)

        self.Wlat = A.alloc([128, 8, 512], BF16)
        self.Wkr = A.alloc([128, 8, 2, 96], BF16)
        self.Wf = A.alloc([128, 8, 512], BF16)
        self.Wg = A.alloc([128, 8, 2048], BF16)
        self.Wuq = A.alloc([128, 2, NH, 96], BF16)
        self.WuqR = A.alloc([128, 2, NH, 96], BF16)
        self.Wuk = A.alloc([128, 2, 512], BF16)
        self.Wuv = A.alloc([128, 2, 512], BF16)
        self.bg_b = A.alloc([128, 2048], BF16)
        self.b_W1 = Buf()
        bW = self.b_W1
        m2 = A.mark()
        stg = Ring([A.alloc([128, 8, 512], F32) for _ in range(2)])
        self.memset("pool", self.Wkr, 0.0, [bW])
        self.memset("pool", self.WuqR, 0.0, [bW])
        win = self.w_in

        def wsrc(c0, c1):
            return win[:, c0:c1].rearrange("(k p) n -> p k n", p=128)
        t, b = stg.next()
        S.dma("sp", t, wsrc(0, 512), writes=[b])
        self.cp("dve", self.Wlat, t, [b], [bW])
        t, b = stg.next()
        S.dma("sp", t[:, :, 0:32], wsrc(512, 544), writes=[b])
        self.cp("dve", self.Wkr[:, :, 0, 64:96], t[:, :, 0:32], [b], [bW])
        self.ts("dve", self.Wkr[:, :, 1, 64:80], t[:, :, 16:32], -1.0, None, ALU.mult, None, [b], [bW])
        self.cp("dve", self.Wkr[:, :, 1, 80:96], t[:, :, 0:16], [b], [bW])
        t, b = stg.next()
        S.dma("sp", t, wsrc(544, 1056), writes=[b])
        self.cp("act", self.Wf, t, [b], [bW])
        for g in range(4):
            t, b = stg.next()
            S.dma("sp", t, wsrc(1056 + g * 512, 1056 + (g + 1) * 512), writes=[b])
            self.cp("dve" if g % 2 == 0 else "act", self.Wg[:, :, g * 512:(g + 1) * 512], t, [b], [bW])
        tq2 = A.alloc([128, 2, 768], F32); btq2 = Buf()
        S.dma("sp", tq2, self.w_uq.rearrange("(j p) n -> p j n", p=128), writes=[btq2])
        tq2v = tq2.rearrange("p j (h d) -> p j h d", h=NH)
        self.cp("dve", self.Wuq, tq2v, [btq2], [bW])
        self.ts("dve", self.WuqR[:, :, :, 64:80], tq2v[:, :, :, 80:96], -1.0, None, ALU.mult, None, [btq2], [bW])
        self.cp("dve", self.WuqR[:, :, :, 80:96], tq2v[:, :, :, 64:80], [btq2], [bW])
        t, b = stg.next()
        S.dma("sp", t[:, 0:2, :], self.w_uk.rearrange("(j p) n -> p j n", p=128), writes=[b])
        self.cp("dve", self.Wuk, t[:, 0:2, :], [b], [bW])
        t, b = stg.next()
        S.dma("sp", t[:, 0:2, :], self.w_uv.rearrange("(j p) n -> p j n", p=128), writes=[b])
        self.cp("dve", self.Wuv, t[:, 0:2, :], [b], [bW])
        t, b = stg.next()
        tb = t[0:1, 0:4, :].rearrange("p a b -> p (a b)")
        S.dma("sp", tb, self.b_gates[0:1, :], writes=[b])
        self.cp("dve", self.bg_b[0:1, :], tb, [b], [bW])
        S.barrier()
        A.reset(m2)
        if "DBG" in self.dbg:
            S.dma("sp", self.DBG[:, 0:64], self.modT, reads=[self.b_modT], writes=[Buf()])
            S.dma("sp", self.DBG[:, 64:128], self.vecs.rearrange("p a b -> p (a b)"), reads=[self.b_vecs], writes=[Buf()])
            S.dma("sp", self.DBG[:, 1024:2048], self.bc[:, 0, :], reads=[self.b_bc], writes=[Buf()])
            S.dma("sp", self.DBG[:, 2048:3072], self.bc[:, 2, :], reads=[self.b_bc], writes=[Buf()])

    def phase1(self):
        S, A = self.S, self.A
        m0 = A.mark()
        bW = self.b_W1
        V = self.vecs
        small = self.small
        xr = Ring([A.alloc([128, D], F32) for _ in range(3)])
        junk = A.alloc([128, D], BF16); b_junk = Buf()
        xnr = Ring([A.alloc([128, D], BF16) for _ in range(2)])
        uTr = Ring([A.alloc([128, 8, CH], BF16) for _ in range(2)])
        latTr = Ring([A.alloc([128, 4, CH], BF16) for _ in range(2)])
        stat = A.alloc([128, NT, 4], F32)
        stat2 = A.alloc([128, NT, 4], F32)
        znr = Ring([A.alloc([128, 512], BF16) for _ in range(2)])
        vsr = Ring([A.alloc([128, 512], BF16) for _ in range(3)])
        usr = Ring([A.alloc([128, 512], BF16) for _ in range(3)])
        sgr = Ring([A.alloc([128, 2048], BF16) for _ in range(3)])
        qtr = Ring([A.alloc([128, CH], BF16) for _ in range(3)])
        ktr = Ring([A.alloc([128, CH], BF16) for _ in range(3)])
        t1r = Ring([A.alloc([128, CH], F32) for _ in range(2)])
        t2r = Ring([A.alloc([128, CH], F32) for _ in range(2)])
        xT = {}
        state = {}

        def load_x(t):
            xt, bx = xr.next()
            S.dma("sp", xt, self.x[t * 128:(t + 1) * 128, :], writes=[bx])
            xT[t] = (xt, bx)

        xns = {}
        zns = {}

        def stageA1(t):
            xt, bx = xT.pop(t)
            bst = Buf()
            self.act(junk, xt, AF.Square, [bx], [b_junk, bst], accum_out=stat[:, t, 0:1])
            self.ts("dve", stat[:, t, 1:2], stat[:, t, 0:1], 1.0 / D, EPS, ALU.mult, ALU.add, [bst], [bst])
            self.tt("pool", stat[:, t, 2:3], stat[:, t, 1:2], self.mhalf[:, 0:1], ALU.pow, [bst, self.b_mhalf], [bst])
            xn, bxn = xnr.next()
            self.act(xn, xt, AF.Copy, [bx, bst], [bxn], scale=stat[:, t, 2:3])
            xns[t] = (xn, bxn)

        def stageA2(t):
            c, s = divmod(t, 4)
            if s == 0:
                state[c] = (uTr.next(), latTr.next())
            (uT, buT), (latT, blatT) = state[c]
            xn, bxn = xns.pop(t)
            pt, bp = self.psum()
            pv = pt[:, :].bitcast(BF16).rearrange("p (k n) -> p k n", k=8)
            for k in range(8):
                self.tr(pv[:, k, :], xn[:, k * 128:(k + 1) * 128], self.ident_b, [bxn, self.b_ident_b], [bp])
            for k in range(8):
                self.ts("dve", uT[:, k, s * 128:(s + 1) * 128], pv[:, k, :], V[:, 0, k:k + 1], V[:, 1, k:k + 1],
                        ALU.mult, ALU.add, [bp, self.b_vecs], [buT])

        def stageB1(t):
            c, s = divmod(t, 4)
            (uT, buT), (latT, blatT) = state[c]
            ts_ = slice(s * 128, (s + 1) * 128)
            pt, bp = self.psum()
            for k in range(8):
                self.mm(pt[:, :], uT[:, k, ts_], self.Wlat[:, k, :], k == 0, k == 7, [buT, bW], [bp])
            bst = Buf()
            self.act(junk[:, 0:256], pt[:, 0:256], AF.Square, [bp], [b_junk, bst], accum_out=stat2[:, t, 0:1])
            self.act(junk[:, 256:512], pt[:, 256:512], AF.Square, [bp], [b_junk, bst], accum_out=stat2[:, t, 1:2])
            self.ts("dve", stat2[:, t, 0:2], stat2[:, t, 0:2], 1.0 / 256, EPS, ALU.mult, ALU.add, [bst], [bst])
            self.tt("pool", stat2[:, t, 2:4], stat2[:, t, 0:2], self.mhalf[:, 0:2], ALU.pow,
                    [bst, self.b_mhalf], [bst])
            zn, bzn = znr.next()
            self.ts("dve", zn[:, 0:256], pt[:, 0:256], stat2[:, t, 2:3], None, ALU.mult, None, [bp, bst], [bzn])
            self.act(zn[:, 256:512], pt[:, 256:512], AF.Copy, [bp, bst], [bzn], scale=stat2[:, t, 3:4])
            zns[t] = (zn, bzn)

        def stageB2(t):
            c, s = divmod(t, 4)
            (uT, buT), (latT, blatT) = state[c]
            ts_ = slice(s * 128, (s + 1) * 128)
            zn, bzn = zns.pop(t)
            pt2, bp2 = self.psum()
            pv2 = pt2[:, 0:256].bitcast(BF16).rearrange("p (k n) -> p k n", k=4)
            for j in range(4):
                self.tr(pv2[:, j, :], zn[:, j * 128:(j + 1) * 128], self.ident_b, [bzn, self.b_ident_b], [bp2])
            for j in range(4):
                self.ts("dve", latT[:, j, ts_], pv2[:, j, :], small[:, 88 + j:89 + j], None, ALU.mult, None,
                        [bp2, self.b_small], [blatT])

        def stageC(t):
            c, s = divmod(t, 4)
            (uT, buT), (latT, blatT) = state[c]
            ts_ = slice(s * 128, (s + 1) * 128)
            rows = slice(t * 128, (t + 1) * 128)
            pt, bp = self.psum()
            for j in range(2):
                self.mm(pt[:, :], latT[:, 2 + j, ts_], self.Wuv[:, j, :], j == 0, j == 1, [blatT, bW], [bp])
            vs, bvs = vsr.next()
            self.cp("act", vs, pt[:, :], [bp], [bvs])
            self.defer(lambda: S.dma("sp", self.VS[rows, :], vs, reads=[bvs], writes=[Buf()]))
            pt, bp = self.psum()
            for k in range(8):
                self.mm(pt[:, :], uT[:, k, ts_], self.Wf[:, k, :], k == 0, k == 7, [buT, bW], [bp])
            us, bus = usr.next()
            self.cp("dve", us, pt[:, :], [bp], [bus])
            self.defer(lambda: S.dma("sp", self.US[rows, :], us, reads=[bus], writes=[Buf()]))
            sg, bsg = sgr.next()
            for g in range(4):
                pt, bp = self.psum()
                gs = slice(g * 512, (g + 1) * 512)
                for k in range(8):
                    self.mm(pt[:, :], uT[:, k, ts_], self.Wg[:, k, gs], k == 0, False, [buT, bW], [bp])
                self.mm(pt[:, :], self.ones_b[0:1, :], self.bg_b[0:1, gs], False, True, [self.b_ones_b, bW], [bp])
                self.act(sg[:, gs], pt[:, :], AF.Sigmoid, [bp], [bsg])
            self.defer(lambda: S.dma("sp", self.SG[rows, :], sg, reads=[bsg], writes=[Buf()]))

        def stageD(c):
            (uT, buT), (latT, blatT) = state[c]
            cs = slice(c * CH, (c + 1) * CH)
            R = slice(64, 96)
            for h in range(NH):
                pa, bpa = self.psum()
                for j in range(2):
                    self.mm(pa[0:96, :], self.Wuq[:, j, h, :], latT[:, j, :], j == 0, j == 1, [bW, blatT], [bpa])
                pb, bpb = self.psum()
                for j in range(2):
                    self.mm(pb[0:96, :], self.WuqR[:, j, h, :], latT[:, j, :], j == 0, j == 1, [bW, blatT], [bpb])
                qt, bqt = qtr.next()
                self.act(qt[0:64, :], pa[0:64, :], AF.Copy, [bpa], [bqt], scale=ATT_SCALE)
                t1, bt1 = t1r.next()
                t2, bt2 = t2r.next()
                self.stt(t1[R, :], pa[R, :], ATT_SCALE, self.cosT[R, cs], ALU.mult, ALU.mult, [bpa, self.b_cosT], [bt1])
                self.stt(t2[R, :], pb[R, :], ATT_SCALE, self.sinT[R, cs], ALU.mult, ALU.mult, [bpb, self.b_sinT], [bt2])
                self.tt("pool", qt[R, :], t1[R, :], t2[R, :], ALU.add, [bt1, bt2], [bqt])
                S.dma("sp", self.QT[h, :, cs], qt[0:96, :], reads=[bqt], writes=[Buf()])
                pk, bpk = self.psum()
                for j in range(2):
                    self.mm(pk[0:64, :], self.Wuk[:, j, h * 64:(h + 1) * 64], latT[:, 2 + j, :], j == 0, j == 1,
                            [bW, blatT], [bpk])
                kt, bkt = ktr.next()
                self.cp("act", kt[0:64, :], pk[0:64, :], [bpk], [bkt])
                S.dma("sp", self.KT[h, :, cs], kt[0:64, :], reads=[bkt], writes=[Buf()])
            pa, bpa = self.psum()
            for k in range(8):
                self.mm(pa[0:96, :], self.Wkr[:, k, 0, :], uT[:, k, :], k == 0, k == 7, [bW, buT], [bpa])
            pb, bpb = self.psum()
            for k in range(8):
                self.mm(pb[0:96, :], self.Wkr[:, k, 1, :], uT[:, k, :], k == 0, k == 7, [bW, buT], [bpb])
            t1, bt1 = t1r.next()
            t2, bt2 = t2r.next()
            self.tt("dve", t1[R, :], pa[R, :], self.cosT[R, cs], ALU.mult, [bpa, self.b_cosT], [bt1])
            self.tt("dve", t2[R, :], pb[R, :], self.sinT[R, cs], ALU.mult, [bpb, self.b_sinT], [bt2])
            kt, bkt = ktr.next()
            self.tt("pool", kt[R, :], t1[R, :], t2[R, :], ALU.add, [bt1, bt2], [bkt])
            S.dma("sp", self.KTR[:, cs], kt[R, :], reads=[bkt], writes=[Buf()])

        load_x(0)
        load_x(1)
        stages = [stageA1, stageA2, stageB1, stageB2, stageC]
        for i in range(NT + len(stages) - 1):
            if i + 2 < NT:
                load_x(i + 2)
            self.flush()
            for k_ in P1_ORDER:
                if 0 <= i - k_ < NT:
                    stages[k_](i - k_)
            tl = i - (len(stages) - 1)
            if 0 <= tl < NT and tl % 4 == 3:
                stageD(tl // 4)
        self.flush()
        S.barrier()
        A.reset(self.m_persist)


    def phase2b(self):
        S, A = self.S, self.A
        m0 = A.mark()
        WCS = A.alloc([128, 4, 2, D], BF16); bWCS = Buf()
        D2 = A.alloc([128, 128], BF16); bD2 = Buf()
        m1 = A.mark()
        wbf = A.alloc([128, 4, D], F32); bwbf = Buf()
        bd = A.alloc([128, 2, 128], F32); bbd = Buf()
        S.dma("sp", wbf, self.w_bf.rearrange("(j p) n -> p j n", p=128), writes=[bwbf])
        S.dma("sp", bd[:, 0, :], self.bdc[:, :], writes=[bbd])
        S.dma("sp", bd[:, 1, :], self.bds[:, :], writes=[bbd])
        S.dma("sp", D2, self.d2[:, :], writes=[bD2])
        for j in range(4):
            for r in range(2):
                for hf in range(2):
                    ps, bps = self.psum()
                    hs = slice(hf * 512, (hf + 1) * 512)
                    self.mm(ps[:, :], bd[:, r, :], wbf[:, j, hs], True, True, [bbd, bwbf], [bps])
                    self.cp("act" if hf else "dve", WCS[:, j, r, hs], ps[:, :], [bps], [bWCS])
        S.barrier()
        A.reset(m1)
        tbr = Ring([A.alloc([128, 2, 128], BF16) for _ in range(6)])
        unr = Ring([A.alloc([128, 512], BF16) for _ in range(6)])
        yor = Ring([A.alloc([128, 2, 512], BF16) for _ in range(4)])
        USv = self.US.rearrange("(n1 n2) c -> n2 n1 c", n2=64)
        ld1 = {}

        def load1(n2):
            tb, btb = tbr.next()
            S.dma("sp", tb, self.tri[n2, :, :, :], writes=[btb])
            un, bun = unr.next()
            S.dma("sp", un, USv[n2, :, :], writes=[bun])
            ld1[n2] = (tb, btb, un, bun)
        PF = 4
        for n2 in range(PF):
            load1(n2)
        for n2 in range(64):
            if n2 + PF < 64:
                load1(n2 + PF)
            self.flush()
            tb, btb, un, bun = ld1.pop(n2)
            yo, byo = yor.next()
            for r in range(2):
                ps, bps = self.psum()
                self.mm(ps[:, :], tb[:, r, :], un, True, True, [btb, bun], [bps])
                self.cp("act" if r else "dve", yo[:, r, :], ps[:, :], [bps], [byo])
            self.defer(lambda yo=yo, byo=byo, n2=n2: S.dma("sp", self.YS[:, :, n2, :], yo, reads=[byo], writes=[Buf()]))
        self.flush()
        S.barrier()
        ysr = Ring([A.alloc([128, 512], BF16) for _ in range(8)])
        gtr = Ring([A.alloc([128, 4, 2, 2, 64], BF16) for _ in range(3)])
        for_ = Ring([A.alloc([128, D], BF16) for _ in range(4)])
        FOv = self.FO.rearrange("(k2 k1) d -> k1 k2 d", k1=128)
        ld2 = {}

        def load2(k1):
            ys, bys = ysr.next()
            S.dma("sp", ys, self.YS[k1, :, :, :].rearrange("r n c -> (r n) c"), writes=[bys])
            ld2[k1] = (ys, bys)
        PF2 = 6
        for k1 in range(PF2):
            load2(k1)
        gts = {}

        def d2stage(kp):
            gt, bgt = gtr.next()
            for pr in range(2):
                k1 = 2 * kp + pr
                if k1 + PF2 < 128:
                    load2(k1 + PF2)
                ys, bys = ld2.pop(k1)
                ps, bps = self.psum()
                for j in range(4):
                    self.mm(ps[:, j * 128:(j + 1) * 128], ys[:, j * 128:(j + 1) * 128], D2, True, True,
                            [bys, bD2], [bps])
                self.cp("dve" if pr else "act", gt[:, :, :, pr, :],
                        ps[:, :].rearrange("p (j q k) -> p j q k", j=4, q=2), [bps], [bgt])
            gts[kp] = (gt, bgt)

        def proj(kp):
            gt, bgt = gts.pop(kp)
            fo, bfo = for_.next()
            for hf in range(2):
                hs = slice(hf * 512, (hf + 1) * 512)
                ps, bps = self.psum()
                n = 0
                for j in range(4):
                    for q in range(2):
                        self.mm(ps[:, :], gt[:, j, q, :, :].rearrange("p a b -> p (a b)"), WCS[:, j, q, hs],
                                n == 0, n == 7, [bgt, bWCS], [bps])
                        n += 1
                self.cp("act" if hf else "dve", fo[:, hs], ps[:, :], [bps], [bfo])
            self.defer(lambda: S.dma("sp", FOv[2 * kp, :, :], fo[0:64, :], reads=[bfo], writes=[Buf()]))
            self.defer(lambda: S.dma("sp", FOv[2 * kp + 1, :, :], fo[64:128, :], reads=[bfo], writes=[Buf()]))

        d2stage(0)
        for kp in range(64):
            self.flush()
            if kp + 1 < 64:
                d2stage(kp + 1)
            proj(kp)
        self.flush()
        S.barrier()
        A.reset(m0)

    def phase2a(self):
        S, A = self.S, self.A
        m0 = A.mark()
        pts = self.pring.items
        sring = Ring(pts[0:6])
        oring = Ring(pts[6:8])
        Vall = A.alloc([128, NT, NH, 65], BF16); bV = Buf()
        vst = Ring([A.alloc([128, 8, 512], BF16) for _ in range(2)])
        Kr = Ring([A.alloc([128, S_TOK], BF16) for _ in range(2)])
        Qr = Ring([A.alloc([128, CH], BF16) for _ in range(3)])
        Pr = Ring([A.alloc([128, CH], BF16) for _ in range(4)])
        srr = Ring([A.alloc([128, CH], F32) for _ in range(2)])
        recr = Ring([A.alloc([128, CH], F32) for _ in range(2)])
        onr = Ring([A.alloc([128, CH], BF16) for _ in range(2)])
        self.memset("pool", Vall[:, :, :, 64:65], 1.0, [bV])
        for g in range(8):
            vs, bvs = vst.next()
            S.dma("sp", vs, self.VS[g * 1024:(g + 1) * 1024, :].rearrange("(t p) n -> p t n", p=128), writes=[bvs])
            self.cp("pool" if g % 2 else "dve", Vall[:, g * 8:(g + 1) * 8, :, 0:64],
                    vs.rearrange("p t (h d) -> p t h d", h=NH), [bvs], [bV])
        kbufs = []
        for i in range(2):
            kb, bk = Kr.next()
            brope = Buf()
            S.dma("sp", kb[64:96, :], self.KTR[:, :], writes=[brope])
            kbufs.append((kb, bk, brope))
        LAG = 2
        cst = Ring([A.alloc([128, 2048], F32) for _ in range(3)])
        cbf = Ring([A.alloc([128, 2048], BF16) for _ in range(2)])
        WGUv = self.WGU.rearrange("r (k f) -> r k f", k=8)
        units = [(e_, kind) for e_ in range(32) for kind in range(3)]
        cld = {}

        def conv_load(u):
            if u >= len(units):
                return
            e_, kind = units[u]
            t_, b_ = cst.next()
            if kind == 0:
                S.dma("sp", t_.rearrange("p (k f) -> p k f", k=8),
                      self.w_eg[e_, :, :].rearrange("(k p) f -> p k f", p=128), writes=[b_])
            elif kind == 1:
                S.dma("sp", t_.rearrange("p (k f) -> p k f", k=8),
                      self.w_eu[e_, :, :].rearrange("(k p) f -> p k f", p=128), writes=[b_])
            else:
                S.dma("sp", t_.rearrange("p (j d) -> p j d", j=2),
                      self.w_ed[e_, :, :].rearrange("(j p) d -> p j d", p=128), writes=[b_])
            cld[u] = (t_, b_)

        def conv_cast(u):
            if u >= len(units):
                return
            e_, kind = units[u]
            t_, b_ = cld.pop(u)
            o_, bo_ = cbf.next()
            self.cp("pool", o_, t_, [b_], [bo_])
            rows = slice(e_ * 128, (e_ + 1) * 128)
            if kind == 0:
                S.dma("sp", WGUv[rows, :, 0:256], o_.rearrange("p (k f) -> p k f", k=8), reads=[bo_], writes=[Buf()])
            elif kind == 1:
                S.dma("sp", WGUv[rows, :, 256:512], o_.rearrange("p (k f) -> p k f", k=8), reads=[bo_], writes=[Buf()])
            else:
                S.dma("sp", self.WD[rows, :], o_, reads=[bo_], writes=[Buf()])

        conv_load(0)
        zt = A.alloc([128, 4096], BF16); bzt = Buf()
        self.memset("pool", zt, 0.0, [bzt])
        XSz = self.XS.rearrange("(p r) d -> p (r d)", p=128)
        nz = (NSLOT // 128) * D // 4096

        def zero_fill(i):
            if 0 <= i < nz:
                S.dma("sp", XSz[:, i * 4096:(i + 1) * 4096], zt, reads=[bzt], writes=[Buf()])

        def load_k(h):
            kb, bk, brope = kbufs[h % 2]
            S.dma("sp", kb[0:64, :], self.KT[h, :, :], writes=[bk])

        load_k(0)
        for h in range(NH):
            kb, bk, brope = kbufs[h % 2]
            if h + 1 < NH:
                load_k(h + 1)
            qts = {}

            def load_q(qc):
                qt, bq = Qr.next()
                S.dma("sp", qt[0:96, :], self.QT[h, :, qc * CH:(qc + 1) * CH], writes=[bq])
                qts[qc] = (qt, bq)
            load_q(0)
            load_q(1)
            for qc in range(NCH):
                if qc + 2 < NCH:
                    load_q(qc + 2)
                conv_load(h * NCH + qc + 1)
                conv_cast(h * NCH + qc)
                zero_fill(h * NCH + qc - 70)
                qt, bq = qts.pop(qc)
                po, bpo = oring.next()
                pend = []

                def pv(kt, pT, bpT):
                    self.mm(po[0:65, :], Vall[:, kt, h, :], pT, kt == 0, kt == NT - 1, [bV, bpT], [bpo])
                for kt in range(NT):
                    ps, bps = sring.next()
                    self.mm(ps[:, :], kb[0:96, kt * 128:(kt + 1) * 128], qt[0:96, :], True, True,
                            [bk, brope, bq], [bps])
                    pT, bpT = Pr.next()
                    self.act(pT, ps[:, :], AF.Exp, [bps], [bpT])
                    pend.append((kt, pT, bpT))
                    if len(pend) > LAG:
                        pv(*pend.pop(0))
                while pend:
                    pv(*pend.pop(0))
                sr, bsr = srr.next()
                self.cp("dve", sr[64:65, :], po[64:65, :], [bpo], [bsr])
                pb, bpb = sring.next()
                self.mm(pb[0:64, :], self.ones_f[64:65, 0:64], sr[64:65, :], True, True, [self.b_ones_f, bsr], [bpb])
                rec, brec = recr.next()
                self.S.op("dve", lambda e, o_=rec[0:64, :], i_=pb[0:64, :]: e.reciprocal(out=o_, in_=i_), [bpb], [brec])
                on, bon = onr.next()
                self.tt("dve", on[0:64, :], po[0:64, :], rec[0:64, :], ALU.mult, [bpo, brec], [bon])
                S.dma("sp", self.OT[h, :, qc * CH:(qc + 1) * CH], on[0:64, :], reads=[bon], writes=[Buf()])
        S.barrier()
        A.reset(m0)

    def phase3(self):
        S, A = self.S, self.A
        V = self.vecs
        self.w12 = A.alloc([128, NT, 2], F32); self.b_w12 = Buf()
        self.slot_i = A.alloc([128, 2, NT], I32); self.b_slot = Buf()
        self.idxw = A.alloc([128, NST], I32); self.b_idxw = Buf()
        self.m_persist = A.mark()
        M12 = A.alloc([128, 2, NT, 32], BF16); bM = Buf()
        rank = A.alloc([128, NT, 32], F32); brank = Buf()
        Rr = A.alloc([128, 32], F32); bR = Buf()
        m0 = A.mark()
        Wba = A.alloc([128, 4, D], BF16)
        Wout = A.alloc([128, 8, D], BF16)
        Wr = A.alloc([128, 8, 36], BF16)
        br_b = A.alloc([128, 36], BF16)
        Ltri = A.alloc([128, 128], BF16)
        AB2 = A.alloc([128, 2, D], F32)
        bW = Buf()
        m1 = A.mark()
        stg = Ring([A.alloc([128, 8, 512], F32) for _ in range(2)])
        for hf in range(2):
            t, b = stg.next()
            S.dma("sp", t[:, 0:4, :], self.w_ba[:, hf * 512:(hf + 1) * 512].rearrange("(j p) n -> p j n", p=128), writes=[b])
            self.cp("dve", Wba[:, :, hf * 512:(hf + 1) * 512], t[:, 0:4, :], [b], [bW])
        for hf in range(2):
            t, b = stg.next()
            S.dma("sp", t, self.w_out[:, hf * 512:(hf + 1) * 512].rearrange("(k p) n -> p k n", p=128), writes=[b])
            self.tt("dve", Wout[:, :, hf * 512:(hf + 1) * 512], t,
                    self.bc[:, 0, hf * 512:(hf + 1) * 512].unsqueeze(1).broadcast_to([128, 8, 512]), ALU.mult,
                    [b, self.b_bc], [bW])
        t, b = stg.next()
        S.dma("sp", t[:, :, 0:36], self.w_rt.rearrange("(k p) n -> p k n", p=128), writes=[b])
        self.cp("dve", Wr, t[:, :, 0:36], [b], [bW])
        t, b = stg.next()
        S.dma("sp", t[0:1, 0, 0:36], self.b_rt[0:1, :], writes=[b])
        self.cp("dve", br_b[0:1, :], t[0:1, 0, 0:36], [b], [bW])
        S.dma("sp", Ltri, self.ltri[:, :], writes=[bW])
        self.memset("dve", Rr, 0.0, [bR])
        diag_r = Ring([A.alloc([128, 128], F32) for _ in range(2)])
        for i in range(2):
            for j in range(8):
                dg, bd = diag_r.next()
                self.ts("dve", dg, self.ident_f, V[:, 2 + i, j:j + 1], None, ALU.mult, None,
                        [self.b_ident_f, self.b_vecs], [bd])
                pt, bp = self.psum()
                self.mm(pt[:, 0:128], self.ones_f, dg, True, True, [self.b_ones_f, bd], [bp])
                self.cp("act", AB2[:, i, j * 128:(j + 1) * 128], pt[:, 0:128], [bp], [bW])
        S.barrier()
        A.reset(m1)
        oTr = Ring([A.alloc([128, 4, CH], BF16) for _ in range(2)])
        for_ = Ring([A.alloc([128, D], BF16) for _ in range(P3_PF + 1)])
        sgr = Ring([A.alloc([128, 2 * D], BF16) for _ in range(P3_PF + 1)])
        xr = Ring([A.alloc([128, D], F32) for _ in range(P3_PF + 3)])
        m1r = Ring([A.alloc([128, D], BF16) for _ in range(2)])
        m2r = Ring([A.alloc([128, D], BF16) for _ in range(2)])
        mgr = Ring([A.alloc([128, D], BF16) for _ in range(2)])
        mTr = Ring([A.alloc([128, 8, 128], BF16) for _ in range(2)])
        h1r = Ring([A.alloc([128, D], F32) for _ in range(4)])
        hnr = Ring([A.alloc([128, D], F32) for _ in range(2)])
        vnr = Ring([A.alloc([128, D], BF16) for _ in range(3)])
        vTr = Ring([A.alloc([128, 8, 128], BF16) for _ in range(2)])
        junk = A.alloc([128, D], BF16); b_junk = Buf()
        stat = A.alloc([128, NT, 4], F32)
        lg4r = Ring([A.alloc([128, 4, 36], F32) for _ in range(2)])
        rs = A.alloc([128, 1024], F32); brs = Buf()

        st = {}
        chunk = {}
        lgs = {}

        def loads(t):
            c, s_ = divmod(t, 4)
            rows = slice(t * 128, (t + 1) * 128)
            if s_ == 0:
                oT, boT = oTr.next()
                for h in range(NH):
                    S.dma("sp", oT[(h % 2) * 64:(h % 2) * 64 + 64, h // 2, :], self.OT[h, :, c * CH:(c + 1) * CH],
                          writes=[boT])
                chunk[c] = (oT, boT)
            fo, bfo = for_.next()
            S.dma("sp", fo, self.FO[rows, :], writes=[bfo])
            sg, bsg = sgr.next()
            S.dma("sp", sg, self.SG[rows, :], writes=[bsg])
            xt, bx = xr.next()
            S.dma("sp", xt, self.x[rows, :], writes=[bx])
            st[t] = dict(fo=(fo, bfo), sg=(sg, bsg), x=(xt, bx))

        def stage1(t):
            c, s_ = divmod(t, 4)
            oT, boT = chunk[c]
            d = st[t]
            fo, bfo = d["fo"]; sg, bsg = d["sg"]
            ts_ = slice(s_ * 128, (s_ + 1) * 128)
            m1, bm1 = m1r.next()
            for hf in range(2):
                hs = slice(hf * 512, (hf + 1) * 512)
                ps, bps = self.psum()
                for j in range(4):
                    self.mm(ps[:, :], oT[:, j, ts_], Wba[:, j, hs], j == 0, j == 3, [boT, bW], [bps])
                self.tt("dve", m1[:, hs], ps[:, :], sg[:, hs], ALU.mult, [bps, bsg], [bm1])
            m2, bm2 = d["m2"]
            mg, bmg = mgr.next()
            self.tt("dve", mg, m1, m2, ALU.add, [bm1, bm2], [bmg])
            d["mg"] = (mg, bmg)

        def stage1p(t):
            d = st[t]
            fo, bfo = d["fo"]; sg, bsg = d["sg"]
            m2, bm2 = m2r.next()
            self.tt("pool", m2, fo, sg[:, D:2 * D], ALU.mult, [bfo, bsg], [bm2])
            d["m2"] = (m2, bm2)

        def stage2(t):
            d = st[t]
            mg, bmg = d["mg"]
            ps, bps = self.psum()
            pv = ps[:, :].bitcast(BF16).rearrange("p (k n) -> p k n", k=8)
            for k in range(8):
                self.tr(pv[:, k, :], mg[:, k * 128:(k + 1) * 128], self.ident_b, [bmg, self.b_ident_b], [bps])
            mT, bmT = mTr.next()
            self.cp("act", mT, pv, [bps], [bmT])
            d["mT"] = (mT, bmT)

        def stage3(t):
            d = st[t]
            mT, bmT = d["mT"]
            xt, bx = d["x"]
            rows = slice(t * 128, (t + 1) * 128)
            h1, bh1 = h1r.next()
            for hf in range(2):
                hs = slice(hf * 512, (hf + 1) * 512)
                ps, bps = self.psum()
                for k in range(8):
                    self.mm(ps[:, :], mT[:, k, :], Wout[:, k, hs], k == 0, k == 7, [bmT, bW], [bps])
                self.tt("dve", h1[:, hs], ps[:, :], xt[:, hs], ALU.add, [bps, bx], [bh1])
            self.defer(lambda: S.dma("sp", self.H1[rows, :], h1, reads=[bh1], writes=[Buf()]))
            bst = Buf()
            self.act(junk, h1, AF.Square, [bh1], [b_junk, bst], accum_out=stat[:, t, 0:1])
            d["h1"] = (h1, bh1, bst)

        def stage3b(t):
            d = st[t]
            h1, bh1, bst = d["h1"]
            self.ts("dve", stat[:, t, 1:2], stat[:, t, 0:1], 1.0 / D, EPS, ALU.mult, ALU.add, [bst], [bst])
            self.tt("pool", stat[:, t, 2:3], stat[:, t, 1:2], self.mhalf[:, 0:1], ALU.pow, [bst, self.b_mhalf], [bst])

        def stage4(t):
            d = st[t]
            h1, bh1, bst = d["h1"]
            rows = slice(t * 128, (t + 1) * 128)
            hn, bhn = hnr.next()
            self.act(hn, h1, AF.Copy, [bh1, bst], [bhn], scale=stat[:, t, 2:3])
            self.tt("dve", hn, hn, AB2[:, 0, :], ALU.mult, [bhn, bW], [bhn])
            vn, bvn = vnr.next()
            self.tt("pool", vn, hn, AB2[:, 1, :], ALU.add, [bhn, bW], [bvn])
            self.defer(lambda: S.dma("sp", self.VN[rows, :], vn, reads=[bvn], writes=[Buf()]))
            d["vn"] = (vn, bvn)

        def stage5(t):
            d = st[t]
            vn, bvn = d["vn"]
            ps, bps = self.psum()
            pv = ps[:, :].bitcast(BF16).rearrange("p (k n) -> p k n", k=8)
            for k in range(8):
                self.tr(pv[:, k, :], vn[:, k * 128:(k + 1) * 128], self.ident_b, [bvn, self.b_ident_b], [bps])
            vT, bvT = vTr.next()
            self.cp("act", vT, pv, [bps], [bvT])
            d["vT"] = (vT, bvT)

        def stageC(t):
            c, s_ = divmod(t, 4)
            d = st.pop(t)
            vT, bvT = d["vT"]
            if s_ == 0:
                lgs[c] = lg4r.next()
            lg4, blg = lgs[c]
            ps, bps = self.psum()
            for k in range(8):
                self.mm(ps[:, 0:36], vT[:, k, :], Wr[:, k, :], k == 0, False, [bvT, bW], [bps])
            self.mm(ps[:, 0:36], self.ones_b[0:1, :], br_b[0:1, :], False, True, [self.b_ones_b, bW], [bps])
            self.cp("dve", lg4[:, s_, :], ps[:, 0:36], [bps], [blg])
            if s_ == 3:
                pending.append(route(c))

        def bc(ap, shape, axis):
            return ap.unsqueeze(axis).broadcast_to(shape)

        def route(c):
            lg4, blg = lgs.pop(c)
            t0 = c * 4
            G = lg4[:, :, 0:4]
            E = lg4[:, :, 4:36].rearrange("p t (g e) -> p t g e", g=4)
            o = [0]

            def sc(n, shape):
                v = rs[:, o[0]:o[0] + n]
                o[0] += n
                if len(shape) == 2:
                    return v.rearrange("p (a b) -> p a b", a=shape[0])
                if len(shape) == 3:
                    return v.rearrange("p (a b c) -> p a b c", a=shape[0], b=shape[1])
                return v
            R_ = [blg, brs]
            W_ = [brs]
            dve = self.S.op
            gmax = sc(4, (4,))
            dve("dve", lambda e: e.reduce_max(out=gmax, in_=G, axis=AX.X), R_, W_)
            gm = sc(16, (4, 4))
            self.tt("dve", gm, G, bc(gmax, [128, 4, 4], 2), ALU.is_ge, R_, W_)
            gd = sc(16, (4, 4))
            self.tt("dve", gd, G, bc(gmax, [128, 4, 4], 2), ALU.subtract, R_, W_)
            ge = sc(16, (4, 4))
            self.act(ge, gd, AF.Exp, R_, W_)
            gsum = sc(4, (4,))
            dve("dve", lambda e: e.reduce_sum(out=gsum, in_=ge, axis=AX.X), R_, W_)
            tmp = sc(128, (4, 4, 8))
            self.tt("dve", tmp, E, bc(gm, [128, 4, 4, 8], 3), ALU.mult, R_, W_)
            esel = sc(32, (4, 8))
            dve("dve", lambda e: e.reduce_sum(out=esel, in_=tmp.rearrange("p t g e -> p t e g"), axis=AX.X), R_, W_)
            yield
            top8 = sc(32, (4, 8))
            for i in range(4):
                dve("dve", lambda e, i=i: e.max(out=top8[:, i, :], in_=esel[:, i, :]), R_, W_)
            sel = sc(32, (4, 8))
            self.tt("dve", sel, esel, top8[:, :, 1:2].broadcast_to([128, 4, 8]), ALU.is_ge, R_, W_)
            sel1 = sc(32, (4, 8))
            self.tt("dve", sel1, esel, top8[:, :, 0:1].broadcast_to([128, 4, 8]), ALU.is_ge, R_, W_)
            sel2 = sc(32, (4, 8))
            self.tt("dve", sel2, sel, sel1, ALU.subtract, R_, W_)
            ed = sc(32, (4, 8))
            self.tt("dve", ed, esel, top8[:, :, 0:1].broadcast_to([128, 4, 8]), ALU.subtract, R_, W_)
            ex = sc(32, (4, 8))
            self.act(ex, ed, AF.Exp, R_, W_)
            yield
            sx = sc(32, (4, 8))
            self.tt("dve", sx, sel, ex, ALU.mult, R_, W_)
            den = sc(4, (4,))
            dve("dve", lambda e: e.reduce_sum(out=den, in_=sx, axis=AX.X), R_, W_)
            gd2 = sc(4, (4,))
            self.tt("dve", gd2, gsum, den, ALU.mult, R_, W_)
            coef = sc(4, (4,))
            dve("dve", lambda e: e.reciprocal(out=coef, in_=gd2), R_, W_)
            w8 = sc(32, (4, 8))
            self.tt("dve", w8, sx, bc(coef, [128, 4, 8], 2), ALU.mult, R_, W_)
            tw = sc(32, (4, 8))
            self.tt("dve", tw, sel1, w8, ALU.mult, R_, W_)
            dve("dve", lambda e: e.reduce_sum(out=self.w12[:, t0:t0 + 4, 0], in_=tw, axis=AX.X), R_, [brs, self.b_w12])
            tw2 = sc(32, (4, 8))
            self.tt("dve", tw2, sel2, w8, ALU.mult, R_, W_)
            dve("dve", lambda e: e.reduce_sum(out=self.w12[:, t0:t0 + 4, 1], in_=tw2, axis=AX.X), R_, [brs, self.b_w12])
            yield
            M1 = M12[:, 0, t0:t0 + 4, :].rearrange("p t (g e) -> p t g e", g=4)
            M2 = M12[:, 1, t0:t0 + 4, :].rearrange("p t (g e) -> p t g e", g=4)
            self.tt("dve", M1, bc(gm, [128, 4, 4, 8], 3), bc(sel1, [128, 4, 4, 8], 2), ALU.mult, R_, [brs, bM])
            self.tt("dve", M2, bc(gm, [128, 4, 4, 8], 3), bc(sel2, [128, 4, 4, 8], 2), ALU.mult, R_, [brs, bM])
            Mt = rs[:, 512:512 + 64].bitcast(BF16).rearrange("p (t e) -> p t e", t=4)
            self.tt("dve", Mt, M12[:, 0, t0:t0 + 4, :], M12[:, 1, t0:t0 + 4, :], ALU.add, [bM, brs], W_)
            for i in range(4):
                ps, bps = self.psum()
                self.mm(ps[:, 0:32], Ltri, Mt[:, i, :], True, True, [bW, brs], [bps])
                self.mm(ps[:, 32:64], self.ones_b, Mt[:, i, :], True, True, [self.b_ones_b, brs], [bps])
                self.tt("dve", rank[:, t0 + i, :], ps[:, 0:32], Rr, ALU.add, [bps, bR], [brank])
                self.tt("dve", Rr, Rr, ps[:, 32:64], ALU.add, [bps, bR], [bR])
            yield

        for t_ in range(P3_PF):
            loads(t_)
        stages = [stage1, stage2, stage3, stage3b, stage4, stage5, stageC]
        order = P3_ORDER
        pending = []

        def pump():
            for g_ in list(pending):
                try:
                    next(g_)
                except StopIteration:
                    pending.remove(g_)
        for i in range(NT + len(stages) - 1):
            if i + P3_PF < NT:
                loads(i + P3_PF)
            self.flush()
            if i < NT:
                stage1p(i)
            for k_ in order:
                if 0 <= i - k_ < NT:
                    stages[k_](i - k_)
            pump()
        while pending:
            pump()
        self.flush()
        S.barrier()
        A.reset(m0)
        cnt = A.alloc([128, 32], F32)
        ci = A.alloc([128, 32], I32)
        pad = A.alloc([128, 32], F32)
        cs = [A.alloc([128, 32], F32) for _ in range(2)]
        base = A.alloc([128, 32], F32)
        sf = A.alloc([128, NT, 32], F32)
        prod = A.alloc([128, NT, 32], F32)
        slf = A.alloc([128, 2, NT], F32)
        tst = A.alloc([128, NST], F32)
        acc = A.alloc([128, NST], F32)
        pio = A.alloc([128, 1], F32)
        bb = Buf()
        RW = ([bb, bR, brank, bM], [bb])
        S.dma("sp", tst, self.tstart[:, :], writes=[bb])
        S.dma("sp", pio, self.piota[:, :], writes=[bb])
        self.ts("dve", cnt, Rr, float(TR - 1), 1.0 / TR, ALU.add, ALU.mult, *RW)
        self.ts("dve", ci, cnt, -0.5 + 0.5 / TR, None, ALU.add, None, *RW)
        self.cp("dve", pad, ci, *RW)
        self.ts("dve", pad, pad, float(TR), None, ALU.mult, None, *RW)
        cur = cs[0]
        self.cp("dve", cur, pad, *RW)
        k = 0
        for sh in (1, 2, 4, 8, 16):
            nxt = cs[1 - k]
            self.cp("dve", nxt[:, 0:sh], cur[:, 0:sh], *RW)
            self.tt("dve", nxt[:, sh:32], cur[:, sh:32], cur[:, 0:32 - sh], ALU.add, *RW)
            cur = nxt
            k = 1 - k
        bend = cur
        self.tt("dve", base, bend, pad, ALU.subtract, *RW)
        self.tt("dve", sf, rank, base.unsqueeze(1).broadcast_to([128, NT, 32]), ALU.add, *RW)
        for q in range(2):
            self.tt("dve", prod, sf, M12[:, q, :, :], ALU.mult, *RW)
            self.S.op("dve", lambda e, q=q: e.reduce_sum(out=slf[:, q, :], in_=prod, axis=AX.X), *RW)
        self.cp("dve", self.slot_i, slf, RW[0], [bb, self.b_slot])
        self.memset("dve", acc, 0.0, [bb])
        for e_ in range(32):
            self.stt(acc, tst, bend[:, e_:e_ + 1], acc, ALU.is_ge, ALU.add, *RW)
        self.ts("dve", acc, acc, 31.0, 128.0, ALU.min, ALU.mult, *RW)
        self.ts("dve", acc, acc, pio[:, 0:1], None, ALU.add, None, *RW)
        self.cp("dve", self.idxw, acc, RW[0], [bb, self.b_idxw])
        if "RT" in self.dbg:
            S.dma("sp", self.RT[:, 0:128], slf.rearrange("p a b -> p (a b)"), reads=[bb], writes=[Buf()])
            S.dma("sp", self.RT[:, 128:256], self.w12.rearrange("p a b -> p (a b)"), reads=[bb, self.b_w12], writes=[Buf()])
            S.dma("sp", self.RT[:, 256:256 + NST], acc, reads=[bb], writes=[Buf()])
            S.dma("sp", self.RT[:, 512:544], Rr, reads=[bb, bR], writes=[Buf()])
            S.dma("sp", self.RT[:, 544:576], bend, reads=[bb], writes=[Buf()])
        S.barrier()
        A.reset(self.m_persist)

    def idma(self, out, in_, idx, gather, reads, writes):
        if gather:
            fn = lambda e: e.indirect_dma_start(out=out, out_offset=None, in_=in_,
                                                in_offset=bass.IndirectOffsetOnAxis(ap=idx, axis=0))
        else:
            fn = lambda e: e.indirect_dma_start(out=out, out_offset=bass.IndirectOffsetOnAxis(ap=idx, axis=0),
                                                in_=in_, in_offset=None)
        return self.S.op("pool", fn, reads, writes, is_dma=True)

    def phase4(self):
        S, A = self.S, self.A
        m0 = A.mark()
        bwgu = self.b_wgu; bwd = self.b_wd
        vr = Ring([A.alloc([128, D], BF16) for _ in range(3)])
        bxs = Buf()
        for t in range(NT):
            v, bv = vr.next()
            S.dma("sp", v, self.VN[t * 128:(t + 1) * 128, :], writes=[bv])
            for q in range(2):
                self.idma(self.XS[:, :], v, self.slot_i[:, q, t:t + 1], False, [bv, self.b_slot], [Buf()])
        S.barrier()
        xsr = Ring([A.alloc([128, D], BF16) for _ in range(10)])
        wgr = Ring([A.alloc([128, 8, 512], BF16) for _ in range(5)])
        wdr = Ring([A.alloc([128, 2, D], BF16) for _ in range(5)])
        xTr = Ring([A.alloc([128, 8, 128], BF16) for _ in range(2)])
        sgl = Ring([A.alloc([128, 256], F32) for _ in range(2)])
        hdr = Ring([A.alloc([128, 256], BF16) for _ in range(2)])
        hTr = Ring([A.alloc([128, 2, 128], BF16) for _ in range(2)])
        ysr = Ring([A.alloc([128, D], BF16) for _ in range(3)])
        ld = {}

        def loads(s_):
            wg, bwg = wgr.next()
            self.idma(wg.rearrange("p k f -> p (k f)"), self.WGU[:, :], self.idxw[:, s_:s_ + 1], True,
                      [bwgu, self.b_idxw], [bwg])
            wd, bwd_ = wdr.next()
            self.idma(wd.rearrange("p j d -> p (j d)"), self.WD[:, :], self.idxw[:, s_:s_ + 1], True,
                      [bwd, self.b_idxw], [bwd_])
            xl = []
            for u in range(TR // 128):
                xs, bx = xsr.next()
                r0 = s_ * TR + u * 128
                S.dma("sp", xs, self.XS[r0:r0 + 128, :], reads=[bxs], writes=[bx])
                xl.append((xs, bx))
            ld[s_] = (xl, wg, bwg, wd, bwd_)

        SUB = TR // 128
        NU = NST * SUB
        stt_ = {}

        def e1(u):
            xl, wg, bwg, wd, bwd_ = ld[u // SUB]
            xs, bx = xl[u % SUB]
            ps, bps = self.psum()
            pv = ps[:, :].bitcast(BF16).rearrange("p (k n) -> p k n", k=8)
            for k in range(8):
                self.tr(pv[:, k, :], xs[:, k * 128:(k + 1) * 128], self.ident_b, [bx, self.b_ident_b], [bps])
            xT, bxT = xTr.next()
            self.cp("act", xT, pv, [bps], [bxT])
            stt_[u] = dict(xT=(xT, bxT))

        def e2(u):
            xl, wg, bwg, wd, bwd_ = ld[u // SUB]
            xT, bxT = stt_[u]["xT"]
            ps, bps = self.psum()
            for k in range(8):
                self.mm(ps[:, :], xT[:, k, :], wg[:, k, :], k == 0, k == 7, [bxT, bwg], [bps])
            sg, bsg = sgl.next()
            self.act(sg, ps[:, 0:256], AF.Silu, [bps], [bsg])
            hd, bhd = hdr.next()
            self.tt("dve", hd, ps[:, 256:512], sg, ALU.mult, [bps, bsg], [bhd])
            stt_[u]["hd"] = (hd, bhd)

        def e3(u):
            hd, bhd = stt_[u]["hd"]
            ps2, bps2 = self.psum()
            pv2 = ps2[:, 0:128].bitcast(BF16).rearrange("p (k n) -> p k n", k=2)
            for j in range(2):
                self.tr(pv2[:, j, :], hd[:, j * 128:(j + 1) * 128], self.ident_b, [bhd, self.b_ident_b], [bps2])
            hT, bhT = hTr.next()
            self.cp("act", hT, pv2, [bps2], [bhT])
            stt_[u]["hT"] = (hT, bhT)

        def e4(u):
            xl, wg, bwg, wd, bwd_ = ld[u // SUB]
            hT, bhT = stt_.pop(u)["hT"]
            r0 = u * 128
            ys, bys = ysr.next()
            for hf in range(2):
                hs = slice(hf * 512, (hf + 1) * 512)
                ps3, bps3 = self.psum()
                for j in range(2):
                    self.mm(ps3[:, :], hT[:, j, :], wd[:, j, hs], j == 0, j == 1, [bhT, bwd_], [bps3])
                self.tt("dve", ys[:, hs], ps3[:, :], self.bc[:, 1, hs], ALU.mult, [bps3, self.b_bc], [bys])
            self.defer(lambda: S.dma("sp", self.YS2[r0:r0 + 128, :], ys, reads=[bys], writes=[Buf()]))
            if u % SUB == SUB - 1:
                ld.pop(u // SUB)

        loads(0)
        loads(1)
        est = [e1, e2, e3, e4]
        eorder = E_ORDER
        for i in range(NU + 3):
            if i % SUB == 0 and i // SUB + 2 < NST:
                loads(i // SUB + 2)
            self.flush()
            for k_ in eorder:
                if 0 <= i - k_ < NU:
                    est[k_](i - k_)
        self.flush()
        S.barrier()
        A.reset(m0)

    def phase5(self):
        S, A = self.S, self.A
        m0 = A.mark()
        y1r = Ring([A.alloc([128, D], BF16) for _ in range(3)])
        y2r = Ring([A.alloc([128, D], BF16) for _ in range(3)])
        h1r = Ring([A.alloc([128, D], F32) for _ in range(3)])
        mr = Ring([A.alloc([128, D], F32) for _ in range(2)])
        h2r = Ring([A.alloc([128, D], F32) for _ in range(4)])
        outr = Ring([A.alloc([128, D], F32) for _ in range(4)])
        junk = A.alloc([128, D], BF16); b_junk = Buf()
        stat = A.alloc([128, NT, 4], F32)
        ld = {}

        def loads(t):
            y1, by1 = y1r.next()
            self.idma(y1, self.YS2[:, :], self.slot_i[:, 0, t:t + 1], True, [self.b_ys2, self.b_slot], [by1])
            y2, by2 = y2r.next()
            self.idma(y2, self.YS2[:, :], self.slot_i[:, 1, t:t + 1], True, [self.b_ys2, self.b_slot], [by2])
            h1, bh1 = h1r.next()
            S.dma("sp", h1, self.H1[t * 128:(t + 1) * 128, :], writes=[bh1])
            ld[t] = (y1, by1, y2, by2, h1, bh1)

        mid = {}
        mid0 = {}

        def s1(t):
            y1, by1, y2, by2, h1, bh1 = ld.pop(t)
            m, bm = mr.next()
            self.act(m, y1, AF.Copy, [by1, self.b_w12], [bm], scale=self.w12[:, t, 0:1])
            self.stt(m, y2, self.w12[:, t, 1:2], m, ALU.mult, ALU.add, [by2, self.b_w12, bm], [bm])
            h2, bh2 = h2r.next()
            self.tt("pool", h2, m, h1, ALU.add, [bm, bh1], [bh2])
            mid0[t] = (h2, bh2)

        def s1b(t):
            h2, bh2 = mid0.pop(t)
            bst = Buf()
            self.act(junk, h2, AF.Square, [bh2], [b_junk, bst], accum_out=stat[:, t, 0:1])
            self.ts("dve", stat[:, t, 1:2], stat[:, t, 0:1], 1.0 / D, EPS, ALU.mult, ALU.add, [bst], [bst])
            self.tt("pool", stat[:, t, 2:3], stat[:, t, 1:2], self.mhalf[:, 0:1], ALU.pow, [bst, self.b_mhalf], [bst])
            mid[t] = (h2, bh2, bst)

        def s2(t):
            h2, bh2, bst = mid.pop(t)
            o, bo = outr.next()
            self.stt(o, h2, stat[:, t, 2:3], self.bc[:, 2, :], ALU.mult, ALU.mult, [bh2, bst, self.b_bc], [bo])
            self.tt("dve", o, o, self.bc[:, 3, :], ALU.add, [bo, self.b_bc], [bo])
            self.defer(lambda: S.dma("sp", self.out[t * 128:(t + 1) * 128, :], o, reads=[bo], writes=[Buf()]))

        loads(0)
        loads(1)
        for i in range(NT + 2):
            if i + 2 < NT:
                loads(i + 2)
            self.flush()
            if 0 <= i - 2 < NT:
                s2(i - 2)
            if 0 <= i - 1 < NT:
                s1b(i - 1)
            if i < NT:
                s1(i)
        self.flush()
        S.barrier()
        A.reset(m0)


def host_inputs(inputs, b):
    f32 = np.float32

    def fm(v):
        v = np.asarray(v, f32).reshape(-1, 128)
        return np.ascontiguousarray(v.T)
    invf = np.zeros((128, 1), f32)
    inv = (10000.0 ** (-np.arange(0, 32, 2, dtype=f32) / 32)).astype(f32)
    for p in range(64, 96):
        invf[p, 0] = inv[(p - 64) % 16]
    m = {
        "x": np.ascontiguousarray(inputs["x"][b]),
        "ct": fm(inputs["c"][b]),
        "pos": np.ascontiguousarray(inputs["positions"][b].reshape(1, S_TOK).astype(np.int32)),
        "w_ada": np.ascontiguousarray(inputs["w_ada"][0]),
        "b_ada_t": fm(inputs["b_ada"][0]),
        "w_adaf": np.ascontiguousarray(inputs["w_ada_final"]),
        "b_adaf_t": fm(inputs["b_ada_final"]),
        "gmix_t": fm(inputs["g_norm_mix"][0]),
        "gffn_t": fm(inputs["g_norm_ffn"][0]),
        "gfin_t": fm(inputs["g_norm_final"]),
        "w_in": np.ascontiguousarray(inputs["w_in"][0]),
        "glat_t": np.ascontiguousarray(np.concatenate([fm(inputs["g_q_lat"][0]), fm(inputs["g_kv_lat"][0])], axis=1)),
        "w_uq": np.ascontiguousarray(inputs["w_uq"][0].reshape(256, 768)),
        "w_uk": np.ascontiguousarray(inputs["w_uk"][0].reshape(256, 512)),
        "w_uv": np.ascontiguousarray(inputs["w_uv"][0].reshape(256, 512)),
        "b_gates": np.ascontiguousarray(inputs["b_gates"][0].reshape(1, 2048)),
        "ident": np.eye(128, dtype=f32),
        "invf": invf,
        "w_bf": np.ascontiguousarray(inputs["w_branch_fourier"][0]),
        "w_ba": np.ascontiguousarray(inputs["w_branch_attn"][0]),
        "w_out": np.ascontiguousarray(inputs["w_out"][0]),
        "w_rt": np.ascontiguousarray(np.concatenate([inputs["w_router_group"][0],
                                                     inputs["w_router_expert"][0].reshape(D, 32)], axis=1)),
        "b_rt": np.ascontiguousarray(np.concatenate([inputs["b_router_group"][0].reshape(1, 4),
                                                     inputs["b_router_expert"][0].reshape(1, 32)], axis=1)),
        "w_eg": np.ascontiguousarray(inputs["w_expert_gate"][0]),
        "w_eu": np.ascontiguousarray(inputs["w_expert_up"][0]),
        "w_ed": np.ascontiguousarray(inputs["w_expert_down"][0]),
    }
    m.update(CONSTS)
    return m


def _make_consts():
    bf = ml_dtypes.bfloat16
    n1 = np.arange(128)[None, :, None]
    n2 = np.arange(64)[:, None, None]
    k1 = np.arange(128)[None, None, :]
    ph = (k1 * (64 * n1 + n2)) % 8192
    al = 2.0 * np.pi * ph / 8192.0
    sc = 1.0 / math.sqrt(8192.0)
    tre = (np.cos(al) * sc).astype(bf)
    tim = (-np.sin(al) * sc).astype(bf)
    a = np.arange(64)
    th = 2.0 * np.pi * ((a[:, None] * a[None, :]) % 64) / 64.0
    c, s_ = np.cos(th), np.sin(th)
    d2 = np.zeros((128, 128))
    d2[0:64, 0:64] = c
    d2[0:64, 64:128] = s_
    d2[64:128, 0:64] = s_
    d2[64:128, 64:128] = -c
    bdc = np.zeros((128, 128), np.float32)
    bds = np.zeros((128, 128), np.float32)
    for g in range(2):
        bdc[g * 64:(g + 1) * 64, g * 64:(g + 1) * 64] = c / 8.0
        bds[g * 64:(g + 1) * 64, g * 64:(g + 1) * 64] = -s_ / 8.0
    ltri = np.triu(np.ones((128, 128), np.float32), 1).astype(bf)
    tstart = np.tile((np.arange(NST, dtype=np.float32) * float(TR))[None, :], (128, 1))
    piota = np.arange(128, dtype=np.float32).reshape(128, 1)
    tri = np.ascontiguousarray(np.stack([tre, tim], axis=2))
    return {"tri": tri, "d2": d2.astype(bf), "bdc": bdc, "bds": bds, "ltri": ltri,
            "tstart": np.ascontiguousarray(tstart), "piota": piota}


CONSTS = _make_consts()


def kernel(**inputs):
    bld = Builder()
    nc = bld.build()
    in_maps = [host_inputs(inputs, b) for b in range(8)]
    res = run_bass_kernel_spmd(nc, in_maps, core_ids=list(range(8)))
    return np.stack([np.asarray(r["out"]) for r in res.results], axis=0).astype(np.float32)
```

```python
import math
from contextlib import ExitStack

import numpy as np
import ml_dtypes

import concourse.bass as bass
import concourse.mybir as mybir
from concourse.bass_utils import run_bass_kernel_spmd

F32 = mybir.dt.float32
BF16 = mybir.dt.bfloat16
I32 = mybir.dt.int32
U32 = mybir.dt.uint32
U8 = mybir.dt.uint8
AF = mybir.ActivationFunctionType
ALU = mybir.AluOpType
AX = mybir.AxisListType
DTSIZE = {F32: 4, BF16: 2, I32: 4, U32: 4, U8: 1}

S_TOK = 8192
D = 1024
NT = 64
NCH = 16
CH = 512
NH = 8
EPS = 1e-6
TWO_PI = 2.0 * math.pi
CW1 = 6.28125
CW2 = TWO_PI - CW1
ATT_SCALE = 96 ** -0.5
TR = 256
NST = 96
P1_ORDER = [4, 3, 2, 1, 0]
P3_ORDER = [3, 4, 2, 0, 1, 5, 6]
P3_PF = 2
E_ORDER = [2, 0, 3, 1]
NSLOT = NST * TR


class Buf:
    __slots__ = ("name", "last_w", "readers")

    def __init__(self, name=""):
        self.name = name
        self.last_w = None
        self.readers = []


class Op:
    __slots__ = ("eng", "fn", "deps", "signal", "sig_idx", "is_dma", "dma_sem", "dma_val", "prev_dma")

    def __init__(self, eng, fn, is_dma=False):
        self.eng = eng
        self.fn = fn
        self.deps = []
        self.signal = False
        self.sig_idx = 0
        self.is_dma = is_dma
        self.dma_sem = None
        self.dma_val = 0
        self.prev_dma = None


ENGS = ("pe", "act", "dve", "pool", "sp")
DMA_RING = 8


def _nop(eng):
    return eng.nop()


_nop.is_nop = True


class Sched:
    def __init__(self, nc, same_engine_sync=True):
        self.nc = nc
        self.ops = {e: [] for e in ENGS}
        self.same = same_engine_sync
        self.dma_ops = {e: [] for e in ENGS}

    def op(self, eng, fn, reads=(), writes=(), is_dma=False):
        o = Op(eng, fn, is_dma)
        for b in reads:
            p = b.last_w
            if p is not None:
                if (not p.is_dma) and p.eng == eng and not is_dma:
                    if self.same and eng != "pe":
                        o.deps.append(p)
                else:
                    o.deps.append(p)
        for b in writes:
            p = b.last_w
            if p is not None and (p.is_dma or is_dma or p.eng != eng):
                o.deps.append(p)
            for r in b.readers:
                if r.is_dma or is_dma or r.eng != eng:
                    o.deps.append(r)
        for b in reads:
            b.readers.append(o)
        for b in writes:
            b.last_w = o
            b.readers = []
        if is_dma:
            lst = self.dma_ops[eng]
            n = len(lst)
            if n >= DMA_RING:
                o.prev_dma = lst[n - DMA_RING]
            o.dma_val = 16 * (n // DMA_RING + 1)
            o.dma_sem = n % DMA_RING
            lst.append(o)
        self.ops[eng].append(o)
        return o

    def dma(self, queue, out, in_, reads=(), writes=(), **kw):
        return self.op(queue, lambda e: e.dma_start(out=out, in_=in_, **kw), reads, writes, is_dma=True)

    def barrier(self):
        lasts = []
        for e in ENGS:
            if self.ops[e]:
                for o in reversed(self.ops[e]):
                    if not o.is_dma and not getattr(o.fn, "is_nop", False):
                        lasts.append(o)
                        break
            lasts.extend(self.dma_ops[e][-DMA_RING:])
        for e in ENGS:
            if True:
                o = self.op(e, _nop)
                for p in lasts:
                    if p.is_dma or p.eng != e:
                        o.deps.append(p)

    def finish(self):
        o = self.op("sp", _nop)
        for e in ENGS:
            o.deps.extend(self.dma_ops[e][-DMA_RING:])

    def emit(self, stack):
        nc = self.nc
        for e in ENGS:
            for o in self.ops[e]:
                for p in o.deps:
                    if not p.is_dma:
                        p.signal = True
        for e in ENGS:
            c = 0
            for o in self.ops[e]:
                if o.signal and not o.is_dma:
                    c += 1
                    o.sig_idx = c
        sems = {e: stack.enter_context(nc.semaphore("s_" + e)) for e in ENGS}
        dsems = {e: [stack.enter_context(nc.semaphore("d_%s_%d" % (e, i))) for i in range(DMA_RING)]
                 for e in ENGS if self.dma_ops[e]}
        block = stack.enter_context(nc.Block())
        engobj = {"pe": "tensor", "act": "scalar", "dve": "vector", "pool": "gpsimd", "sp": "sync"}

        def make(ename):
            ops = self.ops[ename]

            def body(eng):
                waited = {}
                dwaited = {}
                for o in ops:
                    deps = list(o.deps)
                    if o.prev_dma is not None:
                        deps.append(o.prev_dma)
                    for p in deps:
                        if p.is_dma:
                            key = (p.eng, p.dma_sem)
                            if dwaited.get(key, 0) >= p.dma_val:
                                continue
                            eng.wait_ge(dsems[p.eng][p.dma_sem], p.dma_val)
                            dwaited[key] = p.dma_val
                        else:
                            if waited.get(p.eng, 0) >= p.sig_idx:
                                continue
                            eng.wait_ge(sems[p.eng], p.sig_idx)
                            waited[p.eng] = p.sig_idx
                    ins = o.fn(eng)
                    if o.is_dma:
                        ins.then_inc(dsems[ename][o.dma_sem], 16)
                    elif o.signal:
                        ins.then_inc(sems[ename], 1)
            return body

        for ename in ENGS:
            if self.ops[ename]:
                getattr(block, engobj[ename])(make(ename))


class Arena:
    def __init__(self, tensor, size):
        self.t = tensor
        self.size = size
        self.off = 0

    def alloc(self, shape, dt):
        n = 1
        for s in shape[1:]:
            n *= s
        nbytes = n * DTSIZE[dt]
        off = (self.off + 63) // 64 * 64
        assert off + nbytes <= self.size, "arena overflow %d + %d > %d" % (off, nbytes, self.size)
        self.off = off + nbytes
        v = self.t[:, off:off + nbytes].bitcast(dt)
        if len(shape) > 2:
            names = ["d%d" % i for i in range(len(shape) - 1)]
            pat = "p (" + " ".join(names) + ") -> p " + " ".join(names)
            kw = {names[i]: shape[i + 1] for i in range(len(names))}
            v = v.rearrange(pat, **kw)
        return v

    def mark(self):
        return self.off

    def reset(self, m):
        self.off = m


class Ring:
    def __init__(self, items):
        self.items = items
        self.bufs = [Buf() for _ in items]
        self.i = 0

    def next(self):
        i = self.i
        self.i = (i + 1) % len(self.items)
        return self.items[i], self.bufs[i]


class Builder:
    def __init__(self, dbg=(), stop_after=None):
        self.dbg = set(dbg)
        self.stop_after = stop_after
        self.nc = bass.Bass("TRN2", target_bir_lowering=False)
        self.S = Sched(self.nc)
        self.deferred = []

    def din(self, name, shape, dt=F32):
        return self.nc.dram_tensor(name, list(shape), dt, kind="ExternalInput").ap()

    def scratch(self, name, shape, dt):
        kind = "ExternalOutput" if name in self.dbg else "Internal"
        return self.nc.dram_tensor(name, list(shape), dt, kind=kind).ap()

    def mm(self, out, lhsT, rhs, start, stop, reads, writes):
        return self.S.op("pe", lambda e: e.matmul(out, lhsT=lhsT, rhs=rhs, start=start, stop=stop), reads, writes)

    def tr(self, out, in_, ident, reads, writes):
        return self.S.op("pe", lambda e: e.transpose(out=out, in_=in_, identity=ident), reads, writes)

    def act(self, out, in_, func, reads, writes, **kw):
        return self.S.op("act", lambda e: e.activation(out=out, in_=in_, func=func, **kw), reads, writes)

    def ts(self, eng, out, in0, s1, s2, op0, op1, reads, writes):
        if op1 is None:
            return self.S.op(eng, lambda e: e.tensor_scalar(out=out, in0=in0, scalar1=s1, scalar2=None, op0=op0), reads, writes)
        return self.S.op(eng, lambda e: e.tensor_scalar(out=out, in0=in0, scalar1=s1, scalar2=s2, op0=op0, op1=op1), reads, writes)

    def tt(self, eng, out, in0, in1, op, reads, writes):
        return self.S.op(eng, lambda e: e.tensor_tensor(out=out, in0=in0, in1=in1, op=op), reads, writes)

    def stt(self, out, in0, scalar, in1, op0, op1, reads, writes):
        return self.S.op("dve", lambda e: e.scalar_tensor_tensor(out=out, in0=in0, scalar=scalar, in1=in1, op0=op0, op1=op1), reads, writes)

    def cp(self, eng, out, in_, reads, writes):
        if eng == "act":
            return self.S.op("act", lambda e: e.copy(out=out, in_=in_), reads, writes)
        return self.S.op(eng, lambda e: e.tensor_copy(out=out, in_=in_), reads, writes)

    def memset(self, eng, ap, val, writes):
        return self.S.op(eng, lambda e: e.memset(ap, val), (), writes)

    def psum(self):
        return self.pring.next()

    def defer(self, fn):
        self.deferred.append(fn)

    def flush(self):
        d, self.deferred = self.deferred, []
        for f in d:
            f()

    def build(self):
        nc = self.nc
        S = self.S
        with ExitStack() as st:
            self.st = st
            self.b_ys2 = Buf()
            self.b_wgu = Buf()
            self.b_wd = Buf()
            arena_t = st.enter_context(nc.sbuf_tensor("arena", [128, 200 * 1024], U8))
            self.A = Arena(arena_t, 200 * 1024)
            pts = [st.enter_context(nc.psum_tensor("ps%d" % i, [128, 512], F32)) for i in range(8)]
            self.pring = Ring(pts)
            self.declare_io()
            self.phase0()
            if self.stop_after != "p0":
                self.phase1()
            if self.stop_after not in ("p0", "p1"):
                S.barrier()
                self.phase2b()
            if self.stop_after not in ("p0", "p1", "p2b") and "noatt" not in self.dbg:
                S.barrier()
                self.phase2a()
            if self.stop_after not in ("p0", "p1", "p2b", "p2a"):
                S.barrier()
                self.phase3()
            if self.stop_after not in ("p0", "p1", "p2b", "p2a", "p3"):
                self.phase4()
            if self.stop_after not in ("p0", "p1", "p2b", "p2a", "p3", "p4"):
                self.phase5()
            S.barrier()
            S.finish()
            S.emit(st)
        return nc

    def declare_io(self):
        self.x = self.din("x", [S_TOK, D])
        self.ct = self.din("ct", [128, 8])
        self.pos = self.din("pos", [1, S_TOK], I32)
        self.w_ada = self.din("w_ada", [D, 6 * D])
        self.b_ada_t = self.din("b_ada_t", [128, 48])
        self.w_adaf = self.din("w_adaf", [D, 2 * D])
        self.b_adaf_t = self.din("b_adaf_t", [128, 16])
        self.gmix_t = self.din("gmix_t", [128, 8])
        self.gffn_t = self.din("gffn_t", [128, 8])
        self.gfin_t = self.din("gfin_t", [128, 8])
        self.w_in = self.din("w_in", [D, 3104])
        self.glat_t = self.din("glat_t", [128, 4])
        self.w_uq = self.din("w_uq", [256, 768])
        self.w_uk = self.din("w_uk", [256, 512])
        self.w_uv = self.din("w_uv", [256, 512])
        self.b_gates = self.din("b_gates", [1, 2048])
        self.ident = self.din("ident", [128, 128])
        self.invf = self.din("invf", [128, 1])
        self.out = self.nc.dram_tensor("out", [S_TOK, D], F32, kind="ExternalOutput").ap()
        self.US = self.scratch("US", [S_TOK, 512], BF16)
        self.SG = self.scratch("SG", [S_TOK, 2048], BF16)
        self.QT = self.scratch("QT", [NH, 96, S_TOK], BF16)
        self.KT = self.scratch("KT", [NH, 64, S_TOK], BF16)
        self.KTR = self.scratch("KTR", [32, S_TOK], BF16)
        self.VS = self.scratch("VS", [S_TOK, 512], BF16)
        self.DBG = self.scratch("DBG", [128, 4096], F32)
        self.OT = self.scratch("OT", [NH, 64, S_TOK], BF16)
        self.YS = self.scratch("YS", [128, 2, 64, 512], BF16)
        self.FO = self.scratch("FO", [S_TOK, D], BF16)
        self.H1 = self.scratch("H1", [S_TOK, D], F32)
        self.VN = self.scratch("VN", [S_TOK, D], BF16)
        self.XS = self.scratch("XS", [NSLOT, D], BF16)
        self.YS2 = self.scratch("YS2", [NSLOT, D], BF16)
        self.WGU = self.scratch("WGU", [32 * 128, 8 * 512], BF16)
        self.WD = self.scratch("WD", [32 * 128, 2 * D], BF16)
        self.RT = self.scratch("RT", [128, 1024], F32)
        self.w_ba = self.din("w_ba", [512, D])
        self.w_out = self.din("w_out", [D, D])
        self.w_rt = self.din("w_rt", [D, 36])
        self.b_rt = self.din("b_rt", [1, 36])
        self.ltri = self.din("ltri", [128, 128], BF16)
        self.tstart = self.din("tstart", [128, NST])
        self.piota = self.din("piota", [128, 1])
        self.w_eg = self.din("w_eg", [32, D, 256])
        self.w_eu = self.din("w_eu", [32, D, 256])
        self.w_ed = self.din("w_ed", [32, 256, D])
        self.tri = self.din("tri", [64, 128, 2, 128], BF16)
        self.d2 = self.din("d2", [128, 128], BF16)
        self.bdc = self.din("bdc", [128, 128])
        self.bds = self.din("bds", [128, 128])
        self.w_bf = self.din("w_bf", [512, D])

    def phase0(self):
        S, A = self.S, self.A
        self.ident_f = A.alloc([128, 128], F32); self.b_ident_f = Buf()
        self.ident_b = A.alloc([128, 128], BF16); self.b_ident_b = Buf()
        self.ones_f = A.alloc([128, 128], F32); self.b_ones_f = Buf()
        self.ones_b = A.alloc([128, 128], BF16); self.b_ones_b = Buf()
        self.modT = A.alloc([128, 64], F32); self.b_modT = Buf()
        self.vecs = A.alloc([128, 8, 8], F32); self.b_vecs = Buf()
        self.bc = A.alloc([128, 4, D], F32); self.b_bc = Buf()
        self.mhalf = A.alloc([128, 2], F32); self.b_mhalf = Buf()
        cact = A.alloc([128, 8, 2], F32); b_cact = Buf()
        ctt = A.alloc([128, 8], F32); b_ctt = Buf()
        small = A.alloc([128, 128], F32); b_small = Buf()
        self.small = small; self.b_small = b_small
        S.dma("sp", self.ident_f, self.ident[:, :], writes=[self.b_ident_f])
        S.dma("sp", ctt, self.ct[:, :], writes=[b_ctt])
        S.dma("sp", small[:, 0:48], self.b_ada_t[:, :], writes=[b_small])
        S.dma("sp", small[:, 48:64], self.b_adaf_t[:, :], writes=[b_small])
        S.dma("sp", small[:, 64:72], self.gmix_t[:, :], writes=[b_small])
        S.dma("sp", small[:, 72:80], self.gffn_t[:, :], writes=[b_small])
        S.dma("sp", small[:, 80:88], self.gfin_t[:, :], writes=[b_small])
        S.dma("sp", small[:, 88:92], self.glat_t[:, :], writes=[b_small])
        S.dma("sp", small[:, 92:93], self.invf[:, :], writes=[b_small])
        self.cp("dve", self.ident_b, self.ident_f, [self.b_ident_f], [self.b_ident_b])
        self.memset("pool", self.ones_f, 1.0, [self.b_ones_f])
        self.memset("pool", self.ones_b, 1.0, [self.b_ones_b])
        self.memset("pool", self.mhalf, -0.5, [self.b_mhalf])
        self.act(cact[:, :, 0], ctt, AF.Silu, [b_ctt], [b_cact])
        self.act(cact[:, :, 1], ctt, AF.Silu, [b_ctt], [b_cact])

        m0 = A.mark()
        wring = Ring([A.alloc([128, 8, 512], F32) for _ in range(3)])
        Rrow = A.alloc([128, 8192], F32); bRrow = Buf()
        one2 = A.alloc([128, 2], F32); bone2 = Buf()
        self.memset("dve", one2, 1.0, [bone2])
        wl = {}

        def wload(piece):
            wt, bw = wring.next()
            if piece < 12:
                src = self.w_ada[:, piece * 512:(piece + 1) * 512]
            else:
                src = self.w_adaf[:, (piece - 12) * 512:(piece - 11) * 512]
            S.dma("sp", wt, src.rearrange("(k p) n -> p k n", p=128), writes=[bw])
            wl[piece] = (wt, bw)
        wload(0)
        wload(1)
        for piece in range(16):
            if piece + 2 < 16:
                wload(piece + 2)
            wt, bw = wl.pop(piece)
            pt, bp = self.psum()
            for k in range(8):
                self.mm(pt[0:2, :], cact[:, k, :], wt[:, k, :], k == 0, k == 7, [bw, b_cact], [bp])
            self.cp("act" if piece % 2 else "dve", Rrow[0:2, piece * 512:(piece + 1) * 512], pt[0:2, :], [bp], [bRrow])
        pt, bp = self.psum()
        pv = pt[:, 0:128].rearrange("p (a b) -> p a b", b=2)
        for j in range(64):
            self.mm(pv[:, j, :], Rrow[0:1, j * 128:(j + 1) * 128], one2[0:1, :], True, True, [bRrow, bone2], [bp])
        self.tt("dve", self.modT, pv[:, :, 0], small[:, 0:64], ALU.add, [bp, b_small], [self.b_modT])
        S.barrier()
        A.reset(m0)
        mT = self.modT
        V = self.vecs
        bv = self.b_vecs
        self.stt(V[:, 0, :], mT[:, 8:16], 1.0, small[:, 64:72], ALU.add, ALU.mult, [self.b_modT, b_small], [bv])
        self.cp("dve", V[:, 1, :], mT[:, 0:8], [self.b_modT], [bv])
        self.stt(V[:, 2, :], mT[:, 32:40], 1.0, small[:, 72:80], ALU.add, ALU.mult, [self.b_modT, b_small], [bv])
        self.cp("dve", V[:, 3, :], mT[:, 24:32], [self.b_modT], [bv])
        self.stt(V[:, 4, :], mT[:, 56:64], 1.0, small[:, 80:88], ALU.add, ALU.mult, [self.b_modT, b_small], [bv])
        diag_r = Ring([A.alloc([128, 128], F32) for _ in range(2)])
        srcs = [mT[:, 16:24], mT[:, 40:48], V[:, 4, :], mT[:, 48:56]]
        for i, sv in enumerate(srcs):
            for j in range(8):
                dg, bd = diag_r.next()
                self.ts("dve", dg, self.ident_f, sv[:, j:j + 1], None, ALU.mult, None,
                        [self.b_ident_f, self.b_modT, bv], [bd])
                pt, bp = self.psum()
                self.mm(pt[:, 0:128], self.ones_f, dg, True, True, [self.b_ones_f, bd], [bp])
                self.cp("act", self.bc[:, i, j * 128:(j + 1) * 128], pt[:, 0:128], [bp], [self.b_bc])

        self.m_persist = A.mark()
        self.cosT = A.alloc([128, S_TOK], BF16); self.b_cosT = Buf()
        self.sinT = A.alloc([128, S_TOK], BF16); self.b_sinT = Buf()
        m1 = A.mark()
        PW = 2048
        pi_r = Ring([A.alloc([128, PW], I32) for _ in range(2)])
        ang_r = Ring([A.alloc([128, PW], F32) for _ in range(2)])
        ki_r = Ring([A.alloc([128, PW], I32) for _ in range(1)])
        kf_r = Ring([A.alloc([128, PW], F32) for _ in range(1)])
        r_r = Ring([A.alloc([128, PW], F32) for _ in range(2)])
        ab_r = Ring([A.alloc([128, PW], F32) for _ in range(1)])
        R = slice(64, 96)
        invf = small[R, 92:93]
        for pc in range(S_TOK // PW):
            sl = slice(pc * PW, (pc + 1) * PW)
            pi, bpi = pi_r.next()
            S.dma("sp", pi[R, :], self.pos[0:1, sl].partition_broadcast(32), writes=[bpi])
            ang, bang = ang_r.next()
            self.cp("dve", ang[R, :], pi[R, :], [bpi], [bang])
            self.ts("dve", ang[R, :], ang[R, :], invf, None, ALU.mult, None, [bang, b_small], [bang])
            ki, bki = ki_r.next()
            self.ts("dve", ki[R, :], ang[R, :], 1.0 / TWO_PI, None, ALU.mult, None, [bang], [bki])
            kf, bkf = kf_r.next()
            self.cp("dve", kf[R, :], ki[R, :], [bki], [bkf])
            r, br = r_r.next()
            self.stt(r[R, :], kf[R, :], -CW1, ang[R, :], ALU.mult, ALU.add, [bkf, bang], [br])
            self.stt(r[R, :], kf[R, :], -CW2, r[R, :], ALU.mult, ALU.add, [bkf, br], [br])
            self.ts("dve", r[R, :], r[R, :], math.pi, -math.pi, ALU.min, ALU.max, [br], [br])
            self.act(self.sinT[R, sl], r[R, :], AF.Sin, [br], [self.b_sinT])
            ab, bab = ab_r.next()
            self.stt(ab[R, :], r[R, :], -1.0, r[R, :], ALU.mult, ALU.max, [br], [bab])
            self.act(self.cosT[R, sl], ab[R, :], AF.Sin, [bab], [self.b_cosT], scale=-1.0, bias=math.pi / 2)
        S.barrier()
        A.reset(m1)

        self.Wlat = A.alloc([128, 8, 512], BF16)
        self.Wkr = A.alloc([128, 8, 2, 96], BF16)
        self.Wf = A.alloc([128, 8, 512], BF16)
        self.Wg = A.alloc([128, 8, 2048], BF16)
        self.Wuq = A.alloc([128, 2, NH, 96], BF16)
        self.WuqR = A.alloc([128, 2, NH, 96], BF16)
        self.Wuk = A.alloc([128, 2, 512], BF16)
        self.Wuv = A.alloc([128, 2, 512], BF16)
        self.bg_b = A.alloc([128, 2048], BF16)
        self.b_W1 = Buf()
        bW = self.b_W1
        m2 = A.mark()
        stg = Ring([A.alloc([128, 8, 512], F32) for _ in range(2)])
        self.memset("pool", self.Wkr, 0.0, [bW])
        self.memset("pool", self.WuqR, 0.0, [bW])
        win = self.w_in

        def wsrc(c0, c1):
            return win[:, c0:c1].rearrange("(k p) n -> p k n", p=128)
        t, b = stg.next()
        S.dma("sp", t, wsrc(0, 512), writes=[b])
        self.cp("dve", self.Wlat, t, [b], [bW])
        t, b = stg.next()
        S.dma("sp", t[:, :, 0:32], wsrc(512, 544), writes=[b])
        self.cp("dve", self.Wkr[:, :, 0, 64:96], t[:, :, 0:32], [b], [bW])
        self.ts("dve", self.Wkr[:, :, 1, 64:80], t[:, :, 16:32], -1.0, None, ALU.mult, None, [b], [bW])
        self.cp("dve", self.Wkr[:, :, 1, 80:96], t[:, :, 0:16], [b], [bW])
        t, b = stg.next()
        S.dma("sp", t, wsrc(544, 1056), writes=[b])
        self.cp("act", self.Wf, t, [b], [bW])
        for g in range(4):
            t, b = stg.next()
            S.dma("sp", t, wsrc(1056 + g * 512, 1056 + (g + 1) * 512), writes=[b])
            self.cp("dve" if g % 2 == 0 else "act", self.Wg[:, :, g * 512:(g + 1) * 512], t, [b], [bW])
        tq2 = A.alloc([128, 2, 768], F32); btq2 = Buf()
        S.dma("sp", tq2, self.w_uq.rearrange("(j p) n -> p j n", p=128), writes=[btq2])
        tq2v = tq2.rearrange("p j (h d) -> p j h d", h=NH)
        self.cp("dve", self.Wuq, tq2v, [btq2], [bW])
        self.ts("dve", self.WuqR[:, :, :, 64:80], tq2v[:, :, :, 80:96], -1.0, None, ALU.mult, None, [btq2], [bW])
        self.cp("dve", self.WuqR[:, :, :, 80:96], tq2v[:, :, :, 64:80], [btq2], [bW])
        t, b = stg.next()
        S.dma("sp", t[:, 0:2, :], self.w_uk.rearrange("(j p) n -> p j n", p=128), writes=[b])
        self.cp("dve", self.Wuk, t[:, 0:2, :], [b], [bW])
        t, b = stg.next()
        S.dma("sp", t[:, 0:2, :], self.w_uv.rearrange("(j p) n -> p j n", p=128), writes=[b])
        self.cp("dve", self.Wuv, t[:, 0:2, :], [b], [bW])
        t, b = stg.next()
        tb = t[0:1, 0:4, :].rearrange("p a b -> p (a b)")
        S.dma("sp", tb, self.b_gates[0:1, :], writes=[b])
        self.cp("dve", self.bg_b[0:1, :], tb, [b], [bW])
        S.barrier()
        A.reset(m2)
        if "DBG" in self.dbg:
            S.dma("sp", self.DBG[:, 0:64], self.modT, reads=[self.b_modT], writes=[Buf()])
            S.dma("sp", self.DBG[:, 64:128], self.vecs.rearrange("p a b -> p (a b)"), reads=[self.b_vecs], writes=[Buf()])
            S.dma("sp", self.DBG[:, 1024:2048], self.bc[:, 0, :], reads=[self.b_bc], writes=[Buf()])
            S.dma("sp", self.DBG[:, 2048:3072], self.bc[:, 2, :], reads=[self.b_bc], writes=[Buf()])

    def phase1(self):
        S, A = self.S, self.A
        m0 = A.mark()
        bW = self.b_W1
        V = self.vecs
        small = self.small
        xr = Ring([A.alloc([128, D], F32) for _ in range(3)])
        junk = A.alloc([128, D], BF16); b_junk = Buf()
        xnr = Ring([A.alloc([128, D], BF16) for _ in range(2)])
        uTr = Ring([A.alloc([128, 8, CH], BF16) for _ in range(2)])
        latTr = Ring([A.alloc([128, 4, CH], BF16) for _ in range(2)])
        stat = A.alloc([128, NT, 4], F32)
        stat2 = A.alloc([128, NT, 4], F32)
        znr = Ring([A.alloc([128, 512], BF16) for _ in range(2)])
        vsr = Ring([A.alloc([128, 512], BF16) for _ in range(3)])
        usr = Ring([A.alloc([128, 512], BF16) for _ in range(3)])
        sgr = Ring([A.alloc([128, 2048], BF16) for _ in range(3)])
        qtr = Ring([A.alloc([128, CH], BF16) for _ in range(3)])
        ktr = Ring([A.alloc([128, CH], BF16) for _ in range(3)])
        t1r = Ring([A.alloc([128, CH], F32) for _ in range(2)])
        t2r = Ring([A.alloc([128, CH], F32) for _ in range(2)])
        xT = {}
        state = {}

        def load_x(t):
            xt, bx = xr.next()
            S.dma("sp", xt, self.x[t * 128:(t + 1) * 128, :], writes=[bx])
            xT[t] = (xt, bx)

        xns = {}
        zns = {}

        def stageA1(t):
            xt, bx = xT.pop(t)
            bst = Buf()
            self.act(junk, xt, AF.Square, [bx], [b_junk, bst], accum_out=stat[:, t, 0:1])
            self.ts("dve", stat[:, t, 1:2], stat[:, t, 0:1], 1.0 / D, EPS, ALU.mult, ALU.add, [bst], [bst])
            self.tt("pool", stat[:, t, 2:3], stat[:, t, 1:2], self.mhalf[:, 0:1], ALU.pow, [bst, self.b_mhalf], [bst])
            xn, bxn = xnr.next()
            self.act(xn, xt, AF.Copy, [bx, bst], [bxn], scale=stat[:, t, 2:3])
            xns[t] = (xn, bxn)

        def stageA2(t):
            c, s = divmod(t, 4)
            if s == 0:
                state[c] = (uTr.next(), latTr.next())
            (uT, buT), (latT, blatT) = state[c]
            xn, bxn = xns.pop(t)
            pt, bp = self.psum()
            pv = pt[:, :].bitcast(BF16).rearrange("p (k n) -> p k n", k=8)
            for k in range(8):
                self.tr(pv[:, k, :], xn[:, k * 128:(k + 1) * 128], self.ident_b, [bxn, self.b_ident_b], [bp])
            for k in range(8):
                self.ts("dve", uT[:, k, s * 128:(s + 1) * 128], pv[:, k, :], V[:, 0, k:k + 1], V[:, 1, k:k + 1],
                        ALU.mult, ALU.add, [bp, self.b_vecs], [buT])

        def stageB1(t):
            c, s = divmod(t, 4)
            (uT, buT), (latT, blatT) = state[c]
            ts_ = slice(s * 128, (s + 1) * 128)
            pt, bp = self.psum()
            for k in range(8):
                self.mm(pt[:, :], uT[:, k, ts_], self.Wlat[:, k, :], k == 0, k == 7, [buT, bW], [bp])
            bst = Buf()
            self.act(junk[:, 0:256], pt[:, 0:256], AF.Square, [bp], [b_junk, bst], accum_out=stat2[:, t, 0:1])
            self.act(junk[:, 256:512], pt[:, 256:512], AF.Square, [bp], [b_junk, bst], accum_out=stat2[:, t, 1:2])
            self.ts("dve", stat2[:, t, 0:2], stat2[:, t, 0:2], 1.0 / 256, EPS, ALU.mult, ALU.add, [bst], [bst])
            self.tt("pool", stat2[:, t, 2:4], stat2[:, t, 0:2], self.mhalf[:, 0:2], ALU.pow,
                    [bst, self.b_mhalf], [bst])
            zn, bzn = znr.next()
            self.ts("dve", zn[:, 0:256], pt[:, 0:256], stat2[:, t, 2:3], None, ALU.mult, None, [bp, bst], [bzn])
            self.act(zn[:, 256:512], pt[:, 256:512], AF.Copy, [bp, bst], [bzn], scale=stat2[:, t, 3:4])
            zns[t] = (zn, bzn)

        def stageB2(t):
            c, s = divmod(t, 4)
            (uT, buT), (latT, blatT) = state[c]
            ts_ = slice(s * 128, (s + 1) * 128)
            zn, bzn = zns.pop(t)
            pt2, bp2 = self.psum()
            pv2 = pt2[:, 0:256].bitcast(BF16).rearrange("p (k n) -> p k n", k=4)
            for j in range(4):
                self.tr(pv2[:, j, :], zn[:, j * 128:(j + 1) * 128], self.ident_b, [bzn, self.b_ident_b], [bp2])
            for j in range(4):
                self.ts("dve", latT[:, j, ts_], pv2[:, j, :], small[:, 88 + j:89 + j], None, ALU.mult, None,
                        [bp2, self.b_small], [blatT])

        def stageC(t):
            c, s = divmod(t, 4)
            (uT, buT), (latT, blatT) = state[c]
            ts_ = slice(s * 128, (s + 1) * 128)
            rows = slice(t * 128, (t + 1) * 128)
            pt, bp = self.psum()
            for j in range(2):
                self.mm(pt[:, :], latT[:, 2 + j, ts_], self.Wuv[:, j, :], j == 0, j == 1, [blatT, bW], [bp])
            vs, bvs = vsr.next()
            self.cp("act", vs, pt[:, :], [bp], [bvs])
            self.defer(lambda: S.dma("sp", self.VS[rows, :], vs, reads=[bvs], writes=[Buf()]))
            pt, bp = self.psum()
            for k in range(8):
                self.mm(pt[:, :], uT[:, k, ts_], self.Wf[:, k, :], k == 0, k == 7, [buT, bW], [bp])
            us, bus = usr.next()
            self.cp("dve", us, pt[:, :], [bp], [bus])
            self.defer(lambda: S.dma("sp", self.US[rows, :], us, reads=[bus], writes=[Buf()]))
            sg, bsg = sgr.next()
            for g in range(4):
                pt, bp = self.psum()
                gs = slice(g * 512, (g + 1) * 512)
                for k in range(8):
                    self.mm(pt[:, :], uT[:, k, ts_], self.Wg[:, k, gs], k == 0, False, [buT, bW], [bp])
                self.mm(pt[:, :], self.ones_b[0:1, :], self.bg_b[0:1, gs], False, True, [self.b_ones_b, bW], [bp])
                self.act(sg[:, gs], pt[:, :], AF.Sigmoid, [bp], [bsg])
            self.defer(lambda: S.dma("sp", self.SG[rows, :], sg, reads=[bsg], writes=[Buf()]))

        def stageD(c):
            (uT, buT), (latT, blatT) = state[c]
            cs = slice(c * CH, (c + 1) * CH)
            R = slice(64, 96)
            for h in range(NH):
                pa, bpa = self.psum()
                for j in range(2):
                    self.mm(pa[0:96, :], self.Wuq[:, j, h, :], latT[:, j, :], j == 0, j == 1, [bW, blatT], [bpa])
                pb, bpb = self.psum()
                for j in range(2):
                    self.mm(pb[0:96, :], self.WuqR[:, j, h, :], latT[:, j, :], j == 0, j == 1, [bW, blatT], [bpb])
                qt, bqt = qtr.next()
                self.act(qt[0:64, :], pa[0:64, :], AF.Copy, [bpa], [bqt], scale=ATT_SCALE)
                t1, bt1 = t1r.next()
                t2, bt2 = t2r.next()
                self.stt(t1[R, :], pa[R, :], ATT_SCALE, self.cosT[R, cs], ALU.mult, ALU.mult, [bpa, self.b_cosT], [bt1])
                self.stt(t2[R, :], pb[R, :], ATT_SCALE, self.sinT[R, cs], ALU.mult, ALU.mult, [bpb, self.b_sinT], [bt2])
                self.tt("pool", qt[R, :], t1[R, :], t2[R, :], ALU.add, [bt1, bt2], [bqt])
                S.dma("sp", self.QT[h, :, cs], qt[0:96, :], reads=[bqt], writes=[Buf()])
                pk, bpk = self.psum()
                for j in range(2):
                    self.mm(pk[0:64, :], self.Wuk[:, j, h * 64:(h + 1) * 64], latT[:, 2 + j, :], j == 0, j == 1,
                            [bW, blatT], [bpk])
                kt, bkt = ktr.next()
                self.cp("act", kt[0:64, :], pk[0:64, :], [bpk], [bkt])
                S.dma("sp", self.KT[h, :, cs], kt[0:64, :], reads=[bkt], writes=[Buf()])
            pa, bpa = self.psum()
            for k in range(8):
                self.mm(pa[0:96, :], self.Wkr[:, k, 0, :], uT[:, k, :], k == 0, k == 7, [bW, buT], [bpa])
            pb, bpb = self.psum()
            for k in range(8):
                self.mm(pb[0:96, :], self.Wkr[:, k, 1, :], uT[:, k, :], k == 0, k == 7, [bW, buT], [bpb])
            t1, bt1 = t1r.next()
            t2, bt2 = t2r.next()
            self.tt("dve", t1[R, :], pa[R, :], self.cosT[R, cs], ALU.mult, [bpa, self.b_cosT], [bt1])
            self.tt("dve", t2[R, :], pb[R, :], self.sinT[R, cs], ALU.mult, [bpb, self.b_sinT], [bt2])
            kt, bkt = ktr.next()
            self.tt("pool", kt[R, :], t1[R, :], t2[R, :], ALU.add, [bt1, bt2], [bkt])
            S.dma("sp", self.KTR[:, cs], kt[R, :], reads=[bkt], writes=[Buf()])

        load_x(0)
        load_x(1)
        stages = [stageA1, stageA2, stageB1, stageB2, stageC]
        for i in range(NT + len(stages) - 1):
            if i + 2 < NT:
                load_x(i + 2)
            self.flush()
            for k_ in P1_ORDER:
                if 0 <= i - k_ < NT:
                    stages[k_](i - k_)
            tl = i - (len(stages) - 1)
            if 0 <= tl < NT and tl % 4 == 3:
                stageD(tl // 4)
        self.flush()
        S.barrier()
        A.reset(self.m_persist)


    def phase2b(self):
        S, A = self.S, self.A
        m0 = A.mark()
        WCS = A.alloc([128, 4, 2, D], BF16); bWCS = Buf()
        D2 = A.alloc([128, 128], BF16); bD2 = Buf()
        m1 = A.mark()
        wbf = A.alloc([128, 4, D], F32); bwbf = Buf()
        bd = A.alloc([128, 2, 128], F32); bbd = Buf()
        S.dma("sp", wbf, self.w_bf.rearrange("(j p) n -> p j n", p=128), writes=[bwbf])
        S.dma("sp", bd[:, 0, :], self.bdc[:, :], writes=[bbd])
        S.dma("sp", bd[:, 1, :], self.bds[:, :], writes=[bbd])
        S.dma("sp", D2, self.d2[:, :], writes=[bD2])
        for j in range(4):
            for r in range(2):
                for hf in range(2):
                    ps, bps = self.psum()
                    hs = slice(hf * 512, (hf + 1) * 512)
                    self.mm(ps[:, :], bd[:, r, :], wbf[:, j, hs], True, True, [bbd, bwbf], [bps])
                    self.cp("act" if hf else "dve", WCS[:, j, r, hs], ps[:, :], [bps], [bWCS])
        S.barrier()
        A.reset(m1)
        tbr = Ring([A.alloc([128, 2, 128], BF16) for _ in range(6)])
        unr = Ring([A.alloc([128, 512], BF16) for _ in range(6)])
        yor = Ring([A.alloc([128, 2, 512], BF16) for _ in range(4)])
        USv = self.US.rearrange("(n1 n2) c -> n2 n1 c", n2=64)
        ld1 = {}

        def load1(n2):
            tb, btb = tbr.next()
            S.dma("sp", tb, self.tri[n2, :, :, :], writes=[btb])
            un, bun = unr.next()
            S.dma("sp", un, USv[n2, :, :], writes=[bun])
            ld1[n2] = (tb, btb, un, bun)
        PF = 4
        for n2 in range(PF):
            load1(n2)
        for n2 in range(64):
            if n2 + PF < 64:
                load1(n2 + PF)
            self.flush()
            tb, btb, un, bun = ld1.pop(n2)
            yo, byo = yor.next()
            for r in range(2):
                ps, bps = self.psum()
                self.mm(ps[:, :], tb[:, r, :], un, True, True, [btb, bun], [bps])
                self.cp("act" if r else "dve", yo[:, r, :], ps[:, :], [bps], [byo])
            self.defer(lambda yo=yo, byo=byo, n2=n2: S.dma("sp", self.YS[:, :, n2, :], yo, reads=[byo], writes=[Buf()]))
        self.flush()
        S.barrier()
        ysr = Ring([A.alloc([128, 512], BF16) for _ in range(8)])
        gtr = Ring([A.alloc([128, 4, 2, 2, 64], BF16) for _ in range(3)])
        for_ = Ring([A.alloc([128, D], BF16) for _ in range(4)])
        FOv = self.FO.rearrange("(k2 k1) d -> k1 k2 d", k1=128)
        ld2 = {}

        def load2(k1):
            ys, bys = ysr.next()
            S.dma("sp", ys, self.YS[k1, :, :, :].rearrange("r n c -> (r n) c"), writes=[bys])
            ld2[k1] = (ys, bys)
        PF2 = 6
        for k1 in range(PF2):
            load2(k1)
        gts = {}

        def d2stage(kp):
            gt, bgt = gtr.next()
            for pr in range(2):
                k1 = 2 * kp + pr
                if k1 + PF2 < 128:
                    load2(k1 + PF2)
                ys, bys = ld2.pop(k1)
                ps, bps = self.psum()
                for j in range(4):
                    self.mm(ps[:, j * 128:(j + 1) * 128], ys[:, j * 128:(j + 1) * 128], D2, True, True,
                            [bys, bD2], [bps])
                self.cp("dve" if pr else "act", gt[:, :, :, pr, :],
                        ps[:, :].rearrange("p (j q k) -> p j q k", j=4, q=2), [bps], [bgt])
            gts[kp] = (gt, bgt)

        def proj(kp):
            gt, bgt = gts.pop(kp)
            fo, bfo = for_.next()
            for hf in range(2):
                hs = slice(hf * 512, (hf + 1) * 512)
                ps, bps = self.psum()
                n = 0
                for j in range(4):
                    for q in range(2):
                        self.mm(ps[:, :], gt[:, j, q, :, :].rearrange("p a b -> p (a b)"), WCS[:, j, q, hs],
                                n == 0, n == 7, [bgt, bWCS], [bps])
                        n += 1
                self.cp("act" if hf else "dve", fo[:, hs], ps[:, :], [bps], [bfo])
            self.defer(lambda: S.dma("sp", FOv[2 * kp, :, :], fo[0:64, :], reads=[bfo], writes=[Buf()]))
            self.defer(lambda: S.dma("sp", FOv[2 * kp + 1, :, :], fo[64:128, :], reads=[bfo], writes=[Buf()]))

        d2stage(0)
        for kp in range(64):
            self.flush()
            if kp + 1 < 64:
                d2stage(kp + 1)
            proj(kp)
        self.flush()
        S.barrier()
        A.reset(m0)

    def phase2a(self):
        S, A = self.S, self.A
        m0 = A.mark()
        pts = self.pring.items
        sring = Ring(pts[0:6])
        oring = Ring(pts[6:8])
        Vall = A.alloc([128, NT, NH, 65], BF16); bVs = [Buf() for _ in range(8)]
        vst = Ring([A.alloc([128, 8, 512], BF16) for _ in range(2)])
        Kr = Ring([A.alloc([128, S_TOK], BF16) for _ in range(2)])
        Qr = Ring([A.alloc([128, CH], BF16) for _ in range(3)])
        Pr = Ring([A.alloc([128, CH], BF16) for _ in range(4)])
        srr = Ring([A.alloc([128, CH], F32) for _ in range(2)])
        recr = Ring([A.alloc([128, CH], F32) for _ in range(2)])
        onr = Ring([A.alloc([128, CH], BF16) for _ in range(2)])
        self.memset("pool", Vall[:, :, :, 64:65], 1.0, bVs)
        for g in range(8):
            vs, bvs = vst.next()
            S.dma("sp", vs, self.VS[g * 1024:(g + 1) * 1024, :].rearrange("(t p) n -> p t n", p=128), writes=[bvs])
            self.cp("pool" if g % 2 else "dve", Vall[:, g * 8:(g + 1) * 8, :, 0:64],
                    vs.rearrange("p t (h d) -> p t h d", h=NH), [bvs], [bVs[g]])
        kbufs = []
        for i in range(2):
            kb, bk = Kr.next()
            brope = Buf()
            S.dma("sp", kb[64:96, :], self.KTR[:, :], writes=[brope])
            kbufs.append((kb, bk, brope))
        LAG = 2
        cst = Ring([A.alloc([128, 2048], F32) for _ in range(3)])
        cbf = Ring([A.alloc([128, 2048], BF16) for _ in range(2)])
        WGUv = self.WGU.rearrange("r (k f) -> r k f", k=8)
        units = [(e_, kind) for e_ in range(32) for kind in range(3)]
        cld = {}

        def conv_load(u):
            if u >= len(units):
                return
            e_, kind = units[u]
            t_, b_ = cst.next()
            if kind == 0:
                S.dma("sp", t_.rearrange("p (k f) -> p k f", k=8),
                      self.w_eg[e_, :, :].rearrange("(k p) f -> p k f", p=128), writes=[b_])
            elif kind == 1:
                S.dma("sp", t_.rearrange("p (k f) -> p k f", k=8),
                      self.w_eu[e_, :, :].rearrange("(k p) f -> p k f", p=128), writes=[b_])
            else:
                S.dma("sp", t_.rearrange("p (j d) -> p j d", j=2),
                      self.w_ed[e_, :, :].rearrange("(j p) d -> p j d", p=128), writes=[b_])
            cld[u] = (t_, b_)

        def conv_cast(u):
            if u >= len(units):
                return
            e_, kind = units[u]
            t_, b_ = cld.pop(u)
            o_, bo_ = cbf.next()
            self.cp("pool", o_, t_, [b_], [bo_])
            rows = slice(e_ * 128, (e_ + 1) * 128)
            if kind == 0:
                S.dma("sp", WGUv[rows, :, 0:256], o_.rearrange("p (k f) -> p k f", k=8), reads=[bo_], writes=[Buf()])
            elif kind == 1:
                S.dma("sp", WGUv[rows, :, 256:512], o_.rearrange("p (k f) -> p k f", k=8), reads=[bo_], writes=[Buf()])
            else:
                S.dma("sp", self.WD[rows, :], o_, reads=[bo_], writes=[Buf()])

        conv_load(0)
        zt = A.alloc([128, 4096], BF16); bzt = Buf()
        self.memset("pool", zt, 0.0, [bzt])
        XSz = self.XS.rearrange("(p r) d -> p (r d)", p=128)
        nz = (NSLOT // 128) * D // 4096

        def zero_fill(i):
            if 0 <= i < nz:
                S.dma("sp", XSz[:, i * 4096:(i + 1) * 4096], zt, reads=[bzt], writes=[Buf()])

        def load_k(h):
            kb, bk, brope = kbufs[h % 2]
            S.dma("sp", kb[0:64, :], self.KT[h, :, :], writes=[bk])

        load_k(0)
        fin_pending = []
        for h in range(NH):
            kb, bk, brope = kbufs[h % 2]
            if h + 1 < NH:
                load_k(h + 1)
            qts = {}

            def load_q(qc):
                qt, bq = Qr.next()
                S.dma("sp", qt[0:96, :], self.QT[h, :, qc * CH:(qc + 1) * CH], writes=[bq])
                qts[qc] = (qt, bq)
            load_q(0)
            load_q(1)
            for qc in range(NCH):
                if qc + 2 < NCH:
                    load_q(qc + 2)
                conv_load(h * NCH + qc + 1)
                conv_cast(h * NCH + qc)
                zero_fill(h * NCH + qc - 70)
                qt, bq = qts.pop(qc)
                po, bpo = oring.next()
                pend = []

                def pv(kt, pT, bpT):
                    self.mm(po[0:65, :], Vall[:, kt, h, :], pT, kt == 0, kt == NT - 1, [bVs[kt // 8], bpT], [bpo])
                for kt in range(NT):
                    if kt == 3 and fin_pending:
                        fin_pending.pop(0)()
                    ps, bps = sring.next()
                    self.mm(ps[:, :], kb[0:96, kt * 128:(kt + 1) * 128], qt[0:96, :], True, True,
                            [bk, brope, bq], [bps])
                    pT, bpT = Pr.next()
                    self.act(pT, ps[:, :], AF.Exp, [bps], [bpT])
                    pend.append((kt, pT, bpT))
                    if len(pend) > LAG:
                        pv(*pend.pop(0))
                while pend:
                    pv(*pend.pop(0))
                def fin(po=po, bpo=bpo, h=h, qc=qc):
                    sr, bsr = srr.next()
                    self.cp("dve", sr[64:65, :], po[64:65, :], [bpo], [bsr])
                    pb, bpb = sring.next()
                    self.mm(pb[0:64, :], self.ones_f[64:65, 0:64], sr[64:65, :], True, True, [self.b_ones_f, bsr], [bpb])
                    rec, brec = recr.next()
                    self.S.op("dve", lambda e, o_=rec[0:64, :], i_=pb[0:64, :]: e.reciprocal(out=o_, in_=i_), [bpb], [brec])
                    on, bon = onr.next()
                    self.tt("dve", on[0:64, :], po[0:64, :], rec[0:64, :], ALU.mult, [bpo, brec], [bon])
                    S.dma("sp", self.OT[h, :, qc * CH:(qc + 1) * CH], on[0:64, :], reads=[bon], writes=[Buf()])
                fin_pending.append(fin)
        while fin_pending:
            fin_pending.pop(0)()
        S.barrier()
        A.reset(m0)

    def phase3(self):
        S, A = self.S, self.A
        V = self.vecs
        self.w12 = A.alloc([128, NT, 2], F32); self.b_w12 = Buf()
        self.slot_i = A.alloc([128, 2, NT], I32); self.b_slot = Buf()
        self.idxw = A.alloc([128, NST], I32); self.b_idxw = Buf()
        self.m_persist = A.mark()
        M12 = A.alloc([128, 2, NT, 32], BF16); bM = Buf()
        rank = A.alloc([128, NT, 32], F32); brank = Buf()
        Rr = A.alloc([128, 32], F32); bR = Buf()
        m0 = A.mark()
        Wba = A.alloc([128, 4, D], BF16)
        Wout = A.alloc([128, 8, D], BF16)
        Wr = A.alloc([128, 8, 36], BF16)
        br_b = A.alloc([128, 36], BF16)
        Ltri = A.alloc([128, 128], BF16)
        AB2 = A.alloc([128, 2, D], F32)
        bW = Buf()
        m1 = A.mark()
        stg = Ring([A.alloc([128, 8, 512], F32) for _ in range(2)])
        for hf in range(2):
            t, b = stg.next()
            S.dma("sp", t[:, 0:4, :], self.w_ba[:, hf * 512:(hf + 1) * 512].rearrange("(j p) n -> p j n", p=128), writes=[b])
            self.cp("dve", Wba[:, :, hf * 512:(hf + 1) * 512], t[:, 0:4, :], [b], [bW])
        for hf in range(2):
            t, b = stg.next()
            S.dma("sp", t, self.w_out[:, hf * 512:(hf + 1) * 512].rearrange("(k p) n -> p k n", p=128), writes=[b])
            self.tt("dve", Wout[:, :, hf * 512:(hf + 1) * 512], t,
                    self.bc[:, 0, hf * 512:(hf + 1) * 512].unsqueeze(1).broadcast_to([128, 8, 512]), ALU.mult,
                    [b, self.b_bc], [bW])
        t, b = stg.next()
        S.dma("sp", t[:, :, 0:36], self.w_rt.rearrange("(k p) n -> p k n", p=128), writes=[b])
        self.cp("dve", Wr, t[:, :, 0:36], [b], [bW])
        t, b = stg.next()
        S.dma("sp", t[0:1, 0, 0:36], self.b_rt[0:1, :], writes=[b])
        self.cp("dve", br_b[0:1, :], t[0:1, 0, 0:36], [b], [bW])
        S.dma("sp", Ltri, self.ltri[:, :], writes=[bW])
        self.memset("dve", Rr, 0.0, [bR])
        diag_r = Ring([A.alloc([128, 128], F32) for _ in range(2)])
        for i in range(2):
            for j in range(8):
                dg, bd = diag_r.next()
                self.ts("dve", dg, self.ident_f, V[:, 2 + i, j:j + 1], None, ALU.mult, None,
                        [self.b_ident_f, self.b_vecs], [bd])
                pt, bp = self.psum()
                self.mm(pt[:, 0:128], self.ones_f, dg, True, True, [self.b_ones_f, bd], [bp])
                self.cp("act", AB2[:, i, j * 128:(j + 1) * 128], pt[:, 0:128], [bp], [bW])
        S.barrier()
        A.reset(m1)
        oTr = Ring([A.alloc([128, 4, CH], BF16) for _ in range(2)])
        for_ = Ring([A.alloc([128, D], BF16) for _ in range(P3_PF + 1)])
        sgr = Ring([A.alloc([128, 2 * D], BF16) for _ in range(P3_PF + 1)])
        xr = Ring([A.alloc([128, D], F32) for _ in range(P3_PF + 3)])
        m1r = Ring([A.alloc([128, D], BF16) for _ in range(2)])
        m2r = Ring([A.alloc([128, D], BF16) for _ in range(2)])
        mgr = Ring([A.alloc([128, D], BF16) for _ in range(2)])
        mTr = Ring([A.alloc([128, 8, 128], BF16) for _ in range(2)])
        h1r = Ring([A.alloc([128, D], F32) for _ in range(4)])
        hnr = Ring([A.alloc([128, D], F32) for _ in range(2)])
        vnr = Ring([A.alloc([128, D], BF16) for _ in range(3)])
        vTr = Ring([A.alloc([128, 8, 128], BF16) for _ in range(2)])
        junk = A.alloc([128, D], BF16); b_junk = Buf()
        stat = A.alloc([128, NT, 4], F32)
        lg4r = Ring([A.alloc([128, 4, 36], F32) for _ in range(2)])
        rs = A.alloc([128, 1024], F32); brs = Buf()

        st = {}
        chunk = {}
        lgs = {}

        def loads(t):
            c, s_ = divmod(t, 4)
            rows = slice(t * 128, (t + 1) * 128)
            if s_ == 0:
                oT, boT = oTr.next()
                for h in range(NH):
                    S.dma("sp", oT[(h % 2) * 64:(h % 2) * 64 + 64, h // 2, :], self.OT[h, :, c * CH:(c + 1) * CH],
                          writes=[boT])
                chunk[c] = (oT, boT)
            fo, bfo = for_.next()
            S.dma("sp", fo, self.FO[rows, :], writes=[bfo])
            sg, bsg = sgr.next()
            S.dma("sp", sg, self.SG[rows, :], writes=[bsg])
            xt, bx = xr.next()
            S.dma("sp", xt, self.x[rows, :], writes=[bx])
            st[t] = dict(fo=(fo, bfo), sg=(sg, bsg), x=(xt, bx))

        def stage1(t):
            c, s_ = divmod(t, 4)
            oT, boT = chunk[c]
            d = st[t]
            fo, bfo = d["fo"]; sg, bsg = d["sg"]
            ts_ = slice(s_ * 128, (s_ + 1) * 128)
            m1, bm1 = m1r.next()
            for hf in range(2):
                hs = slice(hf * 512, (hf + 1) * 512)
                ps, bps = self.psum()
                for j in range(4):
                    self.mm(ps[:, :], oT[:, j, ts_], Wba[:, j, hs], j == 0, j == 3, [boT, bW], [bps])
                self.tt("dve", m1[:, hs], ps[:, :], sg[:, hs], ALU.mult, [bps, bsg], [bm1])
            m2, bm2 = d["m2"]
            mg, bmg = mgr.next()
            self.tt("dve", mg, m1, m2, ALU.add, [bm1, bm2], [bmg])
            d["mg"] = (mg, bmg)

        def stage1p(t):
            d = st[t]
            fo, bfo = d["fo"]; sg, bsg = d["sg"]
            m2, bm2 = m2r.next()
            self.tt("pool", m2, fo, sg[:, D:2 * D], ALU.mult, [bfo, bsg], [bm2])
            d["m2"] = (m2, bm2)

        def stage2(t):
            d = st[t]
            mg, bmg = d["mg"]
            ps, bps = self.psum()
            pv = ps[:, :].bitcast(BF16).rearrange("p (k n) -> p k n", k=8)
            for k in range(8):
                self.tr(pv[:, k, :], mg[:, k * 128:(k + 1) * 128], self.ident_b, [bmg, self.b_ident_b], [bps])
            mT, bmT = mTr.next()
            self.cp("act", mT, pv, [bps], [bmT])
            d["mT"] = (mT, bmT)

        def stage3(t):
            d = st[t]
            mT, bmT = d["mT"]
            xt, bx = d["x"]
            rows = slice(t * 128, (t + 1) * 128)
            h1, bh1 = h1r.next()
            for hf in range(2):
                hs = slice(hf * 512, (hf + 1) * 512)
                ps, bps = self.psum()
                for k in range(8):
                    self.mm(ps[:, :], mT[:, k, :], Wout[:, k, hs], k == 0, k == 7, [bmT, bW], [bps])
                self.tt("dve", h1[:, hs], ps[:, :], xt[:, hs], ALU.add, [bps, bx], [bh1])
            self.defer(lambda: S.dma("sp", self.H1[rows, :], h1, reads=[bh1], writes=[Buf()]))
            bst = Buf()
            self.act(junk, h1, AF.Square, [bh1], [b_junk, bst], accum_out=stat[:, t, 0:1])
            d["h1"] = (h1, bh1, bst)

        def stage3b(t):
            d = st[t]
            h1, bh1, bst = d["h1"]
            self.ts("dve", stat[:, t, 1:2], stat[:, t, 0:1], 1.0 / D, EPS, ALU.mult, ALU.add, [bst], [bst])
            self.tt("pool", stat[:, t, 2:3], stat[:, t, 1:2], self.mhalf[:, 0:1], ALU.pow, [bst, self.b_mhalf], [bst])

        def stage4(t):
            d = st[t]
            h1, bh1, bst = d["h1"]
            rows = slice(t * 128, (t + 1) * 128)
            hn, bhn = hnr.next()
            self.act(hn, h1, AF.Copy, [bh1, bst], [bhn], scale=stat[:, t, 2:3])
            self.tt("dve", hn, hn, AB2[:, 0, :], ALU.mult, [bhn, bW], [bhn])
            vn, bvn = vnr.next()
            self.tt("pool", vn, hn, AB2[:, 1, :], ALU.add, [bhn, bW], [bvn])
            self.defer(lambda: S.dma("sp", self.VN[rows, :], vn, reads=[bvn], writes=[Buf()]))
            d["vn"] = (vn, bvn)

        def stage5(t):
            d = st[t]
            vn, bvn = d["vn"]
            ps, bps = self.psum()
            pv = ps[:, :].bitcast(BF16).rearrange("p (k n) -> p k n", k=8)
            for k in range(8):
                self.tr(pv[:, k, :], vn[:, k * 128:(k + 1) * 128], self.ident_b, [bvn, self.b_ident_b], [bps])
            vT, bvT = vTr.next()
            self.cp("act", vT, pv, [bps], [bvT])
            d["vT"] = (vT, bvT)

        def stageC(t):
            c, s_ = divmod(t, 4)
            d = st.pop(t)
            vT, bvT = d["vT"]
            if s_ == 0:
                lgs[c] = lg4r.next()
            lg4, blg = lgs[c]
            ps, bps = self.psum()
            for k in range(8):
                self.mm(ps[:, 0:36], vT[:, k, :], Wr[:, k, :], k == 0, False, [bvT, bW], [bps])
            self.mm(ps[:, 0:36], self.ones_b[0:1, :], br_b[0:1, :], False, True, [self.b_ones_b, bW], [bps])
            self.cp("dve", lg4[:, s_, :], ps[:, 0:36], [bps], [blg])
            if s_ == 3:
                pending.append(route(c))

        def bc(ap, shape, axis):
            return ap.unsqueeze(axis).broadcast_to(shape)

        def route(c):
            lg4, blg = lgs.pop(c)
            t0 = c * 4
            G = lg4[:, :, 0:4]
            E = lg4[:, :, 4:36].rearrange("p t (g e) -> p t g e", g=4)
            o = [0]

            def sc(n, shape):
                v = rs[:, o[0]:o[0] + n]
                o[0] += n
                if len(shape) == 2:
                    return v.rearrange("p (a b) -> p a b", a=shape[0])
                if len(shape) == 3:
                    return v.rearrange("p (a b c) -> p a b c", a=shape[0], b=shape[1])
                return v
            R_ = [blg, brs]
            W_ = [brs]
            dve = self.S.op
            gmax = sc(4, (4,))
            dve("dve", lambda e: e.reduce_max(out=gmax, in_=G, axis=AX.X), R_, W_)
            gm = sc(16, (4, 4))
            self.tt("dve", gm, G, bc(gmax, [128, 4, 4], 2), ALU.is_ge, R_, W_)
            gd = sc(16, (4, 4))
            self.tt("dve", gd, G, bc(gmax, [128, 4, 4], 2), ALU.subtract, R_, W_)
            ge = sc(16, (4, 4))
            self.act(ge, gd, AF.Exp, R_, W_)
            gsum = sc(4, (4,))
            dve("dve", lambda e: e.reduce_sum(out=gsum, in_=ge, axis=AX.X), R_, W_)
            tmp = sc(128, (4, 4, 8))
            self.tt("dve", tmp, E, bc(gm, [128, 4, 4, 8], 3), ALU.mult, R_, W_)
            esel = sc(32, (4, 8))
            dve("dve", lambda e: e.reduce_sum(out=esel, in_=tmp.rearrange("p t g e -> p t e g"), axis=AX.X), R_, W_)
            yield
            top8 = sc(32, (4, 8))
            for i in range(4):
                dve("dve", lambda e, i=i: e.max(out=top8[:, i, :], in_=esel[:, i, :]), R_, W_)
            sel = sc(32, (4, 8))
            self.tt("dve", sel, esel, top8[:, :, 1:2].broadcast_to([128, 4, 8]), ALU.is_ge, R_, W_)
            sel1 = sc(32, (4, 8))
            self.tt("dve", sel1, esel, top8[:, :, 0:1].broadcast_to([128, 4, 8]), ALU.is_ge, R_, W_)
            sel2 = sc(32, (4, 8))
            self.tt("dve", sel2, sel, sel1, ALU.subtract, R_, W_)
            ed = sc(32, (4, 8))
            self.tt("dve", ed, esel, top8[:, :, 0:1].broadcast_to([128, 4, 8]), ALU.subtract, R_, W_)
            ex = sc(32, (4, 8))
            self.act(ex, ed, AF.Exp, R_, W_)
            yield
            sx = sc(32, (4, 8))
            self.tt("dve", sx, sel, ex, ALU.mult, R_, W_)
            den = sc(4, (4,))
            dve("dve", lambda e: e.reduce_sum(out=den, in_=sx, axis=AX.X), R_, W_)
            gd2 = sc(4, (4,))
            self.tt("dve", gd2, gsum, den, ALU.mult, R_, W_)
            coef = sc(4, (4,))
            dve("dve", lambda e: e.reciprocal(out=coef, in_=gd2), R_, W_)
            w8 = sc(32, (4, 8))
            self.tt("dve", w8, sx, bc(coef, [128, 4, 8], 2), ALU.mult, R_, W_)
            tw = sc(32, (4, 8))
            self.tt("dve", tw, sel1, w8, ALU.mult, R_, W_)
            dve("dve", lambda e: e.reduce_sum(out=self.w12[:, t0:t0 + 4, 0], in_=tw, axis=AX.X), R_, [brs, self.b_w12])
            tw2 = sc(32, (4, 8))
            self.tt("dve", tw2, sel2, w8, ALU.mult, R_, W_)
            dve("dve", lambda e: e.reduce_sum(out=self.w12[:, t0:t0 + 4, 1], in_=tw2, axis=AX.X), R_, [brs, self.b_w12])
            yield
            M1 = M12[:, 0, t0:t0 + 4, :].rearrange("p t (g e) -> p t g e", g=4)
            M2 = M12[:, 1, t0:t0 + 4, :].rearrange("p t (g e) -> p t g e", g=4)
            self.tt("dve", M1, bc(gm, [128, 4, 4, 8], 3), bc(sel1, [128, 4, 4, 8], 2), ALU.mult, R_, [brs, bM])
            self.tt("dve", M2, bc(gm, [128, 4, 4, 8], 3), bc(sel2, [128, 4, 4, 8], 2), ALU.mult, R_, [brs, bM])
            Mt = rs[:, 512:512 + 64].bitcast(BF16).rearrange("p (t e) -> p t e", t=4)
            self.tt("dve", Mt, M12[:, 0, t0:t0 + 4, :], M12[:, 1, t0:t0 + 4, :], ALU.add, [bM, brs], W_)
            for i in range(4):
                ps, bps = self.psum()
                self.mm(ps[:, 0:32], Ltri, Mt[:, i, :], True, True, [bW, brs], [bps])
                self.mm(ps[:, 32:64], self.ones_b, Mt[:, i, :], True, True, [self.b_ones_b, brs], [bps])
                self.tt("dve", rank[:, t0 + i, :], ps[:, 0:32], Rr, ALU.add, [bps, bR], [brank])
                self.tt("dve", Rr, Rr, ps[:, 32:64], ALU.add, [bps, bR], [bR])
            yield

        for t_ in range(P3_PF):
            loads(t_)
        stages = [stage1, stage2, stage3, stage3b, stage4, stage5, stageC]
        order = P3_ORDER
        pending = []

        def pump():
            for g_ in list(pending):
                try:
                    next(g_)
                except StopIteration:
                    pending.remove(g_)
        for i in range(NT + len(stages) - 1):
            if i + P3_PF < NT:
                loads(i + P3_PF)
            self.flush()
            if i < NT:
                stage1p(i)
            for k_ in order:
                if 0 <= i - k_ < NT:
                    stages[k_](i - k_)
            pump()
        while pending:
            pump()
        self.flush()
        S.barrier()
        A.reset(m0)
        cnt = A.alloc([128, 32], F32)
        ci = A.alloc([128, 32], I32)
        pad = A.alloc([128, 32], F32)
        cs = [A.alloc([128, 32], F32) for _ in range(2)]
        base = A.alloc([128, 32], F32)
        sf = A.alloc([128, NT, 32], F32)
        prod = A.alloc([128, NT, 32], F32)
        slf = A.alloc([128, 2, NT], F32)
        tst = A.alloc([128, NST], F32)
        acc = A.alloc([128, NST], F32)
        pio = A.alloc([128, 1], F32)
        bb = Buf()
        RW = ([bb, bR, brank, bM], [bb])
        S.dma("sp", tst, self.tstart[:, :], writes=[bb])
        S.dma("sp", pio, self.piota[:, :], writes=[bb])
        self.ts("dve", cnt, Rr, float(TR - 1), 1.0 / TR, ALU.add, ALU.mult, *RW)
        self.ts("dve", ci, cnt, -0.5 + 0.5 / TR, None, ALU.add, None, *RW)
        self.cp("dve", pad, ci, *RW)
        self.ts("dve", pad, pad, float(TR), None, ALU.mult, None, *RW)
        cur = cs[0]
        self.cp("dve", cur, pad, *RW)
        k = 0
        for sh in (1, 2, 4, 8, 16):
            nxt = cs[1 - k]
            self.cp("dve", nxt[:, 0:sh], cur[:, 0:sh], *RW)
            self.tt("dve", nxt[:, sh:32], cur[:, sh:32], cur[:, 0:32 - sh], ALU.add, *RW)
            cur = nxt
            k = 1 - k
        bend = cur
        self.tt("dve", base, bend, pad, ALU.subtract, *RW)
        self.tt("dve", sf, rank, base.unsqueeze(1).broadcast_to([128, NT, 32]), ALU.add, *RW)
        for q in range(2):
            self.tt("dve", prod, sf, M12[:, q, :, :], ALU.mult, *RW)
            self.S.op("dve", lambda e, q=q: e.reduce_sum(out=slf[:, q, :], in_=prod, axis=AX.X), *RW)
        self.cp("dve", self.slot_i, slf, RW[0], [bb, self.b_slot])
        self.memset("dve", acc, 0.0, [bb])
        for e_ in range(32):
            self.stt(acc, tst, bend[:, e_:e_ + 1], acc, ALU.is_ge, ALU.add, *RW)
        self.ts("dve", acc, acc, 31.0, 128.0, ALU.min, ALU.mult, *RW)
        self.ts("dve", acc, acc, pio[:, 0:1], None, ALU.add, None, *RW)
        self.cp("dve", self.idxw, acc, RW[0], [bb, self.b_idxw])
        if "RT" in self.dbg:
            S.dma("sp", self.RT[:, 0:128], slf.rearrange("p a b -> p (a b)"), reads=[bb], writes=[Buf()])
            S.dma("sp", self.RT[:, 128:256], self.w12.rearrange("p a b -> p (a b)"), reads=[bb, self.b_w12], writes=[Buf()])
            S.dma("sp", self.RT[:, 256:256 + NST], acc, reads=[bb], writes=[Buf()])
            S.dma("sp", self.RT[:, 512:544], Rr, reads=[bb, bR], writes=[Buf()])
            S.dma("sp", self.RT[:, 544:576], bend, reads=[bb], writes=[Buf()])
        S.barrier()
        A.reset(self.m_persist)

    def idma(self, out, in_, idx, gather, reads, writes):
        if gather:
            fn = lambda e: e.indirect_dma_start(out=out, out_offset=None, in_=in_,
                                                in_offset=bass.IndirectOffsetOnAxis(ap=idx, axis=0))
        else:
            fn = lambda e: e.indirect_dma_start(out=out, out_offset=bass.IndirectOffsetOnAxis(ap=idx, axis=0),
                                                in_=in_, in_offset=None)
        return self.S.op("pool", fn, reads, writes, is_dma=True)

    def phase4(self):
        S, A = self.S, self.A
        m0 = A.mark()
        bwgu = self.b_wgu; bwd = self.b_wd
        vr = Ring([A.alloc([128, D], BF16) for _ in range(3)])
        bxs = Buf()
        for t in range(NT):
            v, bv = vr.next()
            S.dma("sp", v, self.VN[t * 128:(t + 1) * 128, :], writes=[bv])
            for q in range(2):
                self.idma(self.XS[:, :], v, self.slot_i[:, q, t:t + 1], False, [bv, self.b_slot], [Buf()])
        S.barrier()
        xsr = Ring([A.alloc([128, D], BF16) for _ in range(10)])
        wgr = Ring([A.alloc([128, 8, 512], BF16) for _ in range(5)])
        wdr = Ring([A.alloc([128, 2, D], BF16) for _ in range(5)])
        xTr = Ring([A.alloc([128, 8, 128], BF16) for _ in range(2)])
        sgl = Ring([A.alloc([128, 256], F32) for _ in range(2)])
        hdr = Ring([A.alloc([128, 256], BF16) for _ in range(2)])
        hTr = Ring([A.alloc([128, 2, 128], BF16) for _ in range(2)])
        ysr = Ring([A.alloc([128, D], BF16) for _ in range(3)])
        ld = {}

        def loads(s_):
            wg, bwg = wgr.next()
            self.idma(wg.rearrange("p k f -> p (k f)"), self.WGU[:, :], self.idxw[:, s_:s_ + 1], True,
                      [bwgu, self.b_idxw], [bwg])
            wd, bwd_ = wdr.next()
            self.idma(wd.rearrange("p j d -> p (j d)"), self.WD[:, :], self.idxw[:, s_:s_ + 1], True,
                      [bwd, self.b_idxw], [bwd_])
            xl = []
            for u in range(TR // 128):
                xs, bx = xsr.next()
                r0 = s_ * TR + u * 128
                S.dma("sp", xs, self.XS[r0:r0 + 128, :], reads=[bxs], writes=[bx])
                xl.append((xs, bx))
            ld[s_] = (xl, wg, bwg, wd, bwd_)

        SUB = TR // 128
        NU = NST * SUB
        stt_ = {}

        def e1(u):
            xl, wg, bwg, wd, bwd_ = ld[u // SUB]
            xs, bx = xl[u % SUB]
            ps, bps = self.psum()
            pv = ps[:, :].bitcast(BF16).rearrange("p (k n) -> p k n", k=8)
            for k in range(8):
                self.tr(pv[:, k, :], xs[:, k * 128:(k + 1) * 128], self.ident_b, [bx, self.b_ident_b], [bps])
            xT, bxT = xTr.next()
            self.cp("act", xT, pv, [bps], [bxT])
            stt_[u] = dict(xT=(xT, bxT))

        def e2(u):
            xl, wg, bwg, wd, bwd_ = ld[u // SUB]
            xT, bxT = stt_[u]["xT"]
            ps, bps = self.psum()
            for k in range(8):
                self.mm(ps[:, :], xT[:, k, :], wg[:, k, :], k == 0, k == 7, [bxT, bwg], [bps])
            sg, bsg = sgl.next()
            self.act(sg, ps[:, 0:256], AF.Silu, [bps], [bsg])
            hd, bhd = hdr.next()
            self.tt("dve", hd, ps[:, 256:512], sg, ALU.mult, [bps, bsg], [bhd])
            stt_[u]["hd"] = (hd, bhd)

        def e3(u):
            hd, bhd = stt_[u]["hd"]
            ps2, bps2 = self.psum()
            pv2 = ps2[:, 0:128].bitcast(BF16).rearrange("p (k n) -> p k n", k=2)
            for j in range(2):
                self.tr(pv2[:, j, :], hd[:, j * 128:(j + 1) * 128], self.ident_b, [bhd, self.b_ident_b], [bps2])
            hT, bhT = hTr.next()
            self.cp("act", hT, pv2, [bps2], [bhT])
            stt_[u]["hT"] = (hT, bhT)

        def e4(u):
            xl, wg, bwg, wd, bwd_ = ld[u // SUB]
            hT, bhT = stt_.pop(u)["hT"]
            r0 = u * 128
            ys, bys = ysr.next()
            for hf in range(2):
                hs = slice(hf * 512, (hf + 1) * 512)
                ps3, bps3 = self.psum()
                for j in range(2):
                    self.mm(ps3[:, :], hT[:, j, :], wd[:, j, hs], j == 0, j == 1, [bhT, bwd_], [bps3])
                self.tt("dve", ys[:, hs], ps3[:, :], self.bc[:, 1, hs], ALU.mult, [bps3, self.b_bc], [bys])
            self.defer(lambda: S.dma("sp", self.YS2[r0:r0 + 128, :], ys, reads=[bys], writes=[Buf()]))
            if u % SUB == SUB - 1:
                ld.pop(u // SUB)

        loads(0)
        loads(1)
        est = [e1, e2, e3, e4]
        eorder = E_ORDER
        for i in range(NU + 3):
            if i % SUB == 0 and i // SUB + 2 < NST:
                loads(i // SUB + 2)
            self.flush()
            for k_ in eorder:
                if 0 <= i - k_ < NU:
                    est[k_](i - k_)
        self.flush()
        S.barrier()
        A.reset(m0)

    def phase5(self):
        S, A = self.S, self.A
        m0 = A.mark()
        y1r = Ring([A.alloc([128, D], BF16) for _ in range(3)])
        y2r = Ring([A.alloc([128, D], BF16) for _ in range(3)])
        h1r = Ring([A.alloc([128, D], F32) for _ in range(3)])
        mr = Ring([A.alloc([128, D], F32) for _ in range(2)])
        h2r = Ring([A.alloc([128, D], F32) for _ in range(4)])
        outr = Ring([A.alloc([128, D], F32) for _ in range(4)])
        junk = A.alloc([128, D], BF16); b_junk = Buf()
        stat = A.alloc([128, NT, 4], F32)
        ld = {}

        def loads(t):
            y1, by1 = y1r.next()
            self.idma(y1, self.YS2[:, :], self.slot_i[:, 0, t:t + 1], True, [self.b_ys2, self.b_slot], [by1])
            y2, by2 = y2r.next()
            self.idma(y2, self.YS2[:, :], self.slot_i[:, 1, t:t + 1], True, [self.b_ys2, self.b_slot], [by2])
            h1, bh1 = h1r.next()
            S.dma("sp", h1, self.H1[t * 128:(t + 1) * 128, :], writes=[bh1])
            ld[t] = (y1, by1, y2, by2, h1, bh1)

        mid = {}
        mid0 = {}

        def s1(t):
            y1, by1, y2, by2, h1, bh1 = ld.pop(t)
            m, bm = mr.next()
            self.act(m, y1, AF.Copy, [by1, self.b_w12], [bm], scale=self.w12[:, t, 0:1])
            self.stt(m, y2, self.w12[:, t, 1:2], m, ALU.mult, ALU.add, [by2, self.b_w12, bm], [bm])
            h2, bh2 = h2r.next()
            self.tt("pool", h2, m, h1, ALU.add, [bm, bh1], [bh2])
            mid0[t] = (h2, bh2)

        def s1b(t):
            h2, bh2 = mid0.pop(t)
            bst = Buf()
            self.act(junk, h2, AF.Square, [bh2], [b_junk, bst], accum_out=stat[:, t, 0:1])
            self.ts("dve", stat[:, t, 1:2], stat[:, t, 0:1], 1.0 / D, EPS, ALU.mult, ALU.add, [bst], [bst])
            self.tt("pool", stat[:, t, 2:3], stat[:, t, 1:2], self.mhalf[:, 0:1], ALU.pow, [bst, self.b_mhalf], [bst])
            mid[t] = (h2, bh2, bst)

        def s2(t):
            h2, bh2, bst = mid.pop(t)
            o, bo = outr.next()
            self.stt(o, h2, stat[:, t, 2:3], self.bc[:, 2, :], ALU.mult, ALU.mult, [bh2, bst, self.b_bc], [bo])
            self.tt("dve", o, o, self.bc[:, 3, :], ALU.add, [bo, self.b_bc], [bo])
            self.defer(lambda: S.dma("sp", self.out[t * 128:(t + 1) * 128, :], o, reads=[bo], writes=[Buf()]))

        loads(0)
        loads(1)
        for i in range(NT + 2):
            if i + 2 < NT:
                loads(i + 2)
            self.flush()
            if 0 <= i - 2 < NT:
                s2(i - 2)
            if 0 <= i - 1 < NT:
                s1b(i - 1)
            if i < NT:
                s1(i)
        self.flush()
        S.barrier()
        A.reset(m0)


def host_inputs(inputs, b):
    f32 = np.float32

    def fm(v):
        v = np.asarray(v, f32).reshape(-1, 128)
        return np.ascontiguousarray(v.T)
    invf = np.zeros((128, 1), f32)
    inv = (10000.0 ** (-np.arange(0, 32, 2, dtype=f32) / 32)).astype(f32)
    for p in range(64, 96):
        invf[p, 0] = inv[(p - 64) % 16]
    m = {
        "x": np.ascontiguousarray(inputs["x"][b]),
        "ct": fm(inputs["c"][b]),
        "pos": np.ascontiguousarray(inputs["positions"][b].reshape(1, S_TOK).astype(np.int32)),
        "w_ada": np.ascontiguousarray(inputs["w_ada"][0]),
        "b_ada_t": fm(inputs["b_ada"][0]),
        "w_adaf": np.ascontiguousarray(inputs["w_ada_final"]),
        "b_adaf_t": fm(inputs["b_ada_final"]),
        "gmix_t": fm(inputs["g_norm_mix"][0]),
        "gffn_t": fm(inputs["g_norm_ffn"][0]),
        "gfin_t": fm(inputs["g_norm_final"]),
        "w_in": np.ascontiguousarray(inputs["w_in"][0]),
        "glat_t": np.ascontiguousarray(np.concatenate([fm(inputs["g_q_lat"][0]), fm(inputs["g_kv_lat"][0])], axis=1)),
        "w_uq": np.ascontiguousarray(inputs["w_uq"][0].reshape(256, 768)),
        "w_uk": np.ascontiguousarray(inputs["w_uk"][0].reshape(256, 512)),
        "w_uv": np.ascontiguousarray(inputs["w_uv"][0].reshape(256, 512)),
        "b_gates": np.ascontiguousarray(inputs["b_gates"][0].reshape(1, 2048)),
        "ident": np.eye(128, dtype=f32),
        "invf": invf,
        "w_bf": np.ascontiguousarray(inputs["w_branch_fourier"][0]),
        "w_ba": np.ascontiguousarray(inputs["w_branch_attn"][0]),
        "w_out": np.ascontiguousarray(inputs["w_out"][0]),
        "w_rt": np.ascontiguousarray(np.concatenate([inputs["w_router_group"][0],
                                                     inputs["w_router_expert"][0].reshape(D, 32)], axis=1)),
        "b_rt": np.ascontiguousarray(np.concatenate([inputs["b_router_group"][0].reshape(1, 4),
                                                     inputs["b_router_expert"][0].reshape(1, 32)], axis=1)),
        "w_eg": np.ascontiguousarray(inputs["w_expert_gate"][0]),
        "w_eu": np.ascontiguousarray(inputs["w_expert_up"][0]),
        "w_ed": np.ascontiguousarray(inputs["w_expert_down"][0]),
    }
    m.update(CONSTS)
    return m


def _make_consts():
    bf = ml_dtypes.bfloat16
    n1 = np.arange(128)[None, :, None]
    n2 = np.arange(64)[:, None, None]
    k1 = np.arange(128)[None, None, :]
    ph = (k1 * (64 * n1 + n2)) % 8192
    al = 2.0 * np.pi * ph / 8192.0
    sc = 1.0 / math.sqrt(8192.0)
    tre = (np.cos(al) * sc).astype(bf)
    tim = (-np.sin(al) * sc).astype(bf)
    a = np.arange(64)
    th = 2.0 * np.pi * ((a[:, None] * a[None, :]) % 64) / 64.0
    c, s_ = np.cos(th), np.sin(th)
    d2 = np.zeros((128, 128))
    d2[0:64, 0:64] = c
    d2[0:64, 64:128] = s_
    d2[64:128, 0:64] = s_
    d2[64:128, 64:128] = -c
    bdc = np.zeros((128, 128), np.float32)
    bds = np.zeros((128, 128), np.float32)
    for g in range(2):
        bdc[g * 64:(g + 1) * 64, g * 64:(g + 1) * 64] = c / 8.0
        bds[g * 64:(g + 1) * 64, g * 64:(g + 1) * 64] = -s_ / 8.0
    ltri = np.triu(np.ones((128, 128), np.float32), 1).astype(bf)
    tstart = np.tile((np.arange(NST, dtype=np.float32) * float(TR))[None, :], (128, 1))
    piota = np.arange(128, dtype=np.float32).reshape(128, 1)
    tri = np.ascontiguousarray(np.stack([tre, tim], axis=2))
    return {"tri": tri, "d2": d2.astype(bf), "bdc": bdc, "bds": bds, "ltri": ltri,
            "tstart": np.ascontiguousarray(tstart), "piota": piota}


CONSTS = _make_consts()


def kernel(**inputs):
    bld = Builder()
    nc = bld.build()
    in_maps = [host_inputs(inputs, b) for b in range(8)]
    res = run_bass_kernel_spmd(nc, in_maps, core_ids=list(range(8)))
    return np.stack([np.asarray(r["out"]) for r in res.results], axis=0).astype(np.float32)
```

```python
import math
from contextlib import ExitStack

import numpy as np
import ml_dtypes

import concourse.bass as bass
import concourse.mybir as mybir
from concourse.bass_utils import run_bass_kernel_spmd

F32 = mybir.dt.float32
BF16 = mybir.dt.bfloat16
I32 = mybir.dt.int32
U32 = mybir.dt.uint32
U8 = mybir.dt.uint8
AF = mybir.ActivationFunctionType
ALU = mybir.AluOpType
AX = mybir.AxisListType
DTSIZE = {F32: 4, BF16: 2, I32: 4, U32: 4, U8: 1}

S_TOK = 8192
D = 1024
NT = 64
NCH = 16
CH = 512
NH = 8
EPS = 1e-6
TWO_PI = 2.0 * math.pi
CW1 = 6.28125
CW2 = TWO_PI - CW1
ATT_SCALE = 96 ** -0.5
TR = 256
NST = 96
P1_ORDER = [4, 3, 2, 1, 0]
P3_ORDER = [3, 4, 2, 0, 1, 5, 6]
P3_PF = 2
E_ORDER = [3, 2, 1, 0]
NSLOT = NST * TR


class Buf:
    __slots__ = ("name", "last_w", "readers")

    def __init__(self, name=""):
        self.name = name
        self.last_w = None
        self.readers = []


class Op:
    __slots__ = ("eng", "fn", "deps", "signal", "sig_idx", "is_dma", "dma_sem", "dma_val", "prev_dma")

    def __init__(self, eng, fn, is_dma=False):
        self.eng = eng
        self.fn = fn
        self.deps = []
        self.signal = False
        self.sig_idx = 0
        self.is_dma = is_dma
        self.dma_sem = None
        self.dma_val = 0
        self.prev_dma = None


ENGS = ("pe", "act", "dve", "pool", "sp")
DMA_RING = 8


def _nop(eng):
    return eng.nop()


_nop.is_nop = True


class Sched:
    def __init__(self, nc, same_engine_sync=True):
        self.nc = nc
        self.ops = {e: [] for e in ENGS}
        self.same = same_engine_sync
        self.dma_ops = {e: [] for e in ENGS}

    def op(self, eng, fn, reads=(), writes=(), is_dma=False):
        o = Op(eng, fn, is_dma)
        for b in reads:
            p = b.last_w
            if p is not None:
                if (not p.is_dma) and p.eng == eng and not is_dma:
                    if self.same and eng != "pe":
                        o.deps.append(p)
                else:
                    o.deps.append(p)
        for b in writes:
            p = b.last_w
            if p is not None and (p.is_dma or is_dma or p.eng != eng):
                o.deps.append(p)
            for r in b.readers:
                if r.is_dma or is_dma or r.eng != eng:
                    o.deps.append(r)
        for b in reads:
            b.readers.append(o)
        for b in writes:
            b.last_w = o
            b.readers = []
        if is_dma:
            lst = self.dma_ops[eng]
            n = len(lst)
            if n >= DMA_RING:
                o.prev_dma = lst[n - DMA_RING]
            o.dma_val = 16 * (n // DMA_RING + 1)
            o.dma_sem = n % DMA_RING
            lst.append(o)
        self.ops[eng].append(o)
        return o

    def dma(self, queue, out, in_, reads=(), writes=(), **kw):
        return self.op(queue, lambda e: e.dma_start(out=out, in_=in_, **kw), reads, writes, is_dma=True)

    def barrier(self):
        lasts = []
        for e in ENGS:
            if self.ops[e]:
                for o in reversed(self.ops[e]):
                    if not o.is_dma and not getattr(o.fn, "is_nop", False):
                        lasts.append(o)
                        break
            lasts.extend(self.dma_ops[e][-DMA_RING:])
        for e in ENGS:
            if True:
                o = self.op(e, _nop)
                for p in lasts:
                    if p.is_dma or p.eng != e:
                        o.deps.append(p)

    def finish(self):
        o = self.op("sp", _nop)
        for e in ENGS:
            o.deps.extend(self.dma_ops[e][-DMA_RING:])

    def emit(self, stack):
        nc = self.nc
        for e in ENGS:
            for o in self.ops[e]:
                for p in o.deps:
                    if not p.is_dma:
                        p.signal = True
        for e in ENGS:
            c = 0
            for o in self.ops[e]:
                if o.signal and not o.is_dma:
                    c += 1
                    o.sig_idx = c
        sems = {e: stack.enter_context(nc.semaphore("s_" + e)) for e in ENGS}
        dsems = {e: [stack.enter_context(nc.semaphore("d_%s_%d" % (e, i))) for i in range(DMA_RING)]
                 for e in ENGS if self.dma_ops[e]}
        block = stack.enter_context(nc.Block())
        engobj = {"pe": "tensor", "act": "scalar", "dve": "vector", "pool": "gpsimd", "sp": "sync"}

        def make(ename):
            ops = self.ops[ename]

            def body(eng):
                waited = {}
                dwaited = {}
                for o in ops:
                    deps = list(o.deps)
                    if o.prev_dma is not None:
                        deps.append(o.prev_dma)
                    for p in deps:
                        if p.is_dma:
                            key = (p.eng, p.dma_sem)
                            if dwaited.get(key, 0) >= p.dma_val:
                                continue
                            eng.wait_ge(dsems[p.eng][p.dma_sem], p.dma_val)
                            dwaited[key] = p.dma_val
                        else:
                            if waited.get(p.eng, 0) >= p.sig_idx:
                                continue
                            eng.wait_ge(sems[p.eng], p.sig_idx)
                            waited[p.eng] = p.sig_idx
                    ins = o.fn(eng)
                    if o.is_dma:
                        ins.then_inc(dsems[ename][o.dma_sem], 16)
                    elif o.signal:
                        ins.then_inc(sems[ename], 1)
            return body

        for ename in ENGS:
            if self.ops[ename]:
                getattr(block, engobj[ename])(make(ename))


class Arena:
    def __init__(self, tensor, size):
        self.t = tensor
        self.size = size
        self.off = 0

    def alloc(self, shape, dt):
        n = 1
        for s in shape[1:]:
            n *= s
        nbytes = n * DTSIZE[dt]
        off = (self.off + 63) // 64 * 64
        assert off + nbytes <= self.size, "arena overflow %d + %d > %d" % (off, nbytes, self.size)
        self.off = off + nbytes
        v = self.t[:, off:off + nbytes].bitcast(dt)
        if len(shape) > 2:
            names = ["d%d" % i for i in range(len(shape) - 1)]
            pat = "p (" + " ".join(names) + ") -> p " + " ".join(names)
            kw = {names[i]: shape[i + 1] for i in range(len(names))}
            v = v.rearrange(pat, **kw)
        return v

    def mark(self):
        return self.off

    def reset(self, m):
        self.off = m


class Ring:
    def __init__(self, items):
        self.items = items
        self.bufs = [Buf() for _ in items]
        self.i = 0

    def next(self):
        i = self.i
        self.i = (i + 1) % len(self.items)
        return self.items[i], self.bufs[i]


class Builder:
    def __init__(self, dbg=(), stop_after=None):
        self.dbg = set(dbg)
        self.stop_after = stop_after
        self.nc = bass.Bass("TRN2", target_bir_lowering=False)
        self.S = Sched(self.nc)
        self.deferred = []

    def din(self, name, shape, dt=F32):
        return self.nc.dram_tensor(name, list(shape), dt, kind="ExternalInput").ap()

    def scratch(self, name, shape, dt):
        kind = "ExternalOutput" if name in self.dbg else "Internal"
        return self.nc.dram_tensor(name, list(shape), dt, kind=kind).ap()

    def mm(self, out, lhsT, rhs, start, stop, reads, writes):
        return self.S.op("pe", lambda e: e.matmul(out, lhsT=lhsT, rhs=rhs, start=start, stop=stop), reads, writes)

    def tr(self, out, in_, ident, reads, writes):
        return self.S.op("pe", lambda e: e.transpose(out=out, in_=in_, identity=ident), reads, writes)

    def act(self, out, in_, func, reads, writes, **kw):
        return self.S.op("act", lambda e: e.activation(out=out, in_=in_, func=func, **kw), reads, writes)

    def ts(self, eng, out, in0, s1, s2, op0, op1, reads, writes):
        if op1 is None:
            return self.S.op(eng, lambda e: e.tensor_scalar(out=out, in0=in0, scalar1=s1, scalar2=None, op0=op0), reads, writes)
        return self.S.op(eng, lambda e: e.tensor_scalar(out=out, in0=in0, scalar1=s1, scalar2=s2, op0=op0, op1=op1), reads, writes)

    def tt(self, eng, out, in0, in1, op, reads, writes):
        return self.S.op(eng, lambda e: e.tensor_tensor(out=out, in0=in0, in1=in1, op=op), reads, writes)

    def stt(self, out, in0, scalar, in1, op0, op1, reads, writes):
        return self.S.op("dve", lambda e: e.scalar_tensor_tensor(out=out, in0=in0, scalar=scalar, in1=in1, op0=op0, op1=op1), reads, writes)

    def cp(self, eng, out, in_, reads, writes):
        if eng == "act":
            return self.S.op("act", lambda e: e.copy(out=out, in_=in_), reads, writes)
        return self.S.op(eng, lambda e: e.tensor_copy(out=out, in_=in_), reads, writes)

    def memset(self, eng, ap, val, writes):
        return self.S.op(eng, lambda e: e.memset(ap, val), (), writes)

    def psum(self):
        return self.pring.next()

    def defer(self, fn):
        self.deferred.append(fn)

    def flush(self):
        d, self.deferred = self.deferred, []
        for f in d:
            f()

    def build(self):
        nc = self.nc
        S = self.S
        with ExitStack() as st:
            self.st = st
            self.b_ys2 = Buf()
            self.b_wgu = Buf()
            self.b_wd = Buf()
            arena_t = st.enter_context(nc.sbuf_tensor("arena", [128, 200 * 1024], U8))
            self.A = Arena(arena_t, 200 * 1024)
            pts = [st.enter_context(nc.psum_tensor("ps%d" % i, [128, 512], F32)) for i in range(8)]
            self.pring = Ring(pts)
            self.declare_io()
            self.phase0()
            if self.stop_after != "p0":
                self.phase1()
            if self.stop_after not in ("p0", "p1"):
                S.barrier()
                self.phase2b()
            if self.stop_after not in ("p0", "p1", "p2b") and "noatt" not in self.dbg:
                S.barrier()
                self.phase2a()
            if self.stop_after not in ("p0", "p1", "p2b", "p2a"):
                S.barrier()
                self.phase3()
            if self.stop_after not in ("p0", "p1", "p2b", "p2a", "p3"):
                self.phase4()
            if self.stop_after not in ("p0", "p1", "p2b", "p2a", "p3", "p4"):
                self.phase5()
            S.barrier()
            S.finish()
            S.emit(st)
        return nc

    def declare_io(self):
        self.x = self.din("x", [S_TOK, D])
        self.ct = self.din("ct", [128, 8])
        self.pos = self.din("pos", [1, S_TOK], I32)
        self.w_ada = self.din("w_ada", [D, 6 * D])
        self.b_ada_t = self.din("b_ada_t", [128, 48])
        self.w_adaf = self.din("w_adaf", [D, 2 * D])
        self.b_adaf_t = self.din("b_adaf_t", [128, 16])
        self.gmix_t = self.din("gmix_t", [128, 8])
        self.gffn_t = self.din("gffn_t", [128, 8])
        self.gfin_t = self.din("gfin_t", [128, 8])
        self.w_in = self.din("w_in", [D, 3104])
        self.glat_t = self.din("glat_t", [128, 4])
        self.w_uq = self.din("w_uq", [256, 768])
        self.w_uk = self.din("w_uk", [256, 512])
        self.w_uv = self.din("w_uv", [256, 512])
        self.b_gates = self.din("b_gates", [1, 2048])
        self.ident = self.din("ident", [128, 128])
        self.invf = self.din("invf", [128, 1])
        self.out = self.nc.dram_tensor("out", [S_TOK, D], F32, kind="ExternalOutput").ap()
        self.US = self.scratch("US", [S_TOK, 512], BF16)
        self.SG = self.scratch("SG", [S_TOK, 2048], BF16)
        self.QT = self.scratch("QT", [NH, 96, S_TOK], BF16)
        self.KT = self.scratch("KT", [NH, 64, S_TOK], BF16)
        self.KTR = self.scratch("KTR", [32, S_TOK], BF16)
        self.VS = self.scratch("VS", [S_TOK, 512], BF16)
        self.DBG = self.scratch("DBG", [128, 4096], F32)
        self.OT = self.scratch("OT", [NH, 64, S_TOK], BF16)
        self.YS = self.scratch("YS", [128, 2, 64, 512], BF16)
        self.FO = self.scratch("FO", [S_TOK, D], BF16)
        self.H1 = self.scratch("H1", [S_TOK, D], F32)
        self.VN = self.scratch("VN", [S_TOK, D], BF16)
        self.XS = self.scratch("XS", [NSLOT, D], BF16)
        self.YS2 = self.scratch("YS2", [NSLOT, D], BF16)
        self.WGU = self.scratch("WGU", [32 * 128, 8 * 512], BF16)
        self.WD = self.scratch("WD", [32 * 128, 2 * D], BF16)
        self.RT = self.scratch("RT", [128, 1024], F32)
        self.w_ba = self.din("w_ba", [512, D])
        self.w_out = self.din("w_out", [D, D])
        self.w_rt = self.din("w_rt", [D, 36])
        self.b_rt = self.din("b_rt", [1, 36])
        self.ltri = self.din("ltri", [128, 128], BF16)
        self.tstart = self.din("tstart", [128, NST])
        self.piota = self.din("piota", [128, 1])
        self.w_eg = self.din("w_eg", [32, D, 256])
        self.w_eu = self.din("w_eu", [32, D, 256])
        self.w_ed = self.din("w_ed", [32, 256, D])
        self.tri = self.din("tri", [64, 128, 2, 128], BF16)
        self.d2 = self.din("d2", [128, 128], BF16)
        self.bdc = self.din("bdc", [128, 128])
        self.bds = self.din("bds", [128, 128])
        self.w_bf = self.din("w_bf", [512, D])

    def phase0(self):
        S, A = self.S, self.A
        self.ident_f = A.alloc([128, 128], F32); self.b_ident_f = Buf()
        self.ident_b = A.alloc([128, 128], BF16); self.b_ident_b = Buf()
        self.ones_f = A.alloc([128, 128], F32); self.b_ones_f = Buf()
        self.ones_b = A.alloc([128, 128], BF16); self.b_ones_b = Buf()
        self.modT = A.alloc([128, 64], F32); self.b_modT = Buf()
        self.vecs = A.alloc([128, 8, 8], F32); self.b_vecs = Buf()
        self.bc = A.alloc([128, 4, D], F32); self.b_bc = Buf()
        self.mhalf = A.alloc([128, 2], F32); self.b_mhalf = Buf()
        cact = A.alloc([128, 8, 2], F32); b_cact = Buf()
        ctt = A.alloc([128, 8], F32); b_ctt = Buf()
        small = A.alloc([128, 128], F32); b_small = Buf()
        self.small = small; self.b_small = b_small
        S.dma("sp", self.ident_f, self.ident[:, :], writes=[self.b_ident_f])
        S.dma("sp", ctt, self.ct[:, :], writes=[b_ctt])
        S.dma("sp", small[:, 0:48], self.b_ada_t[:, :], writes=[b_small])
        S.dma("sp", small[:, 48:64], self.b_adaf_t[:, :], writes=[b_small])
        S.dma("sp", small[:, 64:72], self.gmix_t[:, :], writes=[b_small])
        S.dma("sp", small[:, 72:80], self.gffn_t[:, :], writes=[b_small])
        S.dma("sp", small[:, 80:88], self.gfin_t[:, :], writes=[b_small])
        S.dma("sp", small[:, 88:92], self.glat_t[:, :], writes=[b_small])
        S.dma("sp", small[:, 92:93], self.invf[:, :], writes=[b_small])
        self.cp("dve", self.ident_b, self.ident_f, [self.b_ident_f], [self.b_ident_b])
        self.memset("pool", self.ones_f, 1.0, [self.b_ones_f])
        self.memset("pool", self.ones_b, 1.0, [self.b_ones_b])
        self.memset("pool", self.mhalf, -0.5, [self.b_mhalf])
        self.act(cact[:, :, 0], ctt, AF.Silu, [b_ctt], [b_cact])
        self.act(cact[:, :, 1], ctt, AF.Silu, [b_ctt], [b_cact])

        m0 = A.mark()
        wring = Ring([A.alloc([128, 8, 512], F32) for _ in range(3)])
        Rrow = A.alloc([128, 8192], F32); bRrow = Buf()
        one2 = A.alloc([128, 2], F32); bone2 = Buf()
        self.memset("dve", one2, 1.0, [bone2])
        wl = {}

        def wload(piece):
            wt, bw = wring.next()
            if piece < 12:
                src = self.w_ada[:, piece * 512:(piece + 1) * 512]
            else:
                src = self.w_adaf[:, (piece - 12) * 512:(piece - 11) * 512]
            S.dma("sp", wt, src.rearrange("(k p) n -> p k n", p=128), writes=[bw])
            wl[piece] = (wt, bw)
        wload(0)
        wload(1)
        for piece in range(16):
            if piece + 2 < 16:
                wload(piece + 2)
            wt, bw = wl.pop(piece)
            pt, bp = self.psum()
            for k in range(8):
                self.mm(pt[0:2, :], cact[:, k, :], wt[:, k, :], k == 0, k == 7, [bw, b_cact], [bp])
            self.cp("act" if piece % 2 else "dve", Rrow[0:2, piece * 512:(piece + 1) * 512], pt[0:2, :], [bp], [bRrow])
        pt, bp = self.psum()
        pv = pt[:, 0:128].rearrange("p (a b) -> p a b", b=2)
        for j in range(64):
            self.mm(pv[:, j, :], Rrow[0:1, j * 128:(j + 1) * 128], one2[0:1, :], True, True, [bRrow, bone2], [bp])
        self.tt("dve", self.modT, pv[:, :, 0], small[:, 0:64], ALU.add, [bp, b_small], [self.b_modT])
        S.barrier()
        A.reset(m0)
        mT = self.modT
        V = self.vecs
        bv = self.b_vecs
        self.stt(V[:, 0, :], mT[:, 8:16], 1.0, small[:, 64:72], ALU.add, ALU.mult, [self.b_modT, b_small], [bv])
        self.cp("dve", V[:, 1, :], mT[:, 0:8], [self.b_modT], [bv])
        self.stt(V[:, 2, :], mT[:, 32:40], 1.0, small[:, 72:80], ALU.add, ALU.mult, [self.b_modT, b_small], [bv])
        self.cp("dve", V[:, 3, :], mT[:, 24:32], [self.b_modT], [bv])
        self.stt(V[:, 4, :], mT[:, 56:64], 1.0, small[:, 80:88], ALU.add, ALU.mult, [self.b_modT, b_small], [bv])
        diag_r = Ring([A.alloc([128, 128], F32) for _ in range(2)])
        srcs = [mT[:, 16:24], mT[:, 40:48], V[:, 4, :], mT[:, 48:56]]
        for i, sv in enumerate(srcs):
            for j in range(8):
                dg, bd = diag_r.next()
                self.ts("dve", dg, self.ident_f, sv[:, j:j + 1], None, ALU.mult, None,
                        [self.b_ident_f, self.b_modT, bv], [bd])
                pt, bp = self.psum()
                self.mm(pt[:, 0:128], self.ones_f, dg, True, True, [self.b_ones_f, bd], [bp])
                self.cp("act", self.bc[:, i, j * 128:(j + 1) * 128], pt[:, 0:128], [bp], [self.b_bc])

        self.m_persist = A.mark()
        self.cosT = A.alloc([128, S_TOK], BF16); self.b_cosT = Buf()
        self.sinT = A.alloc([128, S_TOK], BF16); self.b_sinT = Buf()
        m1 = A.mark()
        PW = 2048
        pi_r = Ring([A.alloc([128, PW], I32) for _ in range(2)])
        ang_r = Ring([A.alloc([128, PW], F32) for _ in range(2)])
        ki_r = Ring([A.alloc([128, PW], I32) for _ in range(1)])
        kf_r = Ring([A.alloc([128, PW], F32) for _ in range(1)])
        r_r = Ring([A.alloc([128, PW], F32) for _ in range(2)])
        ab_r = Ring([A.alloc([128, PW], F32) for _ in range(1)])
        R = slice(64, 96)
        invf = small[R, 92:93]
        for pc in range(S_TOK // PW):
            sl = slice(pc * PW, (pc + 1) * PW)
            pi, bpi = pi_r.next()
            S.dma("sp", pi[R, :], self.pos[0:1, sl].partition_broadcast(32), writes=[bpi])
            ang, bang = ang_r.next()
            self.cp("dve", ang[R, :], pi[R, :], [bpi], [bang])
            self.ts("dve", ang[R, :], ang[R, :], invf, None, ALU.mult, None, [bang, b_small], [bang])
            ki, bki = ki_r.next()
            self.ts("dve", ki[R, :], ang[R, :], 1.0 / TWO_PI, None, ALU.mult, None, [bang], [bki])
            kf, bkf = kf_r.next()
            self.cp("dve", kf[R, :], ki[R, :], [bki], [bkf])
            r, br = r_r.next()
            self.stt(r[R, :], kf[R, :], -CW1, ang[R, :], ALU.mult, ALU.add, [bkf, bang], [br])
            self.stt(r[R, :], kf[R, :], -CW2, r[R, :], ALU.mult, ALU.add, [bkf, br], [br])
            self.ts("dve", r[R, :], r[R, :], math.pi, -math.pi, ALU.min, ALU.max, [br], [br])
            self.act(self.sinT[R, sl], r[R, :], AF.Sin, [br], [self.b_sinT])
            ab, bab = ab_r.next()
            self.stt(ab[R, :], r[R, :], -1.0, r[R, :], ALU.mult, ALU.max, [br], [bab])
            self.act(self.cosT[R, sl], ab[R, :], AF.Sin, [bab], [self.b_cosT], scale=-1.0, bias=math.pi / 2)
        S.barrier()
        A.reset(m1)

        self.Wlat = A.alloc([128, 8, 512], BF16)
        self.Wkr = A.alloc([128, 8, 2, 96], BF16)
        self.Wf = A.alloc([128, 8, 512], BF16)
        self.Wg = A.alloc([128, 8, 2048], BF16)
        self.Wuq = A.alloc([128, 2, NH, 96], BF16)
        self.WuqR = A.alloc([128, 2, NH, 96], BF16)
        self.Wuk = A.alloc([128, 2, 512], BF16)
        self.Wuv = A.alloc([128, 2, 512], BF16)
        self.bg_b = A.alloc([128, 2048], BF16)
        self.b_W1 = Buf()
        bW = self.b_W1
        m2 = A.mark()
        stg = Ring([A.alloc([128, 8, 512], F32) for _ in range(2)])
        self.memset("pool", self.Wkr, 0.0, [bW])
        self.memset("pool", self.WuqR, 0.0, [bW])
        win = self.w_in

        def wsrc(c0, c1):
            return win[:, c0:c1].rearrange("(k p) n -> p k n", p=128)
        t, b = stg.next()
        S.dma("sp", t, wsrc(0, 512), writes=[b])
        self.cp("dve", self.Wlat, t, [b], [bW])
        t, b = stg.next()
        S.dma("sp", t[:, :, 0:32], wsrc(512, 544), writes=[b])
        self.cp("dve", self.Wkr[:, :, 0, 64:96], t[:, :, 0:32], [b], [bW])
        self.ts("dve", self.Wkr[:, :, 1, 64:80], t[:, :, 16:32], -1.0, None, ALU.mult, None, [b], [bW])
        self.cp("dve", self.Wkr[:, :, 1, 80:96], t[:, :, 0:16], [b], [bW])
        t, b = stg.next()
        S.dma("sp", t, wsrc(544, 1056), writes=[b])
        self.cp("act", self.Wf, t, [b], [bW])
        for g in range(4):
            t, b = stg.next()
            S.dma("sp", t, wsrc(1056 + g * 512, 1056 + (g + 1) * 512), writes=[b])
            self.cp("dve" if g % 2 == 0 else "act", self.Wg[:, :, g * 512:(g + 1) * 512], t, [b], [bW])
        tq2 = A.alloc([128, 2, 768], F32); btq2 = Buf()
        S.dma("sp", tq2, self.w_uq.rearrange("(j p) n -> p j n", p=128), writes=[btq2])
        tq2v = tq2.rearrange("p j (h d) -> p j h d", h=NH)
        self.cp("dve", self.Wuq, tq2v, [btq2], [bW])
        self.ts("dve", self.WuqR[:, :, :, 64:80], tq2v[:, :, :, 80:96], -1.0, None, ALU.mult, None, [btq2], [bW])
        self.cp("dve", self.WuqR[:, :, :, 80:96], tq2v[:, :, :, 64:80], [btq2], [bW])
        t, b = stg.next()
        S.dma("sp", t[:, 0:2, :], self.w_uk.rearrange("(j p) n -> p j n", p=128), writes=[b])
        self.cp("dve", self.Wuk, t[:, 0:2, :], [b], [bW])
        t, b = stg.next()
        S.dma("sp", t[:, 0:2, :], self.w_uv.rearrange("(j p) n -> p j n", p=128), writes=[b])
        self.cp("dve", self.Wuv, t[:, 0:2, :], [b], [bW])
        t, b = stg.next()
        tb = t[0:1, 0:4, :].rearrange("p a b -> p (a b)")
        S.dma("sp", tb, self.b_gates[0:1, :], writes=[b])
        self.cp("dve", self.bg_b[0:1, :], tb, [b], [bW])
        S.barrier()
        A.reset(m2)
        if "DBG" in self.dbg:
            S.dma("sp", self.DBG[:, 0:64], self.modT, reads=[self.b_modT], writes=[Buf()])
            S.dma("sp", self.DBG[:, 64:128], self.vecs.rearrange("p a b -> p (a b)"), reads=[self.b_vecs], writes=[Buf()])
            S.dma("sp", self.DBG[:, 1024:2048], self.bc[:, 0, :], reads=[self.b_bc], writes=[Buf()])
            S.dma("sp", self.DBG[:, 2048:3072], self.bc[:, 2, :], reads=[self.b_bc], writes=[Buf()])

    def phase1(self):
        S, A = self.S, self.A
        m0 = A.mark()
        bW = self.b_W1
        V = self.vecs
        small = self.small
        xr = Ring([A.alloc([128, D], F32) for _ in range(3)])
        junk = A.alloc([128, D], BF16); b_junk = Buf()
        xnr = Ring([A.alloc([128, D], BF16) for _ in range(2)])
        uTr = Ring([A.alloc([128, 8, CH], BF16) for _ in range(2)])
        latTr = Ring([A.alloc([128, 4, CH], BF16) for _ in range(2)])
        stat = A.alloc([128, NT, 4], F32)
        stat2 = A.alloc([128, NT, 4], F32)
        znr = Ring([A.alloc([128, 512], BF16) for _ in range(2)])
        vsr = Ring([A.alloc([128, 512], BF16) for _ in range(3)])
        usr = Ring([A.alloc([128, 512], BF16) for _ in range(3)])
        sgr = Ring([A.alloc([128, 2048], BF16) for _ in range(3)])
        qtr = Ring([A.alloc([128, CH], BF16) for _ in range(3)])
        ktr = Ring([A.alloc([128, CH], BF16) for _ in range(3)])
        t1r = Ring([A.alloc([128, CH], F32) for _ in range(2)])
        t2r = Ring([A.alloc([128, CH], F32) for _ in range(2)])
        xT = {}
        state = {}

        def load_x(t):
            xt, bx = xr.next()
            S.dma("sp", xt, self.x[t * 128:(t + 1) * 128, :], writes=[bx])
            xT[t] = (xt, bx)

        xns = {}
        zns = {}

        def stageA1(t):
            xt, bx = xT.pop(t)
            bst = Buf()
            self.act(junk, xt, AF.Square, [bx], [b_junk, bst], accum_out=stat[:, t, 0:1])
            self.ts("dve", stat[:, t, 1:2], stat[:, t, 0:1], 1.0 / D, EPS, ALU.mult, ALU.add, [bst], [bst])
            self.tt("pool", stat[:, t, 2:3], stat[:, t, 1:2], self.mhalf[:, 0:1], ALU.pow, [bst, self.b_mhalf], [bst])
            xn, bxn = xnr.next()
            self.act(xn, xt, AF.Copy, [bx, bst], [bxn], scale=stat[:, t, 2:3])
            xns[t] = (xn, bxn)

        def stageA2(t):
            c, s = divmod(t, 4)
            if s == 0:
                state[c] = (uTr.next(), latTr.next())
            (uT, buT), (latT, blatT) = state[c]
            xn, bxn = xns.pop(t)
            pt, bp = self.psum()
            pv = pt[:, :].bitcast(BF16).rearrange("p (k n) -> p k n", k=8)
            for k in range(8):
                self.tr(pv[:, k, :], xn[:, k * 128:(k + 1) * 128], self.ident_b, [bxn, self.b_ident_b], [bp])
            for k in range(8):
                self.ts("dve", uT[:, k, s * 128:(s + 1) * 128], pv[:, k, :], V[:, 0, k:k + 1], V[:, 1, k:k + 1],
                        ALU.mult, ALU.add, [bp, self.b_vecs], [buT])

        def stageB1(t):
            c, s = divmod(t, 4)
            (uT, buT), (latT, blatT) = state[c]
            ts_ = slice(s * 128, (s + 1) * 128)
            pt, bp = self.psum()
            for k in range(8):
                self.mm(pt[:, :], uT[:, k, ts_], self.Wlat[:, k, :], k == 0, k == 7, [buT, bW], [bp])
            bst = Buf()
            self.act(junk[:, 0:256], pt[:, 0:256], AF.Square, [bp], [b_junk, bst], accum_out=stat2[:, t, 0:1])
            self.act(junk[:, 256:512], pt[:, 256:512], AF.Square, [bp], [b_junk, bst], accum_out=stat2[:, t, 1:2])
            self.ts("dve", stat2[:, t, 0:2], stat2[:, t, 0:2], 1.0 / 256, EPS, ALU.mult, ALU.add, [bst], [bst])
            self.tt("pool", stat2[:, t, 2:4], stat2[:, t, 0:2], self.mhalf[:, 0:2], ALU.pow,
                    [bst, self.b_mhalf], [bst])
            zn, bzn = znr.next()
            self.ts("dve", zn[:, 0:256], pt[:, 0:256], stat2[:, t, 2:3], None, ALU.mult, None, [bp, bst], [bzn])
            self.act(zn[:, 256:512], pt[:, 256:512], AF.Copy, [bp, bst], [bzn], scale=stat2[:, t, 3:4])
            zns[t] = (zn, bzn)

        def stageB2(t):
            c, s = divmod(t, 4)
            (uT, buT), (latT, blatT) = state[c]
            ts_ = slice(s * 128, (s + 1) * 128)
            zn, bzn = zns.pop(t)
            pt2, bp2 = self.psum()
            pv2 = pt2[:, 0:256].bitcast(BF16).rearrange("p (k n) -> p k n", k=4)
            for j in range(4):
                self.tr(pv2[:, j, :], zn[:, j * 128:(j + 1) * 128], self.ident_b, [bzn, self.b_ident_b], [bp2])
            for j in range(4):
                self.ts("dve", latT[:, j, ts_], pv2[:, j, :], small[:, 88 + j:89 + j], None, ALU.mult, None,
                        [bp2, self.b_small], [blatT])

        def stageC(t):
            c, s = divmod(t, 4)
            (uT, buT), (latT, blatT) = state[c]
            ts_ = slice(s * 128, (s + 1) * 128)
            rows = slice(t * 128, (t + 1) * 128)
            pt, bp = self.psum()
            for j in range(2):
                self.mm(pt[:, :], latT[:, 2 + j, ts_], self.Wuv[:, j, :], j == 0, j == 1, [blatT, bW], [bp])
            vs, bvs = vsr.next()
            self.cp("act", vs, pt[:, :], [bp], [bvs])
            self.defer(lambda: S.dma("sp", self.VS[rows, :], vs, reads=[bvs], writes=[Buf()]))
            pt, bp = self.psum()
            for k in range(8):
                self.mm(pt[:, :], uT[:, k, ts_], self.Wf[:, k, :], k == 0, k == 7, [buT, bW], [bp])
            us, bus = usr.next()
            self.cp("dve", us, pt[:, :], [bp], [bus])
            self.defer(lambda: S.dma("sp", self.US[rows, :], us, reads=[bus], writes=[Buf()]))
            sg, bsg = sgr.next()
            for g in range(4):
                pt, bp = self.psum()
                gs = slice(g * 512, (g + 1) * 512)
                for k in range(8):
                    self.mm(pt[:, :], uT[:, k, ts_], self.Wg[:, k, gs], k == 0, False, [buT, bW], [bp])
                self.mm(pt[:, :], self.ones_b[0:1, :], self.bg_b[0:1, gs], False, True, [self.b_ones_b, bW], [bp])
                self.act(sg[:, gs], pt[:, :], AF.Sigmoid, [bp], [bsg])
            self.defer(lambda: S.dma("sp", self.SG[rows, :], sg, reads=[bsg], writes=[Buf()]))

        def stageD(c):
            (uT, buT), (latT, blatT) = state[c]
            cs = slice(c * CH, (c + 1) * CH)
            R = slice(64, 96)
            for h in range(NH):
                pa, bpa = self.psum()
                for j in range(2):
                    self.mm(pa[0:96, :], self.Wuq[:, j, h, :], latT[:, j, :], j == 0, j == 1, [bW, blatT], [bpa])
                pb, bpb = self.psum()
                for j in range(2):
                    self.mm(pb[0:96, :], self.WuqR[:, j, h, :], latT[:, j, :], j == 0, j == 1, [bW, blatT], [bpb])
                qt, bqt = qtr.next()
                self.act(qt[0:64, :], pa[0:64, :], AF.Copy, [bpa], [bqt], scale=ATT_SCALE)
                t1, bt1 = t1r.next()
                t2, bt2 = t2r.next()
                self.stt(t1[R, :], pa[R, :], ATT_SCALE, self.cosT[R, cs], ALU.mult, ALU.mult, [bpa, self.b_cosT], [bt1])
                self.stt(t2[R, :], pb[R, :], ATT_SCALE, self.sinT[R, cs], ALU.mult, ALU.mult, [bpb, self.b_sinT], [bt2])
                self.tt("pool", qt[R, :], t1[R, :], t2[R, :], ALU.add, [bt1, bt2], [bqt])
                S.dma("sp", self.QT[h, :, cs], qt[0:96, :], reads=[bqt], writes=[Buf()])
                pk, bpk = self.psum()
                for j in range(2):
                    self.mm(pk[0:64, :], self.Wuk[:, j, h * 64:(h + 1) * 64], latT[:, 2 + j, :], j == 0, j == 1,
                            [bW, blatT], [bpk])
                kt, bkt = ktr.next()
                self.cp("act", kt[0:64, :], pk[0:64, :], [bpk], [bkt])
                S.dma("sp", self.KT[h, :, cs], kt[0:64, :], reads=[bkt], writes=[Buf()])
            pa, bpa = self.psum()
            for k in range(8):
                self.mm(pa[0:96, :], self.Wkr[:, k, 0, :], uT[:, k, :], k == 0, k == 7, [bW, buT], [bpa])
            pb, bpb = self.psum()
            for k in range(8):
                self.mm(pb[0:96, :], self.Wkr[:, k, 1, :], uT[:, k, :], k == 0, k == 7, [bW, buT], [bpb])
            t1, bt1 = t1r.next()
            t2, bt2 = t2r.next()
            self.tt("dve", t1[R, :], pa[R, :], self.cosT[R, cs], ALU.mult, [bpa, self.b_cosT], [bt1])
            self.tt("dve", t2[R, :], pb[R, :], self.sinT[R, cs], ALU.mult, [bpb, self.b_sinT], [bt2])
            kt, bkt = ktr.next()
            self.tt("pool", kt[R, :], t1[R, :], t2[R, :], ALU.add, [bt1, bt2], [bkt])
            S.dma("sp", self.KTR[:, cs], kt[R, :], reads=[bkt], writes=[Buf()])

        load_x(0)
        load_x(1)
        stages = [stageA1, stageA2, stageB1, stageB2, stageC]
        for i in range(NT + len(stages) - 1):
            if i + 2 < NT:
                load_x(i + 2)
            self.flush()
            for k_ in P1_ORDER:
                if 0 <= i - k_ < NT:
                    stages[k_](i - k_)
            tl = i - (len(stages) - 1)
            if 0 <= tl < NT and tl % 4 == 3:
                stageD(tl // 4)
        self.flush()
        S.barrier()
        A.reset(self.m_persist)


    def phase2b(self):
        S, A = self.S, self.A
        m0 = A.mark()
        WCS = A.alloc([128, 4, 2, D], BF16); bWCS = Buf()
        D2 = A.alloc([128, 128], BF16); bD2 = Buf()
        m1 = A.mark()
        wbf = A.alloc([128, 4, D], F32); bwbf = Buf()
        bd = A.alloc([128, 2, 128], F32); bbd = Buf()
        S.dma("sp", wbf, self.w_bf.rearrange("(j p) n -> p j n", p=128), writes=[bwbf])
        S.dma("sp", bd[:, 0, :], self.bdc[:, :], writes=[bbd])
        S.dma("sp", bd[:, 1, :], self.bds[:, :], writes=[bbd])
        S.dma("sp", D2, self.d2[:, :], writes=[bD2])
        for j in range(4):
            for r in range(2):
                for hf in range(2):
                    ps, bps = self.psum()
                    hs = slice(hf * 512, (hf + 1) * 512)
                    self.mm(ps[:, :], bd[:, r, :], wbf[:, j, hs], True, True, [bbd, bwbf], [bps])
                    self.cp("act" if hf else "dve", WCS[:, j, r, hs], ps[:, :], [bps], [bWCS])
        S.barrier()
        A.reset(m1)
        tbr = Ring([A.alloc([128, 2, 128], BF16) for _ in range(6)])
        unr = Ring([A.alloc([128, 512], BF16) for _ in range(6)])
        yor = Ring([A.alloc([128, 2, 512], BF16) for _ in range(4)])
        USv = self.US.rearrange("(n1 n2) c -> n2 n1 c", n2=64)
        ld1 = {}

        def load1(n2):
            tb, btb = tbr.next()
            S.dma("sp", tb, self.tri[n2, :, :, :], writes=[btb])
            un, bun = unr.next()
            S.dma("sp", un, USv[n2, :, :], writes=[bun])
            ld1[n2] = (tb, btb, un, bun)
        PF = 4
        for n2 in range(PF):
            load1(n2)
        for n2 in range(64):
            if n2 + PF < 64:
                load1(n2 + PF)
            self.flush()
            tb, btb, un, bun = ld1.pop(n2)
            yo, byo = yor.next()
            for r in range(2):
                ps, bps = self.psum()
                self.mm(ps[:, :], tb[:, r, :], un, True, True, [btb, bun], [bps])
                self.cp("act" if r else "dve", yo[:, r, :], ps[:, :], [bps], [byo])
            self.defer(lambda yo=yo, byo=byo, n2=n2: S.dma("sp", self.YS[:, :, n2, :], yo, reads=[byo], writes=[Buf()]))
        self.flush()
        S.barrier()
        ysr = Ring([A.alloc([128, 512], BF16) for _ in range(8)])
        gtr = Ring([A.alloc([128, 4, 2, 2, 64], BF16) for _ in range(3)])
        for_ = Ring([A.alloc([128, D], BF16) for _ in range(4)])
        FOv = self.FO.rearrange("(k2 k1) d -> k1 k2 d", k1=128)
        ld2 = {}

        def load2(k1):
            ys, bys = ysr.next()
            S.dma("sp", ys, self.YS[k1, :, :, :].rearrange("r n c -> (r n) c"), writes=[bys])
            ld2[k1] = (ys, bys)
        PF2 = 6
        for k1 in range(PF2):
            load2(k1)
        gts = {}

        def d2stage(kp):
            gt, bgt = gtr.next()
            for pr in range(2):
                k1 = 2 * kp + pr
                if k1 + PF2 < 128:
                    load2(k1 + PF2)
                ys, bys = ld2.pop(k1)
                ps, bps = self.psum()
                for j in range(4):
                    self.mm(ps[:, j * 128:(j + 1) * 128], ys[:, j * 128:(j + 1) * 128], D2, True, True,
                            [bys, bD2], [bps])
                self.cp("dve" if pr else "act", gt[:, :, :, pr, :],
                        ps[:, :].rearrange("p (j q k) -> p j q k", j=4, q=2), [bps], [bgt])
            gts[kp] = (gt, bgt)

        def proj(kp):
            gt, bgt = gts.pop(kp)
            fo, bfo = for_.next()
            for hf in range(2):
                hs = slice(hf * 512, (hf + 1) * 512)
                ps, bps = self.psum()
                n = 0
                for j in range(4):
                    for q in range(2):
                        self.mm(ps[:, :], gt[:, j, q, :, :].rearrange("p a b -> p (a b)"), WCS[:, j, q, hs],
                                n == 0, n == 7, [bgt, bWCS], [bps])
                        n += 1
                self.cp("act" if hf else "dve", fo[:, hs], ps[:, :], [bps], [bfo])
            self.defer(lambda: S.dma("sp", FOv[2 * kp, :, :], fo[0:64, :], reads=[bfo], writes=[Buf()]))
            self.defer(lambda: S.dma("sp", FOv[2 * kp + 1, :, :], fo[64:128, :], reads=[bfo], writes=[Buf()]))

        d2stage(0)
        for kp in range(64):
            self.flush()
            if kp + 1 < 64:
                d2stage(kp + 1)
            proj(kp)
        self.flush()
        S.barrier()
        A.reset(m0)

    def phase2a(self):
        S, A = self.S, self.A
        m0 = A.mark()
        pts = self.pring.items
        sring = Ring(pts[0:6])
        oring = Ring(pts[6:8])
        Vall = A.alloc([128, NT, NH, 65], BF16); bVs = [Buf() for _ in range(8)]
        vst = Ring([A.alloc([128, 8, 512], BF16) for _ in range(2)])
        Kr = Ring([A.alloc([128, S_TOK], BF16) for _ in range(2)])
        Qr = Ring([A.alloc([128, CH], BF16) for _ in range(3)])
        Pr = Ring([A.alloc([128, CH], BF16) for _ in range(4)])
        srr = Ring([A.alloc([128, CH], F32) for _ in range(2)])
        recr = Ring([A.alloc([128, CH], F32) for _ in range(2)])
        onr = Ring([A.alloc([128, CH], BF16) for _ in range(2)])
        self.memset("pool", Vall[:, :, :, 64:65], 1.0, bVs)
        for g in range(8):
            vs, bvs = vst.next()
            S.dma("sp", vs, self.VS[g * 1024:(g + 1) * 1024, :].rearrange("(t p) n -> p t n", p=128), writes=[bvs])
            self.cp("pool" if g % 2 else "dve", Vall[:, g * 8:(g + 1) * 8, :, 0:64],
                    vs.rearrange("p t (h d) -> p t h d", h=NH), [bvs], [bVs[g]])
        kbufs = []
        for i in range(2):
            kb, bk = Kr.next()
            brope = Buf()
            S.dma("sp", kb[64:96, :], self.KTR[:, :], writes=[brope])
            kbufs.append((kb, bk, brope))
        LAG = 2
        cst = Ring([A.alloc([128, 2048], F32) for _ in range(3)])
        cbf = Ring([A.alloc([128, 2048], BF16) for _ in range(2)])
        WGUv = self.WGU.rearrange("r (k f) -> r k f", k=8)
        units = [(e_, kind) for e_ in range(32) for kind in range(3)]
        cld = {}

        def conv_load(u):
            if u >= len(units):
                return
            e_, kind = units[u]
            t_, b_ = cst.next()
            if kind == 0:
                S.dma("sp", t_.rearrange("p (k f) -> p k f", k=8),
                      self.w_eg[e_, :, :].rearrange("(k p) f -> p k f", p=128), writes=[b_])
            elif kind == 1:
                S.dma("sp", t_.rearrange("p (k f) -> p k f", k=8),
                      self.w_eu[e_, :, :].rearrange("(k p) f -> p k f", p=128), writes=[b_])
            else:
                S.dma("sp", t_.rearrange("p (j d) -> p j d", j=2),
                      self.w_ed[e_, :, :].rearrange("(j p) d -> p j d", p=128), writes=[b_])
            cld[u] = (t_, b_)

        def conv_cast(u):
            if u >= len(units):
                return
            e_, kind = units[u]
            t_, b_ = cld.pop(u)
            o_, bo_ = cbf.next()
            self.cp("pool", o_, t_, [b_], [bo_])
            rows = slice(e_ * 128, (e_ + 1) * 128)
            if kind == 0:
                S.dma("sp", WGUv[rows, :, 0:256], o_.rearrange("p (k f) -> p k f", k=8), reads=[bo_], writes=[Buf()])
            elif kind == 1:
                S.dma("sp", WGUv[rows, :, 256:512], o_.rearrange("p (k f) -> p k f", k=8), reads=[bo_], writes=[Buf()])
            else:
                S.dma("sp", self.WD[rows, :], o_, reads=[bo_], writes=[Buf()])

        conv_load(0)
        zt = A.alloc([128, 4096], BF16); bzt = Buf()
        self.memset("pool", zt, 0.0, [bzt])
        XSz = self.XS.rearrange("(p r) d -> p (r d)", p=128)
        nz = (NSLOT // 128) * D // 4096

        def zero_fill(i):
            if 0 <= i < nz:
                S.dma("sp", XSz[:, i * 4096:(i + 1) * 4096], zt, reads=[bzt], writes=[Buf()])

        def load_k(h):
            kb, bk, brope = kbufs[h % 2]
            S.dma("sp", kb[0:64, :], self.KT[h, :, :], writes=[bk])

        load_k(0)
        fin_pending = []
        for h in range(NH):
            kb, bk, brope = kbufs[h % 2]
            if h + 1 < NH:
                load_k(h + 1)
            qts = {}

            def load_q(qc):
                qt, bq = Qr.next()
                S.dma("sp", qt[0:96, :], self.QT[h, :, qc * CH:(qc + 1) * CH], writes=[bq])
                qts[qc] = (qt, bq)
            load_q(0)
            load_q(1)
            for qc in range(NCH):
                if qc + 2 < NCH:
                    load_q(qc + 2)
                conv_load(h * NCH + qc + 1)
                conv_cast(h * NCH + qc)
                zero_fill(h * NCH + qc - 70)
                qt, bq = qts.pop(qc)
                po, bpo = oring.next()
                pend = []

                def pv(kt, pT, bpT):
                    self.mm(po[0:65, :], Vall[:, kt, h, :], pT, kt == 0, kt == NT - 1, [bVs[kt // 8], bpT], [bpo])
                for kt in range(NT):
                    if kt == 3 and fin_pending:
                        fin_pending.pop(0)()
                    ps, bps = sring.next()
                    self.mm(ps[:, :], kb[0:96, kt * 128:(kt + 1) * 128], qt[0:96, :], True, True,
                            [bk, brope, bq], [bps])
                    pT, bpT = Pr.next()
                    self.act(pT, ps[:, :], AF.Exp, [bps], [bpT])
                    pend.append((kt, pT, bpT))
                    if len(pend) > LAG:
                        pv(*pend.pop(0))
                while pend:
                    pv(*pend.pop(0))
                def fin(po=po, bpo=bpo, h=h, qc=qc):
                    sr, bsr = srr.next()
                    self.cp("dve", sr[64:65, :], po[64:65, :], [bpo], [bsr])
                    pb, bpb = sring.next()
                    self.mm(pb[0:64, :], self.ones_f[64:65, 0:64], sr[64:65, :], True, True, [self.b_ones_f, bsr], [bpb])
                    rec, brec = recr.next()
                    self.S.op("dve", lambda e, o_=rec[0:64, :], i_=pb[0:64, :]: e.reciprocal(out=o_, in_=i_), [bpb], [brec])
                    on, bon = onr.next()
                    self.tt("dve", on[0:64, :], po[0:64, :], rec[0:64, :], ALU.mult, [bpo, brec], [bon])
                    S.dma("sp", self.OT[h, :, qc * CH:(qc + 1) * CH], on[0:64, :], reads=[bon], writes=[Buf()])
                fin_pending.append(fin)
        while fin_pending:
            fin_pending.pop(0)()
        S.barrier()
        A.reset(m0)

    def phase3(self):
        S, A = self.S, self.A
        V = self.vecs
        self.w12 = A.alloc([128, NT, 2], F32); self.b_w12 = Buf()
        self.slot_i = A.alloc([128, 2, NT], I32); self.b_slot = Buf()
        self.idxw = A.alloc([128, NST], I32); self.b_idxw = Buf()
        self.m_persist = A.mark()
        M12 = A.alloc([128, 2, NT, 32], BF16); bM = Buf()
        rank = A.alloc([128, NT, 32], F32); brank = Buf()
        Rr = A.alloc([128, 32], F32); bR = Buf()
        m0 = A.mark()
        Wba = A.alloc([128, 4, D], BF16)
        Wout = A.alloc([128, 8, D], BF16)
        Wr = A.alloc([128, 8, 36], BF16)
        br_b = A.alloc([128, 36], BF16)
        Ltri = A.alloc([128, 128], BF16)
        AB2 = A.alloc([128, 2, D], F32)
        bW = Buf()
        m1 = A.mark()
        stg = Ring([A.alloc([128, 8, 512], F32) for _ in range(2)])
        for hf in range(2):
            t, b = stg.next()
            S.dma("sp", t[:, 0:4, :], self.w_ba[:, hf * 512:(hf + 1) * 512].rearrange("(j p) n -> p j n", p=128), writes=[b])
            self.cp("dve", Wba[:, :, hf * 512:(hf + 1) * 512], t[:, 0:4, :], [b], [bW])
        for hf in range(2):
            t, b = stg.next()
            S.dma("sp", t, self.w_out[:, hf * 512:(hf + 1) * 512].rearrange("(k p) n -> p k n", p=128), writes=[b])
            self.tt("dve", Wout[:, :, hf * 512:(hf + 1) * 512], t,
                    self.bc[:, 0, hf * 512:(hf + 1) * 512].unsqueeze(1).broadcast_to([128, 8, 512]), ALU.mult,
                    [b, self.b_bc], [bW])
        t, b = stg.next()
        S.dma("sp", t[:, :, 0:36], self.w_rt.rearrange("(k p) n -> p k n", p=128), writes=[b])
        self.cp("dve", Wr, t[:, :, 0:36], [b], [bW])
        t, b = stg.next()
        S.dma("sp", t[0:1, 0, 0:36], self.b_rt[0:1, :], writes=[b])
        self.cp("dve", br_b[0:1, :], t[0:1, 0, 0:36], [b], [bW])
        S.dma("sp", Ltri, self.ltri[:, :], writes=[bW])
        self.memset("dve", Rr, 0.0, [bR])
        diag_r = Ring([A.alloc([128, 128], F32) for _ in range(2)])
        for i in range(2):
            for j in range(8):
                dg, bd = diag_r.next()
                self.ts("dve", dg, self.ident_f, V[:, 2 + i, j:j + 1], None, ALU.mult, None,
                        [self.b_ident_f, self.b_vecs], [bd])
                pt, bp = self.psum()
                self.mm(pt[:, 0:128], self.ones_f, dg, True, True, [self.b_ones_f, bd], [bp])
                self.cp("act", AB2[:, i, j * 128:(j + 1) * 128], pt[:, 0:128], [bp], [bW])
        S.barrier()
        A.reset(m1)
        oTr = Ring([A.alloc([128, 4, CH], BF16) for _ in range(2)])
        for_ = Ring([A.alloc([128, D], BF16) for _ in range(P3_PF + 1)])
        sgr = Ring([A.alloc([128, 2 * D], BF16) for _ in range(P3_PF + 1)])
        xr = Ring([A.alloc([128, D], F32) for _ in range(P3_PF + 3)])
        m1r = Ring([A.alloc([128, D], BF16) for _ in range(2)])
        m2r = Ring([A.alloc([128, D], BF16) for _ in range(2)])
        mgr = Ring([A.alloc([128, D], BF16) for _ in range(2)])
        mTr = Ring([A.alloc([128, 8, 128], BF16) for _ in range(2)])
        h1r = Ring([A.alloc([128, D], F32) for _ in range(4)])
        hnr = Ring([A.alloc([128, D], F32) for _ in range(2)])
        vnr = Ring([A.alloc([128, D], BF16) for _ in range(3)])
        vTr = Ring([A.alloc([128, 8, 128], BF16) for _ in range(2)])
        junk = A.alloc([128, D], BF16); b_junk = Buf()
        stat = A.alloc([128, NT, 4], F32)
        lg4r = Ring([A.alloc([128, 4, 36], F32) for _ in range(2)])
        rs = A.alloc([128, 1024], F32); brs = Buf()

        st = {}
        chunk = {}
        lgs = {}

        def loads(t):
            c, s_ = divmod(t, 4)
            rows = slice(t * 128, (t + 1) * 128)
            if s_ == 0:
                oT, boT = oTr.next()
                for h in range(NH):
                    S.dma("sp", oT[(h % 2) * 64:(h % 2) * 64 + 64, h // 2, :], self.OT[h, :, c * CH:(c + 1) * CH],
                          writes=[boT])
                chunk[c] = (oT, boT)
            fo, bfo = for_.next()
            S.dma("sp", fo, self.FO[rows, :], writes=[bfo])
            sg, bsg = sgr.next()
            S.dma("sp", sg, self.SG[rows, :], writes=[bsg])
            xt, bx = xr.next()
            S.dma("sp", xt, self.x[rows, :], writes=[bx])
            st[t] = dict(fo=(fo, bfo), sg=(sg, bsg), x=(xt, bx))

        def stage1(t):
            c, s_ = divmod(t, 4)
            oT, boT = chunk[c]
            d = st[t]
            fo, bfo = d["fo"]; sg, bsg = d["sg"]
            ts_ = slice(s_ * 128, (s_ + 1) * 128)
            m1, bm1 = m1r.next()
            for hf in range(2):
                hs = slice(hf * 512, (hf + 1) * 512)
                ps, bps = self.psum()
                for j in range(4):
                    self.mm(ps[:, :], oT[:, j, ts_], Wba[:, j, hs], j == 0, j == 3, [boT, bW], [bps])
                self.tt("dve", m1[:, hs], ps[:, :], sg[:, hs], ALU.mult, [bps, bsg], [bm1])
            m2, bm2 = d["m2"]
            mg, bmg = mgr.next()
            self.tt("dve", mg, m1, m2, ALU.add, [bm1, bm2], [bmg])
            d["mg"] = (mg, bmg)

        def stage1p(t):
            d = st[t]
            fo, bfo = d["fo"]; sg, bsg = d["sg"]
            m2, bm2 = m2r.next()
            self.tt("pool", m2, fo, sg[:, D:2 * D], ALU.mult, [bfo, bsg], [bm2])
            d["m2"] = (m2, bm2)

        def stage2(t):
            d = st[t]
            mg, bmg = d["mg"]
            ps, bps = self.psum()
            pv = ps[:, :].bitcast(BF16).rearrange("p (k n) -> p k n", k=8)
            for k in range(8):
                self.tr(pv[:, k, :], mg[:, k * 128:(k + 1) * 128], self.ident_b, [bmg, self.b_ident_b], [bps])
            mT, bmT = mTr.next()
            self.cp("act", mT, pv, [bps], [bmT])
            d["mT"] = (mT, bmT)

        def stage3(t):
            d = st[t]
            mT, bmT = d["mT"]
            xt, bx = d["x"]
            rows = slice(t * 128, (t + 1) * 128)
            h1, bh1 = h1r.next()
            for hf in range(2):
                hs = slice(hf * 512, (hf + 1) * 512)
                ps, bps = self.psum()
                for k in range(8):
                    self.mm(ps[:, :], mT[:, k, :], Wout[:, k, hs], k == 0, k == 7, [bmT, bW], [bps])
                self.tt("dve", h1[:, hs], ps[:, :], xt[:, hs], ALU.add, [bps, bx], [bh1])
            self.defer(lambda: S.dma("sp", self.H1[rows, :], h1, reads=[bh1], writes=[Buf()]))
            bst = Buf()
            self.act(junk, h1, AF.Square, [bh1], [b_junk, bst], accum_out=stat[:, t, 0:1])
            d["h1"] = (h1, bh1, bst)

        def stage3b(t):
            d = st[t]
            h1, bh1, bst = d["h1"]
            self.ts("dve", stat[:, t, 1:2], stat[:, t, 0:1], 1.0 / D, EPS, ALU.mult, ALU.add, [bst], [bst])
            self.tt("pool", stat[:, t, 2:3], stat[:, t, 1:2], self.mhalf[:, 0:1], ALU.pow, [bst, self.b_mhalf], [bst])

        def stage4(t):
            d = st[t]
            h1, bh1, bst = d["h1"]
            rows = slice(t * 128, (t + 1) * 128)
            hn, bhn = hnr.next()
            self.act(hn, h1, AF.Copy, [bh1, bst], [bhn], scale=stat[:, t, 2:3])
            self.tt("dve", hn, hn, AB2[:, 0, :], ALU.mult, [bhn, bW], [bhn])
            vn, bvn = vnr.next()
            self.tt("pool", vn, hn, AB2[:, 1, :], ALU.add, [bhn, bW], [bvn])
            self.defer(lambda: S.dma("sp", self.VN[rows, :], vn, reads=[bvn], writes=[Buf()]))
            d["vn"] = (vn, bvn)

        def stage5(t):
            d = st[t]
            vn, bvn = d["vn"]
            ps, bps = self.psum()
            pv = ps[:, :].bitcast(BF16).rearrange("p (k n) -> p k n", k=8)
            for k in range(8):
                self.tr(pv[:, k, :], vn[:, k * 128:(k + 1) * 128], self.ident_b, [bvn, self.b_ident_b], [bps])
            vT, bvT = vTr.next()
            self.cp("act", vT, pv, [bps], [bvT])
            d["vT"] = (vT, bvT)

        def stageC(t):
            c, s_ = divmod(t, 4)
            d = st.pop(t)
            vT, bvT = d["vT"]
            if s_ == 0:
                lgs[c] = lg4r.next()
            lg4, blg = lgs[c]
            ps, bps = self.psum()
            for k in range(8):
                self.mm(ps[:, 0:36], vT[:, k, :], Wr[:, k, :], k == 0, False, [bvT, bW], [bps])
            self.mm(ps[:, 0:36], self.ones_b[0:1, :], br_b[0:1, :], False, True, [self.b_ones_b, bW], [bps])
            self.cp("dve", lg4[:, s_, :], ps[:, 0:36], [bps], [blg])
            if s_ == 3:
                pending.append(route(c))

        def bc(ap, shape, axis):
            return ap.unsqueeze(axis).broadcast_to(shape)

        def route(c):
            lg4, blg = lgs.pop(c)
            t0 = c * 4
            G = lg4[:, :, 0:4]
            E = lg4[:, :, 4:36].rearrange("p t (g e) -> p t g e", g=4)
            o = [0]

            def sc(n, shape):
                v = rs[:, o[0]:o[0] + n]
                o[0] += n
                if len(shape) == 2:
                    return v.rearrange("p (a b) -> p a b", a=shape[0])
                if len(shape) == 3:
                    return v.rearrange("p (a b c) -> p a b c", a=shape[0], b=shape[1])
                return v
            R_ = [blg, brs]
            W_ = [brs]
            dve = self.S.op
            gmax = sc(4, (4,))
            dve("dve", lambda e: e.reduce_max(out=gmax, in_=G, axis=AX.X), R_, W_)
            gm = sc(16, (4, 4))
            self.tt("dve", gm, G, bc(gmax, [128, 4, 4], 2), ALU.is_ge, R_, W_)
            gd = sc(16, (4, 4))
            self.tt("dve", gd, G, bc(gmax, [128, 4, 4], 2), ALU.subtract, R_, W_)
            ge = sc(16, (4, 4))
            self.act(ge, gd, AF.Exp, R_, W_)
            gsum = sc(4, (4,))
            dve("dve", lambda e: e.reduce_sum(out=gsum, in_=ge, axis=AX.X), R_, W_)
            tmp = sc(128, (4, 4, 8))
            self.tt("dve", tmp, E, bc(gm, [128, 4, 4, 8], 3), ALU.mult, R_, W_)
            esel = sc(32, (4, 8))
            dve("dve", lambda e: e.reduce_sum(out=esel, in_=tmp.rearrange("p t g e -> p t e g"), axis=AX.X), R_, W_)
            yield
            top8 = sc(32, (4, 8))
            for i in range(4):
                dve("dve", lambda e, i=i: e.max(out=top8[:, i, :], in_=esel[:, i, :]), R_, W_)
            sel = sc(32, (4, 8))
            self.tt("dve", sel, esel, top8[:, :, 1:2].broadcast_to([128, 4, 8]), ALU.is_ge, R_, W_)
            sel1 = sc(32, (4, 8))
            self.tt("dve", sel1, esel, top8[:, :, 0:1].broadcast_to([128, 4, 8]), ALU.is_ge, R_, W_)
            sel2 = sc(32, (4, 8))
            self.tt("dve", sel2, sel, sel1, ALU.subtract, R_, W_)
            ed = sc(32, (4, 8))
            self.tt("dve", ed, esel, top8[:, :, 0:1].broadcast_to([128, 4, 8]), ALU.subtract, R_, W_)
            ex = sc(32, (4, 8))
            self.act(ex, ed, AF.Exp, R_, W_)
            yield
            sx = sc(32, (4, 8))
            self.tt("dve", sx, sel, ex, ALU.mult, R_, W_)
            den = sc(4, (4,))
            dve("dve", lambda e: e.reduce_sum(out=den, in_=sx, axis=AX.X), R_, W_)
            gd2 = sc(4, (4,))
            self.tt("dve", gd2, gsum, den, ALU.mult, R_, W_)
            coef = sc(4, (4,))
            dve("dve", lambda e: e.reciprocal(out=coef, in_=gd2), R_, W_)
            w8 = sc(32, (4, 8))
            self.tt("dve", w8, sx, bc(coef, [128, 4, 8], 2), ALU.mult, R_, W_)
            tw = sc(32, (4, 8))
            self.tt("dve", tw, sel1, w8, ALU.mult, R_, W_)
            dve("dve", lambda e: e.reduce_sum(out=self.w12[:, t0:t0 + 4, 0], in_=tw, axis=AX.X), R_, [brs, self.b_w12])
            tw2 = sc(32, (4, 8))
            self.tt("dve", tw2, sel2, w8, ALU.mult, R_, W_)
            dve("dve", lambda e: e.reduce_sum(out=self.w12[:, t0:t0 + 4, 1], in_=tw2, axis=AX.X), R_, [brs, self.b_w12])
            yield
            M1 = M12[:, 0, t0:t0 + 4, :].rearrange("p t (g e) -> p t g e", g=4)
            M2 = M12[:, 1, t0:t0 + 4, :].rearrange("p t (g e) -> p t g e", g=4)
            self.tt("dve", M1, bc(gm, [128, 4, 4, 8], 3), bc(sel1, [128, 4, 4, 8], 2), ALU.mult, R_, [brs, bM])
            self.tt("dve", M2, bc(gm, [128, 4, 4, 8], 3), bc(sel2, [128, 4, 4, 8], 2), ALU.mult, R_, [brs, bM])
            Mt = rs[:, 512:512 + 64].bitcast(BF16).rearrange("p (t e) -> p t e", t=4)
            self.tt("dve", Mt, M12[:, 0, t0:t0 + 4, :], M12[:, 1, t0:t0 + 4, :], ALU.add, [bM, brs], W_)
            for i in range(4):
                ps, bps = self.psum()
                self.mm(ps[:, 0:32], Ltri, Mt[:, i, :], True, True, [bW, brs], [bps])
                self.mm(ps[:, 32:64], self.ones_b, Mt[:, i, :], True, True, [self.b_ones_b, brs], [bps])
                self.tt("dve", rank[:, t0 + i, :], ps[:, 0:32], Rr, ALU.add, [bps, bR], [brank])
                self.tt("dve", Rr, Rr, ps[:, 32:64], ALU.add, [bps, bR], [bR])
            yield

        for t_ in range(P3_PF):
            loads(t_)
        stages = [stage1, stage2, stage3, stage3b, stage4, stage5, stageC]
        order = P3_ORDER
        pending = []

        def pump():
            for g_ in list(pending):
                try:
                    next(g_)
                except StopIteration:
                    pending.remove(g_)
        for i in range(NT + len(stages) - 1):
            if i + P3_PF < NT:
                loads(i + P3_PF)
            self.flush()
            if i < NT:
                stage1p(i)
            for k_ in order:
                if 0 <= i - k_ < NT:
                    stages[k_](i - k_)
            pump()
        while pending:
            pump()
        self.flush()
        S.barrier()
        A.reset(m0)
        cnt = A.alloc([128, 32], F32)
        ci = A.alloc([128, 32], I32)
        pad = A.alloc([128, 32], F32)
        cs = [A.alloc([128, 32], F32) for _ in range(2)]
        base = A.alloc([128, 32], F32)
        sf = A.alloc([128, NT, 32], F32)
        prod = A.alloc([128, NT, 32], F32)
        slf = A.alloc([128, 2, NT], F32)
        tst = A.alloc([128, NST], F32)
        acc = A.alloc([128, NST], F32)
        pio = A.alloc([128, 1], F32)
        bb = Buf()
        RW = ([bb, bR, brank, bM], [bb])
        S.dma("sp", tst, self.tstart[:, :], writes=[bb])
        S.dma("sp", pio, self.piota[:, :], writes=[bb])
        self.ts("dve", cnt, Rr, float(TR - 1), 1.0 / TR, ALU.add, ALU.mult, *RW)
        self.ts("dve", ci, cnt, -0.5 + 0.5 / TR, None, ALU.add, None, *RW)
        self.cp("dve", pad, ci, *RW)
        self.ts("dve", pad, pad, float(TR), None, ALU.mult, None, *RW)
        cur = cs[0]
        self.cp("dve", cur, pad, *RW)
        k = 0
        for sh in (1, 2, 4, 8, 16):
            nxt = cs[1 - k]
            self.cp("dve", nxt[:, 0:sh], cur[:, 0:sh], *RW)
            self.tt("dve", nxt[:, sh:32], cur[:, sh:32], cur[:, 0:32 - sh], ALU.add, *RW)
            cur = nxt
            k = 1 - k
        bend = cur
        self.tt("dve", base, bend, pad, ALU.subtract, *RW)
        self.tt("dve", sf, rank, base.unsqueeze(1).broadcast_to([128, NT, 32]), ALU.add, *RW)
        for q in range(2):
            self.tt("dve", prod, sf, M12[:, q, :, :], ALU.mult, *RW)
            self.S.op("dve", lambda e, q=q: e.reduce_sum(out=slf[:, q, :], in_=prod, axis=AX.X), *RW)
        self.cp("dve", self.slot_i, slf, RW[0], [bb, self.b_slot])
        self.memset("dve", acc, 0.0, [bb])
        for e_ in range(32):
            self.stt(acc, tst, bend[:, e_:e_ + 1], acc, ALU.is_ge, ALU.add, *RW)
        self.ts("dve", acc, acc, 31.0, 128.0, ALU.min, ALU.mult, *RW)
        self.ts("dve", acc, acc, pio[:, 0:1], None, ALU.add, None, *RW)
        self.cp("dve", self.idxw, acc, RW[0], [bb, self.b_idxw])
        if "RT" in self.dbg:
            S.dma("sp", self.RT[:, 0:128], slf.rearrange("p a b -> p (a b)"), reads=[bb], writes=[Buf()])
            S.dma("sp", self.RT[:, 128:256], self.w12.rearrange("p a b -> p (a b)"), reads=[bb, self.b_w12], writes=[Buf()])
            S.dma("sp", self.RT[:, 256:256 + NST], acc, reads=[bb], writes=[Buf()])
            S.dma("sp", self.RT[:, 512:544], Rr, reads=[bb, bR], writes=[Buf()])
            S.dma("sp", self.RT[:, 544:576], bend, reads=[bb], writes=[Buf()])
        S.barrier()
        A.reset(self.m_persist)

    def idma(self, out, in_, idx, gather, reads, writes):
        if gather:
            fn = lambda e: e.indirect_dma_start(out=out, out_offset=None, in_=in_,
                                                in_offset=bass.IndirectOffsetOnAxis(ap=idx, axis=0))
        else:
            fn = lambda e: e.indirect_dma_start(out=out, out_offset=bass.IndirectOffsetOnAxis(ap=idx, axis=0),
                                                in_=in_, in_offset=None)
        return self.S.op("pool", fn, reads, writes, is_dma=True)

    def phase4(self):
        S, A = self.S, self.A
        m0 = A.mark()
        bwgu = self.b_wgu; bwd = self.b_wd
        vr = Ring([A.alloc([128, D], BF16) for _ in range(3)])
        bxs = Buf()
        for t in range(NT):
            v, bv = vr.next()
            S.dma("sp", v, self.VN[t * 128:(t + 1) * 128, :], writes=[bv])
            for q in range(2):
                self.idma(self.XS[:, :], v, self.slot_i[:, q, t:t + 1], False, [bv, self.b_slot], [Buf()])
        S.barrier()
        xsr = Ring([A.alloc([128, D], BF16) for _ in range(10)])
        wgr = Ring([A.alloc([128, 8, 512], BF16) for _ in range(5)])
        wdr = Ring([A.alloc([128, 2, D], BF16) for _ in range(5)])
        xTr = Ring([A.alloc([128, 8, 128], BF16) for _ in range(2)])
        sgl = Ring([A.alloc([128, 256], F32) for _ in range(2)])
        hdr = Ring([A.alloc([128, 256], BF16) for _ in range(2)])
        hTr = Ring([A.alloc([128, 2, 128], BF16) for _ in range(2)])
        ysr = Ring([A.alloc([128, D], BF16) for _ in range(3)])
        ld = {}

        def loads(s_):
            wg, bwg = wgr.next()
            self.idma(wg.rearrange("p k f -> p (k f)"), self.WGU[:, :], self.idxw[:, s_:s_ + 1], True,
                      [bwgu, self.b_idxw], [bwg])
            wd, bwd_ = wdr.next()
            self.idma(wd.rearrange("p j d -> p (j d)"), self.WD[:, :], self.idxw[:, s_:s_ + 1], True,
                      [bwd, self.b_idxw], [bwd_])
            xl = []
            for u in range(TR // 128):
                xs, bx = xsr.next()
                r0 = s_ * TR + u * 128
                S.dma("sp", xs, self.XS[r0:r0 + 128, :], reads=[bxs], writes=[bx])
                xl.append((xs, bx))
            ld[s_] = (xl, wg, bwg, wd, bwd_)

        SUB = TR // 128
        NU = NST * SUB
        stt_ = {}

        def e1(u):
            xl, wg, bwg, wd, bwd_ = ld[u // SUB]
            xs, bx = xl[u % SUB]
            ps, bps = self.psum()
            pv = ps[:, :].bitcast(BF16).rearrange("p (k n) -> p k n", k=8)
            for k in range(8):
                self.tr(pv[:, k, :], xs[:, k * 128:(k + 1) * 128], self.ident_b, [bx, self.b_ident_b], [bps])
            xT, bxT = xTr.next()
            self.cp("act", xT, pv, [bps], [bxT])
            stt_[u] = dict(xT=(xT, bxT))

        def e2(u):
            xl, wg, bwg, wd, bwd_ = ld[u // SUB]
            xT, bxT = stt_[u]["xT"]
            ps, bps = self.psum()
            for k in range(8):
                self.mm(ps[:, :], xT[:, k, :], wg[:, k, :], k == 0, k == 7, [bxT, bwg], [bps])
            sg, bsg = sgl.next()
            self.act(sg, ps[:, 0:256], AF.Silu, [bps], [bsg])
            hd, bhd = hdr.next()
            self.tt("dve", hd, ps[:, 256:512], sg, ALU.mult, [bps, bsg], [bhd])
            stt_[u]["hd"] = (hd, bhd)

        def e3(u):
            hd, bhd = stt_[u]["hd"]
            ps2, bps2 = self.psum()
            pv2 = ps2[:, 0:128].bitcast(BF16).rearrange("p (k n) -> p k n", k=2)
            for j in range(2):
                self.tr(pv2[:, j, :], hd[:, j * 128:(j + 1) * 128], self.ident_b, [bhd, self.b_ident_b], [bps2])
            hT, bhT = hTr.next()
            self.cp("act", hT, pv2, [bps2], [bhT])
            stt_[u]["hT"] = (hT, bhT)

        def e4(u):
            xl, wg, bwg, wd, bwd_ = ld[u // SUB]
            hT, bhT = stt_.pop(u)["hT"]
            r0 = u * 128
            ys, bys = ysr.next()
            for hf in range(2):
                hs = slice(hf * 512, (hf + 1) * 512)
                ps3, bps3 = self.psum()
                for j in range(2):
                    self.mm(ps3[:, :], hT[:, j, :], wd[:, j, hs], j == 0, j == 1, [bhT, bwd_], [bps3])
                self.tt("dve", ys[:, hs], ps3[:, :], self.bc[:, 1, hs], ALU.mult, [bps3, self.b_bc], [bys])
            self.defer(lambda: S.dma("sp", self.YS2[r0:r0 + 128, :], ys, reads=[bys], writes=[Buf()]))
            if u % SUB == SUB - 1:
                ld.pop(u // SUB)

        loads(0)
        loads(1)
        est = [e1, e2, e3, e4]
        eorder = E_ORDER
        for i in range(NU + 3):
            if i % SUB == 0 and i // SUB + 2 < NST:
                loads(i // SUB + 2)
            self.flush()
            for k_ in eorder:
                if 0 <= i - k_ < NU:
                    est[k_](i - k_)
        self.flush()
        S.barrier()
        A.reset(m0)

    def phase5(self):
        S, A = self.S, self.A
        m0 = A.mark()
        y1r = Ring([A.alloc([128, D], BF16) for _ in range(3)])
        y2r = Ring([A.alloc([128, D], BF16) for _ in range(3)])
        h1r = Ring([A.alloc([128, D], F32) for _ in range(3)])
        mr = Ring([A.alloc([128, D], F32) for _ in range(2)])
        h2r = Ring([A.alloc([128, D], F32) for _ in range(4)])
        outr = Ring([A.alloc([128, D], F32) for _ in range(4)])
        junk = A.alloc([128, D], BF16); b_junk = Buf()
        stat = A.alloc([128, NT, 4], F32)
        ld = {}

        def loads(t):
            y1, by1 = y1r.next()
            self.idma(y1, self.YS2[:, :], self.slot_i[:, 0, t:t + 1], True, [self.b_ys2, self.b_slot], [by1])
            y2, by2 = y2r.next()
            self.idma(y2, self.YS2[:, :], self.slot_i[:, 1, t:t + 1], True, [self.b_ys2, self.b_slot], [by2])
            h1, bh1 = h1r.next()
            S.dma("sp", h1, self.H1[t * 128:(t + 1) * 128, :], writes=[bh1])
            ld[t] = (y1, by1, y2, by2, h1, bh1)

        mid = {}
        mid0 = {}

        def s1(t):
            y1, by1, y2, by2, h1, bh1 = ld.pop(t)
            m, bm = mr.next()
            self.act(m, y1, AF.Copy, [by1, self.b_w12], [bm], scale=self.w12[:, t, 0:1])
            self.stt(m, y2, self.w12[:, t, 1:2], m, ALU.mult, ALU.add, [by2, self.b_w12, bm], [bm])
            h2, bh2 = h2r.next()
            self.tt("pool", h2, m, h1, ALU.add, [bm, bh1], [bh2])
            mid0[t] = (h2, bh2)

        def s1b(t):
            h2, bh2 = mid0.pop(t)
            bst = Buf()
            self.act(junk, h2, AF.Square, [bh2], [b_junk, bst], accum_out=stat[:, t, 0:1])
            self.ts("dve", stat[:, t, 1:2], stat[:, t, 0:1], 1.0 / D, EPS, ALU.mult, ALU.add, [bst], [bst])
            self.tt("pool", stat[:, t, 2:3], stat[:, t, 1:2], self.mhalf[:, 0:1], ALU.pow, [bst, self.b_mhalf], [bst])
            mid[t] = (h2, bh2, bst)

        def s2(t):
            h2, bh2, bst = mid.pop(t)
            o, bo = outr.next()
            self.stt(o, h2, stat[:, t, 2:3], self.bc[:, 2, :], ALU.mult, ALU.mult, [bh2, bst, self.b_bc], [bo])
            self.tt("dve", o, o, self.bc[:, 3, :], ALU.add, [bo, self.b_bc], [bo])
            self.defer(lambda: S.dma("sp", self.out[t * 128:(t + 1) * 128, :], o, reads=[bo], writes=[Buf()]))

        loads(0)
        loads(1)
        for i in range(NT + 2):
            if i + 2 < NT:
                loads(i + 2)
            self.flush()
            if 0 <= i - 2 < NT:
                s2(i - 2)
            if 0 <= i - 1 < NT:
                s1b(i - 1)
            if i < NT:
                s1(i)
        self.flush()
        S.barrier()
        A.reset(m0)


def host_inputs(inputs, b):
    f32 = np.float32

    def fm(v):
        v = np.asarray(v, f32).reshape(-1, 128)
        return np.ascontiguousarray(v.T)
    invf = np.zeros((128, 1), f32)
    inv = (10000.0 ** (-np.arange(0, 32, 2, dtype=f32) / 32)).astype(f32)
    for p in range(64, 96):
        invf[p, 0] = inv[(p - 64) % 16]
    m = {
        "x": np.ascontiguousarray(inputs["x"][b]),
        "ct": fm(inputs["c"][b]),
        "pos": np.ascontiguousarray(inputs["positions"][b].reshape(1, S_TOK).astype(np.int32)),
        "w_ada": np.ascontiguousarray(inputs["w_ada"][0]),
        "b_ada_t": fm(inputs["b_ada"][0]),
        "w_adaf": np.ascontiguousarray(inputs["w_ada_final"]),
        "b_adaf_t": fm(inputs["b_ada_final"]),
        "gmix_t": fm(inputs["g_norm_mix"][0]),
        "gffn_t": fm(inputs["g_norm_ffn"][0]),
        "gfin_t": fm(inputs["g_norm_final"]),
        "w_in": np.ascontiguousarray(inputs["w_in"][0]),
        "glat_t": np.ascontiguousarray(np.concatenate([fm(inputs["g_q_lat"][0]), fm(inputs["g_kv_lat"][0])], axis=1)),
        "w_uq": np.ascontiguousarray(inputs["w_uq"][0].reshape(256, 768)),
        "w_uk": np.ascontiguousarray(inputs["w_uk"][0].reshape(256, 512)),
        "w_uv": np.ascontiguousarray(inputs["w_uv"][0].reshape(256, 512)),
        "b_gates": np.ascontiguousarray(inputs["b_gates"][0].reshape(1, 2048)),
        "ident": np.eye(128, dtype=f32),
        "invf": invf,
        "w_bf": np.ascontiguousarray(inputs["w_branch_fourier"][0]),
        "w_ba": np.ascontiguousarray(inputs["w_branch_attn"][0]),
        "w_out": np.ascontiguousarray(inputs["w_out"][0]),
        "w_rt": np.ascontiguousarray(np.concatenate([inputs["w_router_group"][0],
                                                     inputs["w_router_expert"][0].reshape(D, 32)], axis=1)),
        "b_rt": np.ascontiguousarray(np.concatenate([inputs["b_router_group"][0].reshape(1, 4),
                                                     inputs["b_router_expert"][0].reshape(1, 32)], axis=1)),
        "w_eg": np.ascontiguousarray(inputs["w_expert_gate"][0]),
        "w_eu": np.ascontiguousarray(inputs["w_expert_up"][0]),
        "w_ed": np.ascontiguousarray(inputs["w_expert_down"][0]),
    }
    m.update(CONSTS)
    return m


def _make_consts():
    bf = ml_dtypes.bfloat16
    n1 = np.arange(128)[None, :, None]
    n2 = np.arange(64)[:, None, None]
    k1 = np.arange(128)[None, None, :]
    ph = (k1 * (64 * n1 + n2)) % 8192
    al = 2.0 * np.pi * ph / 8192.0
    sc = 1.0 / math.sqrt(8192.0)
    tre = (np.cos(al) * sc).astype(bf)
    tim = (-np.sin(al) * sc).astype(bf)
    a = np.arange(64)
    th = 2.0 * np.pi * ((a[:, None] * a[None, :]) % 64) / 64.0
    c, s_ = np.cos(th), np.sin(th)
    d2 = np.zeros((128, 128))
    d2[0:64, 0:64] = c
    d2[0:64, 64:128] = s_
    d2[64:128, 0:64] = s_
    d2[64:128, 64:128] = -c
    bdc = np.zeros((128, 128), np.float32)
    bds = np.zeros((128, 128), np.float32)
    for g in range(2):
        bdc[g * 64:(g + 1) * 64, g * 64:(g + 1) * 64] = c / 8.0
        bds[g * 64:(g + 1) * 64, g * 64:(g + 1) * 64] = -s_ / 8.0
    ltri = np.triu(np.ones((128, 128), np.float32), 1).astype(bf)
    tstart = np.tile((np.arange(NST, dtype=np.float32) * float(TR))[None, :], (128, 1))
    piota = np.arange(128, dtype=np.float32).reshape(128, 1)
    tri = np.ascontiguousarray(np.stack([tre, tim], axis=2))
    return {"tri": tri, "d2": d2.astype(bf), "bdc": bdc, "bds": bds, "ltri": ltri,
            "tstart": np.ascontiguousarray(tstart), "piota": piota}


CONSTS = _make_consts()


def kernel(**inputs):
    bld = Builder()
    nc = bld.build()
    in_maps = [host_inputs(inputs, b) for b in range(8)]
    res = run_bass_kernel_spmd(nc, in_maps, core_ids=list(range(8)))
    return np.stack([np.asarray(r["out"]) for r in res.results], axis=0).astype(np.float32)
```
